# Optimizing a Trainium2 kernel written in Bass

```python
import math
import jax, jax.numpy as jnp
from jax import lax
import numpy as np

D_MODEL = 1024
BATCH = 2
SEQ = 8192
DEPTH = 1

GDN_HEADS = 4
GDN_HEAD_DIM = 128
GDN_WIDTH = GDN_HEADS * GDN_HEAD_DIM
ATT_HEADS = 4
ATT_HEAD_DIM = 128
ATT_WIDTH = ATT_HEADS * ATT_HEAD_DIM
CONV_WIDTH = 4
GDN_CHUNK = 64
DILATED_PATTERNS = ((128, 1), (512, 4), (2048, 16))
ATT_BLOCK = 128
ROPE_THETA = 500000.0
ROPE_DIMS = ATT_HEAD_DIM // 4
IN_SPLITS = (3 * GDN_WIDTH,
             4 * GDN_WIDTH,
             4 * GDN_WIDTH + GDN_HEADS,
             4 * GDN_WIDTH + 2 * GDN_HEADS,
             4 * GDN_WIDTH + 2 * GDN_HEADS + ATT_WIDTH,
             4 * GDN_WIDTH + 2 * GDN_HEADS + 2 * ATT_WIDTH)
IN_COLS = 4 * GDN_WIDTH + 2 * GDN_HEADS + 3 * ATT_WIDTH
N_GROUPS = 4
EXPERTS_PER_GROUP = 8
N_EXPERTS = N_GROUPS * EXPERTS_PER_GROUP
TOP_K_IN_GROUP = 2
EXPERT_FF = 512
MOE_BLOCK = 128
DEEPNORM_ALPHA = (2.0 * DEPTH) ** 0.25
DEEPNORM_BETA = (8.0 * DEPTH) ** -0.25
LN_EPS = 1e-5
RMS_EPS = 1e-6

kernel_name = "hymba_gdn_dilated_hmoe_deepnorm_adaln"

F32 = jnp.float32


def layer_norm(x, g, b):
    xf = x.astype(F32)
    mu = jnp.mean(xf, -1, keepdims=True)
    var = jnp.mean(jnp.square(xf - mu), -1, keepdims=True)
    return ((xf - mu) * lax.rsqrt(var + LN_EPS) * g + b).astype(x.dtype)


def rms_norm(x, g):
    xf = x.astype(F32)
    return xf * lax.rsqrt(jnp.mean(jnp.square(xf), -1, keepdims=True) + RMS_EPS) * g


def l2_normalize(x):
    xf = x.astype(F32)
    return xf * lax.rsqrt(jnp.sum(jnp.square(xf), -1, keepdims=True) + RMS_EPS)


def split_heads(t, n_heads):
    b, s, _ = t.shape
    return t.reshape(b, s, n_heads, -1).transpose(0, 2, 1, 3)


def causal_depthwise_conv(x, w):
    k, ch = w.shape
    return lax.conv_general_dilated(
        x, w[:, None, :].astype(x.dtype), window_strides=(1,), padding=[(k - 1, 0)],
        dimension_numbers=("NWC", "WIO", "NWC"), feature_group_count=ch)


def partial_rope(x, positions):
    half = ROPE_DIMS // 2
    inv_freq = ROPE_THETA ** (-jnp.arange(half, dtype=F32) * 2.0 / ROPE_DIMS)
    ang = positions.astype(F32)[:, None, :, None] * inv_freq
    cos, sin = jnp.cos(ang), jnp.sin(ang)
    xr = x[..., :ROPE_DIMS].astype(F32)
    x1, x2 = xr[..., :half], xr[..., half:]
    rot = jnp.concatenate([x1 * cos - x2 * sin, x2 * cos + x1 * sin], -1)
    return jnp.concatenate([rot, x[..., ROPE_DIMS:].astype(F32)], -1)


def gated_delta_rule_chunked(q, k, v, g, beta):
    b, h, s, dk = q.shape
    dv = v.shape[-1]
    nc = s // GDN_CHUNK
    q = q.reshape(b, h, nc, GDN_CHUNK, dk)
    k = k.reshape(b, h, nc, GDN_CHUNK, dk)
    v = v.reshape(b, h, nc, GDN_CHUNK, dv)
    g = g.reshape(b, h, nc, GDN_CHUNK)
    beta = beta.reshape(b, h, nc, GDN_CHUNK)
    gc = jnp.cumsum(g, -1)
    idx = jnp.arange(GDN_CHUNK)
    lower = idx[:, None] >= idx[None, :]
    strict = idx[:, None] > idx[None, :]
    decay = jnp.exp(jnp.where(lower, gc[..., :, None] - gc[..., None, :], -jnp.inf))
    kb = k * beta[..., None]
    vb = v * beta[..., None]
    a_mat = jnp.where(strict, jnp.einsum("bhnid,bhnjd->bhnij", kb, k) * decay, 0.0)
    t_mat = a_mat + jnp.eye(GDN_CHUNK, dtype=F32)
    rhs = jnp.concatenate([vb, kb * jnp.exp(gc)[..., None]], -1)
    sol = lax.linalg.triangular_solve(t_mat, rhs, left_side=True, lower=True, unit_diagonal=True)
    u, w = sol[..., :dv], sol[..., dv:]
    qk = jnp.where(lower, jnp.einsum("bhnid,bhnjd->bhnij", q, k) * decay, 0.0)
    q_dec = q * jnp.exp(gc)[..., None]
    k_dec = k * jnp.exp(gc[..., -1:] - gc)[..., None]
    g_last = jnp.exp(gc[..., -1])

    def step(state, inp):
        qk_c, qd_c, w_c, u_c, kd_c, gl_c = inp
        v_new = u_c - jnp.einsum("bhcd,bhdv->bhcv", w_c, state)
        o = jnp.einsum("bhcd,bhdv->bhcv", qd_c, state) + jnp.einsum("bhij,bhjv->bhiv", qk_c, v_new)
        state = state * gl_c[..., None, None] + jnp.einsum("bhcd,bhcv->bhdv", kd_c, v_new)
        return state, o

    xs = tuple(jnp.moveaxis(t, 2, 0) for t in (qk, q_dec, w, u, k_dec, g_last))
    _, o = lax.scan(step, jnp.zeros((b, h, dk, dv), F32), xs)
    return jnp.moveaxis(o, 0, 2).reshape(b, h, s, dv)


def banded_attention_stats(q, k, v, span):
    lead = q.shape[:-2]
    length, dh = q.shape[-2], q.shape[-1]
    nb = -(-length // ATT_BLOCK)
    lp = nb * ATT_BLOCK
    padw = [(0, 0)] * len(lead)
    q = jnp.pad(q, padw + [(0, lp - length), (0, 0)])
    k = jnp.pad(k, padw + [(ATT_BLOCK, lp - length), (0, 0)])
    v = jnp.pad(v, padw + [(ATT_BLOCK, lp - length), (0, 0)])
    qb = q.reshape(*lead, nb, ATT_BLOCK, dh)

    def prev_and_current(t):
        prev = t[..., :lp, :].reshape(*lead, nb, ATT_BLOCK, dh)
        cur = t[..., ATT_BLOCK:, :].reshape(*lead, nb, ATT_BLOCK, dh)
        return jnp.concatenate([prev, cur], -2)

    kb, vb = prev_and_current(k), prev_and_current(v)
    scores = jnp.einsum("...nqd,...nkd->...nqk", qb, kb, preferred_element_type=F32)
    qi = jnp.arange(ATT_BLOCK)[:, None] + ATT_BLOCK
    kj = jnp.arange(2 * ATT_BLOCK)[None, :]
    dist = qi - kj
    key_pos = jnp.arange(nb)[:, None, None] * ATT_BLOCK + kj - ATT_BLOCK
    mask = (dist >= 0) & (dist <= span) & (key_pos >= 0)
    scores = jnp.where(mask, scores, -jnp.inf)
    m = jnp.max(scores, -1)
    p = jnp.exp(scores - m[..., None])
    s = jnp.sum(p, -1)
    o = jnp.einsum("...nqk,...nkd->...nqd", p, vb.astype(F32))
    m = m.reshape(*lead, lp)[..., :length]
    s = s.reshape(*lead, lp)[..., :length]
    o = o.reshape(*lead, lp, dh)[..., :length, :]
    return m, s, o


def dilated_sliding_attention(q, k, v):
    b, h, s, dh = q.shape
    ms, ss, os_ = [], [], []
    for window, dil in DILATED_PATTERNS:
        span = window // dil

        def to_classes(t):
            return t.reshape(b, h, s // dil, dil, dh).swapaxes(2, 3)

        m, sd, o = banded_attention_stats(to_classes(q), to_classes(k), to_classes(v), span)
        ms.append(m.swapaxes(2, 3).reshape(b, h, s))
        ss.append(sd.swapaxes(2, 3).reshape(b, h, s))
        os_.append(o.swapaxes(2, 3).reshape(b, h, s, dh))
    m_all = jnp.stack(ms)
    wts = jnp.exp(m_all - jnp.max(m_all, 0, keepdims=True))
    num = jnp.sum(wts[..., None] * jnp.stack(os_), 0)
    den = jnp.sum(wts * jnp.stack(ss), 0)
    return num / den[..., None]


def hybrid_mixer(h, positions, w_in, conv_w, a_log, dt_bias, gdn_norm_w, attn_norm_w, w_o):
    b, s, _ = h.shape
    proj = h @ w_in
    qkv_a, z_a, beta_logit, decay_logit, q_b, k_b, v_b = jnp.split(proj, IN_SPLITS, -1)
    qkv_a = jax.nn.silu(causal_depthwise_conv(qkv_a, conv_w))
    q_a, k_a, v_a = jnp.split(qkv_a, 3, -1)
    q_a = l2_normalize(split_heads(q_a, GDN_HEADS)) * (GDN_HEAD_DIM ** -0.5)
    k_a = l2_normalize(split_heads(k_a, GDN_HEADS))
    v_a = split_heads(v_a, GDN_HEADS).astype(F32)
    beta = jax.nn.sigmoid(beta_logit.astype(F32)).transpose(0, 2, 1)
    g = (-jnp.exp(a_log) * jax.nn.softplus(decay_logit.astype(F32) + dt_bias)).transpose(0, 2, 1)
    o_a = gated_delta_rule_chunked(q_a, k_a, v_a, g, beta).transpose(0, 2, 1, 3)
    z = z_a.reshape(b, s, GDN_HEADS, GDN_HEAD_DIM).astype(F32)
    o_a = rms_norm(o_a, gdn_norm_w) * jax.nn.silu(z)
    q_b = partial_rope(split_heads(q_b, ATT_HEADS), positions) * (ATT_HEAD_DIM ** -0.5)
    k_b = partial_rope(split_heads(k_b, ATT_HEADS), positions)
    v_b = split_heads(v_b, ATT_HEADS).astype(F32)
    o_b = dilated_sliding_attention(q_b, k_b, v_b).transpose(0, 2, 1, 3)
    o_b = rms_norm(o_b, attn_norm_w)
    mixed = jnp.concatenate([o_a.reshape(b, s, GDN_WIDTH), o_b.reshape(b, s, ATT_WIDTH)], -1)
    return mixed.astype(h.dtype) @ w_o


def grouped_expert_mlp(xf, expert_ids, gates, w_gate, w_up, w_down):
    t, d = xf.shape
    n = expert_ids.shape[0]
    top = n // t
    token_ids = jnp.arange(n, dtype=jnp.int32) // top
    order = jnp.argsort(expert_ids)
    e_sorted = expert_ids[order]
    counts = jnp.zeros((N_EXPERTS,), jnp.int32).at[expert_ids].add(1)
    padded = (counts + MOE_BLOCK - 1) // MOE_BLOCK * MOE_BLOCK
    pad_end = jnp.cumsum(padded)
    pad_start = pad_end - padded
    start = jnp.cumsum(counts) - counts
    dest = pad_start[e_sorted] + jnp.arange(n, dtype=jnp.int32) - start[e_sorted]
    n_slots = -(-n // MOE_BLOCK) * MOE_BLOCK + N_EXPERTS * MOE_BLOCK
    n_blocks = n_slots // MOE_BLOCK
    slot_tok = jnp.full((n_slots,), t, jnp.int32).at[dest].set(token_ids[order])
    slot_gate = jnp.zeros((n_slots,), F32).at[dest].set(gates[order])
    blk_expert = jnp.minimum(
        jnp.searchsorted(pad_end, jnp.arange(n_blocks, dtype=jnp.int32) * MOE_BLOCK, side="right"),
        N_EXPERTS - 1)
    x_pad = jnp.concatenate([xf, jnp.zeros((1, d), xf.dtype)], 0)
    xs = x_pad[slot_tok].reshape(n_blocks, MOE_BLOCK, d)

    def expert_block(args):
        xb, e = args
        hid = jax.nn.silu(xb @ w_gate[e]) * (xb @ w_up[e])
        return hid @ w_down[e]

    ys = lax.map(expert_block, (xs, blk_expert)).reshape(n_slots, d)
    y = jnp.zeros((t + 1, d), F32).at[slot_tok].add(ys.astype(F32) * slot_gate[:, None])
    return y[:t]


def hierarchical_moe(h, w_rg, b_rg, w_re, b_re, w_gate, w_up, w_down):
    b, s, d = h.shape
    hf = h.reshape(b * s, d)
    lg = (hf @ w_rg).astype(F32) + b_rg
    pg = jax.nn.softmax(lg, -1)
    grp = jnp.argmax(lg, -1).astype(jnp.int32)
    gate_grp = jnp.take_along_axis(pg, grp[:, None], -1)
    le = ((hf @ w_re).astype(F32) + b_re).reshape(b * s, N_GROUPS, EXPERTS_PER_GROUP)
    le_sel = jnp.take_along_axis(le, grp[:, None, None], 1)[:, 0]
    top_v, top_i = lax.top_k(le_sel, TOP_K_IN_GROUP)
    gates = gate_grp * jax.nn.softmax(top_v, -1)
    experts = grp[:, None] * EXPERTS_PER_GROUP + top_i.astype(jnp.int32)
    y = grouped_expert_mlp(hf, experts.reshape(-1), gates.reshape(-1), w_gate, w_up, w_down)
    return y.reshape(b, s, d).astype(h.dtype)


def setup_inputs(seed: int = 0) -> dict:
    key = jax.random.key(seed)
    ks = jax.random.split(key, 24)
    D, L = D_MODEL, DEPTH
    nrm = jax.random.normal
    x = nrm(ks[0], (BATCH, SEQ, D), F32)
    c = nrm(ks[1], (BATCH, D), F32)
    offs = jax.random.randint(ks[2], (BATCH, 1), 0, 4096, dtype=jnp.int32)
    positions = jnp.arange(SEQ, dtype=jnp.int32)[None, :] + offs
    w_ada = nrm(ks[3], (L, D, 6 * D), F32) * (0.1 * D ** -0.5)
    b_ada = nrm(ks[4], (L, 6 * D), F32) * 0.01
    w_in = nrm(ks[5], (L, D, IN_COLS), F32) * D ** -0.5
    conv_w = nrm(ks[6], (L, CONV_WIDTH, 3 * GDN_WIDTH), F32) * CONV_WIDTH ** -0.5
    a_log = jnp.log(jax.random.uniform(ks[7], (L, GDN_HEADS), F32, 1.0, 16.0))
    dt = jnp.exp(jax.random.uniform(ks[8], (L, GDN_HEADS), F32, math.log(1e-3), math.log(1e-1)))
    dt_bias = dt + jnp.log(-jnp.expm1(-dt))
    gdn_norm_w = 1.0 + 0.02 * nrm(ks[9], (L, GDN_HEAD_DIM), F32)
    attn_norm_w = 1.0 + 0.02 * nrm(ks[10], (L, ATT_HEAD_DIM), F32)
    w_o = nrm(ks[11], (L, D, D), F32) * (D ** -0.5 * DEEPNORM_BETA)
    ln1_g = 1.0 + 0.02 * nrm(ks[12], (L, D), F32)
    ln1_b = 0.02 * nrm(ks[13], (L, D), F32)
    w_router_group = nrm(ks[14], (L, D, N_GROUPS), F32) * D ** -0.5
    b_router_group = 0.01 * nrm(ks[15], (L, N_GROUPS), F32)
    w_router_expert = nrm(ks[16], (L, D, N_EXPERTS), F32) * D ** -0.5
    b_router_expert = 0.01 * nrm(ks[17], (L, N_EXPERTS), F32)
    w_gate = nrm(ks[18], (L, N_EXPERTS, D, EXPERT_FF), F32) * D ** -0.5
    w_up = nrm(ks[19], (L, N_EXPERTS, D, EXPERT_FF), F32) * D ** -0.5
    w_down = nrm(ks[20], (L, N_EXPERTS, EXPERT_FF, D), F32) * (EXPERT_FF ** -0.5 * DEEPNORM_BETA)
    ln2_g = 1.0 + 0.02 * nrm(ks[21], (L, D), F32)
    ln2_b = 0.02 * nrm(ks[22], (L, D), F32)
    return {"x": x, "c": c, "positions": positions, "w_ada": w_ada, "b_ada": b_ada,
            "w_in": w_in, "conv_w": conv_w, "a_log": a_log, "dt_bias": dt_bias,
            "gdn_norm_w": gdn_norm_w, "attn_norm_w": attn_norm_w, "w_o": w_o,
            "ln1_g": ln1_g, "ln1_b": ln1_b, "w_router_group": w_router_group,
            "b_router_group": b_router_group, "w_router_expert": w_router_expert,
            "b_router_expert": b_router_expert, "w_gate": w_gate, "w_up": w_up,
            "w_down": w_down, "ln2_g": ln2_g, "ln2_b": ln2_b}


def reference(x, c, positions, w_ada, b_ada, w_in, conv_w, a_log, dt_bias, gdn_norm_w,
              attn_norm_w, w_o, ln1_g, ln1_b, w_router_group, b_router_group, w_router_expert,
              b_router_expert, w_gate, w_up, w_down, ln2_g, ln2_b):
    for l in range(DEPTH):
        mod = jax.nn.silu(c) @ w_ada[l] + b_ada[l]
        shift1, scale1, gate1, shift2, scale2, gate2 = jnp.split(mod[:, None, :], 6, axis=-1)
        h = x * (1 + scale1) + shift1
        mix = hybrid_mixer(h, positions, w_in[l], conv_w[l], a_log[l], dt_bias[l],
                           gdn_norm_w[l], attn_norm_w[l], w_o[l])
        x = layer_norm(DEEPNORM_ALPHA * x + (1 + gate1) * mix, ln1_g[l], ln1_b[l])
        h = x * (1 + scale2) + shift2
        y = hierarchical_moe(h, w_router_group[l], b_router_group[l], w_router_expert[l],
                             b_router_expert[l], w_gate[l], w_up[l], w_down[l])
        x = layer_norm(DEEPNORM_ALPHA * x + (1 + gate2) * y, ln2_g[l], ln2_b[l])
    return x
```

```python
import math
from contextlib import ExitStack

import numpy as np
import concourse.bass as bass
import concourse.mybir as mybir
from concourse.bass_utils import run_bass_kernel_spmd

F32 = mybir.dt.float32
BF16 = mybir.dt.bfloat16
I32 = mybir.dt.int32
U32 = mybir.dt.uint32
F32R = mybir.dt.float32r
AF = mybir.ActivationFunctionType
ALU = mybir.AluOpType
AX = mybir.AxisListType

D_MODEL = 1024
SEQ = 8192
BATCH = 2
NCORES = 8
T = 512
NCH = SEQ // T
ALPHA = 2.0 ** 0.25
LN_EPS = 1e-5
RMS_EPS = 1e-6
ROPE_THETA = 500000.0
NEG = -30000.0
TWO_PI = 2.0 * math.pi


class Trk:
    def __init__(self, nc, es, n_dma_sems=24, same_engine_sync=True):
        self.nc = nc
        self.e = {"pe": nc.tensor, "dve": nc.vector, "act": nc.scalar, "pool": nc.gpsimd, "sp": nc.sync}
        self.sem = {k: es.enter_context(nc.semaphore("sem_" + k)) for k in self.e}
        self.cnt = {k: 0 for k in self.e}
        self.seen = {k: {} for k in self.e}
        self.last_w = {}
        self.reads = {}
        self.dsem = [es.enter_context(nc.semaphore("dsem%d" % i)) for i in range(n_dma_sems)]
        self.dval = [0] * n_dma_sems
        self.dnext = 0
        self.same = same_engine_sync
        self.all_dma_events = []
        self.ccsem = es.enter_context(nc.semaphore("ccsem"))
        self.ccval = 0

    def _wait(self, eng, ev):
        kind, src, n = ev
        key = (kind, src)
        if self.seen[eng].get(key, 0) >= n:
            return
        if kind == "e":
            if src == eng and not self.same:
                return
            self.e[eng].wait_ge(self.sem[src], n)
        elif kind == "c":
            self.e[eng].wait_ge(self.ccsem, n)
        else:
            self.e[eng].wait_ge(self.dsem[src], n)
        self.seen[eng][key] = n

    def _deps(self, eng, reads, writes):
        deps = []
        for r in reads:
            ev = self.last_w.get(r)
            if ev is not None:
                deps.append(ev)
        for w in writes:
            ev = self.last_w.get(w)
            if ev is not None:
                deps.append(ev)
            for ev2 in self.reads.get(w, {}).values():
                deps.append(ev2)
        for ev in deps:
            self._wait(eng, ev)

    def _record(self, ev, reads, writes):
        for r in reads:
            self.reads.setdefault(r, {})[(ev[0], ev[1])] = ev
        for w in writes:
            self.last_w[w] = ev
            self.reads[w] = {}

    def op(self, eng, fn, reads=(), writes=()):
        self._deps(eng, reads, writes)
        inst = fn(self.e[eng])
        self.cnt[eng] += 1
        inst.then_inc(self.sem[eng], 1)
        ev = ("e", eng, self.cnt[eng])
        self._record(ev, reads, writes)
        return ev

    def ops(self, eng, fns, reads=(), writes=()):
        self._deps(eng, reads, writes)
        inst = None
        for fn in fns:
            inst = fn(self.e[eng])
        self.cnt[eng] += 1
        inst.then_inc(self.sem[eng], 1)
        ev = ("e", eng, self.cnt[eng])
        self._record(ev, reads, writes)
        return ev

    def dma(self, eng, fn, reads=(), writes=()):
        self._deps(eng, reads, writes)
        i = self.dnext
        self.dnext = (self.dnext + 1) % len(self.dsem)
        if self.dval[i] > 0:
            self._wait(eng, ("d", i, self.dval[i]))
        inst = fn(self.e[eng])
        self.dval[i] += 16
        inst.then_inc(self.dsem[i], 16)
        ev = ("d", i, self.dval[i])
        self._record(ev, reads, writes)
        self.all_dma_events.append(ev)
        return ev

    def cc(self, fn, reads=(), writes=()):
        eng = "pool"
        self._deps(eng, reads, writes)
        if self.ccsem is None:
            raise RuntimeError("no cc semaphore")
        if self.ccval > 0:
            self._wait(eng, ("c", 0, self.ccval))
        inst = fn(self.e[eng])
        self.ccval += 1
        inst.then_inc(self.ccsem, 1)
        ev = ("c", 0, self.ccval)
        self._record(ev, reads, writes)
        return ev

    def barrier(self):
        for eng in self.e:
            for i, v in enumerate(self.dval):
                if v > 0:
                    self._wait(eng, ("d", i, v))
            if self.ccval > 0:
                self._wait(eng, ("c", 0, self.ccval))
            for k, n in self.cnt.items():
                if n > 0 and k != eng:
                    self._wait(eng, ("e", k, n))

    def finish(self, eng="sp"):
        for i, v in enumerate(self.dval):
            if v > 0:
                self._wait(eng, ("d", i, v))
        if self.ccval > 0:
            self._wait(eng, ("c", 0, self.ccval))
        for k, n in self.cnt.items():
            if n > 0 and k != eng:
                self._wait(eng, ("e", k, n))


class _Proxy:
    def __init__(self):
        self.calls = []

    def __getattr__(self, name):
        def f(*a, **k):
            self.calls.append((name, a, k))
            return None
        return f


def _freeze(fn):
    p = _Proxy()
    fn(p)
    assert len(p.calls) == 1
    name, a, k = p.calls[0]
    return lambda e: getattr(e, name)(*a, **k)


class Rec:
    HOP = 0.8

    def __init__(self, tk):
        self.tk = tk
        self.L = []

    def op(self, eng, fn, reads=(), writes=(), cost=None):
        self.L.append(("op", eng, _freeze(fn), tuple(reads), tuple(writes), cost if cost is not None else (0.3 if eng == "pe" else 0.6)))

    def ops(self, eng, fns, reads=(), writes=(), cost=None):
        fns = [_freeze(f) for f in fns]
        self.L.append(("ops", eng, fns, tuple(reads), tuple(writes), cost if cost is not None else 0.2 * len(fns)))

    def dma(self, eng, fn, reads=(), writes=(), cost=None):
        self.L.append(("dma", eng, _freeze(fn), tuple(reads), tuple(writes), cost if cost is not None else 2.0))

    def cc(self, fn, reads=(), writes=(), cost=None):
        self.L.append(("cc", "pool", _freeze(fn), tuple(reads), tuple(writes), 5.0))

    def flush(self):
        L = self.L
        self.L = []
        n = len(L)
        preds = [set() for _ in range(n)]
        last_w = {}
        readers = {}
        for i, (kind, eng, fn, reads, writes, cost) in enumerate(L):
            for r in reads:
                if r in last_w:
                    preds[i].add(last_w[r])
            for w in writes:
                if w in last_w:
                    preds[i].add(last_w[w])
                for j in readers.get(w, ()):
                    preds[i].add(j)
            for r in reads:
                readers.setdefault(r, []).append(i)
            for w in writes:
                last_w[w] = i
                readers[w] = []
            preds[i].discard(i)
        succs = [[] for _ in range(n)]
        npred = [len(p) for p in preds]
        for i in range(n):
            for j in preds[i]:
                succs[j].append(i)
        efree = {}
        finish = [0.0] * n
        ready_t = [0.0] * n
        ready = [i for i in range(n) if npred[i] == 0]
        order = []
        import heapq
        while ready:
            best = None
            bkey = None
            for i in ready:
                kind, eng, fn, reads, writes, cost = L[i]
                q = eng if kind in ("op", "ops") else ("q_" + eng)
                st = max(efree.get(q, 0.0), ready_t[i])
                key = (st, i)
                if bkey is None or key < bkey:
                    bkey = key
                    best = i
            i = best
            ready.remove(i)
            kind, eng, fn, reads, writes, cost = L[i]
            q = eng if kind in ("op", "ops") else ("q_" + eng)
            st = bkey[0]
            if kind in ("op", "ops"):
                efree[q] = st + cost
                finish[i] = st + cost
            else:
                efree[q] = st + 0.1
                finish[i] = st + cost
            order.append(i)
            for j in succs[i]:
                npred[j] -= 1
                same = (L[j][1] == eng and L[j][0] in ("op", "ops") and kind in ("op", "ops"))
                ready_t[j] = max(ready_t[j], finish[i] + (0.0 if same else self.HOP))
                if npred[j] == 0:
                    ready.append(j)
        assert len(order) == n
        for i in order:
            kind, eng, fn, reads, writes, cost = L[i]
            if kind == "op":
                self.tk.op(eng, fn, reads, writes)
            elif kind == "ops":
                self.tk.ops(eng, fn, reads, writes)
            elif kind == "dma":
                self.tk.dma(eng, fn, reads, writes)
            else:
                self.tk.cc(fn, reads, writes)


class PsumPool:
    _uid = [0]

    def __init__(self, nc, es, names):
        PsumPool._uid[0] += 1
        u = PsumPool._uid[0]
        self.t = {n: es.enter_context(nc.psum_tensor("ps%d_%s" % (u, n), [128, 512], F32)) for n in names}
        self.rot = [n for n in names if n.startswith("r")]
        self.i = 0

    def next(self):
        n = self.rot[self.i]
        self.i = (self.i + 1) % len(self.rot)
        return n, self.t[n]


def phase1(nc, tk, es, D, nch=NCH, cin=None, on_quarter=None):
    def sb(name, shape, dt=F32):
        return es.enter_context(nc.sbuf_tensor("s_" + name, shape, dt))

    def K(name, c):
        return (name, c % 8)

    ppall = PsumPool(nc, es, ["rb0", "rb1", "rb2", "rb3", "ra0", "ra1", "scan", "ot"])
    PS = ppall.t
    real_tk = tk
    tk = Rec(real_tk)

    class _Sub:
        def __init__(self, names):
            self.rot = names
            self.i = 0

        def next(self):
            n = self.rot[self.i]
            self.i = (self.i + 1) % len(self.rot)
            return n, PS[n]

    pp = _Sub(["rb0", "rb1", "rb2"])

    ppa = _Sub(["ra0", "ra1", "rb3"])

    ident = sb("ident", [128, 128])
    ident_b = sb("ident_b", [128, 128], BF16)
    ones_f = sb("ones_f", [128, 128])
    ones_b = sb("ones_b", [128, 128], BF16)
    mus_t = sb("mus_neg", [128, 128]); mui_t = sb("mu_inc", [128, 128]); mls_t = sb("mls_neg", [128, 128])
    mus_neg = mus_t[:].unsqueeze(1).to_broadcast([128, 4, 128])
    mu_inc = mui_t[:].unsqueeze(1).to_broadcast([128, 4, 128])
    mls_neg = mls_t[:].unsqueeze(1).to_broadcast([128, 4, 128])
    ident4 = ident[:].unsqueeze(1).to_broadcast([128, 4, 128])
    amask_f = sb("amask_f", [128, 256])
    amask = sb("amask", [128, 256], BF16)
    scanm = sb("scanm", [128, T])
    invf = sb("invf", [64, 2])
    win_b = sb("win_b", [128, 8, 1280], BF16)
    xtok = sb("xtok", [128, 8, T])
    wstage = [xtok[:, 4 * i:4 * i + 4, :].rearrange("p a (h c) -> p (a h) c", c=256) for i in range(2)]
    wkeys = ["xtok_a", "xtok_b"]
    sc1 = sb("sc1", [128, 8])
    sh1 = sb("sh1", [128, 8])
    convw = sb("convw", [128, 3, 4])
    pvec = sb("pvec", [128, 4])
    nalog = sb("nalog", [128, 1])
    cT = sb("cT", [128, 8])
    scT = sb("scT", [128, 8, 2])
    bada = sb("bada", [128, 16])
    mod1 = sb("mod1", [128, 16])

    tk.dma("sp", lambda e: e.dma_start(out=ident[:], in_=D["c_ident"][:, :]), writes=["ident"])
    tk.dma("sp", lambda e: e.dma_start(out=amask_f[:], in_=D["c_amask"][:, :]), writes=["amask_f"])
    tk.dma("sp", lambda e: e.dma_start(out=invf[:], in_=D["c_invf"][:, :]), writes=["invf"])
    tk.dma("sp", lambda e: e.dma_start(out=convw[:], in_=D["convw"][:, :, :]), writes=["convw"])
    tk.dma("sp", lambda e: e.dma_start(out=pvec[:], in_=D["pvec"][:, :]), writes=["pvec"])
    tk.dma("sp", lambda e: e.dma_start(out=cT[:], in_=D["cT"][:, :]), writes=["cT"])
    tk.dma("sp", lambda e: e.dma_start(out=bada[:], in_=D["bada1"][:, :]), writes=["bada"])
    for q, (nm, tl) in enumerate([("mus", mus_t), ("mui", mui_t), ("mls", mls_t)]):
        tk.dma("sp", lambda e, tl=tl, q=q: e.dma_start(out=tl[:], in_=D["c_masks"][:, q, :]), writes=[nm])
    tk.op("dve", lambda e: e.tensor_copy(out=ident_b[:], in_=ident[:]), reads=["ident"], writes=["ident_b"])
    tk.op("dve", lambda e: e.tensor_copy(out=amask[:], in_=amask_f[:]), reads=["amask_f"], writes=["amask"])
    tk.op("pool", lambda e: e.memset(ones_f[:], 1.0), writes=["ones_f"])
    tk.op("pool", lambda e: e.memset(ones_b[:], 1.0), writes=["ones_b"])
    tk.op("pool", lambda e: e.memset(scanm[:], 1.0), writes=["scanm"])
    tk.op("pool", lambda e: e.memset(scanm[:, 0:T:64], 0.0), writes=["scanm"])
    tk.op("act", lambda e: e.activation(out=nalog[:], in_=pvec[:, 0:1], func=AF.Exp), reads=["pvec"],
          writes=["nalog"])
    tk.op("dve", lambda e: e.tensor_scalar(out=nalog[:], in0=nalog[:], scalar1=-1.0, scalar2=None, op0=ALU.mult),
          reads=["nalog"], writes=["nalog"])

    for j in range(5):
        st = wstage[j % 2]
        key = wkeys[j % 2]
        tk.dma("sp" if j % 2 == 0 else "pool", lambda e, st=st, j=j: e.dma_start(out=st, in_=D["win"][:, j, :, :]),
               writes=[key])
        tk.op("dve" if j % 2 == 0 else "act", (lambda e, st=st, j=j: e.tensor_copy(out=win_b[:, :, j * 256:(j + 1) * 256], in_=st))
              if j % 2 == 0 else (lambda e, st=st, j=j: e.copy(out=win_b[:, :, j * 256:(j + 1) * 256], in_=st)),
              reads=[key], writes=[("win_b", j)])
    for c0 in (7 * 128, 7 * 128 + 32):
        tk.op("dve", lambda e, c0=c0: e.tensor_scalar(out=win_b[:, :, c0:c0 + 16], in0=win_b[:, :, c0:c0 + 16],
                                                       scalar1=-1.0, scalar2=None, op0=ALU.mult),
              reads=[("win_b", 3)], writes=[("win_b", 3)])

    tk.op("act", lambda e: e.activation(out=scT[:, :, 0], in_=cT[:], func=AF.Silu), reads=["cT"], writes=["scT"])
    tk.op("act", lambda e: e.activation(out=scT[:, :, 1], in_=cT[:], func=AF.Silu), reads=["cT"], writes=["scT"])
    for j in range(8):
        st = wstage[j % 2]
        key = wkeys[j % 2]
        tk.dma("sp" if j % 2 == 0 else "pool", lambda e, st=st, j=j: e.dma_start(out=st, in_=D["wada1"][:, j, :, :]),
               writes=[key])
        for fh in range(2):
            fc = 2 * j + fh
            pn, pt = pp.next()
            tk.ops("pe", [lambda e, st=st, kc=kc, pt=pt, fh=fh: e.matmul(
                pt[:, 0:2], lhsT=st[:, kc, fh * 128:(fh + 1) * 128], rhs=scT[:, kc, :], start=(kc == 0), stop=(kc == 7))
                for kc in range(8)], reads=[key, "scT"], writes=[pn])
            tk.op("dve", lambda e, pt=pt, fc=fc: e.tensor_tensor(out=mod1[:, fc:fc + 1], in0=pt[:, 0:1],
                                                                  in1=bada[:, fc:fc + 1], op=ALU.add),
                  reads=[pn, "bada"], writes=["mod1"])
    tk.op("dve", lambda e: e.tensor_copy(out=sh1[:], in_=mod1[:, 0:8]), reads=["mod1"], writes=["sh1"])
    tk.op("dve", lambda e: e.tensor_scalar(out=sc1[:], in0=mod1[:, 8:16], scalar1=1.0, scalar2=None, op0=ALU.add),
          reads=["mod1"], writes=["sc1"])

    RING = 4096
    qT = sb("qT", [128, RING], BF16)
    kT = sb("kT", [128, RING], BF16)
    vT = sb("vT", [128, RING], BF16)
    vtt = sb("vtt", [128, 8, 128], BF16)
    acc = sb("acc", [128, 2, 2048])
    S = [sb("S%d" % i, [128, 128]) for i in range(2)]
    tk.op("pool", lambda e: e.memset(S[0][:], 0.0), writes=["S0"])

    hT = [sb("hT%d" % i, [128, 8, T], BF16) for i in range(2)]
    craw = [sb("craw%d" % g, [128, T + 3]) for g in range(3)]
    cacc = sb("cacc", [128, T])
    qs = sb("qs", [128, T]); ks = sb("ks", [128, T]); vs = sb("vs", [128, T])
    zs2 = [sb("zs%d" % i, [128, T]) for i in range(2)]
    sq = sb("sq", [128, T]); rn = sb("rn", [128, T])
    p1 = sb("p1", [128, T]); p2 = sb("p2", [128, T]); p3 = sb("p3", [128, T])
    osq = p1; rs = p2; fsq = p1; fr = p2; oa = p3; fo = p3
    qn = sb("qn", [128, T]); kn = sb("kn", [128, T])
    betab = sb("betab", [128, T]); gb = sb("gb", [128, T]); gcb = sb("gcb", [128, T])
    egcb2 = [sb("egcb%d" % i, [128, T]) for i in range(2)]; eglb = sb("eglb", [128, T]); begb = gb
    cols = sb("cols", [128, 5, 4])
    Kbe = sb("Kbe", [128, 4, 128]); Kd2 = [sb("Kd%d" % i, [128, 4, 128]) for i in range(2)]
    Vb = sb("Vb", [128, 4, 128])
    tdf = sb("tdf", [128, 4, 128]); DT = sb("DT", [128, 4, 128]); Dm = sb("Dm", [128, 4, 128])
    E1 = tdf; QKT2 = [sb("QKT%d" % i, [128, 4, 128]) for i in range(2)]
    Bm = [sb("Bm%d" % i, [128, 4, 128]) for i in range(2)]
    BTm = [sb("BTm%d" % i, [128, 4, 128]) for i in range(2)]
    Pm = [sb("Pm%d" % i, [128, 4, 128]) for i in range(2)]
    U2 = [sb("U%d" % i, [128, 4, 128]) for i in range(2)]; WT2 = [sb("WT%d" % i, [128, T]) for i in range(2)]
    QdT2 = [sb("QdT%d" % i, [128, T]) for i in range(2)]
    vnew = sb("vnew", [128, 2, 128])
    oab = sb("oab", [128, T], BF16)
    posi = sb("posi", [64, T], I32); ra = sb("ra", [64, T]); rb = sb("rb", [64, T])
    ri = sb("ri", [64, T], I32)
    sincos = sb("sincos", [64, T])
    rt1 = sb("rt1", [32, T]); rt2 = sb("rt2", [32, T])
    pTt = [sb("pT%d" % i, [128, 256], BF16) for i in range(4)]
    fob = sb("fob", [128, T], BF16)

    x = D["x"]
    mixT = D.get("mixT")
    pos = D["pos"]

    def load_x(c):
        for i in range(2):
            tk.dma("sp", lambda e, i=i: e.dma_start(
                out=xtok[:, 4 * i:4 * i + 4, :],
                in_=x[i * 512:(i + 1) * 512, c * T:(c + 1) * T].rearrange("(kc p) t -> p kc t", p=128)),
                writes=[wkeys[i]])

    def stage_a(c):
        h = hT[c % 2]
        hk = "hT%d" % (c % 2)
        for kc in range(8):
            tk.op("act", lambda e, kc=kc: e.activation(out=h[:, kc, :], in_=xtok[:, kc, :], func=AF.Identity,
                                                       bias=sh1[:, kc:kc + 1], scale=sc1[:, kc:kc + 1]),
                  reads=[wkeys[kc // 4], "sh1", "sc1"], writes=[hk])

    def proj(c, m):
        h = hT[c % 2]
        hk = "hT%d" % (c % 2)
        pn, pt = pp.next()
        tk.ops("pe", [lambda e, kc=kc, pt=pt: e.matmul(pt[:, :], lhsT=win_b[:, kc, m * 128:(m + 1) * 128],
                                                      rhs=h[:, kc, :], start=(kc == 0), stop=(kc == 7))
                      for kc in range(8)], reads=[hk, ("win_b", m // 2)], writes=[pn])
        return pn, pt

    def rope_tables(c):
        tk.dma("sp", lambda e: e.dma_start(out=posi[:], in_=pos[c * T:(c + 1) * T].partition_broadcast(64)),
               writes=["posi"])
        tk.op("dve", lambda e: e.tensor_copy(out=ra[:], in_=posi[:]), reads=["posi"], writes=["ra"])
        tk.op("dve", lambda e: e.tensor_scalar(out=ra[:], in0=ra[:], scalar1=invf[:, 0:1], scalar2=invf[:, 1:2],
                                               op0=ALU.mult, op1=ALU.add), reads=["ra", "invf"], writes=["ra"])
        tk.op("dve", lambda e: e.tensor_scalar(out=ri[:], in0=ra[:], scalar1=1.0 / TWO_PI, scalar2=None,
                                               op0=ALU.mult), reads=["ra"], writes=["ri"])
        tk.op("dve", lambda e: e.tensor_copy(out=rb[:], in_=ri[:]), reads=["ri"], writes=["rb"])
        tk.op("dve", lambda e: e.scalar_tensor_tensor(out=ra[:], in0=rb[:], scalar=-TWO_PI, in1=ra[:], op0=ALU.mult,
                                                      op1=ALU.add), reads=["ra", "rb"], writes=["ra"])
        tk.op("dve", lambda e: e.tensor_scalar(out=rb[:], in0=ra[:], scalar1=math.pi, scalar2=-TWO_PI,
                                               op0=ALU.is_gt, op1=ALU.mult), reads=["ra"], writes=["rb"])
        tk.op("dve", lambda e: e.tensor_tensor(out=ra[:], in0=ra[:], in1=rb[:], op=ALU.add),
              reads=["ra", "rb"], writes=["ra"])
        tk.op("dve", lambda e: e.tensor_scalar(out=rb[:], in0=ra[:], scalar1=-math.pi, scalar2=TWO_PI,
                                               op0=ALU.is_lt, op1=ALU.mult), reads=["ra"], writes=["rb"])
        tk.op("dve", lambda e: e.tensor_tensor(out=ra[:], in0=ra[:], in1=rb[:], op=ALU.add),
              reads=["ra", "rb"], writes=["ra"])
        tk.op("act", lambda e: e.activation(out=sincos[:], in_=ra[:], func=AF.Sin), reads=["ra"], writes=["sincos"])

    def prep_main(c):
        par = c % 2
        U, WT, QdT, QKT, Kd, egcb, zs = U2[par], WT2[par], QdT2[par], QKT2[par], Kd2[par], egcb2[par], zs2[par]
        uk, wk, qdk, qkk, kdk, ek, zk = ("U%d" % par, "WT%d" % par, "QdT%d" % par, "QKT%d" % par, "Kd%d" % par,
                                         "egcb%d" % par, "zs%d" % par)
        stage_a(c)
        yield
        if c + 1 < nch:
            load_x(c + 1)
        yield
        for g, dst, dk_ in ((0, qs, "qs"), (1, ks, "ks"), (2, vs, "vs")):
            pn, pt = proj(c, g)
            ck = "craw%d" % g
            cr = craw[g]
            if c > 0:
                tk.op("dve", lambda e, cr=cr: e.tensor_copy(out=cr[:, 0:3], in_=cr[:, T:T + 3]), reads=[ck],
                      writes=[ck])
            else:
                tk.op("pool", lambda e, cr=cr: e.memset(cr[:, 0:3], 0.0), writes=[ck])
            tk.op("act", lambda e, pt=pt, cr=cr: e.copy(out=cr[:, 3:T + 3], in_=pt[:, :]), reads=[pn],
                  writes=[ck])
            tk.op("dve", lambda e, cr=cr, g=g: e.tensor_scalar(out=cacc[:], in0=cr[:, 0:T],
                                                               scalar1=convw[:, g, 0:1], scalar2=None, op0=ALU.mult),
                  reads=[ck, "convw"], writes=["cacc"])
            for j in range(1, 4):
                tk.op("dve", lambda e, cr=cr, g=g, j=j: e.scalar_tensor_tensor(
                    out=cacc[:], in0=cr[:, j:j + T], scalar=convw[:, g, j:j + 1], in1=cacc[:],
                    op0=ALU.mult, op1=ALU.add), reads=[ck, "convw", "cacc"], writes=["cacc"])
            tk.op("act", lambda e, dst=dst: e.activation(out=dst[:], in_=cacc[:], func=AF.Silu), reads=["cacc"],
                  writes=[dk_])
            yield
        yield
        pn, pt = proj(c, 3)
        tk.op("act", lambda e, pt=pt: e.activation(out=zs[:], in_=pt[:, :], func=AF.Silu), reads=[pn], writes=[zk])
        yield
        for src, sk, dst, dk_, scale in ((qs, "qs", qn, "qn", 128.0 ** -0.5), (ks, "ks", kn, "kn", 1.0)):
            tk.op("act", lambda e, src=src: e.activation(out=sq[:], in_=src[:], func=AF.Square), reads=[sk],
                  writes=["sq"])
            pn, pt = pp.next()
            tk.op("pe", lambda e, pt=pt: e.matmul(pt[:, :], lhsT=ones_f[:], rhs=sq[:], start=True, stop=True),
                  reads=["sq", "ones_f"], writes=[pn])
            tk.op("act", lambda e, pt=pt: e.activation(out=rn[:], in_=pt[:, :], func=AF.Ln, bias=RMS_EPS_AP[:, 0:1],
                                                       scale=1.0), reads=[pn, "epsc"], writes=["rn"])
            tk.op("act", lambda e: e.activation(out=rn[:], in_=rn[:], func=AF.Exp, scale=-0.5), reads=["rn"],
                  writes=["rn"])
            tk.op("dve", lambda e, src=src, dst=dst, scale=scale: e.scalar_tensor_tensor(
                out=dst[:].bitcast(F32R), in0=src[:], scalar=scale, in1=rn[:], op0=ALU.mult, op1=ALU.mult),
                reads=[sk, "rn"], writes=[dk_])
        yield
        pn, pt = proj(c, 8)
        tk.op("act", lambda e, pt=pt: e.activation(out=betab[:], in_=pt[:, :], func=AF.Sigmoid), reads=[pn],
              writes=["betab"])
        pn, pt = proj(c, 9)
        tk.op("act", lambda e, pt=pt: e.activation(out=gb[:], in_=pt[:, :], func=AF.Exp, bias=pvec[:, 1:2], scale=1.0),
              reads=[pn, "pvec"], writes=["gb"])
        tk.op("act", lambda e: e.activation(out=gb[:], in_=gb[:], func=AF.Ln, bias=ONE_AP[:, 0:1], scale=1.0),
              reads=["gb", "epsc"], writes=["gb"])
        tk.op("dve", lambda e: e.tensor_scalar(out=gb[:], in0=gb[:], scalar1=nalog[:, 0:1], scalar2=None, op0=ALU.mult),
              reads=["gb", "nalog"], writes=["gb"])
        tk.op("dve", lambda e: e.tensor_tensor_scan(out=gcb[:], data0=scanm[:], data1=gb[:], initial=0.0,
                                                    op0=ALU.mult, op1=ALU.add), reads=["gb", "scanm"], writes=["gcb"])
        tk.op("act", lambda e: e.activation(out=egcb[:], in_=gcb[:], func=AF.Exp), reads=["gcb"], writes=[ek])
        gc3 = gcb[:].rearrange("p (n t) -> p n t", t=64)
        tk.op("dve", lambda e: e.tensor_tensor(out=eglb[:].rearrange("p (n t) -> p n t", t=64),
                                               in0=gc3[:, :, 63:64].to_broadcast([128, 8, 64]), in1=gc3,
                                               op=ALU.subtract), reads=["gcb"], writes=["eglb"])
        tk.op("act", lambda e: e.activation(out=eglb[:], in_=eglb[:], func=AF.Exp), reads=["eglb"], writes=["eglb"])
        tk.op("dve", lambda e: e.tensor_tensor(out=begb[:], in0=betab[:], in1=egcb[:], op=ALU.mult),
              reads=["betab", ek], writes=["gb"])
        pn, pt = pp.next()
        srcs = [(betab, "betab"), (gcb, "gcb"), (egcb, ek), (eglb, "eglb"), (begb, "gb")]
        tk.ops("pe", [lambda e, q=q, s=s, pt=pt, src=src: e.transpose(
            pt[:, (q * 4 + s) * 16:(q * 4 + s + 1) * 16], src[0:16, s * 128:(s + 1) * 128], ident[0:16, 0:16])
            for q, (src, _) in enumerate(srcs) for s in range(4)],
            reads=[k_ for _, k_ in srcs] + ["ident"], writes=[pn])
        tk.op("dve", lambda e, pt=pt: e.tensor_copy(
            out=cols[:].rearrange("p q s -> p (q s)"),
            in_=pt[:, 0:320].rearrange("p (n w) -> p n w", w=16)[:, :, 0]), reads=[pn], writes=["cols"])

        def colb(q):
            return cols[:, q, :].unsqueeze(2).to_broadcast([128, 4, 128])

        yield
        pn, pt = pp.next()
        tk.ops("pe", [lambda e, s=s, pt=pt: e.transpose(pt[:, s * 128:(s + 1) * 128], kn[:, s * 128:(s + 1) * 128],
                                                        ident[:]) for s in range(4)], reads=["kn", "ident"],
               writes=[pn])
        pt3 = pt[:, :].rearrange("p (s d) -> p s d", d=128)
        tk.op("dve", lambda e, pt3=pt3: e.tensor_tensor(out=Kbe[:].bitcast(F32R), in0=pt3, in1=colb(4), op=ALU.mult),
              reads=[pn, "cols"], writes=["Kbe"])
        tk.op("dve", lambda e, pt3=pt3: e.tensor_tensor(out=Kd[:], in0=pt3, in1=colb(3), op=ALU.mult),
              reads=[pn, "cols"], writes=[kdk])
        pn, pt = pp.next()
        tk.ops("pe", [lambda e, s=s, pt=pt: e.transpose(pt[:, s * 128:(s + 1) * 128], vs[:, s * 128:(s + 1) * 128],
                                                        ident[:]) for s in range(4)], reads=["vs", "ident"],
               writes=[pn])
        pt3 = pt[:, :].rearrange("p (s d) -> p s d", d=128)
        tk.op("dve", lambda e, pt3=pt3: e.tensor_tensor(out=Vb[:].bitcast(F32R), in0=pt3, in1=colb(0), op=ALU.mult),
              reads=[pn, "cols"], writes=["Vb"])
        yield
        gcb3 = gcb[:].rearrange("p (s t) -> p s t", t=128)
        tk.op("dve", lambda e: e.tensor_tensor(out=tdf[:], in0=gcb3, in1=colb(1), op=ALU.subtract),
              reads=["gcb", "cols"], writes=["tdf"])
        tk.op("dve", lambda e: e.tensor_scalar(out=DT[:], in0=tdf[:], scalar1=0.0, scalar2=None, op0=ALU.min),
              reads=["tdf"], writes=["DT"])
        tk.op("act", lambda e: e.activation(out=DT[:], in_=DT[:], func=AF.Exp), reads=["DT"], writes=["DT"])
        tk.op("dve", lambda e: e.tensor_scalar(out=Dm[:], in0=tdf[:], scalar1=0.0, scalar2=None, op0=ALU.max),
              reads=["tdf"], writes=["Dm"])
        tk.op("act", lambda e: e.activation(out=Dm[:], in_=Dm[:], func=AF.Exp, scale=-1.0), reads=["Dm"],
              writes=["Dm"])
        yield
        pnk, ptk = pp.next()
        tk.ops("pe", [lambda e, s=s, ptk=ptk: e.matmul(ptk[:, s * 128:(s + 1) * 128], lhsT=kn[:, s * 128:(s + 1) * 128].bitcast(F32R),
                                                       rhs=kn[:, s * 128:(s + 1) * 128].bitcast(F32R), start=True, stop=True)
                      for s in range(4)], reads=["kn"], writes=[pnk])
        pnq, ptq = pp.next()
        tk.ops("pe", [lambda e, s=s, ptq=ptq: e.matmul(ptq[:, s * 128:(s + 1) * 128], lhsT=kn[:, s * 128:(s + 1) * 128].bitcast(F32R),
                                                       rhs=qn[:, s * 128:(s + 1) * 128].bitcast(F32R), start=True, stop=True)
                      for s in range(4)], reads=["kn", "qn"], writes=[pnq])
        kk3 = ptk[:, :].rearrange("p (s d) -> p s d", d=128)
        qk3 = ptq[:, :].rearrange("p (s d) -> p s d", d=128)
        b3 = betab[:].rearrange("p (s t) -> p s t", t=128)
        tk.op("dve", lambda e: e.tensor_tensor(out=E1[:], in0=DT[:], in1=b3, op=ALU.mult), reads=["DT", "betab"],
              writes=["tdf"])
        tk.op("dve", lambda e: e.tensor_tensor(out=E1[:], in0=E1[:], in1=mus_neg, op=ALU.mult),
              reads=["tdf", "mus"], writes=["tdf"])
        tk.op("dve", lambda e: e.tensor_tensor(out=Bm[0][:].bitcast(F32R), in0=kk3, in1=E1[:], op=ALU.mult), reads=[pnk, "tdf"],
              writes=["Bm0"])
        tk.op("dve", lambda e: e.tensor_tensor(out=DT[:], in0=DT[:], in1=mu_inc, op=ALU.mult),
              reads=["DT", "mui", "tdf"], writes=["DT"])
        tk.op("dve", lambda e: e.tensor_tensor(out=QKT[:], in0=qk3, in1=DT[:], op=ALU.mult), reads=[pnq, "DT"],
              writes=[qkk])
        tk.op("dve", lambda e: e.tensor_tensor(out=Dm[:], in0=Dm[:], in1=mls_neg, op=ALU.mult),
              reads=["Dm", "mls"], writes=["Dm"])
        tk.op("dve", lambda e: e.tensor_tensor(out=Dm[:], in0=Dm[:], in1=colb(0), op=ALU.mult),
              reads=["Dm", "cols"], writes=["Dm"])
        tk.op("dve", lambda e: e.tensor_tensor(out=BTm[0][:].bitcast(F32R), in0=kk3, in1=Dm[:], op=ALU.mult), reads=[pnk, "Dm"],
              writes=["BTm0"])
        tk.op("dve", lambda e: e.tensor_tensor(out=Pm[0][:].bitcast(F32R), in0=Bm[0][:], in1=ident4, op=ALU.add),
              reads=["Bm0", "ident"], writes=["Pm0"])
        yield
        cb = 0
        cp = 0
        for k in range(1, 6):
            nb = 1 - cb
            if k < 5:
                pn1, pt1 = pp.next()
                tk.ops("pe", [lambda e, s=s, pt1=pt1, cb=cb: e.matmul(
                    pt1[:, s * 128:(s + 1) * 128], lhsT=BTm[cb][:, s, :].bitcast(F32R), rhs=Bm[cb][:, s, :].bitcast(F32R), start=True, stop=True)
                    for s in range(4)], reads=["Bm%d" % cb, "BTm%d" % cb], writes=[pn1])
            pn2, pt2 = pp.next()
            tk.ops("pe", [lambda e, s=s, pt2=pt2, cb=cb: e.matmul(
                pt2[:, s * 128:(s + 1) * 128], lhsT=Bm[cb][:, s, :].bitcast(F32R), rhs=BTm[cb][:, s, :].bitcast(F32R), start=True, stop=True)
                for s in range(4)], reads=["Bm%d" % cb, "BTm%d" % cb], writes=[pn2])
            if k < 5:
                tk.op("act", lambda e, pt1=pt1, nb=nb: e.copy(out=Bm[nb][:].rearrange("p s d -> p (s d)").bitcast(F32R), in_=pt1[:, :]),
                      reads=[pn1], writes=["Bm%d" % nb])
            tk.op("dve", lambda e, pt2=pt2, nb=nb: e.tensor_copy(out=BTm[nb][:].rearrange("p s d -> p (s d)").bitcast(F32R),
                                                                 in_=pt2[:, :]), reads=[pn2], writes=["BTm%d" % nb])
            pn3, pt3_ = pp.next()
            tk.ops("pe", [lambda e, s=s, pt3_=pt3_, nb=nb, cp=cp: e.matmul(
                pt3_[:, s * 128:(s + 1) * 128], lhsT=BTm[nb][:, s, :].bitcast(F32R), rhs=Pm[cp][:, s, :].bitcast(F32R), start=True, stop=True)
                for s in range(4)], reads=["BTm%d" % nb, "Pm%d" % cp], writes=[pn3])
            tk.op("dve", lambda e, pt3_=pt3_, cp=cp: e.tensor_tensor(
                out=Pm[1 - cp][:].rearrange("p s d -> p (s d)").bitcast(F32R), in0=Pm[cp][:].rearrange("p s d -> p (s d)"),
                in1=pt3_[:, :], op=ALU.add), reads=[pn3, "Pm%d" % cp], writes=["Pm%d" % (1 - cp)])
            cb = nb
            cp = 1 - cp
            yield
        Pf = Pm[cp]
        pk = "Pm%d" % cp
        yield
        pn, pt = pp.next()
        tk.ops("pe", [lambda e, s=s, pt=pt: e.matmul(pt[:, s * 128:(s + 1) * 128], lhsT=Pf[:, s, :].bitcast(F32R), rhs=Vb[:, s, :].bitcast(F32R),
                                                     start=True, stop=True) for s in range(4)],
               reads=[pk, "Vb"], writes=[pn])
        tk.op("act", lambda e, pt=pt: e.copy(out=U[:].rearrange("p s d -> p (s d)"), in_=pt[:, :]), reads=[pn],
              writes=[uk])
        pn, pt = pp.next()
        tk.ops("pe", [lambda e, s=s, pt=pt: e.matmul(pt[:, s * 128:(s + 1) * 128], lhsT=Kbe[:, s, :].bitcast(F32R), rhs=Pf[:, s, :].bitcast(F32R),
                                                     start=True, stop=True) for s in range(4)],
               reads=[pk, "Kbe"], writes=[pn])
        tk.op("act", lambda e, pt=pt: e.copy(out=WT[:], in_=pt[:, :]), reads=[pn], writes=[wk])
        tk.op("dve", lambda e: e.tensor_tensor(out=QdT[:], in0=qn[:], in1=egcb[:], op=ALU.mult),
              reads=["qn", ek], writes=[qdk])
        yield

    def prep_attn(c):
        ts = slice((c * T) % 4096, (c * T) % 4096 + T)
        rope_tables(c)
        yield
        pnx, ptx = proj(c, 7)
        for m, dstT, dk_, xo in ((4, qT, "qT", 0), (5, kT, "kT", 32)):
            pn, pt = proj(c, m)
            tk.op("act", lambda e, pt=pt, dstT=dstT: e.copy(out=dstT[32:64, ts], in_=pt[32:64, :]), reads=[pn],
                  writes=[K(dk_, c)])
            tk.op("act", lambda e, pt=pt, dstT=dstT: e.copy(out=dstT[64:128, ts], in_=pt[64:128, :]), reads=[pn],
                  writes=[K(dk_, c)])
            tk.op("dve", lambda e, pt=pt: e.tensor_tensor(out=rt1[:], in0=pt[0:32, :], in1=sincos[32:64, :],
                                                          op=ALU.mult), reads=[pn, "sincos"], writes=["rt1"])
            tk.op("dve", lambda e, ptx=ptx, xo=xo: e.tensor_tensor(out=rt2[:], in0=ptx[xo:xo + 32, :],
                                                                   in1=sincos[0:32, :], op=ALU.mult),
                  reads=[pnx, "sincos"], writes=["rt2"])
            tk.op("dve", lambda e, dstT=dstT: e.tensor_tensor(out=dstT[0:32, ts], in0=rt1[:], in1=rt2[:], op=ALU.add),
                  reads=["rt1", "rt2"], writes=[K(dk_, c)])
            yield
        pn, pt = proj(c, 6)
        tk.op("act", lambda e, pt=pt: e.copy(out=vT[:, ts], in_=pt[:, :]), reads=[pn], writes=[K("vT", c)])

        yield

    def scan(c):
        ts = slice(c * T, (c + 1) * T)
        par = c % 2
        U, WT, QdT, QKT, Kd, egcb, zs = U2[par], WT2[par], QdT2[par], QKT2[par], Kd2[par], egcb2[par], zs2[par]
        uk, wk, qdk, qkk, kdk, ek, zk = ("U%d" % par, "WT%d" % par, "QdT%d" % par, "QKT%d" % par, "Kd%d" % par,
                                         "egcb%d" % par, "zs%d" % par)
        ot = PS["ot"]
        sc_ = PS["scan"]
        for n in range(8):
            gi = c * 8 + n
            s, half = n // 2, n % 2
            r0 = 64 * half
            rows = slice(r0, r0 + 64)
            tsl = slice(64 * n, 64 * n + 64)
            Sc, Sn = S[gi % 2], S[(gi + 1) % 2]
            sk, snk = "S%d" % (gi % 2), "S%d" % ((gi + 1) % 2)
            slot = (gi % 2) * 256
            tk.op("pe", lambda e: e.matmul(sc_[rows, slot:slot + 128], lhsT=WT[:, tsl], rhs=Sc[:], start=True, stop=True),
                  reads=[wk, sk], writes=["ps_scan_a%d" % (gi % 2)])
            tk.op("dve", lambda e: e.tensor_tensor(out=vnew[rows, gi % 2, :], in0=U[rows, s, :],
                                                   in1=sc_[rows, slot:slot + 128], op=ALU.subtract),
                  reads=[uk, "ps_scan_a%d" % (gi % 2)], writes=["vnew%d" % (gi % 2)])
            tk.ops("pe", [
                lambda e: e.matmul(ot[:, tsl], lhsT=Sc[:], rhs=QdT[:, tsl], start=True, stop=False),
                lambda e: e.matmul(ot[:, tsl], lhsT=vnew[rows, gi % 2, :], rhs=QKT[rows, s, r0:r0 + 64], start=False,
                                   stop=True),
                lambda e: e.matmul(sc_[:, slot + 128:slot + 256], lhsT=Kd[rows, s, :], rhs=vnew[rows, gi % 2, :],
                                   start=True, stop=True)],
                reads=[sk, qdk, qkk, kdk, "vnew%d" % (gi % 2)], writes=["ps_ot", "ps_scan_b%d" % (gi % 2)])
            tl = 64 * n + 63
            tk.op("dve", lambda e: e.scalar_tensor_tensor(out=Sn[:], in0=Sc[:], scalar=egcb[:, tl:tl + 1],
                                                          in1=sc_[:, slot + 128:slot + 256], op0=ALU.mult, op1=ALU.add),
                  reads=[sk, ek, "ps_scan_b%d" % (gi % 2)], writes=[snk])
            yield
        tk.op("act", lambda e: e.activation(out=osq[:], in_=ot[:, :], func=AF.Square), reads=["ps_ot"], writes=["p1"])
        pn, pt = pp.next()
        tk.op("pe", lambda e: e.matmul(pt[:, :], lhsT=ones_f[:], rhs=osq[:], start=True, stop=True),
              reads=["p1", "ones_f"], writes=[pn])
        tk.op("act", lambda e: e.activation(out=rs[:], in_=pt[:, :], func=AF.Ln, bias=RMS_EPS_AP[:, 0:1],
                                            scale=1.0 / 128.0), reads=[pn, "epsc"], writes=["p2"])
        tk.op("act", lambda e: e.activation(out=rs[:], in_=rs[:], func=AF.Exp, scale=-0.5), reads=["p2"],
              writes=["p2"])
        tk.op("dve", lambda e: e.scalar_tensor_tensor(out=oa[:], in0=ot[:, :], scalar=pvec[:, 2:3], in1=rs[:],
                                                      op0=ALU.mult, op1=ALU.mult), reads=["ps_ot", "pvec", "p2"],
              writes=["p3"])
        tk.op("dve", lambda e: e.tensor_tensor(out=oab[:], in0=oa[:], in1=zs[:], op=ALU.mult), reads=["p3", zk],
              writes=["oab"])
        if cin is None:
            tk.dma("sp", lambda e: e.dma_start(out=mixT[0:128, ts], in_=oab[:]), reads=["oab"], writes=["mixT_a"])
        else:
            tk.dma("sp", lambda e: e.dma_start(out=cin[c // 4][0:128, (c % 4) * T:(c % 4 + 1) * T], in_=oab[:]),
                   reads=["oab"], writes=[("cin_a", c // 4)])
        yield

    att_scale = 128.0 ** -0.5
    pcount = [0]

    def qblock(qsel, kcur, kprev, vcur, vprev, vkeys, accv, first, rkeys):
        i = pcount[0] % 4
        pcount[0] += 1
        pTb = pTt[i]
        pk = "pT%d" % i
        pn, pt = ppa.next()
        if kprev is not None:
            tk.ops("pe", [
                lambda e: e.matmul(pt[:, 0:256], lhsT=ident_b[:], rhs=amask[:, 0:256], start=True, stop=False),
                lambda e: e.matmul(pt[:, 0:128], lhsT=kprev, rhs=qsel, start=False, stop=False),
                lambda e: e.matmul(pt[:, 128:256], lhsT=kcur, rhs=qsel, start=False, stop=True)],
                reads=rkeys + ["ident_b", "amask"], writes=[pn])
            tk.op("act", lambda e: e.activation(out=pTb[:, 0:256], in_=pt[:, 0:256], func=AF.Exp, scale=att_scale),
                  reads=[pn], writes=[pk])
            pn2, pt2 = ppa.next()
            tk.ops("pe", [
                lambda e: e.matmul(pt2[:, 0:128], lhsT=vprev, rhs=pTb[:, 0:128], start=True, stop=False),
                lambda e: e.matmul(pt2[:, 0:128], lhsT=vcur, rhs=pTb[:, 128:256], start=False, stop=True),
                lambda e: e.matmul(pt2[:, 128:256], lhsT=ones_b[:], rhs=pTb[:, 0:128], start=True, stop=False),
                lambda e: e.matmul(pt2[:, 128:256], lhsT=ones_b[:], rhs=pTb[:, 128:256], start=False, stop=True)],
                reads=[pk, "ones_b"] + vkeys, writes=[pn2])
        else:
            tk.ops("pe", [
                lambda e: e.matmul(pt[:, 128:256], lhsT=ident_b[:], rhs=amask[:, 128:256], start=True, stop=False),
                lambda e: e.matmul(pt[:, 128:256], lhsT=kcur, rhs=qsel, start=False, stop=True)],
                reads=rkeys + ["ident_b", "amask"], writes=[pn])
            tk.op("act", lambda e: e.activation(out=pTb[:, 128:256], in_=pt[:, 128:256], func=AF.Exp, scale=att_scale),
                  reads=[pn], writes=[pk])
            pn2, pt2 = ppa.next()
            tk.ops("pe", [
                lambda e: e.matmul(pt2[:, 0:128], lhsT=vcur, rhs=pTb[:, 128:256], start=True, stop=True),
                lambda e: e.matmul(pt2[:, 128:256], lhsT=ones_b[:], rhs=pTb[:, 128:256], start=True, stop=True)],
                reads=[pk, "ones_b"] + vkeys, writes=[pn2])
        src = pt2[:, 0:256].rearrange("p (a q) -> p a q", a=2)
        if first:
            tk.op("dve", lambda e: e.tensor_copy(out=accv, in_=src), reads=[pn2], writes=["acc"])
        else:
            tk.op("dve", lambda e: e.tensor_tensor(out=accv, in0=accv, in1=src, op=ALU.add), reads=[pn2, "acc"],
                  writes=["acc"])

    vcnt = [0]

    def vtrans(vsel, rk):
        i = vcnt[0] % 8
        vcnt[0] += 1
        pn, pt = ppa.next()
        ptb = pt[:, 0:64].bitcast(BF16)
        tk.op("pe", lambda e: e.transpose(ptb, vsel, ident_b[:]), reads=rk + ["ident_b"], writes=[pn])
        tk.op("act", lambda e: e.copy(out=vtt[:, i, :], in_=ptb), reads=[pn], writes=["vtt%d" % i])
        return vtt[:, i, :], "vtt%d" % i

    def rsl(t0, n, step=1):
        r0 = t0 % RING
        return slice(r0, r0 + (n - 1) * step + 1, step)

    def attn(c):
        sc = c // 4
        for jj in range(4):
            j = 4 * c + jj
            t0 = 128 * j
            cprev = (t0 - 128) // T
            vc, vck = vtrans(vT[:, rsl(t0, 128)], [K("vT", c)])
            if j > 0:
                vp, vpk = vtrans(vT[:, rsl(t0 - 128, 128)], [K("vT", cprev)])
            else:
                vp, vpk = None, None
            rk = [K("qT", c), K("kT", c)] + ([K("kT", cprev)] if j > 0 else [])
            qblock(qT[:, rsl(t0, 128)], kT[:, rsl(t0, 128)], kT[:, rsl(t0 - 128, 128)] if j > 0 else None,
                   vc, vp, [k_ for k_ in (vck, vpk) if k_], acc[:, :, (t0 % 2048):(t0 % 2048) + 128], True, rk)
            yield
        for r in range(4):
            t0 = T * c + r
            vc, vck = vtrans(vT[:, rsl(t0, 128, 4)], [K("vT", c)])
            if c > 0:
                vp, vpk = vtrans(vT[:, rsl(t0 - T, 128, 4)], [K("vT", c - 1)])
            else:
                vp, vpk = None, None
            rk = [K("qT", c), K("kT", c)] + ([K("kT", c - 1)] if c > 0 else [])
            a0 = (T * c) % 2048 + r
            qblock(qT[:, rsl(t0, 128, 4)], kT[:, rsl(t0, 128, 4)], kT[:, rsl(t0 - T, 128, 4)] if c > 0 else None,
                   vc, vp, [k_ for k_ in (vck, vpk) if k_], acc[:, :, a0:a0 + 509:4], False, rk)
            yield
        if c % 4 == 3:
            cs = [4 * sc + i for i in range(4)]
            csp = [4 * (sc - 1) + i for i in range(4)] if sc > 0 else []
            for r in range(16):
                t0 = 2048 * sc + r
                vc, vck = vtrans(vT[:, rsl(t0, 128, 16)], [K("vT", ci) for ci in cs])
                if sc > 0:
                    vp, vpk = vtrans(vT[:, rsl(t0 - 2048, 128, 16)], [K("vT", ci) for ci in csp])
                else:
                    vp, vpk = None, None
                rk = [K("qT", ci) for ci in cs] + [K("kT", ci) for ci in cs] + [K("kT", ci) for ci in csp]
                qblock(qT[:, rsl(t0, 128, 16)], kT[:, rsl(t0, 128, 16)],
                       kT[:, rsl(t0 - 2048, 128, 16)] if sc > 0 else None,
                       vc, vp, [k_ for k_ in (vck, vpk) if k_], acc[:, :, r:r + 2033:16], False, rk)
                yield
            for i in range(4):
                a = slice(i * T, (i + 1) * T)
                tsl = slice(2048 * sc + i * T, 2048 * sc + (i + 1) * T)
                tk.op("act", lambda e, a=a: e.activation(out=fr[:], in_=acc[:, 1, a], func=AF.Ln), reads=["acc"],
                      writes=["p2"])
                tk.op("act", lambda e: e.activation(out=fr[:], in_=fr[:], func=AF.Exp, scale=-1.0), reads=["p2"],
                      writes=["p2"])
                tk.op("dve", lambda e, a=a: e.tensor_tensor(out=fo[:], in0=acc[:, 0, a], in1=fr[:], op=ALU.mult),
                      reads=["acc", "p2"], writes=["p3"])
                tk.op("act", lambda e: e.activation(out=fsq[:], in_=fo[:], func=AF.Square), reads=["p3"],
                      writes=["p1"])
                pn, pt = pp.next()
                tk.op("pe", lambda e, pt=pt: e.matmul(pt[:, :], lhsT=ones_f[:], rhs=fsq[:], start=True, stop=True),
                      reads=["p1", "ones_f"], writes=[pn])
                tk.op("act", lambda e, pt=pt: e.activation(out=fr[:], in_=pt[:, :], func=AF.Ln,
                                                           bias=RMS_EPS_AP[:, 0:1], scale=1.0 / 128.0),
                      reads=[pn, "epsc"], writes=["p2"])
                tk.op("act", lambda e: e.activation(out=fr[:], in_=fr[:], func=AF.Exp, scale=-0.5), reads=["p2"],
                      writes=["p2"])
                tk.op("dve", lambda e: e.scalar_tensor_tensor(out=fob[:], in0=fo[:], scalar=pvec[:, 3:4], in1=fr[:],
                                                              op0=ALU.mult, op1=ALU.mult), reads=["p3", "pvec", "p2"],
                      writes=["fob"])
                yield
                if cin is None:
                    tk.dma("sp", lambda e, tsl=tsl: e.dma_start(out=mixT[128:256, tsl], in_=fob[:]), reads=["fob"],
                           writes=["mixT_b"])
                else:
                    tk.dma("sp", lambda e, i=i: e.dma_start(out=cin[sc][128:256, i * T:(i + 1) * T], in_=fob[:]),
                           reads=["fob"], writes=[("cin_b", sc)])
            if on_quarter is not None:
                on_quarter(sc, tk)
        yield

    epsc = sb("epsc", [128, 2])
    tk.op("pool", lambda e: e.memset(epsc[:, 0:1], RMS_EPS), writes=["epsc"])
    tk.op("pool", lambda e: e.memset(epsc[:, 1:2], 1.0), writes=["epsc"])
    RMS_EPS_AP = epsc[:, 0:1]
    ONE_AP = epsc[:, 1:2]

    def run(gen):
        if gen is not None:
            for _ in gen:
                pass

    load_x(0)
    run(prep_main(0))
    tk.flush()
    for c in range(nch):
        run(scan(c))
        run(prep_attn(c))
        run(attn(c))
        if c + 1 < nch:
            run(prep_main(c + 1))
        tk.flush()
    tk = real_tk


def build_phase1(nch=NCH):
    nc = bass.Bass("TRN2", target_bir_lowering=False)
    D = {}

    def din(name, shape, dt=F32):
        D[name] = nc.dram_tensor(name, shape, dt, kind="ExternalInput").ap()

    din("x", [D_MODEL, SEQ]); din("cT", [128, 8]); din("pos", [SEQ], I32)
    din("wada1", [128, 8, 8, 256]); din("bada1", [128, 16]); din("win", [128, 5, 8, 256])
    din("convw", [128, 3, 4]); din("pvec", [128, 4])
    din("c_ident", [128, 128]); din("c_masks", [128, 3, 128]); din("c_amask", [128, 256]); din("c_invf", [64, 2])
    D["mixT"] = nc.dram_tensor("mixT", [256, SEQ], BF16, kind="ExternalOutput").ap()
    with ExitStack() as es:
        tk = Trk(nc, es)
        phase1(nc, tk, es, D, nch=nch)
        tk.finish("sp")
    return nc


def consts():
    p = np.arange(128)[:, None]
    f = np.arange(128)[None, :]
    same = (p // 64) == (f // 64)
    mus = -((f > p) & same).astype(np.float32)
    mui = ((f >= p) & same).astype(np.float32)
    mls = -((f < p) & same).astype(np.float32)
    masks = np.stack([mus, mui, mls], 1).astype(np.float32)
    prev = np.where(p >= f, 0.0, NEG).astype(np.float32)
    cur = np.where(p <= f, 0.0, NEG).astype(np.float32)
    amask = np.concatenate([prev, cur], 1)
    i = (np.arange(64) % 16).astype(np.float32)
    invf = (ROPE_THETA ** (-i * 2.0 / 32.0)).astype(np.float32)
    invf = np.stack([invf, np.where(np.arange(64) < 32, 0.0, math.pi / 2).astype(np.float32)], 1)
    return {"c_ident": np.eye(128, dtype=np.float32), "c_masks": masks, "c_amask": amask, "c_invf": invf}


def phase1_inputs(inp, core):
    b, h = core // 4, core % 4
    l = 0
    w_in = inp["w_in"][l]
    hs = slice(h * 128, (h + 1) * 128)
    qa, ka, va, z = w_in[:, 0:512][:, hs], w_in[:, 512:1024][:, hs], w_in[:, 1024:1536][:, hs], w_in[:, 1536:2048][:, hs]
    beta = w_in[:, 2048 + h:2049 + h]
    dec = w_in[:, 2052 + h:2053 + h]
    qb, kb, vb = w_in[:, 2056:2568][:, hs], w_in[:, 2568:3080][:, hs], w_in[:, 3080:3592][:, hs]
    extra = np.zeros((D_MODEL, 128), np.float32)
    extra[:, 0:16] = qb[:, 16:32]; extra[:, 16:32] = qb[:, 0:16]
    extra[:, 32:48] = kb[:, 16:32]; extra[:, 48:64] = kb[:, 0:16]
    win = np.concatenate([qa, ka, va, z, qb, kb, vb, extra, np.repeat(beta, 128, 1), np.repeat(dec, 128, 1)], 1)
    conv = inp["conv_w"][l]
    convw = np.stack([conv[:, g * 512:(g + 1) * 512][:, hs].T for g in range(3)], 1)
    pvec = np.stack([np.full(128, inp["a_log"][l, h]), np.full(128, inp["dt_bias"][l, h]),
                     inp["gdn_norm_w"][l], inp["attn_norm_w"][l]], 1).astype(np.float32)
    win_l = win.astype(np.float32).reshape(8, 128, 5, 256).transpose(1, 2, 0, 3)
    wada1_l = inp["w_ada"][l][:, 0:2048].reshape(8, 128, 8, 256).transpose(1, 2, 0, 3)
    d = {"x": np.ascontiguousarray(inp["x"][b].T), "cT": np.ascontiguousarray(inp["c"][b].reshape(8, 128).T),
         "pos": np.ascontiguousarray(inp["positions"][b]).astype(np.int32),
         "wada1": np.ascontiguousarray(wada1_l),
         "bada1": np.ascontiguousarray(inp["b_ada"][l][0:2048].reshape(16, 128).T),
         "win": np.ascontiguousarray(win_l), "convw": np.ascontiguousarray(convw.astype(np.float32)),
         "pvec": np.ascontiguousarray(pvec)}
    d.update(consts())
    return d


NT2 = 16
NBLK = 64


def phase2(nc, tk, es, D, ntile=NT2, nblk=NBLK, cout=None):
    real = tk
    tk = Rec(real)

    def mk(stack, pref):
        def sb(name, shape, dt=F32):
            return stack.enter_context(nc.sbuf_tensor(pref + name, shape, dt))
        return sb

    sb = mk(es, "t_")
    pp = PsumPool(nc, es, ["r0", "r1", "r2", "r3", "r4", "r5", "r6", "r7"])

    ident = sb("ident", [128, 128])
    ones_f = sb("ones_f", [128, 128])
    triS = sb("triS", [128, 128])
    kcoff = sb("kcoff", [128, 1])
    thr16 = sb("thr16", [128, 16])
    thr64 = sb("thr64", [128, 64])
    epsc = sb("epsc", [128, 1])
    cT = sb("cT", [128, 8]); scT = sb("scT", [128, 8])
    modb = sb("modb", [128, 4, 1024])
    lnp = sb("lnp", [128, 4, 1024])
    xt2 = [sb("xt%d" % i, [128, 1024]) for i in range(2)]
    rr2 = [sb("rr%d" % i, [128, 1024]) for i in range(2)]
    x12 = [sb("x1%d" % i, [128, 1024]) for i in range(2)]
    h22 = [sb("h2%d" % i, [128, 1024]) for i in range(2)]
    stt = sb("stt", [128, 2, 6]); mv = sb("mv", [128, 2]); rstd = sb("rstd", [128, 1])
    gts = sb("gts", [128, NT2, 2])
    desti = sb("desti", [128, 2, NT2], I32)
    idxw = sb("idxw", [128, 64], I32)

    x = D["x2"]; mixTf = D.get("mixTf"); out = D["out"]
    x1d = D["x1d"]; h2d = D["h2d"]; xs = D["xs"]; ys = D["ys"]

    def layer_norm(src, skey, dst, dkey, gi):
        tk.op("dve", lambda e: e.bn_stats(out=stt[:, 0, :], in_=src[:, 0:512]), reads=[skey], writes=["stt"])
        tk.op("dve", lambda e: e.bn_stats(out=stt[:, 1, :], in_=src[:, 512:1024]), reads=[skey], writes=["stt"])
        tk.op("dve", lambda e: e.bn_aggr(out=mv[:], in_=stt[:].rearrange("p a b -> p (a b)")), reads=["stt"],
              writes=["mv"])
        tk.op("act", lambda e: e.activation(out=rstd[:], in_=mv[:, 1:2], func=AF.Sqrt, bias=epsc[:, 0:1], scale=1.0),
              reads=["mv", "epsc"], writes=["rstd"])
        tk.op("dve", lambda e: e.reciprocal(out=rstd[:], in_=rstd[:]), reads=["rstd"], writes=["rstd"])
        tk.op("dve", lambda e: e.tensor_scalar(out=dst[:], in0=src[:], scalar1=mv[:, 0:1], scalar2=rstd[:, 0:1],
                                               op0=ALU.subtract, op1=ALU.mult), reads=[skey, "mv", "rstd"],
              writes=[dkey])
        tk.op("dve", lambda e: e.tensor_tensor(out=dst[:], in0=dst[:], in1=lnp[:, gi, :], op=ALU.mult),
              reads=[dkey, "lnp"], writes=[dkey])
        tk.op("dve", lambda e: e.tensor_tensor(out=dst[:], in0=dst[:], in1=lnp[:, gi + 1, :], op=ALU.add),
              reads=[dkey, "lnp"], writes=[dkey])

    tk.dma("sp", lambda e: e.dma_start(out=ident[:], in_=D["c_ident"][:, :]), writes=["ident"])
    tk.dma("sp", lambda e: e.dma_start(out=triS[:], in_=D["c_tri"][:, :]), writes=["triS"])
    tk.dma("sp", lambda e: e.dma_start(out=kcoff[:], in_=D["c_iota"][:, :]), writes=["kcoff"])
    tk.dma("sp", lambda e: e.dma_start(out=thr16[:], in_=D["c_thr16"][:, :]), writes=["thr16"])
    tk.dma("sp", lambda e: e.dma_start(out=thr64[:], in_=D["c_thr64"][:, :]), writes=["thr64"])
    tk.dma("sp", lambda e: e.dma_start(out=cT[:], in_=D["cT"][:, :]), writes=["cT"])
    for i in range(4):
        tk.dma("sp", lambda e, i=i: e.dma_start(out=lnp[:, i, :], in_=D["lnp"][i, :].partition_broadcast(128)),
               writes=["lnp"])
    tk.op("pool", lambda e: e.memset(ones_f[:], 1.0), writes=["ones_f"])
    tk.op("pool", lambda e: e.memset(epsc[:], LN_EPS), writes=["epsc"])

    with ExitStack() as esA:
        sa = mk(esA, "a_")
        scR = sa("scR", [128, 8, 128])
        badab = sa("badab", [128, 512])
        wo_b = sa("wo_b", [128, 8, 1024], BF16)
        wr = sa("wr", [128, 8, 36]); brb = sa("brb", [128, 36])
        h2T2 = [sa("h2T%d" % i, [128, 8, 128]) for i in range(2)]
        lg = sa("lg", [128, 36]); gmax = sa("gmax", [128, 1]); ngmax = sa("ngmax", [128, 1]); ohg = sa("ohg", [128, 4])
        ex4 = sa("ex4", [128, 4]); sume = sa("sume", [128, 1]); ggrp = sa("ggrp", [128, 1])
        tmp48 = sa("tmp48", [128, 4, 8]); lesel = sa("lesel", [128, 8]); m8 = sa("m8", [128, 8])
        oh8 = sa("oh8", [128, 2, 8]); e2 = sa("e2", [128, 1]); g1 = sa("g1", [128, 1])
        ohs = sa("ohs", [128, NT2, 2, 32]); ohany = sa("ohany", [128, 32]); cum = sa("cum", [128, 32])
        ranks = sa("ranks", [128, NT2, 32])
        cntb = sa("cntb", [128, 32]); cmp16 = sa("cmp16", [128, 32, 16]); padded = sa("padded", [128, 32])
        onesr = sa("onesr", [128, 32]); padend = sa("padend", [128, 32]); padstart = sa("padstart", [128, 32])
        tmpd = sa("tmpd", [128, NT2, 32]); tmpd2 = sa("tmpd2", [128, NT2, 32])
        destf = sa("destf", [128, 2, NT2])
        cmp64 = sa("cmp64", [128, 64, 32]); blkf = sa("blkf", [128, 64])

        tk.dma("sp", lambda e: e.dma_start(out=wr[:], in_=D["wr"][:, :].rearrange("(kc p) c -> p kc c", p=128)),
               writes=["wr"])
        tk.dma("sp", lambda e: e.dma_start(out=brb[:], in_=D["br"][:].partition_broadcast(128)), writes=["brb"])
        tk.op("pool", lambda e: e.memset(cum[:], 0.0), writes=["cum"])
        tk.op("pool", lambda e: e.memset(onesr[:], 1.0), writes=["onesr"])
        if cout is None:
            mt1 = sa("mt", [128, 8, 128], BF16)
        else:
            mtf = sa("mtf", [128, 8, 2048], BF16)
            qidx = sa("qidx", [128, 8], I32)
            tk.dma("sp", lambda e: e.dma_start(out=qidx[:], in_=D["qidx"][:, :]), writes=["qidx"])
            for kc in range(8):
                tk.dma("pool", lambda e, kc=kc: e.indirect_dma_start(
                    out=mtf[:, kc, :], out_offset=None, in_=cout[:, :],
                    in_offset=bass.IndirectOffsetOnAxis(ap=qidx[:, kc:kc + 1], axis=0)),
                    reads=["qidx"] + [("cout", q_) for q_ in range(4)], writes=["mt"])
        stg = [(xt2[0], "xt0"), (rr2[0], "rr0"), (xt2[1], "xt1"), (rr2[1], "rr1")]
        for kc in range(8):
            st, key = stg[kc % 4]
            tk.dma("sp", lambda e, st=st, kc=kc: e.dma_start(out=st[:], in_=D["wo"][kc * 128:(kc + 1) * 128, :]),
                   writes=[key])
            tk.op("dve", lambda e, st=st, kc=kc: e.tensor_copy(out=wo_b[:, kc, :], in_=st[:]), reads=[key],
                  writes=["wo_b"])
        tk.op("act", lambda e: e.activation(out=scT[:], in_=cT[:], func=AF.Silu), reads=["cT"], writes=["scT"])
        tk.op("dve", lambda e: e.tensor_copy(out=scR[:], in_=scT[:].unsqueeze(2).to_broadcast([128, 8, 128])),
              reads=["scT"], writes=["scR"])
        for blk in range(8):
            pn, pt = pp.next()
            for sub in range(4):
                st, key = stg[sub % 4]
                st3 = st[:].rearrange("p (kc c) -> p kc c", c=128)
                c0 = blk * 512 + sub * 128
                tk.dma("sp" if sub % 2 == 0 else "pool", lambda e, st3=st3, blk=blk, sub=sub: e.dma_start(
                    out=st3, in_=D["wada2"][:, blk * 4 + sub, :, :]), writes=[key])
                tk.ops("pe", [lambda e, kc=kc, pt=pt, st3=st3, sub=sub: e.matmul(
                    pt[:, sub * 128:(sub + 1) * 128], lhsT=scR[:, kc, :], rhs=st3[:, kc, :], start=(kc == 0),
                    stop=(kc == 7)) for kc in range(8)], reads=[key, "scR"], writes=[pn])
            tk.dma("sp", lambda e, blk=blk: e.dma_start(
                out=badab[:], in_=D["bada2"][blk * 512:(blk + 1) * 512].partition_broadcast(128)), writes=["badab"])
            dst = modb[:, blk // 2, (blk % 2) * 512:(blk % 2 + 1) * 512]
            tk.op("dve", lambda e, pt=pt, dst=dst: e.tensor_tensor(out=dst, in0=pt[:, :], in1=badab[:], op=ALU.add),
                  reads=[pn, "badab"], writes=["modb"])
            if blk // 2 != 1:
                tk.op("dve", lambda e, dst=dst: e.tensor_scalar(out=dst, in0=dst, scalar1=1.0, scalar2=None,
                                                                op0=ALU.add), reads=["modb"], writes=["modb"])
        tk.flush()

        for k in range(ntile):
            par = k % 2
            xt, rr, x1, h2, h2T = xt2[par], rr2[par], x12[par], h22[par], h2T2[par]
            xtk, rrk, x1k, h2k, h2Tk = "xt%d" % par, "rr%d" % par, "x1%d" % par, "h2%d" % par, "h2T%d" % par
            tsl = slice(k * 128, (k + 1) * 128)
            if cout is None:
                mt = mt1
                tk.dma("sp", lambda e: e.dma_start(out=mt[:], in_=mixTf[:, tsl].rearrange("(kc p) t -> p kc t", p=128)),
                       writes=["mt"])
            else:
                mt = mtf[:, :, tsl]
            tk.dma("sp", lambda e: e.dma_start(out=xt[:], in_=x[tsl, :]), writes=[xtk])
            pa, pta = pp.next()
            pb, ptb = pp.next()
            for (pn, pt, hs) in ((pa, pta, slice(0, 512)), (pb, ptb, slice(512, 1024))):
                tk.ops("pe", [lambda e, kc=kc, pt=pt, hs=hs: e.matmul(pt[:, :], lhsT=mt[:, kc, :], rhs=wo_b[:, kc, hs],
                                                                     start=(kc == 0), stop=(kc == 7))
                              for kc in range(8)], reads=["mt", "wo_b"], writes=[pn])
                tk.op("dve", lambda e, pt=pt, hs=hs: e.tensor_tensor(out=rr[:, hs], in0=pt[:, :], in1=modb[:, 0, hs],
                                                                     op=ALU.mult), reads=[pn, "modb"], writes=[rrk])
            tk.op("dve", lambda e: e.scalar_tensor_tensor(out=rr[:], in0=xt[:], scalar=ALPHA, in1=rr[:], op0=ALU.mult,
                                                          op1=ALU.add), reads=[xtk, rrk], writes=[rrk])
            layer_norm(rr, rrk, x1, x1k, 0)
            tk.dma("sp", lambda e: e.dma_start(out=x1d[tsl, :], in_=x1[:]), reads=[x1k], writes=["x1d"])
            tk.op("dve", lambda e: e.tensor_tensor(out=h2[:], in0=x1[:], in1=modb[:, 2, :], op=ALU.mult),
                  reads=[x1k, "modb"], writes=[h2k])
            tk.op("dve", lambda e: e.tensor_tensor(out=h2[:], in0=h2[:], in1=modb[:, 1, :], op=ALU.add),
                  reads=[h2k, "modb"], writes=[h2k])
            tk.dma("sp", lambda e: e.dma_start(out=h2d[tsl, :], in_=h2[:]), reads=[h2k], writes=["h2d"])
            for half in range(2):
                pn, pt = pp.next()
                tk.ops("pe", [lambda e, j=j, pt=pt, half=half: e.transpose(
                    pt[:, j * 128:(j + 1) * 128], h2[:, (half * 4 + j) * 128:(half * 4 + j + 1) * 128], ident[:])
                    for j in range(4)], reads=[h2k, "ident"], writes=[pn])
                tk.op("act", lambda e, pt=pt, half=half: e.copy(
                    out=h2T[:, half * 4:(half + 1) * 4, :].rearrange("p a b -> p (a b)"), in_=pt[:, :]), reads=[pn],
                    writes=[h2Tk])
            pn, pt = pp.next()
            tk.ops("pe", [lambda e, kc=kc, pt=pt: e.matmul(pt[:, 0:36], lhsT=h2T[:, kc, :], rhs=wr[:, kc, :],
                                                          start=(kc == 0), stop=(kc == 7)) for kc in range(8)],
                   reads=[h2Tk, "wr"], writes=[pn])
            tk.op("dve", lambda e, pt=pt: e.tensor_tensor(out=lg[:], in0=pt[:, 0:36], in1=brb[:], op=ALU.add),
                  reads=[pn, "brb"], writes=["lg"])
            tk.op("dve", lambda e: e.tensor_reduce(out=gmax[:], in_=lg[:, 0:4], axis=AX.X, op=ALU.max), reads=["lg"],
                  writes=["gmax"])
            tk.op("dve", lambda e: e.tensor_scalar(out=ohg[:], in0=lg[:, 0:4], scalar1=gmax[:, 0:1], scalar2=None,
                                                   op0=ALU.is_equal), reads=["lg", "gmax"], writes=["ohg"])
            tk.op("dve", lambda e: e.tensor_scalar(out=ngmax[:], in0=gmax[:], scalar1=-1.0, scalar2=None, op0=ALU.mult),
                  reads=["gmax"], writes=["ngmax"])
            tk.op("act", lambda e: e.activation(out=ex4[:], in_=lg[:, 0:4], func=AF.Exp, bias=ngmax[:, 0:1], scale=1.0),
                  reads=["lg", "ngmax"], writes=["ex4"])
            tk.op("dve", lambda e: e.tensor_reduce(out=sume[:], in_=ex4[:], axis=AX.X, op=ALU.add), reads=["ex4"],
                  writes=["sume"])
            tk.op("dve", lambda e: e.reciprocal(out=ggrp[:], in_=sume[:]), reads=["sume"], writes=["ggrp"])
            le3 = lg[:, 4:36].rearrange("p (g e) -> p g e", e=8)
            tk.op("dve", lambda e: e.tensor_tensor(out=tmp48[:], in0=le3,
                                                   in1=ohg[:].unsqueeze(2).to_broadcast([128, 4, 8]), op=ALU.mult),
                  reads=["lg", "ohg"], writes=["tmp48"])
            tk.op("dve", lambda e: e.tensor_reduce(out=lesel[:], in_=tmp48[:].rearrange("p g e -> p e g"), axis=AX.X,
                                                   op=ALU.add), reads=["tmp48"], writes=["lesel"])
            tk.op("dve", lambda e: e.max(out=m8[:], in_=lesel[:]), reads=["lesel"], writes=["m8"])
            for j in range(2):
                tk.op("dve", lambda e, j=j: e.tensor_scalar(out=oh8[:, j, :], in0=lesel[:], scalar1=m8[:, j:j + 1],
                                                            scalar2=None, op0=ALU.is_equal), reads=["lesel", "m8"],
                      writes=["oh8"])
            tk.op("dve", lambda e: e.tensor_tensor(out=e2[:], in0=m8[:, 1:2], in1=m8[:, 0:1], op=ALU.subtract),
                  reads=["m8"], writes=["e2"])
            tk.op("act", lambda e: e.activation(out=e2[:], in_=e2[:], func=AF.Exp), reads=["e2"], writes=["e2"])
            tk.op("dve", lambda e: e.tensor_scalar(out=g1[:], in0=e2[:], scalar1=1.0, scalar2=None, op0=ALU.add),
                  reads=["e2"], writes=["g1"])
            tk.op("dve", lambda e: e.reciprocal(out=g1[:], in_=g1[:]), reads=["g1"], writes=["g1"])
            tk.op("dve", lambda e: e.tensor_tensor(out=gts[:, k, 0:1], in0=g1[:], in1=ggrp[:], op=ALU.mult),
                  reads=["g1", "ggrp"], writes=["gts"])
            tk.op("dve", lambda e: e.tensor_tensor(out=e2[:], in0=e2[:], in1=g1[:], op=ALU.mult), reads=["e2", "g1"],
                  writes=["e2"])
            tk.op("dve", lambda e: e.tensor_tensor(out=gts[:, k, 1:2], in0=e2[:], in1=ggrp[:], op=ALU.mult),
                  reads=["e2", "ggrp"], writes=["gts"])
            for j in range(2):
                tk.op("dve", lambda e, j=j: e.tensor_tensor(
                    out=ohs[:, k, j, :].rearrange("p (g e) -> p g e", e=8),
                    in0=ohg[:].unsqueeze(2).to_broadcast([128, 4, 8]),
                    in1=oh8[:, j, :].unsqueeze(1).to_broadcast([128, 4, 8]), op=ALU.mult), reads=["ohg", "oh8"],
                    writes=["ohs"])
            tk.op("dve", lambda e: e.tensor_tensor(out=ohany[:], in0=ohs[:, k, 0, :], in1=ohs[:, k, 1, :], op=ALU.add),
                  reads=["ohs"], writes=["ohany"])
            pn, pt = pp.next()
            tk.ops("pe", [lambda e, pt=pt: e.matmul(pt[:, 0:32], lhsT=triS[:], rhs=ohany[:], start=True, stop=False),
                          lambda e, pt=pt: e.matmul(pt[:, 0:32], lhsT=ones_f[:], rhs=cum[:], start=False, stop=True)],
                   reads=["triS", "ohany", "ones_f", "cum"], writes=[pn])
            tk.op("act", lambda e, pt=pt: e.copy(out=ranks[:, k, :], in_=pt[:, 0:32]), reads=[pn], writes=["ranks"])
            tk.op("dve", lambda e: e.tensor_tensor(out=cum[:], in0=cum[:], in1=ohany[:], op=ALU.add),
                  reads=["cum", "ohany"], writes=["cum"])
            if k % 4 == 3:
                tk.flush()

        pn, pt = pp.next()
        tk.op("pe", lambda e: e.matmul(pt[:, 0:32], lhsT=ones_f[:], rhs=cum[:], start=True, stop=True),
              reads=["ones_f", "cum"], writes=[pn])
        tk.op("dve", lambda e: e.tensor_copy(out=cntb[:], in_=pt[:, 0:32]), reads=[pn], writes=["cntb"])
        tk.op("dve", lambda e: e.tensor_tensor(out=cmp16[:], in0=cntb[:].unsqueeze(2).to_broadcast([128, 32, 16]),
                                               in1=thr16[:].unsqueeze(1).to_broadcast([128, 32, 16]), op=ALU.is_gt),
              reads=["cntb", "thr16"], writes=["cmp16"])
        tk.op("dve", lambda e: e.tensor_reduce(out=padded[:], in_=cmp16[:], axis=AX.X, op=ALU.add), reads=["cmp16"],
              writes=["padded"])
        tk.op("dve", lambda e: e.tensor_scalar(out=padded[:], in0=padded[:], scalar1=128.0, scalar2=None, op0=ALU.mult),
              reads=["padded"], writes=["padded"])
        tk.op("dve", lambda e: e.tensor_tensor_scan(out=padend[:], data0=onesr[:], data1=padded[:], initial=0.0,
                                                    op0=ALU.mult, op1=ALU.add), reads=["onesr", "padded"],
              writes=["padend"])
        tk.op("dve", lambda e: e.tensor_tensor(out=padstart[:], in0=padend[:], in1=padded[:], op=ALU.subtract),
              reads=["padend", "padded"], writes=["padstart"])
        tk.op("dve", lambda e: e.tensor_tensor(out=tmpd[:, 0:ntile, :], in0=ranks[:, 0:ntile, :],
                                               in1=padstart[:].unsqueeze(1).to_broadcast([128, ntile, 32]), op=ALU.add),
              reads=["ranks", "padstart"], writes=["tmpd"])
        for j in range(2):
            tk.op("dve", lambda e, j=j: e.tensor_tensor(out=tmpd2[:, 0:ntile, :], in0=tmpd[:, 0:ntile, :],
                                                        in1=ohs[:, 0:ntile, j, :], op=ALU.mult), reads=["tmpd", "ohs"],
                  writes=["tmpd2"])
            tk.op("dve", lambda e, j=j: e.tensor_reduce(out=destf[:, j, 0:ntile], in_=tmpd2[:, 0:ntile, :], axis=AX.X,
                                                        op=ALU.add), reads=["tmpd2"], writes=["destf"])
        tk.op("dve", lambda e: e.tensor_copy(out=desti[:, :, 0:ntile], in_=destf[:, :, 0:ntile]), reads=["destf"],
              writes=["desti"])
        tk.op("dve", lambda e: e.tensor_tensor(out=cmp64[:], in0=padend[:].unsqueeze(1).to_broadcast([128, 64, 32]),
                                               in1=thr64[:].unsqueeze(2).to_broadcast([128, 64, 32]), op=ALU.is_le),
              reads=["padend", "thr64"], writes=["cmp64"])
        tk.op("dve", lambda e: e.tensor_reduce(out=blkf[:], in_=cmp64[:], axis=AX.X, op=ALU.add), reads=["cmp64"],
              writes=["blkf"])
        tk.op("dve", lambda e: e.tensor_scalar(out=blkf[:], in0=blkf[:], scalar1=128.0, scalar2=kcoff[:, 0:1],
                                               op0=ALU.mult, op1=ALU.add), reads=["blkf", "kcoff"], writes=["blkf"])
        tk.op("dve", lambda e: e.tensor_copy(out=idxw[:], in_=blkf[:]), reads=["blkf"], writes=["idxw"])
        for k in range(ntile):
            par = k % 2
            h2, h2k = h22[par], "h2%d" % par
            tsl = slice(k * 128, (k + 1) * 128)
            tk.dma("sp", lambda e: e.dma_start(out=h2[:], in_=h2d[tsl, :]), reads=["h2d"], writes=[h2k])
            for j in range(2):
                tk.dma("pool", lambda e, j=j: e.indirect_dma_start(
                    out=xs[:, :], out_offset=bass.IndirectOffsetOnAxis(ap=desti[:, j, k:k + 1], axis=0),
                    in_=h2[:, :], in_offset=None), reads=[h2k, "desti"], writes=[("xs", k, j)])
        tk.flush()
        real.barrier()
    xs_keys = [("xs", k, j) for k in range(ntile) for j in range(2)]

    with ExitStack() as esB:
        sbb = mk(esB, "b_")
        W = []
        for i in range(2):
            wg2 = sbb("wg%d" % i, [128, 4096]); wu2 = sbb("wu%d" % i, [128, 4096]); wd2 = sbb("wd%d" % i, [128, 4096])
            W.append((wg2, wu2, wd2))
        xsb2 = [sbb("xsb%d" % i, [128, 1024]) for i in range(2)]
        xsT2 = [sbb("xsT%d" % i, [128, 8, 128]) for i in range(2)]
        gsil2 = [sbb("gsil%d" % i, [128, 512]) for i in range(2)]
        hid2 = [sbb("hid%d" % i, [128, 512]) for i in range(2)]
        hidT2 = [sbb("hidT%d" % i, [128, 4, 128]) for i in range(2)]
        ysb2 = [sbb("ysb%d" % i, [128, 1024]) for i in range(2)]
        wgd = D["wgate"]; wud = D["wup"]; wdd = D["wdown"]
        breg = nc.gpsimd.to_reg(32 * 128 - 1)
        for b in range(nblk):
            par = b % 2
            wg2, wu2, wd2 = W[par]
            wg = wg2[:].rearrange("p (a b) -> p a b", b=512)
            wu = wu2[:].rearrange("p (a b) -> p a b", b=512)
            wd = wd2[:].rearrange("p (a b) -> p a b", b=1024)
            xsb, xsT, gsil, hid, hidT, ysb = xsb2[par], xsT2[par], gsil2[par], hid2[par], hidT2[par], ysb2[par]
            sfx = str(par)
            for (dst, dkey, src) in ((wg2, "wg" + sfx, wgd), (wu2, "wu" + sfx, wud), (wd2, "wd" + sfx, wdd)):
                tk.dma("pool", lambda e, dst=dst, src=src: e.indirect_dma_start(
                    out=dst[:, :].bitcast(F32R), out_offset=None, in_=src[:, :].bitcast(F32R),
                    in_offset=bass.IndirectOffsetOnAxis(ap=idxw[:, b:b + 1], axis=0), bounds_check=breg,
                    oob_is_err=False), reads=["idxw"], writes=[dkey], cost=8.0)
            tk.dma("sp", lambda e: e.dma_start(out=xsb[:], in_=xs[b * 128:(b + 1) * 128, :]),
                   reads=xs_keys if b == 0 else [], writes=["xsb" + sfx])
            for half in range(2):
                pn, pt = pp.next()
                tk.ops("pe", [lambda e, j=j, pt=pt, half=half: e.transpose(
                    pt[:, j * 128:(j + 1) * 128], xsb[:, (half * 4 + j) * 128:(half * 4 + j + 1) * 128], ident[:])
                    for j in range(4)], reads=["xsb" + sfx, "ident"], writes=[pn])
                tk.op("act", lambda e, pt=pt, half=half: e.copy(
                    out=xsT[:, half * 4:(half + 1) * 4, :].rearrange("p a b -> p (a b)").bitcast(F32R), in_=pt[:, :]),
                    reads=[pn], writes=["xsT" + sfx])
            pg, ptg = pp.next()
            tk.ops("pe", [lambda e, kc=kc: e.matmul(ptg[:, :], lhsT=xsT[:, kc, :].bitcast(F32R),
                                                   rhs=wg[:, kc, :].bitcast(F32R), start=(kc == 0), stop=(kc == 7))
                          for kc in range(8)], reads=["xsT" + sfx, "wg" + sfx], writes=[pg], cost=3.0)
            pu, ptu = pp.next()
            tk.ops("pe", [lambda e, kc=kc: e.matmul(ptu[:, :], lhsT=xsT[:, kc, :].bitcast(F32R),
                                                   rhs=wu[:, kc, :].bitcast(F32R), start=(kc == 0), stop=(kc == 7))
                          for kc in range(8)], reads=["xsT" + sfx, "wu" + sfx], writes=[pu], cost=3.0)
            tk.op("act", lambda e: e.activation(out=gsil[:], in_=ptg[:, :], func=AF.Silu), reads=[pg],
                  writes=["gsil" + sfx])
            tk.op("dve", lambda e: e.tensor_tensor(out=hid[:], in0=gsil[:], in1=ptu[:, :], op=ALU.mult),
                  reads=["gsil" + sfx, pu], writes=["hid" + sfx])
            pn, pt = pp.next()
            tk.ops("pe", [lambda e, j=j, pt=pt: e.transpose(pt[:, j * 128:(j + 1) * 128],
                                                            hid[:, j * 128:(j + 1) * 128], ident[:])
                          for j in range(4)], reads=["hid" + sfx, "ident"], writes=[pn])
            tk.op("act", lambda e, pt=pt: e.copy(out=hidT[:].rearrange("p a b -> p (a b)").bitcast(F32R),
                                                 in_=pt[:, :]), reads=[pn], writes=["hidT" + sfx])
            for half in range(2):
                pn, pt = pp.next()
                tk.ops("pe", [lambda e, fc=fc, pt=pt, half=half: e.matmul(
                    pt[:, :], lhsT=hidT[:, fc, :].bitcast(F32R),
                    rhs=wd[:, fc, half * 512:(half + 1) * 512].bitcast(F32R), start=(fc == 0), stop=(fc == 3))
                    for fc in range(4)], reads=["hidT" + sfx, "wd" + sfx], writes=[pn], cost=1.5)
                if half == 0:
                    tk.op("act", lambda e, pt=pt: e.copy(out=ysb[:, 0:512], in_=pt[:, :]), reads=[pn],
                          writes=["ysb" + sfx])
                else:
                    tk.op("dve", lambda e, pt=pt: e.tensor_copy(out=ysb[:, 512:1024], in_=pt[:, :]), reads=[pn],
                          writes=["ysb" + sfx])
            tk.dma("sp", lambda e: e.dma_start(out=ys[b * 128:(b + 1) * 128, :], in_=ysb[:]), reads=["ysb" + sfx],
                   writes=[("ys", b)])
            if b % 8 == 7:
                tk.flush()
        tk.flush()
        real.barrier()
    ys_keys = [("ys", b) for b in range(nblk)]

    with ExitStack() as esC:
        sc_ = mk(esC, "c_")
        y02 = [sc_("y0%d" % i, [128, 1024]) for i in range(2)]
        y12 = [sc_("y1%d" % i, [128, 1024]) for i in range(2)]
        for k in range(ntile):
            par = k % 2
            x1, rr, xt, y0, y1 = x12[par], rr2[par], xt2[par], y02[par], y12[par]
            x1k, rrk, xtk, y0k, y1k = "x1%d" % par, "rr%d" % par, "xt%d" % par, "y0%d" % par, "y1%d" % par
            tsl = slice(k * 128, (k + 1) * 128)
            tk.dma("sp", lambda e: e.dma_start(out=x1[:], in_=x1d[tsl, :]), reads=["x1d"], writes=[x1k])
            for j, (yt_, yk) in enumerate(((y0, y0k), (y1, y1k))):
                tk.dma("pool", lambda e, j=j, yt_=yt_: e.indirect_dma_start(
                    out=yt_[:, :], out_offset=None, in_=ys[:, :],
                    in_offset=bass.IndirectOffsetOnAxis(ap=desti[:, j, k:k + 1], axis=0)),
                    reads=(ys_keys if k == 0 else []) + ["desti"], writes=[yk])
            tk.op("dve", lambda e: e.tensor_scalar(out=y0[:], in0=y0[:], scalar1=gts[:, k, 0:1], scalar2=None,
                                                   op0=ALU.mult), reads=[y0k, "gts"], writes=[y0k])
            tk.op("dve", lambda e: e.scalar_tensor_tensor(out=y0[:], in0=y1[:], scalar=gts[:, k, 1:2], in1=y0[:],
                                                          op0=ALU.mult, op1=ALU.add), reads=[y0k, y1k, "gts"],
                  writes=[y0k])
            tk.op("dve", lambda e: e.tensor_tensor(out=y0[:], in0=y0[:], in1=modb[:, 3, :], op=ALU.mult),
                  reads=[y0k, "modb"], writes=[y0k])
            tk.op("dve", lambda e: e.scalar_tensor_tensor(out=rr[:], in0=x1[:], scalar=ALPHA, in1=y0[:], op0=ALU.mult,
                                                          op1=ALU.add), reads=[x1k, y0k], writes=[rrk])
            layer_norm(rr, rrk, xt, xtk, 2)
            tk.dma("sp", lambda e: e.dma_start(out=out[tsl, :], in_=xt[:]), reads=[xtk], writes=["out"])
            if k % 2 == 1:
                tk.flush()
        tk.flush()


def consts2():
    p = np.arange(128)
    tri = (p[:, None] < p[None, :]).astype(np.float32)
    thr16 = np.broadcast_to((128.0 * np.arange(16, dtype=np.float32))[None, :], (128, 16))
    thr64 = np.broadcast_to((128.0 * np.arange(64, dtype=np.float32))[None, :], (128, 64))
    return {"c_ident": np.eye(128, dtype=np.float32), "c_tri": tri, "c_iota": p.astype(np.float32)[:, None].copy(),
            "c_thr16": np.ascontiguousarray(thr16), "c_thr64": np.ascontiguousarray(thr64)}


def build_phase2(ntile=NT2, nblk=NBLK):
    nc = bass.Bass("TRN2", target_bir_lowering=False)
    D = {}

    def din(name, shape, dt=F32):
        D[name] = nc.dram_tensor(name, shape, dt, kind="ExternalInput").ap()

    din("mixTf", [1024, 2048], BF16); din("x2", [2048, 1024]); din("cT", [128, 8])
    din("wada2", [128, 32, 8, 128]); din("bada2", [4096]); din("wo", [1024, 1024]); din("lnp", [4, 1024])
    din("wr", [1024, 36]); din("br", [36])
    din("wgate", [4096, 4096]); din("wup", [4096, 4096]); din("wdown", [4096, 4096])
    din("c_ident", [128, 128]); din("c_tri", [128, 128]); din("c_iota", [128, 1]); din("c_thr16", [128, 16])
    din("c_thr64", [128, 64])
    for nm in ("x1d", "h2d"):
        D[nm] = nc.dram_tensor(nm, [2048, 1024], F32, kind="Internal").ap()
    for nm in ("xs", "ys"):
        D[nm] = nc.dram_tensor(nm, [NBLK * 128, 1024], F32, kind="Internal").ap()
    D["out"] = nc.dram_tensor("out", [2048, 1024], F32, kind="ExternalOutput").ap()
    with ExitStack() as es:
        tk = Trk(nc, es)
        phase2(nc, tk, es, D, ntile=ntile, nblk=nblk)
        tk.finish("sp")
    return nc


def phase2_shared_inputs(inp):
    l = 0
    wg = inp["w_gate"][l].reshape(32, 8, 128, 512).transpose(0, 2, 1, 3).reshape(4096, 4096)
    wu = inp["w_up"][l].reshape(32, 8, 128, 512).transpose(0, 2, 1, 3).reshape(4096, 4096)
    wd = inp["w_down"][l].reshape(32, 4, 128, 1024).transpose(0, 2, 1, 3).reshape(4096, 4096)
    d = {"wada2": np.ascontiguousarray(inp["w_ada"][l][:, 2048:6144].reshape(8, 128, 32, 128).transpose(1, 2, 0, 3)),
         "bada2": np.ascontiguousarray(inp["b_ada"][l][2048:6144]),
         "wo": np.ascontiguousarray(inp["w_o"][l]),
         "lnp": np.ascontiguousarray(np.stack([inp["ln1_g"][l], inp["ln1_b"][l], inp["ln2_g"][l], inp["ln2_b"][l]])),
         "wr": np.ascontiguousarray(np.concatenate([inp["w_router_group"][l], inp["w_router_expert"][l]], 1)),
         "br": np.ascontiguousarray(np.concatenate([inp["b_router_group"][l], inp["b_router_expert"][l]])),
         "wgate": np.ascontiguousarray(wg), "wup": np.ascontiguousarray(wu), "wdown": np.ascontiguousarray(wd)}
    d.update(consts2())
    return d


def phase2_inputs(inp, core, shared, mixTf):
    b, q = core // 4, core % 4
    d = dict(shared)
    d["x2"] = np.ascontiguousarray(inp["x"][b, q * 2048:(q + 1) * 2048])
    d["cT"] = np.ascontiguousarray(inp["c"][b].reshape(8, 128).T)
    d["mixTf"] = mixTf
    return d


def build_fused():
    nc = bass.Bass("TRN2", target_bir_lowering=False)
    D = {}

    def din(name, shape, dt=F32):
        D[name] = nc.dram_tensor(name, shape, dt, kind="ExternalInput").ap()

    din("x", [D_MODEL, SEQ]); din("cT", [128, 8]); din("pos", [SEQ], I32)
    din("wada1", [128, 8, 8, 256]); din("bada1", [128, 16]); din("win", [128, 5, 8, 256])
    din("convw", [128, 3, 4]); din("pvec", [128, 4])
    din("c_ident", [128, 128]); din("c_masks", [128, 3, 128]); din("c_amask", [128, 256]); din("c_invf", [64, 2])
    din("x2", [2048, 1024]); din("qidx", [128, 8], I32)
    din("wada2", [128, 32, 8, 128]); din("bada2", [4096]); din("wo", [1024, 1024]); din("lnp", [4, 1024])
    din("wr", [1024, 36]); din("br", [36])
    din("wgate", [4096, 4096]); din("wup", [4096, 4096]); din("wdown", [4096, 4096])
    din("c_tri", [128, 128]); din("c_iota", [128, 1]); din("c_thr16", [128, 16]); din("c_thr64", [128, 64])
    cin = [nc.dram_tensor("cin%d" % q, [256, 2048], BF16, kind="Internal").ap() for q in range(4)]
    cout = nc.dram_tensor("cout", [4096, 2048], BF16, kind="Internal").ap()
    for nm in ("x1d", "h2d"):
        D[nm] = nc.dram_tensor(nm, [2048, 1024], F32, kind="Internal").ap()
    for nm in ("xs", "ys"):
        D[nm] = nc.dram_tensor(nm, [NBLK * 128, 1024], F32, kind="Internal").ap()
    D["out"] = nc.dram_tensor("out", [2048, 1024], F32, kind="ExternalOutput").ap()
    groups = [[0, 1, 2, 3], [4, 5, 6, 7]]
    with ExitStack() as es0:
        tk = Trk(nc, es0)

        def on_quarter(sc, tk):
            tk.cc(lambda e: e.collective_compute("AllGather", ALU.bypass, replica_groups=groups,
                                                 ins=[cin[sc][:, :]], outs=[cout[sc * 1024:(sc + 1) * 1024, :]]),
                  reads=[("cin_a", sc), ("cin_b", sc)], writes=[("cout", sc)])

        with ExitStack() as es1:
            phase1(nc, tk, es1, D, cin=cin, on_quarter=on_quarter)
        tk.barrier()
        with ExitStack() as es2:
            phase2(nc, tk, es2, D, cout=cout)
        tk.finish("sp")
    return nc


def fused_inputs(inp, core, shared):
    b, q = core // 4, core % 4
    d = dict(shared)
    d.update(phase1_inputs(inp, core))
    d["x2"] = np.ascontiguousarray(inp["x"][b, q * 2048:(q + 1) * 2048])
    kc = np.arange(8)[None, :]
    p = np.arange(128)[:, None]
    row = np.where(kc < 4, kc * 256 + p, (kc - 4) * 256 + 128 + p)
    d["qidx"] = np.ascontiguousarray((q * 1024 + row).astype(np.int32))
    return d


def kernel(**inputs):
    inp = {k: np.asarray(v) for k, v in inputs.items()}
    shared = phase2_shared_inputs(inp)
    nc = build_fused()
    maps = [fused_inputs(inp, core, shared) for core in range(NCORES)]
    res = run_bass_kernel_spmd(nc, maps, core_ids=list(range(NCORES)))
    out = np.zeros((BATCH, SEQ, D_MODEL), np.float32)
    for core in range(NCORES):
        b, q = core // 4, core % 4
        out[b, q * 2048:(q + 1) * 2048] = np.asarray(res.results[core]["out"])
    return out
```

```python
import math
from contextlib import ExitStack

import numpy as np
import concourse.bass as bass
import concourse.mybir as mybir
from concourse.bass_utils import run_bass_kernel_spmd

F32 = mybir.dt.float32
BF16 = mybir.dt.bfloat16
I32 = mybir.dt.int32
U32 = mybir.dt.uint32
F32R = mybir.dt.float32r
AF = mybir.ActivationFunctionType
ALU = mybir.AluOpType
AX = mybir.AxisListType

D_MODEL = 1024
SEQ = 8192
BATCH = 2
NCORES = 8
T = 512
NCH = SEQ // T
ALPHA = 2.0 ** 0.25
LN_EPS = 1e-5
RMS_EPS = 1e-6
ROPE_THETA = 500000.0
NEG = -30000.0
TWO_PI = 2.0 * math.pi


class Trk:
    def __init__(self, nc, es, n_dma_sems=24, same_engine_sync=True):
        self.nc = nc
        self.e = {"pe": nc.tensor, "dve": nc.vector, "act": nc.scalar, "pool": nc.gpsimd, "sp": nc.sync}
        self.sem = {k: es.enter_context(nc.semaphore("sem_" + k)) for k in self.e}
        self.cnt = {k: 0 for k in self.e}
        self.seen = {k: {} for k in self.e}
        self.last_w = {}
        self.reads = {}
        self.dsem = [es.enter_context(nc.semaphore("dsem%d" % i)) for i in range(n_dma_sems)]
        self.dval = [0] * n_dma_sems
        self.dnext = 0
        self.same = same_engine_sync
        self.all_dma_events = []
        self.ccsem = es.enter_context(nc.semaphore("ccsem"))
        self.ccval = 0

    def _wait(self, eng, ev):
        kind, src, n = ev
        key = (kind, src)
        if self.seen[eng].get(key, 0) >= n:
            return
        if kind == "e":
            if src == eng and not self.same:
                return
            self.e[eng].wait_ge(self.sem[src], n)
        elif kind == "c":
            self.e[eng].wait_ge(self.ccsem, n)
        else:
            self.e[eng].wait_ge(self.dsem[src], n)
        self.seen[eng][key] = n

    def _deps(self, eng, reads, writes):
        deps = []
        for r in reads:
            ev = self.last_w.get(r)
            if ev is not None:
                deps.append(ev)
        for w in writes:
            ev = self.last_w.get(w)
            if ev is not None:
                deps.append(ev)
            for ev2 in self.reads.get(w, {}).values():
                deps.append(ev2)
        for ev in deps:
            self._wait(eng, ev)

    def _record(self, ev, reads, writes):
        for r in reads:
            self.reads.setdefault(r, {})[(ev[0], ev[1])] = ev
        for w in writes:
            self.last_w[w] = ev
            self.reads[w] = {}

    def op(self, eng, fn, reads=(), writes=()):
        self._deps(eng, reads, writes)
        inst = fn(self.e[eng])
        self.cnt[eng] += 1
        inst.then_inc(self.sem[eng], 1)
        ev = ("e", eng, self.cnt[eng])
        self._record(ev, reads, writes)
        return ev

    def ops(self, eng, fns, reads=(), writes=()):
        self._deps(eng, reads, writes)
        inst = None
        for fn in fns:
            inst = fn(self.e[eng])
        self.cnt[eng] += 1
        inst.then_inc(self.sem[eng], 1)
        ev = ("e", eng, self.cnt[eng])
        self._record(ev, reads, writes)
        return ev

    def dma(self, eng, fn, reads=(), writes=()):
        self._deps(eng, reads, writes)
        i = self.dnext
        self.dnext = (self.dnext + 1) % len(self.dsem)
        if self.dval[i] > 0:
            self._wait(eng, ("d", i, self.dval[i]))
        inst = fn(self.e[eng])
        self.dval[i] += 16
        inst.then_inc(self.dsem[i], 16)
        ev = ("d", i, self.dval[i])
        self._record(ev, reads, writes)
        self.all_dma_events.append(ev)
        return ev

    def cc(self, fn, reads=(), writes=()):
        eng = "pool"
        self._deps(eng, reads, writes)
        if self.ccsem is None:
            raise RuntimeError("no cc semaphore")
        if self.ccval > 0:
            self._wait(eng, ("c", 0, self.ccval))
        inst = fn(self.e[eng])
        self.ccval += 1
        inst.then_inc(self.ccsem, 1)
        ev = ("c", 0, self.ccval)
        self._record(ev, reads, writes)
        return ev

    def barrier(self):
        for eng in self.e:
            for i, v in enumerate(self.dval):
                if v > 0:
                    self._wait(eng, ("d", i, v))
            if self.ccval > 0:
                self._wait(eng, ("c", 0, self.ccval))
            for k, n in self.cnt.items():
                if n > 0 and k != eng:
                    self._wait(eng, ("e", k, n))

    def finish(self, eng="sp"):
        for i, v in enumerate(self.dval):
            if v > 0:
                self._wait(eng, ("d", i, v))
        if self.ccval > 0:
            self._wait(eng, ("c", 0, self.ccval))
        for k, n in self.cnt.items():
            if n > 0 and k != eng:
                self._wait(eng, ("e", k, n))


class _Proxy:
    def __init__(self):
        self.calls = []

    def __getattr__(self, name):
        def f(*a, **k):
            self.calls.append((name, a, k))
            return None
        return f


def _freeze(fn):
    p = _Proxy()
    fn(p)
    assert len(p.calls) == 1
    name, a, k = p.calls[0]
    return lambda e: getattr(e, name)(*a, **k)


class Rec:
    HOP = 0.8

    def __init__(self, tk):
        self.tk = tk
        self.L = []

    def op(self, eng, fn, reads=(), writes=(), cost=None):
        self.L.append(("op", eng, _freeze(fn), tuple(reads), tuple(writes), cost if cost is not None else (0.3 if eng == "pe" else 0.6)))

    def ops(self, eng, fns, reads=(), writes=(), cost=None):
        fns = [_freeze(f) for f in fns]
        self.L.append(("ops", eng, fns, tuple(reads), tuple(writes), cost if cost is not None else 0.2 * len(fns)))

    def dma(self, eng, fn, reads=(), writes=(), cost=None):
        self.L.append(("dma", eng, _freeze(fn), tuple(reads), tuple(writes), cost if cost is not None else 2.0))

    def cc(self, fn, reads=(), writes=(), cost=None):
        self.L.append(("cc", "pool", _freeze(fn), tuple(reads), tuple(writes), 5.0))

    def flush(self):
        L = self.L
        self.L = []
        n = len(L)
        preds = [set() for _ in range(n)]
        last_w = {}
        readers = {}
        for i, (kind, eng, fn, reads, writes, cost) in enumerate(L):
            for r in reads:
                if r in last_w:
                    preds[i].add(last_w[r])
            for w in writes:
                if w in last_w:
                    preds[i].add(last_w[w])
                for j in readers.get(w, ()):
                    preds[i].add(j)
            for r in reads:
                readers.setdefault(r, []).append(i)
            for w in writes:
                last_w[w] = i
                readers[w] = []
            preds[i].discard(i)
        succs = [[] for _ in range(n)]
        npred = [len(p) for p in preds]
        for i in range(n):
            for j in preds[i]:
                succs[j].append(i)
        efree = {}
        finish = [0.0] * n
        ready_t = [0.0] * n
        ready = [i for i in range(n) if npred[i] == 0]
        order = []
        import heapq
        while ready:
            best = None
            bkey = None
            for i in ready:
                kind, eng, fn, reads, writes, cost = L[i]
                q = eng if kind in ("op", "ops") else ("q_" + eng)
                st = max(efree.get(q, 0.0), ready_t[i])
                key = (st, i)
                if bkey is None or key < bkey:
                    bkey = key
                    best = i
            i = best
            ready.remove(i)
            kind, eng, fn, reads, writes, cost = L[i]
            q = eng if kind in ("op", "ops") else ("q_" + eng)
            st = bkey[0]
            if kind in ("op", "ops"):
                efree[q] = st + cost
                finish[i] = st + cost
            else:
                efree[q] = st + 0.1
                finish[i] = st + cost
            order.append(i)
            for j in succs[i]:
                npred[j] -= 1
                same = (L[j][1] == eng and L[j][0] in ("op", "ops") and kind in ("op", "ops"))
                ready_t[j] = max(ready_t[j], finish[i] + (0.0 if same else self.HOP))
                if npred[j] == 0:
                    ready.append(j)
        assert len(order) == n
        for i in order:
            kind, eng, fn, reads, writes, cost = L[i]
            if kind == "op":
                self.tk.op(eng, fn, reads, writes)
            elif kind == "ops":
                self.tk.ops(eng, fn, reads, writes)
            elif kind == "dma":
                self.tk.dma(eng, fn, reads, writes)
            else:
                self.tk.cc(fn, reads, writes)


class PsumPool:
    _uid = [0]

    def __init__(self, nc, es, names):
        PsumPool._uid[0] += 1
        u = PsumPool._uid[0]
        self.t = {n: es.enter_context(nc.psum_tensor("ps%d_%s" % (u, n), [128, 512], F32)) for n in names}
        self.rot = [n for n in names if n.startswith("r")]
        self.i = 0

    def next(self):
        n = self.rot[self.i]
        self.i = (self.i + 1) % len(self.rot)
        return n, self.t[n]


def phase1(nc, tk, es, D, nch=NCH, cin=None, on_quarter=None):
    def sb(name, shape, dt=F32):
        return es.enter_context(nc.sbuf_tensor("s_" + name, shape, dt))

    def K(name, c):
        return (name, c % 8)

    ppall = PsumPool(nc, es, ["rb0", "rb1", "rb2", "rb3", "ra0", "ra1", "scan", "ot"])
    PS = ppall.t
    real_tk = tk
    tk = Rec(real_tk)

    class _Sub:
        def __init__(self, names):
            self.rot = names
            self.i = 0

        def next(self):
            n = self.rot[self.i]
            self.i = (self.i + 1) % len(self.rot)
            return n, PS[n]

    pp = _Sub(["rb0", "rb1", "rb2", "rb3"])
    ppa = _Sub(["ra0", "ra1"])

    ident = sb("ident", [128, 128])
    ident_b = sb("ident_b", [128, 128], BF16)
    ones_f = sb("ones_f", [128, 128])
    ones_b = sb("ones_b", [128, 128], BF16)
    mus_t = sb("mus_neg", [128, 128]); mui_t = sb("mu_inc", [128, 128]); mls_t = sb("mls_neg", [128, 128])
    mus_neg = mus_t[:].unsqueeze(1).to_broadcast([128, 4, 128])
    mu_inc = mui_t[:].unsqueeze(1).to_broadcast([128, 4, 128])
    mls_neg = mls_t[:].unsqueeze(1).to_broadcast([128, 4, 128])
    ident4 = ident[:].unsqueeze(1).to_broadcast([128, 4, 128])
    amask_f = sb("amask_f", [128, 256])
    amask = sb("amask", [128, 256], BF16)
    scanm = sb("scanm", [128, T])
    invf = sb("invf", [64, 2])
    win_b = sb("win_b", [128, 8, 1280], BF16)
    xtok = sb("xtok", [128, 8, T])
    wstage = [xtok[:, 4 * i:4 * i + 4, :].rearrange("p a (h c) -> p (a h) c", c=256) for i in range(2)]
    wkeys = ["xtok_a", "xtok_b"]
    sc1 = sb("sc1", [128, 8])
    sh1 = sb("sh1", [128, 8])
    convw = sb("convw", [128, 3, 4])
    pvec = sb("pvec", [128, 4])
    nalog = sb("nalog", [128, 1])
    cT = sb("cT", [128, 8])
    scT = sb("scT", [128, 8, 2])
    bada = sb("bada", [128, 16])
    mod1 = sb("mod1", [128, 16])

    tk.dma("sp", lambda e: e.dma_start(out=ident[:], in_=D["c_ident"][:, :]), writes=["ident"])
    tk.dma("sp", lambda e: e.dma_start(out=amask_f[:], in_=D["c_amask"][:, :]), writes=["amask_f"])
    tk.dma("sp", lambda e: e.dma_start(out=invf[:], in_=D["c_invf"][:, :]), writes=["invf"])
    tk.dma("sp", lambda e: e.dma_start(out=convw[:], in_=D["convw"][:, :, :]), writes=["convw"])
    tk.dma("sp", lambda e: e.dma_start(out=pvec[:], in_=D["pvec"][:, :]), writes=["pvec"])
    tk.dma("sp", lambda e: e.dma_start(out=cT[:], in_=D["cT"][:, :]), writes=["cT"])
    tk.dma("sp", lambda e: e.dma_start(out=bada[:], in_=D["bada1"][:, :]), writes=["bada"])
    for q, (nm, tl) in enumerate([("mus", mus_t), ("mui", mui_t), ("mls", mls_t)]):
        tk.dma("sp", lambda e, tl=tl, q=q: e.dma_start(out=tl[:], in_=D["c_masks"][:, q, :]), writes=[nm])
    tk.op("dve", lambda e: e.tensor_copy(out=ident_b[:], in_=ident[:]), reads=["ident"], writes=["ident_b"])
    tk.op("dve", lambda e: e.tensor_copy(out=amask[:], in_=amask_f[:]), reads=["amask_f"], writes=["amask"])
    tk.op("pool", lambda e: e.memset(ones_f[:], 1.0), writes=["ones_f"])
    tk.op("pool", lambda e: e.memset(ones_b[:], 1.0), writes=["ones_b"])
    tk.op("pool", lambda e: e.memset(scanm[:], 1.0), writes=["scanm"])
    tk.op("pool", lambda e: e.memset(scanm[:, 0:T:64], 0.0), writes=["scanm"])
    tk.op("act", lambda e: e.activation(out=nalog[:], in_=pvec[:, 0:1], func=AF.Exp), reads=["pvec"],
          writes=["nalog"])
    tk.op("dve", lambda e: e.tensor_scalar(out=nalog[:], in0=nalog[:], scalar1=-1.0, scalar2=None, op0=ALU.mult),
          reads=["nalog"], writes=["nalog"])

    for j in range(5):
        st = wstage[j % 2]
        key = wkeys[j % 2]
        tk.dma("sp" if j % 2 == 0 else "pool", lambda e, st=st, j=j: e.dma_start(out=st, in_=D["win"][:, j, :, :]),
               writes=[key])
        tk.op("dve" if j % 2 == 0 else "act", (lambda e, st=st, j=j: e.tensor_copy(out=win_b[:, :, j * 256:(j + 1) * 256], in_=st))
              if j % 2 == 0 else (lambda e, st=st, j=j: e.copy(out=win_b[:, :, j * 256:(j + 1) * 256], in_=st)),
              reads=[key], writes=[("win_b", j)])
    for c0 in (7 * 128, 7 * 128 + 32):
        tk.op("dve", lambda e, c0=c0: e.tensor_scalar(out=win_b[:, :, c0:c0 + 16], in0=win_b[:, :, c0:c0 + 16],
                                                       scalar1=-1.0, scalar2=None, op0=ALU.mult),
              reads=[("win_b", 3)], writes=[("win_b", 3)])

    tk.op("act", lambda e: e.activation(out=scT[:, :, 0], in_=cT[:], func=AF.Silu), reads=["cT"], writes=["scT"])
    tk.op("act", lambda e: e.activation(out=scT[:, :, 1], in_=cT[:], func=AF.Silu), reads=["cT"], writes=["scT"])
    for j in range(8):
        st = wstage[j % 2]
        key = wkeys[j % 2]
        tk.dma("sp" if j % 2 == 0 else "pool", lambda e, st=st, j=j: e.dma_start(out=st, in_=D["wada1"][:, j, :, :]),
               writes=[key])
        for fh in range(2):
            fc = 2 * j + fh
            pn, pt = pp.next()
            tk.ops("pe", [lambda e, st=st, kc=kc, pt=pt, fh=fh: e.matmul(
                pt[:, 0:2], lhsT=st[:, kc, fh * 128:(fh + 1) * 128], rhs=scT[:, kc, :], start=(kc == 0), stop=(kc == 7))
                for kc in range(8)], reads=[key, "scT"], writes=[pn])
            tk.op("dve", lambda e, pt=pt, fc=fc: e.tensor_tensor(out=mod1[:, fc:fc + 1], in0=pt[:, 0:1],
                                                                  in1=bada[:, fc:fc + 1], op=ALU.add),
                  reads=[pn, "bada"], writes=["mod1"])
    tk.op("dve", lambda e: e.tensor_copy(out=sh1[:], in_=mod1[:, 0:8]), reads=["mod1"], writes=["sh1"])
    tk.op("dve", lambda e: e.tensor_scalar(out=sc1[:], in0=mod1[:, 8:16], scalar1=1.0, scalar2=None, op0=ALU.add),
          reads=["mod1"], writes=["sc1"])

    RING = 4096
    qT = sb("qT", [128, RING], BF16)
    kT = sb("kT", [128, RING], BF16)
    vT = sb("vT", [128, RING], BF16)
    vtt = sb("vtt", [128, 4, 128], BF16)
    acc = sb("acc", [128, 2, 2048])
    S = [sb("S%d" % i, [128, 128]) for i in range(2)]
    tk.op("pool", lambda e: e.memset(S[0][:], 0.0), writes=["S0"])

    hT = [sb("hT%d" % i, [128, 8, T], BF16) for i in range(2)]
    craw = [sb("craw%d" % g, [128, T + 3]) for g in range(3)]
    cacc = sb("cacc", [128, T])
    qs = sb("qs", [128, T]); ks = sb("ks", [128, T]); vs = sb("vs", [128, T])
    zs2 = [sb("zs%d" % i, [128, T]) for i in range(2)]
    sq = sb("sq", [128, T]); rn = sb("rn", [128, T])
    p1 = sb("p1", [128, T]); p2 = sb("p2", [128, T]); p3 = sb("p3", [128, T])
    osq = p1; rs = p2; fsq = p1; fr = p2; oa = p3; fo = p3
    qn = sb("qn", [128, T]); kn = sb("kn", [128, T])
    betab = sb("betab", [128, T]); gb = sb("gb", [128, T]); gcb = sb("gcb", [128, T])
    egcb2 = [sb("egcb%d" % i, [128, T]) for i in range(2)]; eglb = sb("eglb", [128, T]); begb = gb
    cols = sb("cols", [128, 5, 4])
    Kbe = sb("Kbe", [128, 4, 128]); Kd2 = [sb("Kd%d" % i, [128, 4, 128]) for i in range(2)]
    Vb = sb("Vb", [128, 4, 128])
    tdf = sb("tdf", [128, 4, 128]); DT = sb("DT", [128, 4, 128]); Dm = sb("Dm", [128, 4, 128])
    E1 = tdf; QKT2 = [sb("QKT%d" % i, [128, 4, 128]) for i in range(2)]
    Bm = [sb("Bm%d" % i, [128, 4, 128]) for i in range(2)]
    BTm = [sb("BTm%d" % i, [128, 4, 128]) for i in range(2)]
    Pm = [sb("Pm%d" % i, [128, 4, 128]) for i in range(2)]
    U2 = [sb("U%d" % i, [128, 4, 128]) for i in range(2)]; WT2 = [sb("WT%d" % i, [128, T]) for i in range(2)]
    QdT2 = [sb("QdT%d" % i, [128, T]) for i in range(2)]
    vnew = sb("vnew", [128, 2, 128])
    oab = sb("oab", [128, T], BF16)
    posi = sb("posi", [64, T], I32); ra = sb("ra", [64, T]); rb = sb("rb", [64, T])
    ri = sb("ri", [64, T], I32)
    sincos = sb("sincos", [64, T])
    rt1 = sb("rt1", [32, T]); rt2 = sb("rt2", [32, T])
    pTt = [sb("pT%d" % i, [128, 256], BF16) for i in range(2)]
    fob = sb("fob", [128, T], BF16)

    x = D["x"]
    mixT = D.get("mixT")
    pos = D["pos"]

    def load_x(c):
        for i in range(2):
            tk.dma("sp", lambda e, i=i: e.dma_start(
                out=xtok[:, 4 * i:4 * i + 4, :],
                in_=x[i * 512:(i + 1) * 512, c * T:(c + 1) * T].rearrange("(kc p) t -> p kc t", p=128)),
                writes=[wkeys[i]])

    def stage_a(c):
        h = hT[c % 2]
        hk = "hT%d" % (c % 2)
        for kc in range(8):
            tk.op("act", lambda e, kc=kc: e.activation(out=h[:, kc, :], in_=xtok[:, kc, :], func=AF.Identity,
                                                       bias=sh1[:, kc:kc + 1], scale=sc1[:, kc:kc + 1]),
                  reads=[wkeys[kc // 4], "sh1", "sc1"], writes=[hk])

    def proj(c, m):
        h = hT[c % 2]
        hk = "hT%d" % (c % 2)
        pn, pt = pp.next()
        tk.ops("pe", [lambda e, kc=kc, pt=pt: e.matmul(pt[:, :], lhsT=win_b[:, kc, m * 128:(m + 1) * 128],
                                                      rhs=h[:, kc, :], start=(kc == 0), stop=(kc == 7))
                      for kc in range(8)], reads=[hk, ("win_b", m // 2)], writes=[pn])
        return pn, pt

    def rope_tables(c):
        tk.dma("sp", lambda e: e.dma_start(out=posi[:], in_=pos[c * T:(c + 1) * T].partition_broadcast(64)),
               writes=["posi"])
        tk.op("dve", lambda e: e.tensor_copy(out=ra[:], in_=posi[:]), reads=["posi"], writes=["ra"])
        tk.op("dve", lambda e: e.tensor_scalar(out=ra[:], in0=ra[:], scalar1=invf[:, 0:1], scalar2=invf[:, 1:2],
                                               op0=ALU.mult, op1=ALU.add), reads=["ra", "invf"], writes=["ra"])
        tk.op("dve", lambda e: e.tensor_scalar(out=ri[:], in0=ra[:], scalar1=1.0 / TWO_PI, scalar2=None,
                                               op0=ALU.mult), reads=["ra"], writes=["ri"])
        tk.op("dve", lambda e: e.tensor_copy(out=rb[:], in_=ri[:]), reads=["ri"], writes=["rb"])
        tk.op("dve", lambda e: e.scalar_tensor_tensor(out=ra[:], in0=rb[:], scalar=-TWO_PI, in1=ra[:], op0=ALU.mult,
                                                      op1=ALU.add), reads=["ra", "rb"], writes=["ra"])
        tk.op("dve", lambda e: e.tensor_scalar(out=rb[:], in0=ra[:], scalar1=math.pi, scalar2=-TWO_PI,
                                               op0=ALU.is_gt, op1=ALU.mult), reads=["ra"], writes=["rb"])
        tk.op("dve", lambda e: e.tensor_tensor(out=ra[:], in0=ra[:], in1=rb[:], op=ALU.add),
              reads=["ra", "rb"], writes=["ra"])
        tk.op("dve", lambda e: e.tensor_scalar(out=rb[:], in0=ra[:], scalar1=-math.pi, scalar2=TWO_PI,
                                               op0=ALU.is_lt, op1=ALU.mult), reads=["ra"], writes=["rb"])
        tk.op("dve", lambda e: e.tensor_tensor(out=ra[:], in0=ra[:], in1=rb[:], op=ALU.add),
              reads=["ra", "rb"], writes=["ra"])
        tk.op("act", lambda e: e.activation(out=sincos[:], in_=ra[:], func=AF.Sin), reads=["ra"], writes=["sincos"])

    def prep_main(c):
        par = c % 2
        U, WT, QdT, QKT, Kd, egcb, zs = U2[par], WT2[par], QdT2[par], QKT2[par], Kd2[par], egcb2[par], zs2[par]
        uk, wk, qdk, qkk, kdk, ek, zk = ("U%d" % par, "WT%d" % par, "QdT%d" % par, "QKT%d" % par, "Kd%d" % par,
                                         "egcb%d" % par, "zs%d" % par)
        stage_a(c)
        yield
        if c + 1 < nch:
            load_x(c + 1)
        yield
        for g, dst, dk_ in ((0, qs, "qs"), (1, ks, "ks"), (2, vs, "vs")):
            pn, pt = proj(c, g)
            ck = "craw%d" % g
            cr = craw[g]
            if c > 0:
                tk.op("dve", lambda e, cr=cr: e.tensor_copy(out=cr[:, 0:3], in_=cr[:, T:T + 3]), reads=[ck],
                      writes=[ck])
            else:
                tk.op("pool", lambda e, cr=cr: e.memset(cr[:, 0:3], 0.0), writes=[ck])
            tk.op("act", lambda e, pt=pt, cr=cr: e.copy(out=cr[:, 3:T + 3], in_=pt[:, :]), reads=[pn],
                  writes=[ck])
            tk.op("dve", lambda e, cr=cr, g=g: e.tensor_scalar(out=cacc[:], in0=cr[:, 0:T],
                                                               scalar1=convw[:, g, 0:1], scalar2=None, op0=ALU.mult),
                  reads=[ck, "convw"], writes=["cacc"])
            for j in range(1, 4):
                tk.op("dve", lambda e, cr=cr, g=g, j=j: e.scalar_tensor_tensor(
                    out=cacc[:], in0=cr[:, j:j + T], scalar=convw[:, g, j:j + 1], in1=cacc[:],
                    op0=ALU.mult, op1=ALU.add), reads=[ck, "convw", "cacc"], writes=["cacc"])
            tk.op("act", lambda e, dst=dst: e.activation(out=dst[:], in_=cacc[:], func=AF.Silu), reads=["cacc"],
                  writes=[dk_])
            yield
        yield
        pn, pt = proj(c, 3)
        tk.op("act", lambda e, pt=pt: e.activation(out=zs[:], in_=pt[:, :], func=AF.Silu), reads=[pn], writes=[zk])
        yield
        for src, sk, dst, dk_, scale in ((qs, "qs", qn, "qn", 128.0 ** -0.5), (ks, "ks", kn, "kn", 1.0)):
            tk.op("act", lambda e, src=src: e.activation(out=sq[:], in_=src[:], func=AF.Square), reads=[sk],
                  writes=["sq"])
            pn, pt = pp.next()
            tk.op("pe", lambda e, pt=pt: e.matmul(pt[:, :], lhsT=ones_f[:], rhs=sq[:], start=True, stop=True),
                  reads=["sq", "ones_f"], writes=[pn])
            tk.op("act", lambda e, pt=pt: e.activation(out=rn[:], in_=pt[:, :], func=AF.Ln, bias=RMS_EPS_AP[:, 0:1],
                                                       scale=1.0), reads=[pn, "epsc"], writes=["rn"])
            tk.op("act", lambda e: e.activation(out=rn[:], in_=rn[:], func=AF.Exp, scale=-0.5), reads=["rn"],
                  writes=["rn"])
            tk.op("dve", lambda e, src=src, dst=dst, scale=scale: e.scalar_tensor_tensor(
                out=dst[:].bitcast(F32R), in0=src[:], scalar=scale, in1=rn[:], op0=ALU.mult, op1=ALU.mult),
                reads=[sk, "rn"], writes=[dk_])
        yield
        pn, pt = proj(c, 8)
        tk.op("act", lambda e, pt=pt: e.activation(out=betab[:], in_=pt[:, :], func=AF.Sigmoid), reads=[pn],
              writes=["betab"])
        pn, pt = proj(c, 9)
        tk.op("act", lambda e, pt=pt: e.activation(out=gb[:], in_=pt[:, :], func=AF.Exp, bias=pvec[:, 1:2], scale=1.0),
              reads=[pn, "pvec"], writes=["gb"])
        tk.op("act", lambda e: e.activation(out=gb[:], in_=gb[:], func=AF.Ln, bias=ONE_AP[:, 0:1], scale=1.0),
              reads=["gb", "epsc"], writes=["gb"])
        tk.op("dve", lambda e: e.tensor_scalar(out=gb[:], in0=gb[:], scalar1=nalog[:, 0:1], scalar2=None, op0=ALU.mult),
              reads=["gb", "nalog"], writes=["gb"])
        tk.op("dve", lambda e: e.tensor_tensor_scan(out=gcb[:], data0=scanm[:], data1=gb[:], initial=0.0,
                                                    op0=ALU.mult, op1=ALU.add), reads=["gb", "scanm"], writes=["gcb"])
        tk.op("act", lambda e: e.activation(out=egcb[:], in_=gcb[:], func=AF.Exp), reads=["gcb"], writes=[ek])
        gc3 = gcb[:].rearrange("p (n t) -> p n t", t=64)
        tk.op("dve", lambda e: e.tensor_tensor(out=eglb[:].rearrange("p (n t) -> p n t", t=64),
                                               in0=gc3[:, :, 63:64].to_broadcast([128, 8, 64]), in1=gc3,
                                               op=ALU.subtract), reads=["gcb"], writes=["eglb"])
        tk.op("act", lambda e: e.activation(out=eglb[:], in_=eglb[:], func=AF.Exp), reads=["eglb"], writes=["eglb"])
        tk.op("dve", lambda e: e.tensor_tensor(out=begb[:], in0=betab[:], in1=egcb[:], op=ALU.mult),
              reads=["betab", ek], writes=["gb"])
        pn, pt = pp.next()
        srcs = [(betab, "betab"), (gcb, "gcb"), (egcb, ek), (eglb, "eglb"), (begb, "gb")]
        tk.ops("pe", [lambda e, q=q, s=s, pt=pt, src=src: e.transpose(
            pt[:, (q * 4 + s) * 16:(q * 4 + s + 1) * 16], src[0:16, s * 128:(s + 1) * 128], ident[0:16, 0:16])
            for q, (src, _) in enumerate(srcs) for s in range(4)],
            reads=[k_ for _, k_ in srcs] + ["ident"], writes=[pn])
        tk.op("dve", lambda e, pt=pt: e.tensor_copy(
            out=cols[:].rearrange("p q s -> p (q s)"),
            in_=pt[:, 0:320].rearrange("p (n w) -> p n w", w=16)[:, :, 0]), reads=[pn], writes=["cols"])

        def colb(q):
            return cols[:, q, :].unsqueeze(2).to_broadcast([128, 4, 128])

        yield
        pn, pt = pp.next()
        tk.ops("pe", [lambda e, s=s, pt=pt: e.transpose(pt[:, s * 128:(s + 1) * 128], kn[:, s * 128:(s + 1) * 128],
                                                        ident[:]) for s in range(4)], reads=["kn", "ident"],
               writes=[pn])
        pt3 = pt[:, :].rearrange("p (s d) -> p s d", d=128)
        tk.op("dve", lambda e, pt3=pt3: e.tensor_tensor(out=Kbe[:].bitcast(F32R), in0=pt3, in1=colb(4), op=ALU.mult),
              reads=[pn, "cols"], writes=["Kbe"])
        tk.op("dve", lambda e, pt3=pt3: e.tensor_tensor(out=Kd[:], in0=pt3, in1=colb(3), op=ALU.mult),
              reads=[pn, "cols"], writes=[kdk])
        pn, pt = pp.next()
        tk.ops("pe", [lambda e, s=s, pt=pt: e.transpose(pt[:, s * 128:(s + 1) * 128], vs[:, s * 128:(s + 1) * 128],
                                                        ident[:]) for s in range(4)], reads=["vs", "ident"],
               writes=[pn])
        pt3 = pt[:, :].rearrange("p (s d) -> p s d", d=128)
        tk.op("dve", lambda e, pt3=pt3: e.tensor_tensor(out=Vb[:].bitcast(F32R), in0=pt3, in1=colb(0), op=ALU.mult),
              reads=[pn, "cols"], writes=["Vb"])
        yield
        gcb3 = gcb[:].rearrange("p (s t) -> p s t", t=128)
        tk.op("dve", lambda e: e.tensor_tensor(out=tdf[:], in0=gcb3, in1=colb(1), op=ALU.subtract),
              reads=["gcb", "cols"], writes=["tdf"])
        tk.op("dve", lambda e: e.tensor_scalar(out=DT[:], in0=tdf[:], scalar1=0.0, scalar2=None, op0=ALU.min),
              reads=["tdf"], writes=["DT"])
        tk.op("act", lambda e: e.activation(out=DT[:], in_=DT[:], func=AF.Exp), reads=["DT"], writes=["DT"])
        tk.op("dve", lambda e: e.tensor_scalar(out=Dm[:], in0=tdf[:], scalar1=0.0, scalar2=None, op0=ALU.max),
              reads=["tdf"], writes=["Dm"])
        tk.op("act", lambda e: e.activation(out=Dm[:], in_=Dm[:], func=AF.Exp, scale=-1.0), reads=["Dm"],
              writes=["Dm"])
        yield
        pnk, ptk = pp.next()
        tk.ops("pe", [lambda e, s=s, ptk=ptk: e.matmul(ptk[:, s * 128:(s + 1) * 128], lhsT=kn[:, s * 128:(s + 1) * 128].bitcast(F32R),
                                                       rhs=kn[:, s * 128:(s + 1) * 128].bitcast(F32R), start=True, stop=True)
                      for s in range(4)], reads=["kn"], writes=[pnk])
        pnq, ptq = pp.next()
        tk.ops("pe", [lambda e, s=s, ptq=ptq: e.matmul(ptq[:, s * 128:(s + 1) * 128], lhsT=kn[:, s * 128:(s + 1) * 128].bitcast(F32R),
                                                       rhs=qn[:, s * 128:(s + 1) * 128].bitcast(F32R), start=True, stop=True)
                      for s in range(4)], reads=["kn", "qn"], writes=[pnq])
        kk3 = ptk[:, :].rearrange("p (s d) -> p s d", d=128)
        qk3 = ptq[:, :].rearrange("p (s d) -> p s d", d=128)
        b3 = betab[:].rearrange("p (s t) -> p s t", t=128)
        tk.op("dve", lambda e: e.tensor_tensor(out=E1[:], in0=DT[:], in1=b3, op=ALU.mult), reads=["DT", "betab"],
              writes=["tdf"])
        tk.op("dve", lambda e: e.tensor_tensor(out=E1[:], in0=E1[:], in1=mus_neg, op=ALU.mult),
              reads=["tdf", "mus"], writes=["tdf"])
        tk.op("dve", lambda e: e.tensor_tensor(out=Bm[0][:].bitcast(F32R), in0=kk3, in1=E1[:], op=ALU.mult), reads=[pnk, "tdf"],
              writes=["Bm0"])
        tk.op("dve", lambda e: e.tensor_tensor(out=DT[:], in0=DT[:], in1=mu_inc, op=ALU.mult),
              reads=["DT", "mui", "tdf"], writes=["DT"])
        tk.op("dve", lambda e: e.tensor_tensor(out=QKT[:], in0=qk3, in1=DT[:], op=ALU.mult), reads=[pnq, "DT"],
              writes=[qkk])
        tk.op("dve", lambda e: e.tensor_tensor(out=Dm[:], in0=Dm[:], in1=mls_neg, op=ALU.mult),
              reads=["Dm", "mls"], writes=["Dm"])
        tk.op("dve", lambda e: e.tensor_tensor(out=Dm[:], in0=Dm[:], in1=colb(0), op=ALU.mult),
              reads=["Dm", "cols"], writes=["Dm"])
        tk.op("dve", lambda e: e.tensor_tensor(out=BTm[0][:].bitcast(F32R), in0=kk3, in1=Dm[:], op=ALU.mult), reads=[pnk, "Dm"],
              writes=["BTm0"])
        tk.op("dve", lambda e: e.tensor_tensor(out=Pm[0][:].bitcast(F32R), in0=Bm[0][:], in1=ident4, op=ALU.add),
              reads=["Bm0", "ident"], writes=["Pm0"])
        yield
        cb = 0
        cp = 0
        for k in range(1, 6):
            nb = 1 - cb
            if k < 5:
                pn1, pt1 = pp.next()
                tk.ops("pe", [lambda e, s=s, pt1=pt1, cb=cb: e.matmul(
                    pt1[:, s * 128:(s + 1) * 128], lhsT=BTm[cb][:, s, :].bitcast(F32R), rhs=Bm[cb][:, s, :].bitcast(F32R), start=True, stop=True)
                    for s in range(4)], reads=["Bm%d" % cb, "BTm%d" % cb], writes=[pn1])
            pn2, pt2 = pp.next()
            tk.ops("pe", [lambda e, s=s, pt2=pt2, cb=cb: e.matmul(
                pt2[:, s * 128:(s + 1) * 128], lhsT=Bm[cb][:, s, :].bitcast(F32R), rhs=BTm[cb][:, s, :].bitcast(F32R), start=True, stop=True)
                for s in range(4)], reads=["Bm%d" % cb, "BTm%d" % cb], writes=[pn2])
            if k < 5:
                tk.op("act", lambda e, pt1=pt1, nb=nb: e.copy(out=Bm[nb][:].rearrange("p s d -> p (s d)").bitcast(F32R), in_=pt1[:, :]),
                      reads=[pn1], writes=["Bm%d" % nb])
            tk.op("dve", lambda e, pt2=pt2, nb=nb: e.tensor_copy(out=BTm[nb][:].rearrange("p s d -> p (s d)").bitcast(F32R),
                                                                 in_=pt2[:, :]), reads=[pn2], writes=["BTm%d" % nb])
            pn3, pt3_ = pp.next()
            tk.ops("pe", [lambda e, s=s, pt3_=pt3_, nb=nb, cp=cp: e.matmul(
                pt3_[:, s * 128:(s + 1) * 128], lhsT=BTm[nb][:, s, :].bitcast(F32R), rhs=Pm[cp][:, s, :].bitcast(F32R), start=True, stop=True)
                for s in range(4)], reads=["BTm%d" % nb, "Pm%d" % cp], writes=[pn3])
            tk.op("dve", lambda e, pt3_=pt3_, cp=cp: e.tensor_tensor(
                out=Pm[1 - cp][:].rearrange("p s d -> p (s d)").bitcast(F32R), in0=Pm[cp][:].rearrange("p s d -> p (s d)"),
                in1=pt3_[:, :], op=ALU.add), reads=[pn3, "Pm%d" % cp], writes=["Pm%d" % (1 - cp)])
            cb = nb
            cp = 1 - cp
            yield
        Pf = Pm[cp]
        pk = "Pm%d" % cp
        yield
        pn, pt = pp.next()
        tk.ops("pe", [lambda e, s=s, pt=pt: e.matmul(pt[:, s * 128:(s + 1) * 128], lhsT=Pf[:, s, :].bitcast(F32R), rhs=Vb[:, s, :].bitcast(F32R),
                                                     start=True, stop=True) for s in range(4)],
               reads=[pk, "Vb"], writes=[pn])
        tk.op("act", lambda e, pt=pt: e.copy(out=U[:].rearrange("p s d -> p (s d)"), in_=pt[:, :]), reads=[pn],
              writes=[uk])
        pn, pt = pp.next()
        tk.ops("pe", [lambda e, s=s, pt=pt: e.matmul(pt[:, s * 128:(s + 1) * 128], lhsT=Kbe[:, s, :].bitcast(F32R), rhs=Pf[:, s, :].bitcast(F32R),
                                                     start=True, stop=True) for s in range(4)],
               reads=[pk, "Kbe"], writes=[pn])
        tk.op("act", lambda e, pt=pt: e.copy(out=WT[:], in_=pt[:, :]), reads=[pn], writes=[wk])
        tk.op("dve", lambda e: e.tensor_tensor(out=QdT[:], in0=qn[:], in1=egcb[:], op=ALU.mult),
              reads=["qn", ek], writes=[qdk])
        yield

    def prep_attn(c):
        ts = slice((c * T) % 4096, (c * T) % 4096 + T)
        rope_tables(c)
        yield
        pnx, ptx = proj(c, 7)
        for m, dstT, dk_, xo in ((4, qT, "qT", 0), (5, kT, "kT", 32)):
            pn, pt = proj(c, m)
            tk.op("act", lambda e, pt=pt, dstT=dstT: e.copy(out=dstT[32:64, ts], in_=pt[32:64, :]), reads=[pn],
                  writes=[K(dk_, c)])
            tk.op("act", lambda e, pt=pt, dstT=dstT: e.copy(out=dstT[64:128, ts], in_=pt[64:128, :]), reads=[pn],
                  writes=[K(dk_, c)])
            tk.op("dve", lambda e, pt=pt: e.tensor_tensor(out=rt1[:], in0=pt[0:32, :], in1=sincos[32:64, :],
                                                          op=ALU.mult), reads=[pn, "sincos"], writes=["rt1"])
            tk.op("dve", lambda e, ptx=ptx, xo=xo: e.tensor_tensor(out=rt2[:], in0=ptx[xo:xo + 32, :],
                                                                   in1=sincos[0:32, :], op=ALU.mult),
                  reads=[pnx, "sincos"], writes=["rt2"])
            tk.op("dve", lambda e, dstT=dstT: e.tensor_tensor(out=dstT[0:32, ts], in0=rt1[:], in1=rt2[:], op=ALU.add),
                  reads=["rt1", "rt2"], writes=[K(dk_, c)])
            yield
        pn, pt = proj(c, 6)
        tk.op("act", lambda e, pt=pt: e.copy(out=vT[:, ts], in_=pt[:, :]), reads=[pn], writes=[K("vT", c)])

        yield

    def scan(c):
        ts = slice(c * T, (c + 1) * T)
        par = c % 2
        U, WT, QdT, QKT, Kd, egcb, zs = U2[par], WT2[par], QdT2[par], QKT2[par], Kd2[par], egcb2[par], zs2[par]
        uk, wk, qdk, qkk, kdk, ek, zk = ("U%d" % par, "WT%d" % par, "QdT%d" % par, "QKT%d" % par, "Kd%d" % par,
                                         "egcb%d" % par, "zs%d" % par)
        ot = PS["ot"]
        sc_ = PS["scan"]
        for n in range(8):
            gi = c * 8 + n
            s, half = n // 2, n % 2
            r0 = 64 * half
            rows = slice(r0, r0 + 64)
            tsl = slice(64 * n, 64 * n + 64)
            Sc, Sn = S[gi % 2], S[(gi + 1) % 2]
            sk, snk = "S%d" % (gi % 2), "S%d" % ((gi + 1) % 2)
            slot = (gi % 2) * 256
            tk.op("pe", lambda e: e.matmul(sc_[rows, slot:slot + 128], lhsT=WT[:, tsl], rhs=Sc[:], start=True, stop=True),
                  reads=[wk, sk], writes=["ps_scan_a%d" % (gi % 2)])
            tk.op("dve", lambda e: e.tensor_tensor(out=vnew[rows, gi % 2, :], in0=U[rows, s, :],
                                                   in1=sc_[rows, slot:slot + 128], op=ALU.subtract),
                  reads=[uk, "ps_scan_a%d" % (gi % 2)], writes=["vnew%d" % (gi % 2)])
            tk.ops("pe", [
                lambda e: e.matmul(ot[:, tsl], lhsT=Sc[:], rhs=QdT[:, tsl], start=True, stop=False),
                lambda e: e.matmul(ot[:, tsl], lhsT=vnew[rows, gi % 2, :], rhs=QKT[rows, s, r0:r0 + 64], start=False,
                                   stop=True),
                lambda e: e.matmul(sc_[:, slot + 128:slot + 256], lhsT=Kd[rows, s, :], rhs=vnew[rows, gi % 2, :],
                                   start=True, stop=True)],
                reads=[sk, qdk, qkk, kdk, "vnew%d" % (gi % 2)], writes=["ps_ot", "ps_scan_b%d" % (gi % 2)])
            tl = 64 * n + 63
            tk.op("dve", lambda e: e.scalar_tensor_tensor(out=Sn[:], in0=Sc[:], scalar=egcb[:, tl:tl + 1],
                                                          in1=sc_[:, slot + 128:slot + 256], op0=ALU.mult, op1=ALU.add),
                  reads=[sk, ek, "ps_scan_b%d" % (gi % 2)], writes=[snk])
            yield
        tk.op("act", lambda e: e.activation(out=osq[:], in_=ot[:, :], func=AF.Square), reads=["ps_ot"], writes=["p1"])
        pn, pt = ppa.next()
        tk.op("pe", lambda e: e.matmul(pt[:, :], lhsT=ones_f[:], rhs=osq[:], start=True, stop=True),
              reads=["p1", "ones_f"], writes=[pn])
        tk.op("act", lambda e: e.activation(out=rs[:], in_=pt[:, :], func=AF.Ln, bias=RMS_EPS_AP[:, 0:1],
                                            scale=1.0 / 128.0), reads=[pn, "epsc"], writes=["p2"])
        tk.op("act", lambda e: e.activation(out=rs[:], in_=rs[:], func=AF.Exp, scale=-0.5), reads=["p2"],
              writes=["p2"])
        tk.op("dve", lambda e: e.scalar_tensor_tensor(out=oa[:], in0=ot[:, :], scalar=pvec[:, 2:3], in1=rs[:],
                                                      op0=ALU.mult, op1=ALU.mult), reads=["ps_ot", "pvec", "p2"],
              writes=["p3"])
        tk.op("dve", lambda e: e.tensor_tensor(out=oab[:], in0=oa[:], in1=zs[:], op=ALU.mult), reads=["p3", zk],
              writes=["oab"])
        if cin is None:
            tk.dma("sp", lambda e: e.dma_start(out=mixT[0:128, ts], in_=oab[:]), reads=["oab"], writes=["mixT_a"])
        else:
            tk.dma("sp", lambda e: e.dma_start(out=cin[c // 4][0:128, (c % 4) * T:(c % 4 + 1) * T], in_=oab[:]),
                   reads=["oab"], writes=[("cin_a", c // 4)])
        yield

    att_scale = 128.0 ** -0.5
    pcount = [0]

    def qblock(qsel, kcur, kprev, vcur, vprev, vkeys, accv, first, rkeys):
        i = pcount[0] % 2
        pcount[0] += 1
        pTb = pTt[i]
        pk = "pT%d" % i
        pn, pt = ppa.next()
        if kprev is not None:
            tk.ops("pe", [
                lambda e: e.matmul(pt[:, 0:256], lhsT=ident_b[:], rhs=amask[:, 0:256], start=True, stop=False),
                lambda e: e.matmul(pt[:, 0:128], lhsT=kprev, rhs=qsel, start=False, stop=False),
                lambda e: e.matmul(pt[:, 128:256], lhsT=kcur, rhs=qsel, start=False, stop=True)],
                reads=rkeys + ["ident_b", "amask"], writes=[pn])
            tk.op("act", lambda e: e.activation(out=pTb[:, 0:256], in_=pt[:, 0:256], func=AF.Exp, scale=att_scale),
                  reads=[pn], writes=[pk])
            pn2, pt2 = ppa.next()
            tk.ops("pe", [
                lambda e: e.matmul(pt2[:, 0:128], lhsT=vprev, rhs=pTb[:, 0:128], start=True, stop=False),
                lambda e: e.matmul(pt2[:, 0:128], lhsT=vcur, rhs=pTb[:, 128:256], start=False, stop=True),
                lambda e: e.matmul(pt2[:, 128:256], lhsT=ones_b[:], rhs=pTb[:, 0:128], start=True, stop=False),
                lambda e: e.matmul(pt2[:, 128:256], lhsT=ones_b[:], rhs=pTb[:, 128:256], start=False, stop=True)],
                reads=[pk, "ones_b"] + vkeys, writes=[pn2])
        else:
            tk.ops("pe", [
                lambda e: e.matmul(pt[:, 128:256], lhsT=ident_b[:], rhs=amask[:, 128:256], start=True, stop=False),
                lambda e: e.matmul(pt[:, 128:256], lhsT=kcur, rhs=qsel, start=False, stop=True)],
                reads=rkeys + ["ident_b", "amask"], writes=[pn])
            tk.op("act", lambda e: e.activation(out=pTb[:, 128:256], in_=pt[:, 128:256], func=AF.Exp, scale=att_scale),
                  reads=[pn], writes=[pk])
            pn2, pt2 = ppa.next()
            tk.ops("pe", [
                lambda e: e.matmul(pt2[:, 0:128], lhsT=vcur, rhs=pTb[:, 128:256], start=True, stop=True),
                lambda e: e.matmul(pt2[:, 128:256], lhsT=ones_b[:], rhs=pTb[:, 128:256], start=True, stop=True)],
                reads=[pk, "ones_b"] + vkeys, writes=[pn2])
        src = pt2[:, 0:256].rearrange("p (a q) -> p a q", a=2)
        if first:
            tk.op("dve", lambda e: e.tensor_copy(out=accv, in_=src), reads=[pn2], writes=["acc"])
        else:
            tk.op("dve", lambda e: e.tensor_tensor(out=accv, in0=accv, in1=src, op=ALU.add), reads=[pn2, "acc"],
                  writes=["acc"])

    vcnt = [0]

    def vtrans(vsel, rk):
        i = vcnt[0] % 4
        vcnt[0] += 1
        pn, pt = ppa.next()
        ptb = pt[:, 0:64].bitcast(BF16)
        tk.op("pe", lambda e: e.transpose(ptb, vsel, ident_b[:]), reads=rk + ["ident_b"], writes=[pn])
        tk.op("act", lambda e: e.copy(out=vtt[:, i, :], in_=ptb), reads=[pn], writes=["vtt%d" % i])
        return vtt[:, i, :], "vtt%d" % i

    def rsl(t0, n, step=1):
        r0 = t0 % RING
        return slice(r0, r0 + (n - 1) * step + 1, step)

    def attn(c):
        sc = c // 4
        for jj in range(4):
            j = 4 * c + jj
            t0 = 128 * j
            cprev = (t0 - 128) // T
            vc, vck = vtrans(vT[:, rsl(t0, 128)], [K("vT", c)])
            if j > 0:
                vp, vpk = vtrans(vT[:, rsl(t0 - 128, 128)], [K("vT", cprev)])
            else:
                vp, vpk = None, None
            rk = [K("qT", c), K("kT", c)] + ([K("kT", cprev)] if j > 0 else [])
            qblock(qT[:, rsl(t0, 128)], kT[:, rsl(t0, 128)], kT[:, rsl(t0 - 128, 128)] if j > 0 else None,
                   vc, vp, [k_ for k_ in (vck, vpk) if k_], acc[:, :, (t0 % 2048):(t0 % 2048) + 128], True, rk)
            yield
        for r in range(4):
            t0 = T * c + r
            vc, vck = vtrans(vT[:, rsl(t0, 128, 4)], [K("vT", c)])
            if c > 0:
                vp, vpk = vtrans(vT[:, rsl(t0 - T, 128, 4)], [K("vT", c - 1)])
            else:
                vp, vpk = None, None
            rk = [K("qT", c), K("kT", c)] + ([K("kT", c - 1)] if c > 0 else [])
            a0 = (T * c) % 2048 + r
            qblock(qT[:, rsl(t0, 128, 4)], kT[:, rsl(t0, 128, 4)], kT[:, rsl(t0 - T, 128, 4)] if c > 0 else None,
                   vc, vp, [k_ for k_ in (vck, vpk) if k_], acc[:, :, a0:a0 + 509:4], False, rk)
            yield
        if c % 4 == 3:
            cs = [4 * sc + i for i in range(4)]
            csp = [4 * (sc - 1) + i for i in range(4)] if sc > 0 else []
            for r in range(16):
                t0 = 2048 * sc + r
                vc, vck = vtrans(vT[:, rsl(t0, 128, 16)], [K("vT", ci) for ci in cs])
                if sc > 0:
                    vp, vpk = vtrans(vT[:, rsl(t0 - 2048, 128, 16)], [K("vT", ci) for ci in csp])
                else:
                    vp, vpk = None, None
                rk = [K("qT", ci) for ci in cs] + [K("kT", ci) for ci in cs] + [K("kT", ci) for ci in csp]
                qblock(qT[:, rsl(t0, 128, 16)], kT[:, rsl(t0, 128, 16)],
                       kT[:, rsl(t0 - 2048, 128, 16)] if sc > 0 else None,
                       vc, vp, [k_ for k_ in (vck, vpk) if k_], acc[:, :, r:r + 2033:16], False, rk)
                yield
            for i in range(4):
                a = slice(i * T, (i + 1) * T)
                tsl = slice(2048 * sc + i * T, 2048 * sc + (i + 1) * T)
                tk.op("act", lambda e, a=a: e.activation(out=fr[:], in_=acc[:, 1, a], func=AF.Ln), reads=["acc"],
                      writes=["p2"])
                tk.op("act", lambda e: e.activation(out=fr[:], in_=fr[:], func=AF.Exp, scale=-1.0), reads=["p2"],
                      writes=["p2"])
                tk.op("dve", lambda e, a=a: e.tensor_tensor(out=fo[:], in0=acc[:, 0, a], in1=fr[:], op=ALU.mult),
                      reads=["acc", "p2"], writes=["p3"])
                tk.op("act", lambda e: e.activation(out=fsq[:], in_=fo[:], func=AF.Square), reads=["p3"],
                      writes=["p1"])
                pn, pt = ppa.next()
                tk.op("pe", lambda e, pt=pt: e.matmul(pt[:, :], lhsT=ones_f[:], rhs=fsq[:], start=True, stop=True),
                      reads=["p1", "ones_f"], writes=[pn])
                tk.op("act", lambda e, pt=pt: e.activation(out=fr[:], in_=pt[:, :], func=AF.Ln,
                                                           bias=RMS_EPS_AP[:, 0:1], scale=1.0 / 128.0),
                      reads=[pn, "epsc"], writes=["p2"])
                tk.op("act", lambda e: e.activation(out=fr[:], in_=fr[:], func=AF.Exp, scale=-0.5), reads=["p2"],
                      writes=["p2"])
                tk.op("dve", lambda e: e.scalar_tensor_tensor(out=fob[:], in0=fo[:], scalar=pvec[:, 3:4], in1=fr[:],
                                                              op0=ALU.mult, op1=ALU.mult), reads=["p3", "pvec", "p2"],
                      writes=["fob"])
                yield
                if cin is None:
                    tk.dma("sp", lambda e, tsl=tsl: e.dma_start(out=mixT[128:256, tsl], in_=fob[:]), reads=["fob"],
                           writes=["mixT_b"])
                else:
                    tk.dma("sp", lambda e, i=i: e.dma_start(out=cin[sc][128:256, i * T:(i + 1) * T], in_=fob[:]),
                           reads=["fob"], writes=[("cin_b", sc)])
            if on_quarter is not None:
                on_quarter(sc, tk)
        yield

    epsc = sb("epsc", [128, 2])
    tk.op("pool", lambda e: e.memset(epsc[:, 0:1], RMS_EPS), writes=["epsc"])
    tk.op("pool", lambda e: e.memset(epsc[:, 1:2], 1.0), writes=["epsc"])
    RMS_EPS_AP = epsc[:, 0:1]
    ONE_AP = epsc[:, 1:2]

    def run(gen):
        if gen is not None:
            for _ in gen:
                pass

    load_x(0)
    run(prep_main(0))
    tk.flush()
    for c in range(nch):
        run(scan(c))
        run(prep_attn(c))
        run(attn(c))
        if c + 1 < nch:
            run(prep_main(c + 1))
        tk.flush()
    tk = real_tk


def build_phase1(nch=NCH):
    nc = bass.Bass("TRN2", target_bir_lowering=False)
    D = {}

    def din(name, shape, dt=F32):
        D[name] = nc.dram_tensor(name, shape, dt, kind="ExternalInput").ap()

    din("x", [D_MODEL, SEQ]); din("cT", [128, 8]); din("pos", [SEQ], I32)
    din("wada1", [128, 8, 8, 256]); din("bada1", [128, 16]); din("win", [128, 5, 8, 256])
    din("convw", [128, 3, 4]); din("pvec", [128, 4])
    din("c_ident", [128, 128]); din("c_masks", [128, 3, 128]); din("c_amask", [128, 256]); din("c_invf", [64, 2])
    D["mixT"] = nc.dram_tensor("mixT", [256, SEQ], BF16, kind="ExternalOutput").ap()
    with ExitStack() as es:
        tk = Trk(nc, es)
        phase1(nc, tk, es, D, nch=nch)
        tk.finish("sp")
    return nc


def consts():
    p = np.arange(128)[:, None]
    f = np.arange(128)[None, :]
    same = (p // 64) == (f // 64)
    mus = -((f > p) & same).astype(np.float32)
    mui = ((f >= p) & same).astype(np.float32)
    mls = -((f < p) & same).astype(np.float32)
    masks = np.stack([mus, mui, mls], 1).astype(np.float32)
    prev = np.where(p >= f, 0.0, NEG).astype(np.float32)
    cur = np.where(p <= f, 0.0, NEG).astype(np.float32)
    amask = np.concatenate([prev, cur], 1)
    i = (np.arange(64) % 16).astype(np.float32)
    invf = (ROPE_THETA ** (-i * 2.0 / 32.0)).astype(np.float32)
    invf = np.stack([invf, np.where(np.arange(64) < 32, 0.0, math.pi / 2).astype(np.float32)], 1)
    return {"c_ident": np.eye(128, dtype=np.float32), "c_masks": masks, "c_amask": amask, "c_invf": invf}


def phase1_inputs(inp, core):
    b, h = core // 4, core % 4
    l = 0
    w_in = inp["w_in"][l]
    hs = slice(h * 128, (h + 1) * 128)
    qa, ka, va, z = w_in[:, 0:512][:, hs], w_in[:, 512:1024][:, hs], w_in[:, 1024:1536][:, hs], w_in[:, 1536:2048][:, hs]
    beta = w_in[:, 2048 + h:2049 + h]
    dec = w_in[:, 2052 + h:2053 + h]
    qb, kb, vb = w_in[:, 2056:2568][:, hs], w_in[:, 2568:3080][:, hs], w_in[:, 3080:3592][:, hs]
    extra = np.zeros((D_MODEL, 128), np.float32)
    extra[:, 0:16] = qb[:, 16:32]; extra[:, 16:32] = qb[:, 0:16]
    extra[:, 32:48] = kb[:, 16:32]; extra[:, 48:64] = kb[:, 0:16]
    win = np.concatenate([qa, ka, va, z, qb, kb, vb, extra, np.repeat(beta, 128, 1), np.repeat(dec, 128, 1)], 1)
    conv = inp["conv_w"][l]
    convw = np.stack([conv[:, g * 512:(g + 1) * 512][:, hs].T for g in range(3)], 1)
    pvec = np.stack([np.full(128, inp["a_log"][l, h]), np.full(128, inp["dt_bias"][l, h]),
                     inp["gdn_norm_w"][l], inp["attn_norm_w"][l]], 1).astype(np.float32)
    win_l = win.astype(np.float32).reshape(8, 128, 5, 256).transpose(1, 2, 0, 3)
    wada1_l = inp["w_ada"][l][:, 0:2048].reshape(8, 128, 8, 256).transpose(1, 2, 0, 3)
    d = {"x": np.ascontiguousarray(inp["x"][b].T), "cT": np.ascontiguousarray(inp["c"][b].reshape(8, 128).T),
         "pos": np.ascontiguousarray(inp["positions"][b]).astype(np.int32),
         "wada1": np.ascontiguousarray(wada1_l),
         "bada1": np.ascontiguousarray(inp["b_ada"][l][0:2048].reshape(16, 128).T),
         "win": np.ascontiguousarray(win_l), "convw": np.ascontiguousarray(convw.astype(np.float32)),
         "pvec": np.ascontiguousarray(pvec)}
    d.update(consts())
    return d


NT2 = 16
NBLK = 64


def phase2(nc, tk, es, D, ntile=NT2, nblk=NBLK, cout=None):
    real = tk
    tk = Rec(real)

    def mk(stack, pref):
        def sb(name, shape, dt=F32):
            return stack.enter_context(nc.sbuf_tensor(pref + name, shape, dt))
        return sb

    sb = mk(es, "t_")
    pp = PsumPool(nc, es, ["r0", "r1", "r2", "r3", "r4", "r5", "r6", "r7"])

    ident = sb("ident", [128, 128])
    ones_f = sb("ones_f", [128, 128])
    triS = sb("triS", [128, 128])
    kcoff = sb("kcoff", [128, 1])
    thr16 = sb("thr16", [128, 16])
    thr64 = sb("thr64", [128, 64])
    epsc = sb("epsc", [128, 1])
    cT = sb("cT", [128, 8]); scT = sb("scT", [128, 8])
    modb = sb("modb", [128, 4, 1024])
    lnp = sb("lnp", [128, 4, 1024])
    xt2 = [sb("xt%d" % i, [128, 1024]) for i in range(2)]
    rr2 = [sb("rr%d" % i, [128, 1024]) for i in range(2)]
    x12 = [sb("x1%d" % i, [128, 1024]) for i in range(2)]
    h22 = [sb("h2%d" % i, [128, 1024]) for i in range(2)]
    stt = sb("stt", [128, 2, 6]); mv = sb("mv", [128, 2]); rstd = sb("rstd", [128, 1])
    gts = sb("gts", [128, NT2, 2])
    desti = sb("desti", [128, 2, NT2], I32)
    idxw = sb("idxw", [128, 64], I32)

    x = D["x2"]; mixTf = D.get("mixTf"); out = D["out"]
    x1d = D["x1d"]; h2d = D["h2d"]; xs = D["xs"]; ys = D["ys"]

    def layer_norm(src, skey, dst, dkey, gi):
        tk.op("dve", lambda e: e.bn_stats(out=stt[:, 0, :], in_=src[:, 0:512]), reads=[skey], writes=["stt"])
        tk.op("dve", lambda e: e.bn_stats(out=stt[:, 1, :], in_=src[:, 512:1024]), reads=[skey], writes=["stt"])
        tk.op("dve", lambda e: e.bn_aggr(out=mv[:], in_=stt[:].rearrange("p a b -> p (a b)")), reads=["stt"],
              writes=["mv"])
        tk.op("act", lambda e: e.activation(out=rstd[:], in_=mv[:, 1:2], func=AF.Sqrt, bias=epsc[:, 0:1], scale=1.0),
              reads=["mv", "epsc"], writes=["rstd"])
        tk.op("dve", lambda e: e.reciprocal(out=rstd[:], in_=rstd[:]), reads=["rstd"], writes=["rstd"])
        tk.op("dve", lambda e: e.tensor_scalar(out=dst[:], in0=src[:], scalar1=mv[:, 0:1], scalar2=rstd[:, 0:1],
                                               op0=ALU.subtract, op1=ALU.mult), reads=[skey, "mv", "rstd"],
              writes=[dkey])
        tk.op("dve", lambda e: e.tensor_tensor(out=dst[:], in0=dst[:], in1=lnp[:, gi, :], op=ALU.mult),
              reads=[dkey, "lnp"], writes=[dkey])
        tk.op("dve", lambda e: e.tensor_tensor(out=dst[:], in0=dst[:], in1=lnp[:, gi + 1, :], op=ALU.add),
              reads=[dkey, "lnp"], writes=[dkey])

    tk.dma("sp", lambda e: e.dma_start(out=ident[:], in_=D["c_ident"][:, :]), writes=["ident"])
    tk.dma("sp", lambda e: e.dma_start(out=triS[:], in_=D["c_tri"][:, :]), writes=["triS"])
    tk.dma("sp", lambda e: e.dma_start(out=kcoff[:], in_=D["c_iota"][:, :]), writes=["kcoff"])
    tk.dma("sp", lambda e: e.dma_start(out=thr16[:], in_=D["c_thr16"][:, :]), writes=["thr16"])
    tk.dma("sp", lambda e: e.dma_start(out=thr64[:], in_=D["c_thr64"][:, :]), writes=["thr64"])
    tk.dma("sp", lambda e: e.dma_start(out=cT[:], in_=D["cT"][:, :]), writes=["cT"])
    for i in range(4):
        tk.dma("sp", lambda e, i=i: e.dma_start(out=lnp[:, i, :], in_=D["lnp"][i, :].partition_broadcast(128)),
               writes=["lnp"])
    tk.op("pool", lambda e: e.memset(ones_f[:], 1.0), writes=["ones_f"])
    tk.op("pool", lambda e: e.memset(epsc[:], LN_EPS), writes=["epsc"])

    with ExitStack() as esA:
        sa = mk(esA, "a_")
        scR = sa("scR", [128, 8, 128])
        badab = sa("badab", [128, 512])
        wo_b = sa("wo_b", [128, 8, 1024], BF16)
        wr = sa("wr", [128, 8, 36]); brb = sa("brb", [128, 36])
        h2T2 = [sa("h2T%d" % i, [128, 8, 128]) for i in range(2)]
        lg = sa("lg", [128, 36]); gmax = sa("gmax", [128, 1]); ngmax = sa("ngmax", [128, 1]); ohg = sa("ohg", [128, 4])
        ex4 = sa("ex4", [128, 4]); sume = sa("sume", [128, 1]); ggrp = sa("ggrp", [128, 1])
        tmp48 = sa("tmp48", [128, 4, 8]); lesel = sa("lesel", [128, 8]); m8 = sa("m8", [128, 8])
        oh8 = sa("oh8", [128, 2, 8]); e2 = sa("e2", [128, 1]); g1 = sa("g1", [128, 1])
        ohs = sa("ohs", [128, NT2, 2, 32]); ohany = sa("ohany", [128, 32]); cum = sa("cum", [128, 32])
        ranks = sa("ranks", [128, NT2, 32])
        cntb = sa("cntb", [128, 32]); cmp16 = sa("cmp16", [128, 32, 16]); padded = sa("padded", [128, 32])
        onesr = sa("onesr", [128, 32]); padend = sa("padend", [128, 32]); padstart = sa("padstart", [128, 32])
        tmpd = sa("tmpd", [128, NT2, 32]); tmpd2 = sa("tmpd2", [128, NT2, 32])
        destf = sa("destf", [128, 2, NT2])
        cmp64 = sa("cmp64", [128, 64, 32]); blkf = sa("blkf", [128, 64])

        tk.dma("sp", lambda e: e.dma_start(out=wr[:], in_=D["wr"][:, :].rearrange("(kc p) c -> p kc c", p=128)),
               writes=["wr"])
        tk.dma("sp", lambda e: e.dma_start(out=brb[:], in_=D["br"][:].partition_broadcast(128)), writes=["brb"])
        tk.op("pool", lambda e: e.memset(cum[:], 0.0), writes=["cum"])
        tk.op("pool", lambda e: e.memset(onesr[:], 1.0), writes=["onesr"])
        if cout is None:
            mt1 = sa("mt", [128, 8, 128], BF16)
        else:
            mtf = sa("mtf", [128, 8, 2048], BF16)
            qidx = sa("qidx", [128, 8], I32)
            tk.dma("sp", lambda e: e.dma_start(out=qidx[:], in_=D["qidx"][:, :]), writes=["qidx"])
            for kc in range(8):
                tk.dma("pool", lambda e, kc=kc: e.indirect_dma_start(
                    out=mtf[:, kc, :], out_offset=None, in_=cout[:, :],
                    in_offset=bass.IndirectOffsetOnAxis(ap=qidx[:, kc:kc + 1], axis=0)),
                    reads=["qidx"] + [("cout", q_) for q_ in range(4)], writes=["mt"])
        stg = [(xt2[0], "xt0"), (rr2[0], "rr0"), (xt2[1], "xt1"), (rr2[1], "rr1")]
        for kc in range(8):
            st, key = stg[kc % 4]
            tk.dma("sp", lambda e, st=st, kc=kc: e.dma_start(out=st[:], in_=D["wo"][kc * 128:(kc + 1) * 128, :]),
                   writes=[key])
            tk.op("dve", lambda e, st=st, kc=kc: e.tensor_copy(out=wo_b[:, kc, :], in_=st[:]), reads=[key],
                  writes=["wo_b"])
        tk.op("act", lambda e: e.activation(out=scT[:], in_=cT[:], func=AF.Silu), reads=["cT"], writes=["scT"])
        tk.op("dve", lambda e: e.tensor_copy(out=scR[:], in_=scT[:].unsqueeze(2).to_broadcast([128, 8, 128])),
              reads=["scT"], writes=["scR"])
        for blk in range(8):
            pn, pt = pp.next()
            for sub in range(4):
                st, key = stg[sub % 4]
                st3 = st[:].rearrange("p (kc c) -> p kc c", c=128)
                c0 = blk * 512 + sub * 128
                tk.dma("sp" if sub % 2 == 0 else "pool", lambda e, st3=st3, blk=blk, sub=sub: e.dma_start(
                    out=st3, in_=D["wada2"][:, blk * 4 + sub, :, :]), writes=[key])
                tk.ops("pe", [lambda e, kc=kc, pt=pt, st3=st3, sub=sub: e.matmul(
                    pt[:, sub * 128:(sub + 1) * 128], lhsT=scR[:, kc, :], rhs=st3[:, kc, :], start=(kc == 0),
                    stop=(kc == 7)) for kc in range(8)], reads=[key, "scR"], writes=[pn])
            tk.dma("sp", lambda e, blk=blk: e.dma_start(
                out=badab[:], in_=D["bada2"][blk * 512:(blk + 1) * 512].partition_broadcast(128)), writes=["badab"])
            dst = modb[:, blk // 2, (blk % 2) * 512:(blk % 2 + 1) * 512]
            tk.op("dve", lambda e, pt=pt, dst=dst: e.tensor_tensor(out=dst, in0=pt[:, :], in1=badab[:], op=ALU.add),
                  reads=[pn, "badab"], writes=["modb"])
            if blk // 2 != 1:
                tk.op("dve", lambda e, dst=dst: e.tensor_scalar(out=dst, in0=dst, scalar1=1.0, scalar2=None,
                                                                op0=ALU.add), reads=["modb"], writes=["modb"])
        tk.flush()

        for k in range(ntile):
            par = k % 2
            xt, rr, x1, h2, h2T = xt2[par], rr2[par], x12[par], h22[par], h2T2[par]
            xtk, rrk, x1k, h2k, h2Tk = "xt%d" % par, "rr%d" % par, "x1%d" % par, "h2%d" % par, "h2T%d" % par
            tsl = slice(k * 128, (k + 1) * 128)
            if cout is None:
                mt = mt1
                tk.dma("sp", lambda e: e.dma_start(out=mt[:], in_=mixTf[:, tsl].rearrange("(kc p) t -> p kc t", p=128)),
                       writes=["mt"])
            else:
                mt = mtf[:, :, tsl]
            tk.dma("sp", lambda e: e.dma_start(out=xt[:], in_=x[tsl, :]), writes=[xtk])
            pa, pta = pp.next()
            pb, ptb = pp.next()
            for (pn, pt, hs) in ((pa, pta, slice(0, 512)), (pb, ptb, slice(512, 1024))):
                tk.ops("pe", [lambda e, kc=kc, pt=pt, hs=hs: e.matmul(pt[:, :], lhsT=mt[:, kc, :], rhs=wo_b[:, kc, hs],
                                                                     start=(kc == 0), stop=(kc == 7))
                              for kc in range(8)], reads=["mt", "wo_b"], writes=[pn])
                tk.op("dve", lambda e, pt=pt, hs=hs: e.tensor_tensor(out=rr[:, hs], in0=pt[:, :], in1=modb[:, 0, hs],
                                                                     op=ALU.mult), reads=[pn, "modb"], writes=[rrk])
            tk.op("dve", lambda e: e.scalar_tensor_tensor(out=rr[:], in0=xt[:], scalar=ALPHA, in1=rr[:], op0=ALU.mult,
                                                          op1=ALU.add), reads=[xtk, rrk], writes=[rrk])
            layer_norm(rr, rrk, x1, x1k, 0)
            tk.dma("sp", lambda e: e.dma_start(out=x1d[tsl, :], in_=x1[:]), reads=[x1k], writes=["x1d"])
            tk.op("dve", lambda e: e.tensor_tensor(out=h2[:], in0=x1[:], in1=modb[:, 2, :], op=ALU.mult),
                  reads=[x1k, "modb"], writes=[h2k])
            tk.op("dve", lambda e: e.tensor_tensor(out=h2[:], in0=h2[:], in1=modb[:, 1, :], op=ALU.add),
                  reads=[h2k, "modb"], writes=[h2k])
            tk.dma("sp", lambda e: e.dma_start(out=h2d[tsl, :], in_=h2[:]), reads=[h2k], writes=["h2d"])
            for half in range(2):
                pn, pt = pp.next()
                tk.ops("pe", [lambda e, j=j, pt=pt, half=half: e.transpose(
                    pt[:, j * 128:(j + 1) * 128], h2[:, (half * 4 + j) * 128:(half * 4 + j + 1) * 128], ident[:])
                    for j in range(4)], reads=[h2k, "ident"], writes=[pn])
                tk.op("act", lambda e, pt=pt, half=half: e.copy(
                    out=h2T[:, half * 4:(half + 1) * 4, :].rearrange("p a b -> p (a b)"), in_=pt[:, :]), reads=[pn],
                    writes=[h2Tk])
            pn, pt = pp.next()
            tk.ops("pe", [lambda e, kc=kc, pt=pt: e.matmul(pt[:, 0:36], lhsT=h2T[:, kc, :], rhs=wr[:, kc, :],
                                                          start=(kc == 0), stop=(kc == 7)) for kc in range(8)],
                   reads=[h2Tk, "wr"], writes=[pn])
            tk.op("dve", lambda e, pt=pt: e.tensor_tensor(out=lg[:], in0=pt[:, 0:36], in1=brb[:], op=ALU.add),
                  reads=[pn, "brb"], writes=["lg"])
            tk.op("dve", lambda e: e.tensor_reduce(out=gmax[:], in_=lg[:, 0:4], axis=AX.X, op=ALU.max), reads=["lg"],
                  writes=["gmax"])
            tk.op("dve", lambda e: e.tensor_scalar(out=ohg[:], in0=lg[:, 0:4], scalar1=gmax[:, 0:1], scalar2=None,
                                                   op0=ALU.is_equal), reads=["lg", "gmax"], writes=["ohg"])
            tk.op("dve", lambda e: e.tensor_scalar(out=ngmax[:], in0=gmax[:], scalar1=-1.0, scalar2=None, op0=ALU.mult),
                  reads=["gmax"], writes=["ngmax"])
            tk.op("act", lambda e: e.activation(out=ex4[:], in_=lg[:, 0:4], func=AF.Exp, bias=ngmax[:, 0:1], scale=1.0),
                  reads=["lg", "ngmax"], writes=["ex4"])
            tk.op("dve", lambda e: e.tensor_reduce(out=sume[:], in_=ex4[:], axis=AX.X, op=ALU.add), reads=["ex4"],
                  writes=["sume"])
            tk.op("dve", lambda e: e.reciprocal(out=ggrp[:], in_=sume[:]), reads=["sume"], writes=["ggrp"])
            le3 = lg[:, 4:36].rearrange("p (g e) -> p g e", e=8)
            tk.op("dve", lambda e: e.tensor_tensor(out=tmp48[:], in0=le3,
                                                   in1=ohg[:].unsqueeze(2).to_broadcast([128, 4, 8]), op=ALU.mult),
                  reads=["lg", "ohg"], writes=["tmp48"])
            tk.op("dve", lambda e: e.tensor_reduce(out=lesel[:], in_=tmp48[:].rearrange("p g e -> p e g"), axis=AX.X,
                                                   op=ALU.add), reads=["tmp48"], writes=["lesel"])
            tk.op("dve", lambda e: e.max(out=m8[:], in_=lesel[:]), reads=["lesel"], writes=["m8"])
            for j in range(2):
                tk.op("dve", lambda e, j=j: e.tensor_scalar(out=oh8[:, j, :], in0=lesel[:], scalar1=m8[:, j:j + 1],
                                                            scalar2=None, op0=ALU.is_equal), reads=["lesel", "m8"],
                      writes=["oh8"])
            tk.op("dve", lambda e: e.tensor_tensor(out=e2[:], in0=m8[:, 1:2], in1=m8[:, 0:1], op=ALU.subtract),
                  reads=["m8"], writes=["e2"])
            tk.op("act", lambda e: e.activation(out=e2[:], in_=e2[:], func=AF.Exp), reads=["e2"], writes=["e2"])
            tk.op("dve", lambda e: e.tensor_scalar(out=g1[:], in0=e2[:], scalar1=1.0, scalar2=None, op0=ALU.add),
                  reads=["e2"], writes=["g1"])
            tk.op("dve", lambda e: e.reciprocal(out=g1[:], in_=g1[:]), reads=["g1"], writes=["g1"])
            tk.op("dve", lambda e: e.tensor_tensor(out=gts[:, k, 0:1], in0=g1[:], in1=ggrp[:], op=ALU.mult),
                  reads=["g1", "ggrp"], writes=["gts"])
            tk.op("dve", lambda e: e.tensor_tensor(out=e2[:], in0=e2[:], in1=g1[:], op=ALU.mult), reads=["e2", "g1"],
                  writes=["e2"])
            tk.op("dve", lambda e: e.tensor_tensor(out=gts[:, k, 1:2], in0=e2[:], in1=ggrp[:], op=ALU.mult),
                  reads=["e2", "ggrp"], writes=["gts"])
            for j in range(2):
                tk.op("dve", lambda e, j=j: e.tensor_tensor(
                    out=ohs[:, k, j, :].rearrange("p (g e) -> p g e", e=8),
                    in0=ohg[:].unsqueeze(2).to_broadcast([128, 4, 8]),
                    in1=oh8[:, j, :].unsqueeze(1).to_broadcast([128, 4, 8]), op=ALU.mult), reads=["ohg", "oh8"],
                    writes=["ohs"])
            tk.op("dve", lambda e: e.tensor_tensor(out=ohany[:], in0=ohs[:, k, 0, :], in1=ohs[:, k, 1, :], op=ALU.add),
                  reads=["ohs"], writes=["ohany"])
            pn, pt = pp.next()
            tk.ops("pe", [lambda e, pt=pt: e.matmul(pt[:, 0:32], lhsT=triS[:], rhs=ohany[:], start=True, stop=False),
                          lambda e, pt=pt: e.matmul(pt[:, 0:32], lhsT=ones_f[:], rhs=cum[:], start=False, stop=True)],
                   reads=["triS", "ohany", "ones_f", "cum"], writes=[pn])
            tk.op("act", lambda e, pt=pt: e.copy(out=ranks[:, k, :], in_=pt[:, 0:32]), reads=[pn], writes=["ranks"])
            tk.op("dve", lambda e: e.tensor_tensor(out=cum[:], in0=cum[:], in1=ohany[:], op=ALU.add),
                  reads=["cum", "ohany"], writes=["cum"])
            if k % 8 == 7:
                tk.flush()

        pn, pt = pp.next()
        tk.op("pe", lambda e: e.matmul(pt[:, 0:32], lhsT=ones_f[:], rhs=cum[:], start=True, stop=True),
              reads=["ones_f", "cum"], writes=[pn])
        tk.op("dve", lambda e: e.tensor_copy(out=cntb[:], in_=pt[:, 0:32]), reads=[pn], writes=["cntb"])
        tk.op("dve", lambda e: e.tensor_tensor(out=cmp16[:], in0=cntb[:].unsqueeze(2).to_broadcast([128, 32, 16]),
                                               in1=thr16[:].unsqueeze(1).to_broadcast([128, 32, 16]), op=ALU.is_gt),
              reads=["cntb", "thr16"], writes=["cmp16"])
        tk.op("dve", lambda e: e.tensor_reduce(out=padded[:], in_=cmp16[:], axis=AX.X, op=ALU.add), reads=["cmp16"],
              writes=["padded"])
        tk.op("dve", lambda e: e.tensor_scalar(out=padded[:], in0=padded[:], scalar1=128.0, scalar2=None, op0=ALU.mult),
              reads=["padded"], writes=["padded"])
        tk.op("dve", lambda e: e.tensor_tensor_scan(out=padend[:], data0=onesr[:], data1=padded[:], initial=0.0,
                                                    op0=ALU.mult, op1=ALU.add), reads=["onesr", "padded"],
              writes=["padend"])
        tk.op("dve", lambda e: e.tensor_tensor(out=padstart[:], in0=padend[:], in1=padded[:], op=ALU.subtract),
              reads=["padend", "padded"], writes=["padstart"])
        tk.op("dve", lambda e: e.tensor_tensor(out=tmpd[:, 0:ntile, :], in0=ranks[:, 0:ntile, :],
                                               in1=padstart[:].unsqueeze(1).to_broadcast([128, ntile, 32]), op=ALU.add),
              reads=["ranks", "padstart"], writes=["tmpd"])
        for j in range(2):
            tk.op("dve", lambda e, j=j: e.tensor_tensor(out=tmpd2[:, 0:ntile, :], in0=tmpd[:, 0:ntile, :],
                                                        in1=ohs[:, 0:ntile, j, :], op=ALU.mult), reads=["tmpd", "ohs"],
                  writes=["tmpd2"])
            tk.op("dve", lambda e, j=j: e.tensor_reduce(out=destf[:, j, 0:ntile], in_=tmpd2[:, 0:ntile, :], axis=AX.X,
                                                        op=ALU.add), reads=["tmpd2"], writes=["destf"])
        tk.op("dve", lambda e: e.tensor_copy(out=desti[:, :, 0:ntile], in_=destf[:, :, 0:ntile]), reads=["destf"],
              writes=["desti"])
        tk.op("dve", lambda e: e.tensor_tensor(out=cmp64[:], in0=padend[:].unsqueeze(1).to_broadcast([128, 64, 32]),
                                               in1=thr64[:].unsqueeze(2).to_broadcast([128, 64, 32]), op=ALU.is_le),
              reads=["padend", "thr64"], writes=["cmp64"])
        tk.op("dve", lambda e: e.tensor_reduce(out=blkf[:], in_=cmp64[:], axis=AX.X, op=ALU.add), reads=["cmp64"],
              writes=["blkf"])
        tk.op("dve", lambda e: e.tensor_scalar(out=blkf[:], in0=blkf[:], scalar1=128.0, scalar2=kcoff[:, 0:1],
                                               op0=ALU.mult, op1=ALU.add), reads=["blkf", "kcoff"], writes=["blkf"])
        tk.op("dve", lambda e: e.tensor_copy(out=idxw[:], in_=blkf[:]), reads=["blkf"], writes=["idxw"])
        for k in range(ntile):
            par = k % 2
            h2, h2k = h22[par], "h2%d" % par
            tsl = slice(k * 128, (k + 1) * 128)
            tk.dma("sp", lambda e: e.dma_start(out=h2[:], in_=h2d[tsl, :]), reads=["h2d"], writes=[h2k])
            for j in range(2):
                tk.dma("pool", lambda e, j=j: e.indirect_dma_start(
                    out=xs[:, :], out_offset=bass.IndirectOffsetOnAxis(ap=desti[:, j, k:k + 1], axis=0),
                    in_=h2[:, :], in_offset=None), reads=[h2k, "desti"], writes=[("xs", k, j)])
        tk.flush()
        real.barrier()
    xs_keys = [("xs", k, j) for k in range(ntile) for j in range(2)]

    with ExitStack() as esB:
        sbb = mk(esB, "b_")
        W = []
        for i in range(2):
            wg2 = sbb("wg%d" % i, [128, 4096]); wu2 = sbb("wu%d" % i, [128, 4096]); wd2 = sbb("wd%d" % i, [128, 4096])
            W.append((wg2, wu2, wd2))
        xsb2 = [sbb("xsb%d" % i, [128, 1024]) for i in range(2)]
        xsT2 = [sbb("xsT%d" % i, [128, 8, 128]) for i in range(2)]
        gsil2 = [sbb("gsil%d" % i, [128, 512]) for i in range(2)]
        hid2 = [sbb("hid%d" % i, [128, 512]) for i in range(2)]
        hidT2 = [sbb("hidT%d" % i, [128, 4, 128]) for i in range(2)]
        ysb2 = [sbb("ysb%d" % i, [128, 1024]) for i in range(2)]
        wgd = D["wgate"]; wud = D["wup"]; wdd = D["wdown"]
        breg = nc.gpsimd.to_reg(32 * 128 - 1)
        for b in range(nblk):
            par = b % 2
            wg2, wu2, wd2 = W[par]
            wg = wg2[:].rearrange("p (a b) -> p a b", b=512)
            wu = wu2[:].rearrange("p (a b) -> p a b", b=512)
            wd = wd2[:].rearrange("p (a b) -> p a b", b=1024)
            xsb, xsT, gsil, hid, hidT, ysb = xsb2[par], xsT2[par], gsil2[par], hid2[par], hidT2[par], ysb2[par]
            sfx = str(par)
            for (dst, dkey, src) in ((wg2, "wg" + sfx, wgd), (wu2, "wu" + sfx, wud), (wd2, "wd" + sfx, wdd)):
                tk.dma("pool", lambda e, dst=dst, src=src: e.indirect_dma_start(
                    out=dst[:, :].bitcast(F32R), out_offset=None, in_=src[:, :].bitcast(F32R),
                    in_offset=bass.IndirectOffsetOnAxis(ap=idxw[:, b:b + 1], axis=0), bounds_check=breg,
                    oob_is_err=False), reads=["idxw"], writes=[dkey], cost=8.0)
            tk.dma("sp", lambda e: e.dma_start(out=xsb[:], in_=xs[b * 128:(b + 1) * 128, :]),
                   reads=xs_keys if b == 0 else [], writes=["xsb" + sfx])
            for half in range(2):
                pn, pt = pp.next()
                tk.ops("pe", [lambda e, j=j, pt=pt, half=half: e.transpose(
                    pt[:, j * 128:(j + 1) * 128], xsb[:, (half * 4 + j) * 128:(half * 4 + j + 1) * 128], ident[:])
                    for j in range(4)], reads=["xsb" + sfx, "ident"], writes=[pn])
                tk.op("act", lambda e, pt=pt, half=half: e.copy(
                    out=xsT[:, half * 4:(half + 1) * 4, :].rearrange("p a b -> p (a b)").bitcast(F32R), in_=pt[:, :]),
                    reads=[pn], writes=["xsT" + sfx])
            pg, ptg = pp.next()
            tk.ops("pe", [lambda e, kc=kc: e.matmul(ptg[:, :], lhsT=xsT[:, kc, :].bitcast(F32R),
                                                   rhs=wg[:, kc, :].bitcast(F32R), start=(kc == 0), stop=(kc == 7))
                          for kc in range(8)], reads=["xsT" + sfx, "wg" + sfx], writes=[pg], cost=3.0)
            pu, ptu = pp.next()
            tk.ops("pe", [lambda e, kc=kc: e.matmul(ptu[:, :], lhsT=xsT[:, kc, :].bitcast(F32R),
                                                   rhs=wu[:, kc, :].bitcast(F32R), start=(kc == 0), stop=(kc == 7))
                          for kc in range(8)], reads=["xsT" + sfx, "wu" + sfx], writes=[pu], cost=3.0)
            tk.op("act", lambda e: e.activation(out=gsil[:], in_=ptg[:, :], func=AF.Silu), reads=[pg],
                  writes=["gsil" + sfx])
            tk.op("dve", lambda e: e.tensor_tensor(out=hid[:], in0=gsil[:], in1=ptu[:, :], op=ALU.mult),
                  reads=["gsil" + sfx, pu], writes=["hid" + sfx])
            pn, pt = pp.next()
            tk.ops("pe", [lambda e, j=j, pt=pt: e.transpose(pt[:, j * 128:(j + 1) * 128],
                                                            hid[:, j * 128:(j + 1) * 128], ident[:])
                          for j in range(4)], reads=["hid" + sfx, "ident"], writes=[pn])
            tk.op("act", lambda e, pt=pt: e.copy(out=hidT[:].rearrange("p a b -> p (a b)").bitcast(F32R),
                                                 in_=pt[:, :]), reads=[pn], writes=["hidT" + sfx])
            for half in range(2):
                pn, pt = pp.next()
                tk.ops("pe", [lambda e, fc=fc, pt=pt, half=half: e.matmul(
                    pt[:, :], lhsT=hidT[:, fc, :].bitcast(F32R),
                    rhs=wd[:, fc, half * 512:(half + 1) * 512].bitcast(F32R), start=(fc == 0), stop=(fc == 3))
                    for fc in range(4)], reads=["hidT" + sfx, "wd" + sfx], writes=[pn], cost=1.5)
                if half == 0:
                    tk.op("act", lambda e, pt=pt: e.copy(out=ysb[:, 0:512], in_=pt[:, :]), reads=[pn],
                          writes=["ysb" + sfx])
                else:
                    tk.op("dve", lambda e, pt=pt: e.tensor_copy(out=ysb[:, 512:1024], in_=pt[:, :]), reads=[pn],
                          writes=["ysb" + sfx])
            tk.dma("sp", lambda e: e.dma_start(out=ys[b * 128:(b + 1) * 128, :], in_=ysb[:]), reads=["ysb" + sfx],
                   writes=[("ys", b)])
            if b % 16 == 15:
                tk.flush()
        tk.flush()
        real.barrier()
    ys_keys = [("ys", b) for b in range(nblk)]

    with ExitStack() as esC:
        sc_ = mk(esC, "c_")
        y02 = [sc_("y0%d" % i, [128, 1024]) for i in range(2)]
        y12 = [sc_("y1%d" % i, [128, 1024]) for i in range(2)]
        for k in range(ntile):
            par = k % 2
            x1, rr, xt, y0, y1 = x12[par], rr2[par], xt2[par], y02[par], y12[par]
            x1k, rrk, xtk, y0k, y1k = "x1%d" % par, "rr%d" % par, "xt%d" % par, "y0%d" % par, "y1%d" % par
            tsl = slice(k * 128, (k + 1) * 128)
            tk.dma("sp", lambda e: e.dma_start(out=x1[:], in_=x1d[tsl, :]), reads=["x1d"], writes=[x1k])
            for j, (yt_, yk) in enumerate(((y0, y0k), (y1, y1k))):
                tk.dma("pool", lambda e, j=j, yt_=yt_: e.indirect_dma_start(
                    out=yt_[:, :], out_offset=None, in_=ys[:, :],
                    in_offset=bass.IndirectOffsetOnAxis(ap=desti[:, j, k:k + 1], axis=0)),
                    reads=(ys_keys if k == 0 else []) + ["desti"], writes=[yk])
            tk.op("dve", lambda e: e.tensor_scalar(out=y0[:], in0=y0[:], scalar1=gts[:, k, 0:1], scalar2=None,
                                                   op0=ALU.mult), reads=[y0k, "gts"], writes=[y0k])
            tk.op("dve", lambda e: e.scalar_tensor_tensor(out=y0[:], in0=y1[:], scalar=gts[:, k, 1:2], in1=y0[:],
                                                          op0=ALU.mult, op1=ALU.add), reads=[y0k, y1k, "gts"],
                  writes=[y0k])
            tk.op("dve", lambda e: e.tensor_tensor(out=y0[:], in0=y0[:], in1=modb[:, 3, :], op=ALU.mult),
                  reads=[y0k, "modb"], writes=[y0k])
            tk.op("dve", lambda e: e.scalar_tensor_tensor(out=rr[:], in0=x1[:], scalar=ALPHA, in1=y0[:], op0=ALU.mult,
                                                          op1=ALU.add), reads=[x1k, y0k], writes=[rrk])
            layer_norm(rr, rrk, xt, xtk, 2)
            tk.dma("sp", lambda e: e.dma_start(out=out[tsl, :], in_=xt[:]), reads=[xtk], writes=["out"])
            if k % 8 == 7:
                tk.flush()
        tk.flush()


def consts2():
    p = np.arange(128)
    tri = (p[:, None] < p[None, :]).astype(np.float32)
    thr16 = np.broadcast_to((128.0 * np.arange(16, dtype=np.float32))[None, :], (128, 16))
    thr64 = np.broadcast_to((128.0 * np.arange(64, dtype=np.float32))[None, :], (128, 64))
    return {"c_ident": np.eye(128, dtype=np.float32), "c_tri": tri, "c_iota": p.astype(np.float32)[:, None].copy(),
            "c_thr16": np.ascontiguousarray(thr16), "c_thr64": np.ascontiguousarray(thr64)}


def build_phase2(ntile=NT2, nblk=NBLK):
    nc = bass.Bass("TRN2", target_bir_lowering=False)
    D = {}

    def din(name, shape, dt=F32):
        D[name] = nc.dram_tensor(name, shape, dt, kind="ExternalInput").ap()

    din("mixTf", [1024, 2048], BF16); din("x2", [2048, 1024]); din("cT", [128, 8])
    din("wada2", [128, 32, 8, 128]); din("bada2", [4096]); din("wo", [1024, 1024]); din("lnp", [4, 1024])
    din("wr", [1024, 36]); din("br", [36])
    din("wgate", [4096, 4096]); din("wup", [4096, 4096]); din("wdown", [4096, 4096])
    din("c_ident", [128, 128]); din("c_tri", [128, 128]); din("c_iota", [128, 1]); din("c_thr16", [128, 16])
    din("c_thr64", [128, 64])
    for nm in ("x1d", "h2d"):
        D[nm] = nc.dram_tensor(nm, [2048, 1024], F32, kind="Internal").ap()
    for nm in ("xs", "ys"):
        D[nm] = nc.dram_tensor(nm, [NBLK * 128, 1024], F32, kind="Internal").ap()
    D["out"] = nc.dram_tensor("out", [2048, 1024], F32, kind="ExternalOutput").ap()
    with ExitStack() as es:
        tk = Trk(nc, es)
        phase2(nc, tk, es, D, ntile=ntile, nblk=nblk)
        tk.finish("sp")
    return nc


def phase2_shared_inputs(inp):
    l = 0
    wg = inp["w_gate"][l].reshape(32, 8, 128, 512).transpose(0, 2, 1, 3).reshape(4096, 4096)
    wu = inp["w_up"][l].reshape(32, 8, 128, 512).transpose(0, 2, 1, 3).reshape(4096, 4096)
    wd = inp["w_down"][l].reshape(32, 4, 128, 1024).transpose(0, 2, 1, 3).reshape(4096, 4096)
    d = {"wada2": np.ascontiguousarray(inp["w_ada"][l][:, 2048:6144].reshape(8, 128, 32, 128).transpose(1, 2, 0, 3)),
         "bada2": np.ascontiguousarray(inp["b_ada"][l][2048:6144]),
         "wo": np.ascontiguousarray(inp["w_o"][l]),
         "lnp": np.ascontiguousarray(np.stack([inp["ln1_g"][l], inp["ln1_b"][l], inp["ln2_g"][l], inp["ln2_b"][l]])),
         "wr": np.ascontiguousarray(np.concatenate([inp["w_router_group"][l], inp["w_router_expert"][l]], 1)),
         "br": np.ascontiguousarray(np.concatenate([inp["b_router_group"][l], inp["b_router_expert"][l]])),
         "wgate": np.ascontiguousarray(wg), "wup": np.ascontiguousarray(wu), "wdown": np.ascontiguousarray(wd)}
    d.update(consts2())
    return d


def phase2_inputs(inp, core, shared, mixTf):
    b, q = core // 4, core % 4
    d = dict(shared)
    d["x2"] = np.ascontiguousarray(inp["x"][b, q * 2048:(q + 1) * 2048])
    d["cT"] = np.ascontiguousarray(inp["c"][b].reshape(8, 128).T)
    d["mixTf"] = mixTf
    return d


def build_fused():
    nc = bass.Bass("TRN2", target_bir_lowering=False)
    D = {}

    def din(name, shape, dt=F32):
        D[name] = nc.dram_tensor(name, shape, dt, kind="ExternalInput").ap()

    din("x", [D_MODEL, SEQ]); din("cT", [128, 8]); din("pos", [SEQ], I32)
    din("wada1", [128, 8, 8, 256]); din("bada1", [128, 16]); din("win", [128, 5, 8, 256])
    din("convw", [128, 3, 4]); din("pvec", [128, 4])
    din("c_ident", [128, 128]); din("c_masks", [128, 3, 128]); din("c_amask", [128, 256]); din("c_invf", [64, 2])
    din("x2", [2048, 1024]); din("qidx", [128, 8], I32)
    din("wada2", [128, 32, 8, 128]); din("bada2", [4096]); din("wo", [1024, 1024]); din("lnp", [4, 1024])
    din("wr", [1024, 36]); din("br", [36])
    din("wgate", [4096, 4096]); din("wup", [4096, 4096]); din("wdown", [4096, 4096])
    din("c_tri", [128, 128]); din("c_iota", [128, 1]); din("c_thr16", [128, 16]); din("c_thr64", [128, 64])
    cin = [nc.dram_tensor("cin%d" % q, [256, 2048], BF16, kind="Internal").ap() for q in range(4)]
    cout = nc.dram_tensor("cout", [4096, 2048], BF16, kind="Internal").ap()
    for nm in ("x1d", "h2d"):
        D[nm] = nc.dram_tensor(nm, [2048, 1024], F32, kind="Internal").ap()
    for nm in ("xs", "ys"):
        D[nm] = nc.dram_tensor(nm, [NBLK * 128, 1024], F32, kind="Internal").ap()
    D["out"] = nc.dram_tensor("out", [2048, 1024], F32, kind="ExternalOutput").ap()
    groups = [[0, 1, 2, 3], [4, 5, 6, 7]]
    with ExitStack() as es0:
        tk = Trk(nc, es0)

        def on_quarter(sc, tk):
            tk.cc(lambda e: e.collective_compute("AllGather", ALU.bypass, replica_groups=groups,
                                                 ins=[cin[sc][:, :]], outs=[cout[sc * 1024:(sc + 1) * 1024, :]]),
                  reads=[("cin_a", sc), ("cin_b", sc)], writes=[("cout", sc)])

        with ExitStack() as es1:
            phase1(nc, tk, es1, D, cin=cin, on_quarter=on_quarter)
        tk.barrier()
        with ExitStack() as es2:
            phase2(nc, tk, es2, D, cout=cout)
        tk.finish("sp")
    return nc


def fused_inputs(inp, core, shared):
    b, q = core // 4, core % 4
    d = dict(shared)
    d.update(phase1_inputs(inp, core))
    d["x2"] = np.ascontiguousarray(inp["x"][b, q * 2048:(q + 1) * 2048])
    kc = np.arange(8)[None, :]
    p = np.arange(128)[:, None]
    row = np.where(kc < 4, kc * 256 + p, (kc - 4) * 256 + 128 + p)
    d["qidx"] = np.ascontiguousarray((q * 1024 + row).astype(np.int32))
    return d


def kernel(**inputs):
    inp = {k: np.asarray(v) for k, v in inputs.items()}
    shared = phase2_shared_inputs(inp)
    nc = build_fused()
    maps = [fused_inputs(inp, core, shared) for core in range(NCORES)]
    res = run_bass_kernel_spmd(nc, maps, core_ids=list(range(NCORES)))
    out = np.zeros((BATCH, SEQ, D_MODEL), np.float32)
    for core in range(NCORES):
        b, q = core // 4, core % 4
        out[b, q * 2048:(q + 1) * 2048] = np.asarray(res.results[core]["out"])
    return out
```

```python
import math
from contextlib import ExitStack

import numpy as np
import concourse.bass as bass
import concourse.mybir as mybir
from concourse.bass_utils import run_bass_kernel_spmd

F32 = mybir.dt.float32
BF16 = mybir.dt.bfloat16
I32 = mybir.dt.int32
U32 = mybir.dt.uint32
F32R = mybir.dt.float32r
AF = mybir.ActivationFunctionType
ALU = mybir.AluOpType
AX = mybir.AxisListType

D_MODEL = 1024
SEQ = 8192
BATCH = 2
NCORES = 8
T = 512
NCH = SEQ // T
ALPHA = 2.0 ** 0.25
LN_EPS = 1e-5
RMS_EPS = 1e-6
ROPE_THETA = 500000.0
NEG = -30000.0
TWO_PI = 2.0 * math.pi


class Trk:
    def __init__(self, nc, es, n_dma_sems=24, same_engine_sync=True):
        self.nc = nc
        self.e = {"pe": nc.tensor, "dve": nc.vector, "act": nc.scalar, "pool": nc.gpsimd, "sp": nc.sync}
        self.sem = {k: es.enter_context(nc.semaphore("sem_" + k)) for k in self.e}
        self.cnt = {k: 0 for k in self.e}
        self.seen = {k: {} for k in self.e}
        self.last_w = {}
        self.reads = {}
        self.dsem = [es.enter_context(nc.semaphore("dsem%d" % i)) for i in range(n_dma_sems)]
        self.dval = [0] * n_dma_sems
        self.dnext = 0
        self.same = same_engine_sync
        self.all_dma_events = []
        self.ccsem = es.enter_context(nc.semaphore("ccsem"))
        self.ccval = 0

    def _wait(self, eng, ev):
        kind, src, n = ev
        key = (kind, src)
        if self.seen[eng].get(key, 0) >= n:
            return
        if kind == "e":
            if src == eng and not self.same:
                return
            self.e[eng].wait_ge(self.sem[src], n)
        elif kind == "c":
            self.e[eng].wait_ge(self.ccsem, n)
        else:
            self.e[eng].wait_ge(self.dsem[src], n)
        self.seen[eng][key] = n

    def _deps(self, eng, reads, writes):
        deps = []
        for r in reads:
            ev = self.last_w.get(r)
            if ev is not None:
                deps.append(ev)
        for w in writes:
            ev = self.last_w.get(w)
            if ev is not None:
                deps.append(ev)
            for ev2 in self.reads.get(w, {}).values():
                deps.append(ev2)
        for ev in deps:
            self._wait(eng, ev)

    def _record(self, ev, reads, writes):
        for r in reads:
            self.reads.setdefault(r, {})[(ev[0], ev[1])] = ev
        for w in writes:
            self.last_w[w] = ev
            self.reads[w] = {}

    def op(self, eng, fn, reads=(), writes=()):
        self._deps(eng, reads, writes)
        inst = fn(self.e[eng])
        self.cnt[eng] += 1
        inst.then_inc(self.sem[eng], 1)
        ev = ("e", eng, self.cnt[eng])
        self._record(ev, reads, writes)
        return ev

    def ops(self, eng, fns, reads=(), writes=()):
        self._deps(eng, reads, writes)
        inst = None
        for fn in fns:
            inst = fn(self.e[eng])
        self.cnt[eng] += 1
        inst.then_inc(self.sem[eng], 1)
        ev = ("e", eng, self.cnt[eng])
        self._record(ev, reads, writes)
        return ev

    def dma(self, eng, fn, reads=(), writes=()):
        self._deps(eng, reads, writes)
        i = self.dnext
        self.dnext = (self.dnext + 1) % len(self.dsem)
        if self.dval[i] > 0:
            self._wait(eng, ("d", i, self.dval[i]))
        inst = fn(self.e[eng])
        self.dval[i] += 16
        inst.then_inc(self.dsem[i], 16)
        ev = ("d", i, self.dval[i])
        self._record(ev, reads, writes)
        self.all_dma_events.append(ev)
        return ev

    def cc(self, fn, reads=(), writes=()):
        eng = "pool"
        self._deps(eng, reads, writes)
        if self.ccsem is None:
            raise RuntimeError("no cc semaphore")
        if self.ccval > 0:
            self._wait(eng, ("c", 0, self.ccval))
        inst = fn(self.e[eng])
        self.ccval += 1
        inst.then_inc(self.ccsem, 1)
        ev = ("c", 0, self.ccval)
        self._record(ev, reads, writes)
        return ev

    def barrier(self):
        for eng in self.e:
            for i, v in enumerate(self.dval):
                if v > 0:
                    self._wait(eng, ("d", i, v))
            if self.ccval > 0:
                self._wait(eng, ("c", 0, self.ccval))
            for k, n in self.cnt.items():
                if n > 0 and k != eng:
                    self._wait(eng, ("e", k, n))

    def finish(self, eng="sp"):
        for i, v in enumerate(self.dval):
            if v > 0:
                self._wait(eng, ("d", i, v))
        if self.ccval > 0:
            self._wait(eng, ("c", 0, self.ccval))
        for k, n in self.cnt.items():
            if n > 0 and k != eng:
                self._wait(eng, ("e", k, n))


class _Proxy:
    def __init__(self):
        self.calls = []

    def __getattr__(self, name):
        def f(*a, **k):
            self.calls.append((name, a, k))
            return None
        return f


def _freeze(fn):
    p = _Proxy()
    fn(p)
    assert len(p.calls) == 1
    name, a, k = p.calls[0]
    return lambda e: getattr(e, name)(*a, **k)


class Rec:
    HOP = 0.8

    def __init__(self, tk):
        self.tk = tk
        self.L = []

    def op(self, eng, fn, reads=(), writes=(), cost=None):
        self.L.append(("op", eng, _freeze(fn), tuple(reads), tuple(writes), cost if cost is not None else (0.3 if eng == "pe" else 0.6)))

    def ops(self, eng, fns, reads=(), writes=(), cost=None):
        fns = [_freeze(f) for f in fns]
        self.L.append(("ops", eng, fns, tuple(reads), tuple(writes), cost if cost is not None else 0.2 * len(fns)))

    def dma(self, eng, fn, reads=(), writes=(), cost=None):
        self.L.append(("dma", eng, _freeze(fn), tuple(reads), tuple(writes), cost if cost is not None else 2.0))

    def cc(self, fn, reads=(), writes=(), cost=None):
        self.L.append(("cc", "pool", _freeze(fn), tuple(reads), tuple(writes), 5.0))

    def flush(self):
        L = self.L
        self.L = []
        n = len(L)
        preds = [set() for _ in range(n)]
        last_w = {}
        readers = {}
        for i, (kind, eng, fn, reads, writes, cost) in enumerate(L):
            for r in reads:
                if r in last_w:
                    preds[i].add(last_w[r])
            for w in writes:
                if w in last_w:
                    preds[i].add(last_w[w])
                for j in readers.get(w, ()):
                    preds[i].add(j)
            for r in reads:
                readers.setdefault(r, []).append(i)
            for w in writes:
                last_w[w] = i
                readers[w] = []
            preds[i].discard(i)
        succs = [[] for _ in range(n)]
        npred = [len(p) for p in preds]
        for i in range(n):
            for j in preds[i]:
                succs[j].append(i)
        efree = {}
        finish = [0.0] * n
        ready_t = [0.0] * n
        ready = [i for i in range(n) if npred[i] == 0]
        order = []
        import heapq
        while ready:
            best = None
            bkey = None
            for i in ready:
                kind, eng, fn, reads, writes, cost = L[i]
                q = eng if kind in ("op", "ops") else ("q_" + eng)
                st = max(efree.get(q, 0.0), ready_t[i])
                key = (st, i)
                if bkey is None or key < bkey:
                    bkey = key
                    best = i
            i = best
            ready.remove(i)
            kind, eng, fn, reads, writes, cost = L[i]
            q = eng if kind in ("op", "ops") else ("q_" + eng)
            st = bkey[0]
            if kind in ("op", "ops"):
                efree[q] = st + cost
                finish[i] = st + cost
            else:
                efree[q] = st + 0.1
                finish[i] = st + cost
            order.append(i)
            for j in succs[i]:
                npred[j] -= 1
                same = (L[j][1] == eng and L[j][0] in ("op", "ops") and kind in ("op", "ops"))
                ready_t[j] = max(ready_t[j], finish[i] + (0.0 if same else self.HOP))
                if npred[j] == 0:
                    ready.append(j)
        assert len(order) == n
        for i in order:
            kind, eng, fn, reads, writes, cost = L[i]
            if kind == "op":
                self.tk.op(eng, fn, reads, writes)
            elif kind == "ops":
                self.tk.ops(eng, fn, reads, writes)
            elif kind == "dma":
                self.tk.dma(eng, fn, reads, writes)
            else:
                self.tk.cc(fn, reads, writes)


class PsumPool:
    _uid = [0]

    def __init__(self, nc, es, names):
        PsumPool._uid[0] += 1
        u = PsumPool._uid[0]
        self.t = {n: es.enter_context(nc.psum_tensor("ps%d_%s" % (u, n), [128, 512], F32)) for n in names}
        self.rot = [n for n in names if n.startswith("r")]
        self.i = 0

    def next(self):
        n = self.rot[self.i]
        self.i = (self.i + 1) % len(self.rot)
        return n, self.t[n]


def phase1(nc, tk, es, D, nch=NCH, cin=None, on_quarter=None):
    def sb(name, shape, dt=F32):
        return es.enter_context(nc.sbuf_tensor("s_" + name, shape, dt))

    def K(name, c):
        return (name, c % 8)

    ppall = PsumPool(nc, es, ["rb0", "rb1", "rb2", "rb3", "ra0", "ra1", "scan", "ot"])
    PS = ppall.t
    real_tk = tk
    tk = Rec(real_tk)

    class _Sub:
        def __init__(self, names):
            self.rot = names
            self.i = 0

        def next(self):
            n = self.rot[self.i]
            self.i = (self.i + 1) % len(self.rot)
            return n, PS[n]

    pp = _Sub(["rb0", "rb1", "rb2", "rb3"])
    ppa = _Sub(["ra0", "ra1"])

    ident = sb("ident", [128, 128])
    ident_b = sb("ident_b", [128, 128], BF16)
    ones_f = sb("ones_f", [128, 128])
    ones_b = sb("ones_b", [128, 128], BF16)
    mus_t = sb("mus_neg", [128, 128]); mui_t = sb("mu_inc", [128, 128]); mls_t = sb("mls_neg", [128, 128])
    mus_neg = mus_t[:].unsqueeze(1).to_broadcast([128, 4, 128])
    mu_inc = mui_t[:].unsqueeze(1).to_broadcast([128, 4, 128])
    mls_neg = mls_t[:].unsqueeze(1).to_broadcast([128, 4, 128])
    ident4 = ident[:].unsqueeze(1).to_broadcast([128, 4, 128])
    amask_f = sb("amask_f", [128, 256])
    amask = sb("amask", [128, 256], BF16)
    scanm = sb("scanm", [128, T])
    invf = sb("invf", [64, 2])
    win_b = sb("win_b", [128, 8, 1280], BF16)
    xtok = sb("xtok", [128, 8, T])
    wstage = [xtok[:, 4 * i:4 * i + 4, :].rearrange("p a (h c) -> p (a h) c", c=256) for i in range(2)]
    wkeys = ["xtok_a", "xtok_b"]
    sc1 = sb("sc1", [128, 8])
    sh1 = sb("sh1", [128, 8])
    convw = sb("convw", [128, 3, 4])
    pvec = sb("pvec", [128, 4])
    nalog = sb("nalog", [128, 1])
    cT = sb("cT", [128, 8])
    scT = sb("scT", [128, 8, 2])
    bada = sb("bada", [128, 16])
    mod1 = sb("mod1", [128, 16])

    tk.dma("sp", lambda e: e.dma_start(out=ident[:], in_=D["c_ident"][:, :]), writes=["ident"])
    tk.dma("sp", lambda e: e.dma_start(out=amask_f[:], in_=D["c_amask"][:, :]), writes=["amask_f"])
    tk.dma("sp", lambda e: e.dma_start(out=invf[:], in_=D["c_invf"][:, :]), writes=["invf"])
    tk.dma("sp", lambda e: e.dma_start(out=convw[:], in_=D["convw"][:, :, :]), writes=["convw"])
    tk.dma("sp", lambda e: e.dma_start(out=pvec[:], in_=D["pvec"][:, :]), writes=["pvec"])
    tk.dma("sp", lambda e: e.dma_start(out=cT[:], in_=D["cT"][:, :]), writes=["cT"])
    tk.dma("sp", lambda e: e.dma_start(out=bada[:], in_=D["bada1"][:, :]), writes=["bada"])
    for q, (nm, tl) in enumerate([("mus", mus_t), ("mui", mui_t), ("mls", mls_t)]):
        tk.dma("sp", lambda e, tl=tl, q=q: e.dma_start(out=tl[:], in_=D["c_masks"][:, q, :]), writes=[nm])
    tk.op("dve", lambda e: e.tensor_copy(out=ident_b[:], in_=ident[:]), reads=["ident"], writes=["ident_b"])
    tk.op("dve", lambda e: e.tensor_copy(out=amask[:], in_=amask_f[:]), reads=["amask_f"], writes=["amask"])
    tk.op("pool", lambda e: e.memset(ones_f[:], 1.0), writes=["ones_f"])
    tk.op("pool", lambda e: e.memset(ones_b[:], 1.0), writes=["ones_b"])
    tk.op("pool", lambda e: e.memset(scanm[:], 1.0), writes=["scanm"])
    tk.op("pool", lambda e: e.memset(scanm[:, 0:T:64], 0.0), writes=["scanm"])
    tk.op("act", lambda e: e.activation(out=nalog[:], in_=pvec[:, 0:1], func=AF.Exp), reads=["pvec"],
          writes=["nalog"])
    tk.op("dve", lambda e: e.tensor_scalar(out=nalog[:], in0=nalog[:], scalar1=-1.0, scalar2=None, op0=ALU.mult),
          reads=["nalog"], writes=["nalog"])

    for j in range(5):
        st = wstage[j % 2]
        key = wkeys[j % 2]
        tk.dma("sp" if j % 2 == 0 else "pool", lambda e, st=st, j=j: e.dma_start(out=st, in_=D["win"][:, j, :, :]),
               writes=[key])
        tk.op("dve" if j % 2 == 0 else "act", (lambda e, st=st, j=j: e.tensor_copy(out=win_b[:, :, j * 256:(j + 1) * 256], in_=st))
              if j % 2 == 0 else (lambda e, st=st, j=j: e.copy(out=win_b[:, :, j * 256:(j + 1) * 256], in_=st)),
              reads=[key], writes=[("win_b", j)])
    for c0 in (7 * 128, 7 * 128 + 32):
        tk.op("dve", lambda e, c0=c0: e.tensor_scalar(out=win_b[:, :, c0:c0 + 16], in0=win_b[:, :, c0:c0 + 16],
                                                       scalar1=-1.0, scalar2=None, op0=ALU.mult),
              reads=[("win_b", 3)], writes=[("win_b", 3)])

    tk.op("act", lambda e: e.activation(out=scT[:, :, 0], in_=cT[:], func=AF.Silu), reads=["cT"], writes=["scT"])
    tk.op("act", lambda e: e.activation(out=scT[:, :, 1], in_=cT[:], func=AF.Silu), reads=["cT"], writes=["scT"])
    for j in range(8):
        st = wstage[j % 2]
        key = wkeys[j % 2]
        tk.dma("sp" if j % 2 == 0 else "pool", lambda e, st=st, j=j: e.dma_start(out=st, in_=D["wada1"][:, j, :, :]),
               writes=[key])
        for fh in range(2):
            fc = 2 * j + fh
            pn, pt = pp.next()
            tk.ops("pe", [lambda e, st=st, kc=kc, pt=pt, fh=fh: e.matmul(
                pt[:, 0:2], lhsT=st[:, kc, fh * 128:(fh + 1) * 128], rhs=scT[:, kc, :], start=(kc == 0), stop=(kc == 7))
                for kc in range(8)], reads=[key, "scT"], writes=[pn])
            tk.op("dve", lambda e, pt=pt, fc=fc: e.tensor_tensor(out=mod1[:, fc:fc + 1], in0=pt[:, 0:1],
                                                                  in1=bada[:, fc:fc + 1], op=ALU.add),
                  reads=[pn, "bada"], writes=["mod1"])
    tk.op("dve", lambda e: e.tensor_copy(out=sh1[:], in_=mod1[:, 0:8]), reads=["mod1"], writes=["sh1"])
    tk.op("dve", lambda e: e.tensor_scalar(out=sc1[:], in0=mod1[:, 8:16], scalar1=1.0, scalar2=None, op0=ALU.add),
          reads=["mod1"], writes=["sc1"])

    RING = 4096
    qT = sb("qT", [128, RING], BF16)
    kT = sb("kT", [128, RING], BF16)
    vT = sb("vT", [128, RING], BF16)
    vtt = sb("vtt", [128, 4, 128], BF16)
    acc = sb("acc", [128, 2, 2048])
    S = [sb("S%d" % i, [128, 128]) for i in range(2)]
    tk.op("pool", lambda e: e.memset(S[0][:], 0.0), writes=["S0"])

    hT = [sb("hT%d" % i, [128, 8, T], BF16) for i in range(2)]
    craw = [sb("craw%d" % g, [128, T + 3]) for g in range(3)]
    cacc = sb("cacc", [128, T])
    qs = sb("qs", [128, T]); ks = sb("ks", [128, T]); vs = sb("vs", [128, T])
    zs2 = [sb("zs%d" % i, [128, T]) for i in range(2)]
    sq = sb("sq", [128, T]); rn = sb("rn", [128, T])
    p1 = sb("p1", [128, T]); p2 = sb("p2", [128, T]); p3 = sb("p3", [128, T])
    osq = p1; rs = p2; fsq = p1; fr = p2; oa = p3; fo = p3
    qn = sb("qn", [128, T]); kn = sb("kn", [128, T])
    betab = sb("betab", [128, T]); gb = sb("gb", [128, T]); gcb = sb("gcb", [128, T])
    egcb2 = [sb("egcb%d" % i, [128, T]) for i in range(2)]; eglb = sb("eglb", [128, T]); begb = gb
    cols = sb("cols", [128, 5, 4])
    Kbe = sb("Kbe", [128, 4, 128]); Kd2 = [sb("Kd%d" % i, [128, 4, 128]) for i in range(2)]
    Vb = sb("Vb", [128, 4, 128])
    tdf = sb("tdf", [128, 4, 128]); DT = sb("DT", [128, 4, 128]); Dm = sb("Dm", [128, 4, 128])
    E1 = tdf; QKT2 = [sb("QKT%d" % i, [128, 4, 128]) for i in range(2)]
    Bm = [sb("Bm%d" % i, [128, 4, 128]) for i in range(2)]
    BTm = [sb("BTm%d" % i, [128, 4, 128]) for i in range(2)]
    Pm = [sb("Pm%d" % i, [128, 4, 128]) for i in range(2)]
    U2 = [sb("U%d" % i, [128, 4, 128]) for i in range(2)]; WT2 = [sb("WT%d" % i, [128, T]) for i in range(2)]
    QdT2 = [sb("QdT%d" % i, [128, T]) for i in range(2)]
    vnew = sb("vnew", [128, 2, 128])
    oab = sb("oab", [128, T], BF16)
    posi = sb("posi", [64, T], I32); ra = sb("ra", [64, T]); rb = sb("rb", [64, T])
    ri = sb("ri", [64, T], I32)
    sincos = sb("sincos", [64, T])
    rt1 = sb("rt1", [32, T]); rt2 = sb("rt2", [32, T])
    pTt = [sb("pT%d" % i, [128, 256], BF16) for i in range(2)]
    fob = sb("fob", [128, T], BF16)

    x = D["x"]
    mixT = D.get("mixT")
    pos = D["pos"]

    def load_x(c):
        for i in range(2):
            tk.dma("sp", lambda e, i=i: e.dma_start(
                out=xtok[:, 4 * i:4 * i + 4, :],
                in_=x[i * 512:(i + 1) * 512, c * T:(c + 1) * T].rearrange("(kc p) t -> p kc t", p=128)),
                writes=[wkeys[i]])

    def stage_a(c):
        h = hT[c % 2]
        hk = "hT%d" % (c % 2)
        for kc in range(8):
            tk.op("act", lambda e, kc=kc: e.activation(out=h[:, kc, :], in_=xtok[:, kc, :], func=AF.Identity,
                                                       bias=sh1[:, kc:kc + 1], scale=sc1[:, kc:kc + 1]),
                  reads=[wkeys[kc // 4], "sh1", "sc1"], writes=[hk])

    def proj(c, m):
        h = hT[c % 2]
        hk = "hT%d" % (c % 2)
        pn, pt = pp.next()
        tk.ops("pe", [lambda e, kc=kc, pt=pt: e.matmul(pt[:, :], lhsT=win_b[:, kc, m * 128:(m + 1) * 128],
                                                      rhs=h[:, kc, :], start=(kc == 0), stop=(kc == 7))
                      for kc in range(8)], reads=[hk, ("win_b", m // 2)], writes=[pn])
        return pn, pt

    def rope_tables(c):
        tk.dma("sp", lambda e: e.dma_start(out=posi[:], in_=pos[c * T:(c + 1) * T].partition_broadcast(64)),
               writes=["posi"])
        tk.op("dve", lambda e: e.tensor_copy(out=ra[:], in_=posi[:]), reads=["posi"], writes=["ra"])
        tk.op("dve", lambda e: e.tensor_scalar(out=ra[:], in0=ra[:], scalar1=invf[:, 0:1], scalar2=invf[:, 1:2],
                                               op0=ALU.mult, op1=ALU.add), reads=["ra", "invf"], writes=["ra"])
        tk.op("dve", lambda e: e.tensor_scalar(out=ri[:], in0=ra[:], scalar1=1.0 / TWO_PI, scalar2=None,
                                               op0=ALU.mult), reads=["ra"], writes=["ri"])
        tk.op("dve", lambda e: e.tensor_copy(out=rb[:], in_=ri[:]), reads=["ri"], writes=["rb"])
        tk.op("dve", lambda e: e.scalar_tensor_tensor(out=ra[:], in0=rb[:], scalar=-TWO_PI, in1=ra[:], op0=ALU.mult,
                                                      op1=ALU.add), reads=["ra", "rb"], writes=["ra"])
        tk.op("dve", lambda e: e.tensor_scalar(out=rb[:], in0=ra[:], scalar1=math.pi, scalar2=-TWO_PI,
                                               op0=ALU.is_gt, op1=ALU.mult), reads=["ra"], writes=["rb"])
        tk.op("dve", lambda e: e.tensor_tensor(out=ra[:], in0=ra[:], in1=rb[:], op=ALU.add),
              reads=["ra", "rb"], writes=["ra"])
        tk.op("dve", lambda e: e.tensor_scalar(out=rb[:], in0=ra[:], scalar1=-math.pi, scalar2=TWO_PI,
                                               op0=ALU.is_lt, op1=ALU.mult), reads=["ra"], writes=["rb"])
        tk.op("dve", lambda e: e.tensor_tensor(out=ra[:], in0=ra[:], in1=rb[:], op=ALU.add),
              reads=["ra", "rb"], writes=["ra"])
        tk.op("act", lambda e: e.activation(out=sincos[:], in_=ra[:], func=AF.Sin), reads=["ra"], writes=["sincos"])

    def prep_main(c):
        par = c % 2
        U, WT, QdT, QKT, Kd, egcb, zs = U2[par], WT2[par], QdT2[par], QKT2[par], Kd2[par], egcb2[par], zs2[par]
        uk, wk, qdk, qkk, kdk, ek, zk = ("U%d" % par, "WT%d" % par, "QdT%d" % par, "QKT%d" % par, "Kd%d" % par,
                                         "egcb%d" % par, "zs%d" % par)
        stage_a(c)
        yield
        if c + 1 < nch:
            load_x(c + 1)
        yield
        for g, dst, dk_ in ((0, qs, "qs"), (1, ks, "ks"), (2, vs, "vs")):
            pn, pt = proj(c, g)
            ck = "craw%d" % g
            cr = craw[g]
            if c > 0:
                tk.op("dve", lambda e, cr=cr: e.tensor_copy(out=cr[:, 0:3], in_=cr[:, T:T + 3]), reads=[ck],
                      writes=[ck])
            else:
                tk.op("pool", lambda e, cr=cr: e.memset(cr[:, 0:3], 0.0), writes=[ck])
            tk.op("act", lambda e, pt=pt, cr=cr: e.copy(out=cr[:, 3:T + 3], in_=pt[:, :]), reads=[pn],
                  writes=[ck])
            tk.op("dve", lambda e, cr=cr, g=g: e.tensor_scalar(out=cacc[:], in0=cr[:, 0:T],
                                                               scalar1=convw[:, g, 0:1], scalar2=None, op0=ALU.mult),
                  reads=[ck, "convw"], writes=["cacc"])
            for j in range(1, 4):
                tk.op("dve", lambda e, cr=cr, g=g, j=j: e.scalar_tensor_tensor(
                    out=cacc[:], in0=cr[:, j:j + T], scalar=convw[:, g, j:j + 1], in1=cacc[:],
                    op0=ALU.mult, op1=ALU.add), reads=[ck, "convw", "cacc"], writes=["cacc"])
            tk.op("act", lambda e, dst=dst: e.activation(out=dst[:], in_=cacc[:], func=AF.Silu), reads=["cacc"],
                  writes=[dk_])
            yield
        yield
        pn, pt = proj(c, 3)
        tk.op("act", lambda e, pt=pt: e.activation(out=zs[:], in_=pt[:, :], func=AF.Silu), reads=[pn], writes=[zk])
        yield
        for src, sk, dst, dk_, scale in ((qs, "qs", qn, "qn", 128.0 ** -0.5), (ks, "ks", kn, "kn", 1.0)):
            tk.op("act", lambda e, src=src: e.activation(out=sq[:], in_=src[:], func=AF.Square), reads=[sk],
                  writes=["sq"])
            pn, pt = pp.next()
            tk.op("pe", lambda e, pt=pt: e.matmul(pt[:, :], lhsT=ones_f[:], rhs=sq[:], start=True, stop=True),
                  reads=["sq", "ones_f"], writes=[pn])
            tk.op("act", lambda e, pt=pt: e.activation(out=rn[:], in_=pt[:, :], func=AF.Ln, bias=RMS_EPS_AP[:, 0:1],
                                                       scale=1.0), reads=[pn, "epsc"], writes=["rn"])
            tk.op("act", lambda e: e.activation(out=rn[:], in_=rn[:], func=AF.Exp, scale=-0.5), reads=["rn"],
                  writes=["rn"])
            tk.op("dve", lambda e, src=src, dst=dst, scale=scale: e.scalar_tensor_tensor(
                out=dst[:].bitcast(F32R), in0=src[:], scalar=scale, in1=rn[:], op0=ALU.mult, op1=ALU.mult),
                reads=[sk, "rn"], writes=[dk_])
        yield
        pn, pt = proj(c, 8)
        tk.op("act", lambda e, pt=pt: e.activation(out=betab[:], in_=pt[:, :], func=AF.Sigmoid), reads=[pn],
              writes=["betab"])
        pn, pt = proj(c, 9)
        tk.op("act", lambda e, pt=pt: e.activation(out=gb[:], in_=pt[:, :], func=AF.Exp, bias=pvec[:, 1:2], scale=1.0),
              reads=[pn, "pvec"], writes=["gb"])
        tk.op("act", lambda e: e.activation(out=gb[:], in_=gb[:], func=AF.Ln, bias=ONE_AP[:, 0:1], scale=1.0),
              reads=["gb", "epsc"], writes=["gb"])
        tk.op("dve", lambda e: e.tensor_scalar(out=gb[:], in0=gb[:], scalar1=nalog[:, 0:1], scalar2=None, op0=ALU.mult),
              reads=["gb", "nalog"], writes=["gb"])
        tk.op("dve", lambda e: e.tensor_tensor_scan(out=gcb[:], data0=scanm[:], data1=gb[:], initial=0.0,
                                                    op0=ALU.mult, op1=ALU.add), reads=["gb", "scanm"], writes=["gcb"])
        tk.op("act", lambda e: e.activation(out=egcb[:], in_=gcb[:], func=AF.Exp), reads=["gcb"], writes=[ek])
        gc3 = gcb[:].rearrange("p (n t) -> p n t", t=64)
        tk.op("dve", lambda e: e.tensor_tensor(out=eglb[:].rearrange("p (n t) -> p n t", t=64),
                                               in0=gc3[:, :, 63:64].to_broadcast([128, 8, 64]), in1=gc3,
                                               op=ALU.subtract), reads=["gcb"], writes=["eglb"])
        tk.op("act", lambda e: e.activation(out=eglb[:], in_=eglb[:], func=AF.Exp), reads=["eglb"], writes=["eglb"])
        tk.op("dve", lambda e: e.tensor_tensor(out=begb[:], in0=betab[:], in1=egcb[:], op=ALU.mult),
              reads=["betab", ek], writes=["gb"])
        pn, pt = pp.next()
        srcs = [(betab, "betab"), (gcb, "gcb"), (egcb, ek), (eglb, "eglb"), (begb, "gb")]
        tk.ops("pe", [lambda e, q=q, s=s, pt=pt, src=src: e.transpose(
            pt[:, (q * 4 + s) * 16:(q * 4 + s + 1) * 16], src[0:16, s * 128:(s + 1) * 128], ident[0:16, 0:16])
            for q, (src, _) in enumerate(srcs) for s in range(4)],
            reads=[k_ for _, k_ in srcs] + ["ident"], writes=[pn])
        tk.op("dve", lambda e, pt=pt: e.tensor_copy(
            out=cols[:].rearrange("p q s -> p (q s)"),
            in_=pt[:, 0:320].rearrange("p (n w) -> p n w", w=16)[:, :, 0]), reads=[pn], writes=["cols"])

        def colb(q):
            return cols[:, q, :].unsqueeze(2).to_broadcast([128, 4, 128])

        yield
        pn, pt = pp.next()
        tk.ops("pe", [lambda e, s=s, pt=pt: e.transpose(pt[:, s * 128:(s + 1) * 128], kn[:, s * 128:(s + 1) * 128],
                                                        ident[:]) for s in range(4)], reads=["kn", "ident"],
               writes=[pn])
        pt3 = pt[:, :].rearrange("p (s d) -> p s d", d=128)
        tk.op("dve", lambda e, pt3=pt3: e.tensor_tensor(out=Kbe[:].bitcast(F32R), in0=pt3, in1=colb(4), op=ALU.mult),
              reads=[pn, "cols"], writes=["Kbe"])
        tk.op("dve", lambda e, pt3=pt3: e.tensor_tensor(out=Kd[:], in0=pt3, in1=colb(3), op=ALU.mult),
              reads=[pn, "cols"], writes=[kdk])
        pn, pt = pp.next()
        tk.ops("pe", [lambda e, s=s, pt=pt: e.transpose(pt[:, s * 128:(s + 1) * 128], vs[:, s * 128:(s + 1) * 128],
                                                        ident[:]) for s in range(4)], reads=["vs", "ident"],
               writes=[pn])
        pt3 = pt[:, :].rearrange("p (s d) -> p s d", d=128)
        tk.op("dve", lambda e, pt3=pt3: e.tensor_tensor(out=Vb[:].bitcast(F32R), in0=pt3, in1=colb(0), op=ALU.mult),
              reads=[pn, "cols"], writes=["Vb"])
        yield
        gcb3 = gcb[:].rearrange("p (s t) -> p s t", t=128)
        tk.op("dve", lambda e: e.tensor_tensor(out=tdf[:], in0=gcb3, in1=colb(1), op=ALU.subtract),
              reads=["gcb", "cols"], writes=["tdf"])
        tk.op("dve", lambda e: e.tensor_scalar(out=DT[:], in0=tdf[:], scalar1=0.0, scalar2=None, op0=ALU.min),
              reads=["tdf"], writes=["DT"])
        tk.op("act", lambda e: e.activation(out=DT[:], in_=DT[:], func=AF.Exp), reads=["DT"], writes=["DT"])
        tk.op("dve", lambda e: e.tensor_scalar(out=Dm[:], in0=tdf[:], scalar1=0.0, scalar2=None, op0=ALU.max),
              reads=["tdf"], writes=["Dm"])
        tk.op("act", lambda e: e.activation(out=Dm[:], in_=Dm[:], func=AF.Exp, scale=-1.0), reads=["Dm"],
              writes=["Dm"])
        yield
        pnk, ptk = pp.next()
        tk.ops("pe", [lambda e, s=s, ptk=ptk: e.matmul(ptk[:, s * 128:(s + 1) * 128], lhsT=kn[:, s * 128:(s + 1) * 128].bitcast(F32R),
                                                       rhs=kn[:, s * 128:(s + 1) * 128].bitcast(F32R), start=True, stop=True)
                      for s in range(4)], reads=["kn"], writes=[pnk])
        pnq, ptq = pp.next()
        tk.ops("pe", [lambda e, s=s, ptq=ptq: e.matmul(ptq[:, s * 128:(s + 1) * 128], lhsT=kn[:, s * 128:(s + 1) * 128].bitcast(F32R),
                                                       rhs=qn[:, s * 128:(s + 1) * 128].bitcast(F32R), start=True, stop=True)
                      for s in range(4)], reads=["kn", "qn"], writes=[pnq])
        kk3 = ptk[:, :].rearrange("p (s d) -> p s d", d=128)
        qk3 = ptq[:, :].rearrange("p (s d) -> p s d", d=128)
        b3 = betab[:].rearrange("p (s t) -> p s t", t=128)
        tk.op("dve", lambda e: e.tensor_tensor(out=E1[:], in0=DT[:], in1=b3, op=ALU.mult), reads=["DT", "betab"],
              writes=["tdf"])
        tk.op("dve", lambda e: e.tensor_tensor(out=E1[:], in0=E1[:], in1=mus_neg, op=ALU.mult),
              reads=["tdf", "mus"], writes=["tdf"])
        tk.op("dve", lambda e: e.tensor_tensor(out=Bm[0][:].bitcast(F32R), in0=kk3, in1=E1[:], op=ALU.mult), reads=[pnk, "tdf"],
              writes=["Bm0"])
        tk.op("dve", lambda e: e.tensor_tensor(out=DT[:], in0=DT[:], in1=mu_inc, op=ALU.mult),
              reads=["DT", "mui", "tdf"], writes=["DT"])
        tk.op("dve", lambda e: e.tensor_tensor(out=QKT[:], in0=qk3, in1=DT[:], op=ALU.mult), reads=[pnq, "DT"],
              writes=[qkk])
        tk.op("dve", lambda e: e.tensor_tensor(out=Dm[:], in0=Dm[:], in1=mls_neg, op=ALU.mult),
              reads=["Dm", "mls"], writes=["Dm"])
        tk.op("dve", lambda e: e.tensor_tensor(out=Dm[:], in0=Dm[:], in1=colb(0), op=ALU.mult),
              reads=["Dm", "cols"], writes=["Dm"])
        tk.op("dve", lambda e: e.tensor_tensor(out=BTm[0][:].bitcast(F32R), in0=kk3, in1=Dm[:], op=ALU.mult), reads=[pnk, "Dm"],
              writes=["BTm0"])
        tk.op("dve", lambda e: e.tensor_tensor(out=Pm[0][:].bitcast(F32R), in0=Bm[0][:], in1=ident4, op=ALU.add),
              reads=["Bm0", "ident"], writes=["Pm0"])
        yield
        cb = 0
        cp = 0
        for k in range(1, 6):
            nb = 1 - cb
            if k < 5:
                pn1, pt1 = pp.next()
                tk.ops("pe", [lambda e, s=s, pt1=pt1, cb=cb: e.matmul(
                    pt1[:, s * 128:(s + 1) * 128], lhsT=BTm[cb][:, s, :].bitcast(F32R), rhs=Bm[cb][:, s, :].bitcast(F32R), start=True, stop=True)
                    for s in range(4)], reads=["Bm%d" % cb, "BTm%d" % cb], writes=[pn1])
            pn2, pt2 = pp.next()
            tk.ops("pe", [lambda e, s=s, pt2=pt2, cb=cb: e.matmul(
                pt2[:, s * 128:(s + 1) * 128], lhsT=Bm[cb][:, s, :].bitcast(F32R), rhs=BTm[cb][:, s, :].bitcast(F32R), start=True, stop=True)
                for s in range(4)], reads=["Bm%d" % cb, "BTm%d" % cb], writes=[pn2])
            if k < 5:
                tk.op("act", lambda e, pt1=pt1, nb=nb: e.copy(out=Bm[nb][:].rearrange("p s d -> p (s d)").bitcast(F32R), in_=pt1[:, :]),
                      reads=[pn1], writes=["Bm%d" % nb])
            tk.op("dve", lambda e, pt2=pt2, nb=nb: e.tensor_copy(out=BTm[nb][:].rearrange("p s d -> p (s d)").bitcast(F32R),
                                                                 in_=pt2[:, :]), reads=[pn2], writes=["BTm%d" % nb])
            pn3, pt3_ = pp.next()
            tk.ops("pe", [lambda e, s=s, pt3_=pt3_, nb=nb, cp=cp: e.matmul(
                pt3_[:, s * 128:(s + 1) * 128], lhsT=BTm[nb][:, s, :].bitcast(F32R), rhs=Pm[cp][:, s, :].bitcast(F32R), start=True, stop=True)
                for s in range(4)], reads=["BTm%d" % nb, "Pm%d" % cp], writes=[pn3])
            tk.op("dve", lambda e, pt3_=pt3_, cp=cp: e.tensor_tensor(
                out=Pm[1 - cp][:].rearrange("p s d -> p (s d)").bitcast(F32R), in0=Pm[cp][:].rearrange("p s d -> p (s d)"),
                in1=pt3_[:, :], op=ALU.add), reads=[pn3, "Pm%d" % cp], writes=["Pm%d" % (1 - cp)])
            cb = nb
            cp = 1 - cp
            yield
        Pf = Pm[cp]
        pk = "Pm%d" % cp
        yield
        pn, pt = pp.next()
        tk.ops("pe", [lambda e, s=s, pt=pt: e.matmul(pt[:, s * 128:(s + 1) * 128], lhsT=Pf[:, s, :].bitcast(F32R), rhs=Vb[:, s, :].bitcast(F32R),
                                                     start=True, stop=True) for s in range(4)],
               reads=[pk, "Vb"], writes=[pn])
        tk.op("act", lambda e, pt=pt: e.copy(out=U[:].rearrange("p s d -> p (s d)"), in_=pt[:, :]), reads=[pn],
              writes=[uk])
        pn, pt = pp.next()
        tk.ops("pe", [lambda e, s=s, pt=pt: e.matmul(pt[:, s * 128:(s + 1) * 128], lhsT=Kbe[:, s, :].bitcast(F32R), rhs=Pf[:, s, :].bitcast(F32R),
                                                     start=True, stop=True) for s in range(4)],
               reads=[pk, "Kbe"], writes=[pn])
        tk.op("act", lambda e, pt=pt: e.copy(out=WT[:], in_=pt[:, :]), reads=[pn], writes=[wk])
        tk.op("dve", lambda e: e.tensor_tensor(out=QdT[:], in0=qn[:], in1=egcb[:], op=ALU.mult),
              reads=["qn", ek], writes=[qdk])
        yield

    def prep_attn(c):
        ts = slice((c * T) % 4096, (c * T) % 4096 + T)
        rope_tables(c)
        yield
        pnx, ptx = proj(c, 7)
        for m, dstT, dk_, xo in ((4, qT, "qT", 0), (5, kT, "kT", 32)):
            pn, pt = proj(c, m)
            tk.op("act", lambda e, pt=pt, dstT=dstT: e.copy(out=dstT[32:64, ts], in_=pt[32:64, :]), reads=[pn],
                  writes=[K(dk_, c)])
            tk.op("act", lambda e, pt=pt, dstT=dstT: e.copy(out=dstT[64:128, ts], in_=pt[64:128, :]), reads=[pn],
                  writes=[K(dk_, c)])
            tk.op("dve", lambda e, pt=pt: e.tensor_tensor(out=rt1[:], in0=pt[0:32, :], in1=sincos[32:64, :],
                                                          op=ALU.mult), reads=[pn, "sincos"], writes=["rt1"])
            tk.op("dve", lambda e, ptx=ptx, xo=xo: e.tensor_tensor(out=rt2[:], in0=ptx[xo:xo + 32, :],
                                                                   in1=sincos[0:32, :], op=ALU.mult),
                  reads=[pnx, "sincos"], writes=["rt2"])
            tk.op("dve", lambda e, dstT=dstT: e.tensor_tensor(out=dstT[0:32, ts], in0=rt1[:], in1=rt2[:], op=ALU.add),
                  reads=["rt1", "rt2"], writes=[K(dk_, c)])
            yield
        pn, pt = proj(c, 6)
        tk.op("act", lambda e, pt=pt: e.copy(out=vT[:, ts], in_=pt[:, :]), reads=[pn], writes=[K("vT", c)])

        yield

    def scan(c):
        ts = slice(c * T, (c + 1) * T)
        par = c % 2
        U, WT, QdT, QKT, Kd, egcb, zs = U2[par], WT2[par], QdT2[par], QKT2[par], Kd2[par], egcb2[par], zs2[par]
        uk, wk, qdk, qkk, kdk, ek, zk = ("U%d" % par, "WT%d" % par, "QdT%d" % par, "QKT%d" % par, "Kd%d" % par,
                                         "egcb%d" % par, "zs%d" % par)
        ot = PS["ot"]
        sc_ = PS["scan"]
        for n in range(8):
            gi = c * 8 + n
            s, half = n // 2, n % 2
            r0 = 64 * half
            rows = slice(r0, r0 + 64)
            tsl = slice(64 * n, 64 * n + 64)
            Sc, Sn = S[gi % 2], S[(gi + 1) % 2]
            sk, snk = "S%d" % (gi % 2), "S%d" % ((gi + 1) % 2)
            slot = (gi % 2) * 256
            tk.op("pe", lambda e: e.matmul(sc_[rows, slot:slot + 128], lhsT=WT[:, tsl], rhs=Sc[:], start=True, stop=True),
                  reads=[wk, sk], writes=["ps_scan_a%d" % (gi % 2)])
            tk.op("dve", lambda e: e.tensor_tensor(out=vnew[rows, gi % 2, :], in0=U[rows, s, :],
                                                   in1=sc_[rows, slot:slot + 128], op=ALU.subtract),
                  reads=[uk, "ps_scan_a%d" % (gi % 2)], writes=["vnew%d" % (gi % 2)])
            tk.ops("pe", [
                lambda e: e.matmul(ot[:, tsl], lhsT=Sc[:], rhs=QdT[:, tsl], start=True, stop=False),
                lambda e: e.matmul(ot[:, tsl], lhsT=vnew[rows, gi % 2, :], rhs=QKT[rows, s, r0:r0 + 64], start=False,
                                   stop=True),
                lambda e: e.matmul(sc_[:, slot + 128:slot + 256], lhsT=Kd[rows, s, :], rhs=vnew[rows, gi % 2, :],
                                   start=True, stop=True)],
                reads=[sk, qdk, qkk, kdk, "vnew%d" % (gi % 2)], writes=["ps_ot", "ps_scan_b%d" % (gi % 2)])
            tl = 64 * n + 63
            tk.op("dve", lambda e: e.scalar_tensor_tensor(out=Sn[:], in0=Sc[:], scalar=egcb[:, tl:tl + 1],
                                                          in1=sc_[:, slot + 128:slot + 256], op0=ALU.mult, op1=ALU.add),
                  reads=[sk, ek, "ps_scan_b%d" % (gi % 2)], writes=[snk])
            yield
        tk.op("act", lambda e: e.activation(out=osq[:], in_=ot[:, :], func=AF.Square), reads=["ps_ot"], writes=["p1"])
        pn, pt = ppa.next()
        tk.op("pe", lambda e: e.matmul(pt[:, :], lhsT=ones_f[:], rhs=osq[:], start=True, stop=True),
              reads=["p1", "ones_f"], writes=[pn])
        tk.op("act", lambda e: e.activation(out=rs[:], in_=pt[:, :], func=AF.Ln, bias=RMS_EPS_AP[:, 0:1],
                                            scale=1.0 / 128.0), reads=[pn, "epsc"], writes=["p2"])
        tk.op("act", lambda e: e.activation(out=rs[:], in_=rs[:], func=AF.Exp, scale=-0.5), reads=["p2"],
              writes=["p2"])
        tk.op("dve", lambda e: e.scalar_tensor_tensor(out=oa[:], in0=ot[:, :], scalar=pvec[:, 2:3], in1=rs[:],
                                                      op0=ALU.mult, op1=ALU.mult), reads=["ps_ot", "pvec", "p2"],
              writes=["p3"])
        tk.op("dve", lambda e: e.tensor_tensor(out=oab[:], in0=oa[:], in1=zs[:], op=ALU.mult), reads=["p3", zk],
              writes=["oab"])
        if cin is None:
            tk.dma("sp", lambda e: e.dma_start(out=mixT[0:128, ts], in_=oab[:]), reads=["oab"], writes=["mixT_a"])
        else:
            tk.dma("sp", lambda e: e.dma_start(out=cin[c // 4][0:128, (c % 4) * T:(c % 4 + 1) * T], in_=oab[:]),
                   reads=["oab"], writes=[("cin_a", c // 4)])
        yield

    att_scale = 128.0 ** -0.5
    pcount = [0]

    def qblock(qsel, kcur, kprev, vcur, vprev, vkeys, accv, first, rkeys):
        i = pcount[0] % 2
        pcount[0] += 1
        pTb = pTt[i]
        pk = "pT%d" % i
        pn, pt = ppa.next()
        if kprev is not None:
            tk.ops("pe", [
                lambda e: e.matmul(pt[:, 0:256], lhsT=ident_b[:], rhs=amask[:, 0:256], start=True, stop=False),
                lambda e: e.matmul(pt[:, 0:128], lhsT=kprev, rhs=qsel, start=False, stop=False),
                lambda e: e.matmul(pt[:, 128:256], lhsT=kcur, rhs=qsel, start=False, stop=True)],
                reads=rkeys + ["ident_b", "amask"], writes=[pn])
            tk.op("act", lambda e: e.activation(out=pTb[:, 0:256], in_=pt[:, 0:256], func=AF.Exp, scale=att_scale),
                  reads=[pn], writes=[pk])
            pn2, pt2 = ppa.next()
            tk.ops("pe", [
                lambda e: e.matmul(pt2[:, 0:128], lhsT=vprev, rhs=pTb[:, 0:128], start=True, stop=False),
                lambda e: e.matmul(pt2[:, 0:128], lhsT=vcur, rhs=pTb[:, 128:256], start=False, stop=True),
                lambda e: e.matmul(pt2[:, 128:256], lhsT=ones_b[:], rhs=pTb[:, 0:128], start=True, stop=False),
                lambda e: e.matmul(pt2[:, 128:256], lhsT=ones_b[:], rhs=pTb[:, 128:256], start=False, stop=True)],
                reads=[pk, "ones_b"] + vkeys, writes=[pn2])
        else:
            tk.ops("pe", [
                lambda e: e.matmul(pt[:, 128:256], lhsT=ident_b[:], rhs=amask[:, 128:256], start=True, stop=False),
                lambda e: e.matmul(pt[:, 128:256], lhsT=kcur, rhs=qsel, start=False, stop=True)],
                reads=rkeys + ["ident_b", "amask"], writes=[pn])
            tk.op("act", lambda e: e.activation(out=pTb[:, 128:256], in_=pt[:, 128:256], func=AF.Exp, scale=att_scale),
                  reads=[pn], writes=[pk])
            pn2, pt2 = ppa.next()
            tk.ops("pe", [
                lambda e: e.matmul(pt2[:, 0:128], lhsT=vcur, rhs=pTb[:, 128:256], start=True, stop=True),
                lambda e: e.matmul(pt2[:, 128:256], lhsT=ones_b[:], rhs=pTb[:, 128:256], start=True, stop=True)],
                reads=[pk, "ones_b"] + vkeys, writes=[pn2])
        src = pt2[:, 0:256].rearrange("p (a q) -> p a q", a=2)
        if first:
            tk.op("dve", lambda e: e.tensor_copy(out=accv, in_=src), reads=[pn2], writes=["acc"])
        else:
            tk.op("dve", lambda e: e.tensor_tensor(out=accv, in0=accv, in1=src, op=ALU.add), reads=[pn2, "acc"],
                  writes=["acc"])

    vcnt = [0]

    def vtrans(vsel, rk):
        i = vcnt[0] % 4
        vcnt[0] += 1
        pn, pt = ppa.next()
        ptb = pt[:, 0:64].bitcast(BF16)
        tk.op("pe", lambda e: e.transpose(ptb, vsel, ident_b[:]), reads=rk + ["ident_b"], writes=[pn])
        tk.op("act", lambda e: e.copy(out=vtt[:, i, :], in_=ptb), reads=[pn], writes=["vtt%d" % i])
        return vtt[:, i, :], "vtt%d" % i

    def rsl(t0, n, step=1):
        r0 = t0 % RING
        return slice(r0, r0 + (n - 1) * step + 1, step)

    def attn(c):
        sc = c // 4
        for jj in range(4):
            j = 4 * c + jj
            t0 = 128 * j
            cprev = (t0 - 128) // T
            vc, vck = vtrans(vT[:, rsl(t0, 128)], [K("vT", c)])
            if j > 0:
                vp, vpk = vtrans(vT[:, rsl(t0 - 128, 128)], [K("vT", cprev)])
            else:
                vp, vpk = None, None
            rk = [K("qT", c), K("kT", c)] + ([K("kT", cprev)] if j > 0 else [])
            qblock(qT[:, rsl(t0, 128)], kT[:, rsl(t0, 128)], kT[:, rsl(t0 - 128, 128)] if j > 0 else None,
                   vc, vp, [k_ for k_ in (vck, vpk) if k_], acc[:, :, (t0 % 2048):(t0 % 2048) + 128], True, rk)
            yield
        for r in range(4):
            t0 = T * c + r
            vc, vck = vtrans(vT[:, rsl(t0, 128, 4)], [K("vT", c)])
            if c > 0:
                vp, vpk = vtrans(vT[:, rsl(t0 - T, 128, 4)], [K("vT", c - 1)])
            else:
                vp, vpk = None, None
            rk = [K("qT", c), K("kT", c)] + ([K("kT", c - 1)] if c > 0 else [])
            a0 = (T * c) % 2048 + r
            qblock(qT[:, rsl(t0, 128, 4)], kT[:, rsl(t0, 128, 4)], kT[:, rsl(t0 - T, 128, 4)] if c > 0 else None,
                   vc, vp, [k_ for k_ in (vck, vpk) if k_], acc[:, :, a0:a0 + 509:4], False, rk)
            yield
        if c % 4 == 3:
            cs = [4 * sc + i for i in range(4)]
            csp = [4 * (sc - 1) + i for i in range(4)] if sc > 0 else []
            for r in range(16):
                t0 = 2048 * sc + r
                vc, vck = vtrans(vT[:, rsl(t0, 128, 16)], [K("vT", ci) for ci in cs])
                if sc > 0:
                    vp, vpk = vtrans(vT[:, rsl(t0 - 2048, 128, 16)], [K("vT", ci) for ci in csp])
                else:
                    vp, vpk = None, None
                rk = [K("qT", ci) for ci in cs] + [K("kT", ci) for ci in cs] + [K("kT", ci) for ci in csp]
                qblock(qT[:, rsl(t0, 128, 16)], kT[:, rsl(t0, 128, 16)],
                       kT[:, rsl(t0 - 2048, 128, 16)] if sc > 0 else None,
                       vc, vp, [k_ for k_ in (vck, vpk) if k_], acc[:, :, r:r + 2033:16], False, rk)
                yield
            for i in range(4):
                a = slice(i * T, (i + 1) * T)
                tsl = slice(2048 * sc + i * T, 2048 * sc + (i + 1) * T)
                tk.op("act", lambda e, a=a: e.activation(out=fr[:], in_=acc[:, 1, a], func=AF.Ln), reads=["acc"],
                      writes=["p2"])
                tk.op("act", lambda e: e.activation(out=fr[:], in_=fr[:], func=AF.Exp, scale=-1.0), reads=["p2"],
                      writes=["p2"])
                tk.op("dve", lambda e, a=a: e.tensor_tensor(out=fo[:], in0=acc[:, 0, a], in1=fr[:], op=ALU.mult),
                      reads=["acc", "p2"], writes=["p3"])
                tk.op("act", lambda e: e.activation(out=fsq[:], in_=fo[:], func=AF.Square), reads=["p3"],
                      writes=["p1"])
                pn, pt = ppa.next()
                tk.op("pe", lambda e, pt=pt: e.matmul(pt[:, :], lhsT=ones_f[:], rhs=fsq[:], start=True, stop=True),
                      reads=["p1", "ones_f"], writes=[pn])
                tk.op("act", lambda e, pt=pt: e.activation(out=fr[:], in_=pt[:, :], func=AF.Ln,
                                                           bias=RMS_EPS_AP[:, 0:1], scale=1.0 / 128.0),
                      reads=[pn, "epsc"], writes=["p2"])
                tk.op("act", lambda e: e.activation(out=fr[:], in_=fr[:], func=AF.Exp, scale=-0.5), reads=["p2"],
                      writes=["p2"])
                tk.op("dve", lambda e: e.scalar_tensor_tensor(out=fob[:], in0=fo[:], scalar=pvec[:, 3:4], in1=fr[:],
                                                              op0=ALU.mult, op1=ALU.mult), reads=["p3", "pvec", "p2"],
                      writes=["fob"])
                yield
                if cin is None:
                    tk.dma("sp", lambda e, tsl=tsl: e.dma_start(out=mixT[128:256, tsl], in_=fob[:]), reads=["fob"],
                           writes=["mixT_b"])
                else:
                    tk.dma("sp", lambda e, i=i: e.dma_start(out=cin[sc][128:256, i * T:(i + 1) * T], in_=fob[:]),
                           reads=["fob"], writes=[("cin_b", sc)])
            if on_quarter is not None:
                on_quarter(sc, tk)
        yield

    epsc = sb("epsc", [128, 2])
    tk.op("pool", lambda e: e.memset(epsc[:, 0:1], RMS_EPS), writes=["epsc"])
    tk.op("pool", lambda e: e.memset(epsc[:, 1:2], 1.0), writes=["epsc"])
    RMS_EPS_AP = epsc[:, 0:1]
    ONE_AP = epsc[:, 1:2]

    def run(gen):
        if gen is not None:
            for _ in gen:
                pass

    load_x(0)
    run(prep_main(0))
    tk.flush()
    for c in range(nch):
        run(scan(c))
        run(prep_attn(c))
        run(attn(c))
        if c + 1 < nch:
            run(prep_main(c + 1))
        if c % 2 == 1 or c == nch - 1:
            tk.flush()
    tk = real_tk


def build_phase1(nch=NCH):
    nc = bass.Bass("TRN2", target_bir_lowering=False)
    D = {}

    def din(name, shape, dt=F32):
        D[name] = nc.dram_tensor(name, shape, dt, kind="ExternalInput").ap()

    din("x", [D_MODEL, SEQ]); din("cT", [128, 8]); din("pos", [SEQ], I32)
    din("wada1", [128, 8, 8, 256]); din("bada1", [128, 16]); din("win", [128, 5, 8, 256])
    din("convw", [128, 3, 4]); din("pvec", [128, 4])
    din("c_ident", [128, 128]); din("c_masks", [128, 3, 128]); din("c_amask", [128, 256]); din("c_invf", [64, 2])
    D["mixT"] = nc.dram_tensor("mixT", [256, SEQ], BF16, kind="ExternalOutput").ap()
    with ExitStack() as es:
        tk = Trk(nc, es)
        phase1(nc, tk, es, D, nch=nch)
        tk.finish("sp")
    return nc


def consts():
    p = np.arange(128)[:, None]
    f = np.arange(128)[None, :]
    same = (p // 64) == (f // 64)
    mus = -((f > p) & same).astype(np.float32)
    mui = ((f >= p) & same).astype(np.float32)
    mls = -((f < p) & same).astype(np.float32)
    masks = np.stack([mus, mui, mls], 1).astype(np.float32)
    prev = np.where(p >= f, 0.0, NEG).astype(np.float32)
    cur = np.where(p <= f, 0.0, NEG).astype(np.float32)
    amask = np.concatenate([prev, cur], 1)
    i = (np.arange(64) % 16).astype(np.float32)
    invf = (ROPE_THETA ** (-i * 2.0 / 32.0)).astype(np.float32)
    invf = np.stack([invf, np.where(np.arange(64) < 32, 0.0, math.pi / 2).astype(np.float32)], 1)
    return {"c_ident": np.eye(128, dtype=np.float32), "c_masks": masks, "c_amask": amask, "c_invf": invf}


def phase1_inputs(inp, core):
    b, h = core // 4, core % 4
    l = 0
    w_in = inp["w_in"][l]
    hs = slice(h * 128, (h + 1) * 128)
    qa, ka, va, z = w_in[:, 0:512][:, hs], w_in[:, 512:1024][:, hs], w_in[:, 1024:1536][:, hs], w_in[:, 1536:2048][:, hs]
    beta = w_in[:, 2048 + h:2049 + h]
    dec = w_in[:, 2052 + h:2053 + h]
    qb, kb, vb = w_in[:, 2056:2568][:, hs], w_in[:, 2568:3080][:, hs], w_in[:, 3080:3592][:, hs]
    extra = np.zeros((D_MODEL, 128), np.float32)
    extra[:, 0:16] = qb[:, 16:32]; extra[:, 16:32] = qb[:, 0:16]
    extra[:, 32:48] = kb[:, 16:32]; extra[:, 48:64] = kb[:, 0:16]
    win = np.concatenate([qa, ka, va, z, qb, kb, vb, extra, np.repeat(beta, 128, 1), np.repeat(dec, 128, 1)], 1)
    conv = inp["conv_w"][l]
    convw = np.stack([conv[:, g * 512:(g + 1) * 512][:, hs].T for g in range(3)], 1)
    pvec = np.stack([np.full(128, inp["a_log"][l, h]), np.full(128, inp["dt_bias"][l, h]),
                     inp["gdn_norm_w"][l], inp["attn_norm_w"][l]], 1).astype(np.float32)
    win_l = win.astype(np.float32).reshape(8, 128, 5, 256).transpose(1, 2, 0, 3)
    wada1_l = inp["w_ada"][l][:, 0:2048].reshape(8, 128, 8, 256).transpose(1, 2, 0, 3)
    d = {"x": np.ascontiguousarray(inp["x"][b].T), "cT": np.ascontiguousarray(inp["c"][b].reshape(8, 128).T),
         "pos": np.ascontiguousarray(inp["positions"][b]).astype(np.int32),
         "wada1": np.ascontiguousarray(wada1_l),
         "bada1": np.ascontiguousarray(inp["b_ada"][l][0:2048].reshape(16, 128).T),
         "win": np.ascontiguousarray(win_l), "convw": np.ascontiguousarray(convw.astype(np.float32)),
         "pvec": np.ascontiguousarray(pvec)}
    d.update(consts())
    return d


NT2 = 16
NBLK = 64


def phase2(nc, tk, es, D, ntile=NT2, nblk=NBLK, cout=None):
    real = tk
    tk = Rec(real)

    def mk(stack, pref):
        def sb(name, shape, dt=F32):
            return stack.enter_context(nc.sbuf_tensor(pref + name, shape, dt))
        return sb

    sb = mk(es, "t_")
    pp = PsumPool(nc, es, ["r0", "r1", "r2", "r3", "r4", "r5", "r6", "r7"])

    ident = sb("ident", [128, 128])
    ones_f = sb("ones_f", [128, 128])
    triS = sb("triS", [128, 128])
    kcoff = sb("kcoff", [128, 1])
    thr16 = sb("thr16", [128, 16])
    thr64 = sb("thr64", [128, 64])
    epsc = sb("epsc", [128, 1])
    cT = sb("cT", [128, 8]); scT = sb("scT", [128, 8])
    modb = sb("modb", [128, 4, 1024])
    lnp = sb("lnp", [128, 4, 1024])
    xt2 = [sb("xt%d" % i, [128, 1024]) for i in range(2)]
    rr2 = [sb("rr%d" % i, [128, 1024]) for i in range(2)]
    x12 = [sb("x1%d" % i, [128, 1024]) for i in range(2)]
    h22 = [sb("h2%d" % i, [128, 1024]) for i in range(2)]
    stt = sb("stt", [128, 2, 6]); mv = sb("mv", [128, 2]); rstd = sb("rstd", [128, 1])
    gts = sb("gts", [128, NT2, 2])
    desti = sb("desti", [128, 2, NT2], I32)
    idxw = sb("idxw", [128, 64], I32)

    x = D["x2"]; mixTf = D.get("mixTf"); out = D["out"]
    x1d = D["x1d"]; h2d = D["h2d"]; xs = D["xs"]; ys = D["ys"]

    def layer_norm(src, skey, dst, dkey, gi):
        tk.op("dve", lambda e: e.bn_stats(out=stt[:, 0, :], in_=src[:, 0:512]), reads=[skey], writes=["stt"])
        tk.op("dve", lambda e: e.bn_stats(out=stt[:, 1, :], in_=src[:, 512:1024]), reads=[skey], writes=["stt"])
        tk.op("dve", lambda e: e.bn_aggr(out=mv[:], in_=stt[:].rearrange("p a b -> p (a b)")), reads=["stt"],
              writes=["mv"])
        tk.op("act", lambda e: e.activation(out=rstd[:], in_=mv[:, 1:2], func=AF.Sqrt, bias=epsc[:, 0:1], scale=1.0),
              reads=["mv", "epsc"], writes=["rstd"])
        tk.op("dve", lambda e: e.reciprocal(out=rstd[:], in_=rstd[:]), reads=["rstd"], writes=["rstd"])
        tk.op("dve", lambda e: e.tensor_scalar(out=dst[:], in0=src[:], scalar1=mv[:, 0:1], scalar2=rstd[:, 0:1],
                                               op0=ALU.subtract, op1=ALU.mult), reads=[skey, "mv", "rstd"],
              writes=[dkey])
        tk.op("dve", lambda e: e.tensor_tensor(out=dst[:], in0=dst[:], in1=lnp[:, gi, :], op=ALU.mult),
              reads=[dkey, "lnp"], writes=[dkey])
        tk.op("dve", lambda e: e.tensor_tensor(out=dst[:], in0=dst[:], in1=lnp[:, gi + 1, :], op=ALU.add),
              reads=[dkey, "lnp"], writes=[dkey])

    tk.dma("sp", lambda e: e.dma_start(out=ident[:], in_=D["c_ident"][:, :]), writes=["ident"])
    tk.dma("sp", lambda e: e.dma_start(out=triS[:], in_=D["c_tri"][:, :]), writes=["triS"])
    tk.dma("sp", lambda e: e.dma_start(out=kcoff[:], in_=D["c_iota"][:, :]), writes=["kcoff"])
    tk.dma("sp", lambda e: e.dma_start(out=thr16[:], in_=D["c_thr16"][:, :]), writes=["thr16"])
    tk.dma("sp", lambda e: e.dma_start(out=thr64[:], in_=D["c_thr64"][:, :]), writes=["thr64"])
    tk.dma("sp", lambda e: e.dma_start(out=cT[:], in_=D["cT"][:, :]), writes=["cT"])
    for i in range(4):
        tk.dma("sp", lambda e, i=i: e.dma_start(out=lnp[:, i, :], in_=D["lnp"][i, :].partition_broadcast(128)),
               writes=["lnp"])
    tk.op("pool", lambda e: e.memset(ones_f[:], 1.0), writes=["ones_f"])
    tk.op("pool", lambda e: e.memset(epsc[:], LN_EPS), writes=["epsc"])

    with ExitStack() as esA:
        sa = mk(esA, "a_")
        scR = sa("scR", [128, 8, 128])
        badab = sa("badab", [128, 512])
        wo_b = sa("wo_b", [128, 8, 1024], BF16)
        wr = sa("wr", [128, 8, 36]); brb = sa("brb", [128, 36])
        h2T2 = [sa("h2T%d" % i, [128, 8, 128]) for i in range(2)]
        lg = sa("lg", [128, 36]); gmax = sa("gmax", [128, 1]); ngmax = sa("ngmax", [128, 1]); ohg = sa("ohg", [128, 4])
        ex4 = sa("ex4", [128, 4]); sume = sa("sume", [128, 1]); ggrp = sa("ggrp", [128, 1])
        tmp48 = sa("tmp48", [128, 4, 8]); lesel = sa("lesel", [128, 8]); m8 = sa("m8", [128, 8])
        oh8 = sa("oh8", [128, 2, 8]); e2 = sa("e2", [128, 1]); g1 = sa("g1", [128, 1])
        ohs = sa("ohs", [128, NT2, 2, 32]); ohany = sa("ohany", [128, 32]); cum = sa("cum", [128, 32])
        ranks = sa("ranks", [128, NT2, 32])
        cntb = sa("cntb", [128, 32]); cmp16 = sa("cmp16", [128, 32, 16]); padded = sa("padded", [128, 32])
        onesr = sa("onesr", [128, 32]); padend = sa("padend", [128, 32]); padstart = sa("padstart", [128, 32])
        tmpd = sa("tmpd", [128, NT2, 32]); tmpd2 = sa("tmpd2", [128, NT2, 32])
        destf = sa("destf", [128, 2, NT2])
        cmp64 = sa("cmp64", [128, 64, 32]); blkf = sa("blkf", [128, 64])

        tk.dma("sp", lambda e: e.dma_start(out=wr[:], in_=D["wr"][:, :].rearrange("(kc p) c -> p kc c", p=128)),
               writes=["wr"])
        tk.dma("sp", lambda e: e.dma_start(out=brb[:], in_=D["br"][:].partition_broadcast(128)), writes=["brb"])
        tk.op("pool", lambda e: e.memset(cum[:], 0.0), writes=["cum"])
        tk.op("pool", lambda e: e.memset(onesr[:], 1.0), writes=["onesr"])
        if cout is None:
            mt1 = sa("mt", [128, 8, 128], BF16)
        else:
            mtf = sa("mtf", [128, 8, 2048], BF16)
            qidx = sa("qidx", [128, 8], I32)
            tk.dma("sp", lambda e: e.dma_start(out=qidx[:], in_=D["qidx"][:, :]), writes=["qidx"])
            for kc in range(8):
                tk.dma("pool", lambda e, kc=kc: e.indirect_dma_start(
                    out=mtf[:, kc, :], out_offset=None, in_=cout[:, :],
                    in_offset=bass.IndirectOffsetOnAxis(ap=qidx[:, kc:kc + 1], axis=0)),
                    reads=["qidx"] + [("cout", q_) for q_ in range(4)], writes=["mt"])
        stg = [(xt2[0], "xt0"), (rr2[0], "rr0"), (xt2[1], "xt1"), (rr2[1], "rr1")]
        for kc in range(8):
            st, key = stg[kc % 4]
            tk.dma("sp", lambda e, st=st, kc=kc: e.dma_start(out=st[:], in_=D["wo"][kc * 128:(kc + 1) * 128, :]),
                   writes=[key])
            tk.op("dve", lambda e, st=st, kc=kc: e.tensor_copy(out=wo_b[:, kc, :], in_=st[:]), reads=[key],
                  writes=["wo_b"])
        tk.op("act", lambda e: e.activation(out=scT[:], in_=cT[:], func=AF.Silu), reads=["cT"], writes=["scT"])
        tk.op("dve", lambda e: e.tensor_copy(out=scR[:], in_=scT[:].unsqueeze(2).to_broadcast([128, 8, 128])),
              reads=["scT"], writes=["scR"])
        for blk in range(8):
            pn, pt = pp.next()
            for sub in range(4):
                st, key = stg[sub % 4]
                st3 = st[:].rearrange("p (kc c) -> p kc c", c=128)
                c0 = blk * 512 + sub * 128
                tk.dma("sp" if sub % 2 == 0 else "pool", lambda e, st3=st3, blk=blk, sub=sub: e.dma_start(
                    out=st3, in_=D["wada2"][:, blk * 4 + sub, :, :]), writes=[key])
                tk.ops("pe", [lambda e, kc=kc, pt=pt, st3=st3, sub=sub: e.matmul(
                    pt[:, sub * 128:(sub + 1) * 128], lhsT=scR[:, kc, :], rhs=st3[:, kc, :], start=(kc == 0),
                    stop=(kc == 7)) for kc in range(8)], reads=[key, "scR"], writes=[pn])
            tk.dma("sp", lambda e, blk=blk: e.dma_start(
                out=badab[:], in_=D["bada2"][blk * 512:(blk + 1) * 512].partition_broadcast(128)), writes=["badab"])
            dst = modb[:, blk // 2, (blk % 2) * 512:(blk % 2 + 1) * 512]
            tk.op("dve", lambda e, pt=pt, dst=dst: e.tensor_tensor(out=dst, in0=pt[:, :], in1=badab[:], op=ALU.add),
                  reads=[pn, "badab"], writes=["modb"])
            if blk // 2 != 1:
                tk.op("dve", lambda e, dst=dst: e.tensor_scalar(out=dst, in0=dst, scalar1=1.0, scalar2=None,
                                                                op0=ALU.add), reads=["modb"], writes=["modb"])
        tk.flush()

        for k in range(ntile):
            par = k % 2
            xt, rr, x1, h2, h2T = xt2[par], rr2[par], x12[par], h22[par], h2T2[par]
            xtk, rrk, x1k, h2k, h2Tk = "xt%d" % par, "rr%d" % par, "x1%d" % par, "h2%d" % par, "h2T%d" % par
            tsl = slice(k * 128, (k + 1) * 128)
            if cout is None:
                mt = mt1
                tk.dma("sp", lambda e: e.dma_start(out=mt[:], in_=mixTf[:, tsl].rearrange("(kc p) t -> p kc t", p=128)),
                       writes=["mt"])
            else:
                mt = mtf[:, :, tsl]
            tk.dma("sp", lambda e: e.dma_start(out=xt[:], in_=x[tsl, :]), writes=[xtk])
            pa, pta = pp.next()
            pb, ptb = pp.next()
            for (pn, pt, hs) in ((pa, pta, slice(0, 512)), (pb, ptb, slice(512, 1024))):
                tk.ops("pe", [lambda e, kc=kc, pt=pt, hs=hs: e.matmul(pt[:, :], lhsT=mt[:, kc, :], rhs=wo_b[:, kc, hs],
                                                                     start=(kc == 0), stop=(kc == 7))
                              for kc in range(8)], reads=["mt", "wo_b"], writes=[pn])
                tk.op("dve", lambda e, pt=pt, hs=hs: e.tensor_tensor(out=rr[:, hs], in0=pt[:, :], in1=modb[:, 0, hs],
                                                                     op=ALU.mult), reads=[pn, "modb"], writes=[rrk])
            tk.op("dve", lambda e: e.scalar_tensor_tensor(out=rr[:], in0=xt[:], scalar=ALPHA, in1=rr[:], op0=ALU.mult,
                                                          op1=ALU.add), reads=[xtk, rrk], writes=[rrk])
            layer_norm(rr, rrk, x1, x1k, 0)
            tk.dma("sp", lambda e: e.dma_start(out=x1d[tsl, :], in_=x1[:]), reads=[x1k], writes=["x1d"])
            tk.op("dve", lambda e: e.tensor_tensor(out=h2[:], in0=x1[:], in1=modb[:, 2, :], op=ALU.mult),
                  reads=[x1k, "modb"], writes=[h2k])
            tk.op("dve", lambda e: e.tensor_tensor(out=h2[:], in0=h2[:], in1=modb[:, 1, :], op=ALU.add),
                  reads=[h2k, "modb"], writes=[h2k])
            tk.dma("sp", lambda e: e.dma_start(out=h2d[tsl, :], in_=h2[:]), reads=[h2k], writes=["h2d"])
            for half in range(2):
                pn, pt = pp.next()
                tk.ops("pe", [lambda e, j=j, pt=pt, half=half: e.transpose(
                    pt[:, j * 128:(j + 1) * 128], h2[:, (half * 4 + j) * 128:(half * 4 + j + 1) * 128], ident[:])
                    for j in range(4)], reads=[h2k, "ident"], writes=[pn])
                tk.op("act", lambda e, pt=pt, half=half: e.copy(
                    out=h2T[:, half * 4:(half + 1) * 4, :].rearrange("p a b -> p (a b)"), in_=pt[:, :]), reads=[pn],
                    writes=[h2Tk])
            pn, pt = pp.next()
            tk.ops("pe", [lambda e, kc=kc, pt=pt: e.matmul(pt[:, 0:36], lhsT=h2T[:, kc, :], rhs=wr[:, kc, :],
                                                          start=(kc == 0), stop=(kc == 7)) for kc in range(8)],
                   reads=[h2Tk, "wr"], writes=[pn])
            tk.op("dve", lambda e, pt=pt: e.tensor_tensor(out=lg[:], in0=pt[:, 0:36], in1=brb[:], op=ALU.add),
                  reads=[pn, "brb"], writes=["lg"])
            tk.op("dve", lambda e: e.tensor_reduce(out=gmax[:], in_=lg[:, 0:4], axis=AX.X, op=ALU.max), reads=["lg"],
                  writes=["gmax"])
            tk.op("dve", lambda e: e.tensor_scalar(out=ohg[:], in0=lg[:, 0:4], scalar1=gmax[:, 0:1], scalar2=None,
                                                   op0=ALU.is_equal), reads=["lg", "gmax"], writes=["ohg"])
            tk.op("dve", lambda e: e.tensor_scalar(out=ngmax[:], in0=gmax[:], scalar1=-1.0, scalar2=None, op0=ALU.mult),
                  reads=["gmax"], writes=["ngmax"])
            tk.op("act", lambda e: e.activation(out=ex4[:], in_=lg[:, 0:4], func=AF.Exp, bias=ngmax[:, 0:1], scale=1.0),
                  reads=["lg", "ngmax"], writes=["ex4"])
            tk.op("dve", lambda e: e.tensor_reduce(out=sume[:], in_=ex4[:], axis=AX.X, op=ALU.add), reads=["ex4"],
                  writes=["sume"])
            tk.op("dve", lambda e: e.reciprocal(out=ggrp[:], in_=sume[:]), reads=["sume"], writes=["ggrp"])
            le3 = lg[:, 4:36].rearrange("p (g e) -> p g e", e=8)
            tk.op("dve", lambda e: e.tensor_tensor(out=tmp48[:], in0=le3,
                                                   in1=ohg[:].unsqueeze(2).to_broadcast([128, 4, 8]), op=ALU.mult),
                  reads=["lg", "ohg"], writes=["tmp48"])
            tk.op("dve", lambda e: e.tensor_reduce(out=lesel[:], in_=tmp48[:].rearrange("p g e -> p e g"), axis=AX.X,
                                                   op=ALU.add), reads=["tmp48"], writes=["lesel"])
            tk.op("dve", lambda e: e.max(out=m8[:], in_=lesel[:]), reads=["lesel"], writes=["m8"])
            for j in range(2):
                tk.op("dve", lambda e, j=j: e.tensor_scalar(out=oh8[:, j, :], in0=lesel[:], scalar1=m8[:, j:j + 1],
                                                            scalar2=None, op0=ALU.is_equal), reads=["lesel", "m8"],
                      writes=["oh8"])
            tk.op("dve", lambda e: e.tensor_tensor(out=e2[:], in0=m8[:, 1:2], in1=m8[:, 0:1], op=ALU.subtract),
                  reads=["m8"], writes=["e2"])
            tk.op("act", lambda e: e.activation(out=e2[:], in_=e2[:], func=AF.Exp), reads=["e2"], writes=["e2"])
            tk.op("dve", lambda e: e.tensor_scalar(out=g1[:], in0=e2[:], scalar1=1.0, scalar2=None, op0=ALU.add),
                  reads=["e2"], writes=["g1"])
            tk.op("dve", lambda e: e.reciprocal(out=g1[:], in_=g1[:]), reads=["g1"], writes=["g1"])
            tk.op("dve", lambda e: e.tensor_tensor(out=gts[:, k, 0:1], in0=g1[:], in1=ggrp[:], op=ALU.mult),
                  reads=["g1", "ggrp"], writes=["gts"])
            tk.op("dve", lambda e: e.tensor_tensor(out=e2[:], in0=e2[:], in1=g1[:], op=ALU.mult), reads=["e2", "g1"],
                  writes=["e2"])
            tk.op("dve", lambda e: e.tensor_tensor(out=gts[:, k, 1:2], in0=e2[:], in1=ggrp[:], op=ALU.mult),
                  reads=["e2", "ggrp"], writes=["gts"])
            for j in range(2):
                tk.op("dve", lambda e, j=j: e.tensor_tensor(
                    out=ohs[:, k, j, :].rearrange("p (g e) -> p g e", e=8),
                    in0=ohg[:].unsqueeze(2).to_broadcast([128, 4, 8]),
                    in1=oh8[:, j, :].unsqueeze(1).to_broadcast([128, 4, 8]), op=ALU.mult), reads=["ohg", "oh8"],
                    writes=["ohs"])
            tk.op("dve", lambda e: e.tensor_tensor(out=ohany[:], in0=ohs[:, k, 0, :], in1=ohs[:, k, 1, :], op=ALU.add),
                  reads=["ohs"], writes=["ohany"])
            pn, pt = pp.next()
            tk.ops("pe", [lambda e, pt=pt: e.matmul(pt[:, 0:32], lhsT=triS[:], rhs=ohany[:], start=True, stop=False),
                          lambda e, pt=pt: e.matmul(pt[:, 0:32], lhsT=ones_f[:], rhs=cum[:], start=False, stop=True)],
                   reads=["triS", "ohany", "ones_f", "cum"], writes=[pn])
            tk.op("act", lambda e, pt=pt: e.copy(out=ranks[:, k, :], in_=pt[:, 0:32]), reads=[pn], writes=["ranks"])
            tk.op("dve", lambda e: e.tensor_tensor(out=cum[:], in0=cum[:], in1=ohany[:], op=ALU.add),
                  reads=["cum", "ohany"], writes=["cum"])
            if k % 8 == 7:
                tk.flush()

        pn, pt = pp.next()
        tk.op("pe", lambda e: e.matmul(pt[:, 0:32], lhsT=ones_f[:], rhs=cum[:], start=True, stop=True),
              reads=["ones_f", "cum"], writes=[pn])
        tk.op("dve", lambda e: e.tensor_copy(out=cntb[:], in_=pt[:, 0:32]), reads=[pn], writes=["cntb"])
        tk.op("dve", lambda e: e.tensor_tensor(out=cmp16[:], in0=cntb[:].unsqueeze(2).to_broadcast([128, 32, 16]),
                                               in1=thr16[:].unsqueeze(1).to_broadcast([128, 32, 16]), op=ALU.is_gt),
              reads=["cntb", "thr16"], writes=["cmp16"])
        tk.op("dve", lambda e: e.tensor_reduce(out=padded[:], in_=cmp16[:], axis=AX.X, op=ALU.add), reads=["cmp16"],
              writes=["padded"])
        tk.op("dve", lambda e: e.tensor_scalar(out=padded[:], in0=padded[:], scalar1=128.0, scalar2=None, op0=ALU.mult),
              reads=["padded"], writes=["padded"])
        tk.op("dve", lambda e: e.tensor_tensor_scan(out=padend[:], data0=onesr[:], data1=padded[:], initial=0.0,
                                                    op0=ALU.mult, op1=ALU.add), reads=["onesr", "padded"],
              writes=["padend"])
        tk.op("dve", lambda e: e.tensor_tensor(out=padstart[:], in0=padend[:], in1=padded[:], op=ALU.subtract),
              reads=["padend", "padded"], writes=["padstart"])
        tk.op("dve", lambda e: e.tensor_tensor(out=tmpd[:, 0:ntile, :], in0=ranks[:, 0:ntile, :],
                                               in1=padstart[:].unsqueeze(1).to_broadcast([128, ntile, 32]), op=ALU.add),
              reads=["ranks", "padstart"], writes=["tmpd"])
        for j in range(2):
            tk.op("dve", lambda e, j=j: e.tensor_tensor(out=tmpd2[:, 0:ntile, :], in0=tmpd[:, 0:ntile, :],
                                                        in1=ohs[:, 0:ntile, j, :], op=ALU.mult), reads=["tmpd", "ohs"],
                  writes=["tmpd2"])
            tk.op("dve", lambda e, j=j: e.tensor_reduce(out=destf[:, j, 0:ntile], in_=tmpd2[:, 0:ntile, :], axis=AX.X,
                                                        op=ALU.add), reads=["tmpd2"], writes=["destf"])
        tk.op("dve", lambda e: e.tensor_copy(out=desti[:, :, 0:ntile], in_=destf[:, :, 0:ntile]), reads=["destf"],
              writes=["desti"])
        tk.op("dve", lambda e: e.tensor_tensor(out=cmp64[:], in0=padend[:].unsqueeze(1).to_broadcast([128, 64, 32]),
                                               in1=thr64[:].unsqueeze(2).to_broadcast([128, 64, 32]), op=ALU.is_le),
              reads=["padend", "thr64"], writes=["cmp64"])
        tk.op("dve", lambda e: e.tensor_reduce(out=blkf[:], in_=cmp64[:], axis=AX.X, op=ALU.add), reads=["cmp64"],
              writes=["blkf"])
        tk.op("dve", lambda e: e.tensor_scalar(out=blkf[:], in0=blkf[:], scalar1=128.0, scalar2=kcoff[:, 0:1],
                                               op0=ALU.mult, op1=ALU.add), reads=["blkf", "kcoff"], writes=["blkf"])
        tk.op("dve", lambda e: e.tensor_copy(out=idxw[:], in_=blkf[:]), reads=["blkf"], writes=["idxw"])
        for k in range(ntile):
            par = k % 2
            h2, h2k = h22[par], "h2%d" % par
            tsl = slice(k * 128, (k + 1) * 128)
            tk.dma("sp", lambda e: e.dma_start(out=h2[:], in_=h2d[tsl, :]), reads=["h2d"], writes=[h2k])
            for j in range(2):
                tk.dma("pool", lambda e, j=j: e.indirect_dma_start(
                    out=xs[:, :], out_offset=bass.IndirectOffsetOnAxis(ap=desti[:, j, k:k + 1], axis=0),
                    in_=h2[:, :], in_offset=None), reads=[h2k, "desti"], writes=[("xs", k, j)])
        tk.flush()
        real.barrier()
    xs_keys = [("xs", k, j) for k in range(ntile) for j in range(2)]

    with ExitStack() as esB:
        sbb = mk(esB, "b_")
        W = []
        for i in range(2):
            wg2 = sbb("wg%d" % i, [128, 4096]); wu2 = sbb("wu%d" % i, [128, 4096]); wd2 = sbb("wd%d" % i, [128, 4096])
            W.append((wg2, wu2, wd2))
        xsb2 = [sbb("xsb%d" % i, [128, 1024]) for i in range(2)]
        xsT2 = [sbb("xsT%d" % i, [128, 8, 128]) for i in range(2)]
        gsil2 = [sbb("gsil%d" % i, [128, 512]) for i in range(2)]
        hid2 = [sbb("hid%d" % i, [128, 512]) for i in range(2)]
        hidT2 = [sbb("hidT%d" % i, [128, 4, 128]) for i in range(2)]
        ysb2 = [sbb("ysb%d" % i, [128, 1024]) for i in range(2)]
        wgd = D["wgate"]; wud = D["wup"]; wdd = D["wdown"]
        breg = nc.gpsimd.to_reg(32 * 128 - 1)
        for b in range(nblk):
            par = b % 2
            wg2, wu2, wd2 = W[par]
            wg = wg2[:].rearrange("p (a b) -> p a b", b=512)
            wu = wu2[:].rearrange("p (a b) -> p a b", b=512)
            wd = wd2[:].rearrange("p (a b) -> p a b", b=1024)
            xsb, xsT, gsil, hid, hidT, ysb = xsb2[par], xsT2[par], gsil2[par], hid2[par], hidT2[par], ysb2[par]
            sfx = str(par)
            for (dst, dkey, src) in ((wg2, "wg" + sfx, wgd), (wu2, "wu" + sfx, wud), (wd2, "wd" + sfx, wdd)):
                tk.dma("pool", lambda e, dst=dst, src=src: e.indirect_dma_start(
                    out=dst[:, :].bitcast(F32R), out_offset=None, in_=src[:, :].bitcast(F32R),
                    in_offset=bass.IndirectOffsetOnAxis(ap=idxw[:, b:b + 1], axis=0), bounds_check=breg,
                    oob_is_err=False), reads=["idxw"], writes=[dkey], cost=8.0)
            tk.dma("sp", lambda e: e.dma_start(out=xsb[:], in_=xs[b * 128:(b + 1) * 128, :]),
                   reads=xs_keys if b == 0 else [], writes=["xsb" + sfx])
            for half in range(2):
                pn, pt = pp.next()
                tk.ops("pe", [lambda e, j=j, pt=pt, half=half: e.transpose(
                    pt[:, j * 128:(j + 1) * 128], xsb[:, (half * 4 + j) * 128:(half * 4 + j + 1) * 128], ident[:])
                    for j in range(4)], reads=["xsb" + sfx, "ident"], writes=[pn])
                tk.op("act", lambda e, pt=pt, half=half: e.copy(
                    out=xsT[:, half * 4:(half + 1) * 4, :].rearrange("p a b -> p (a b)").bitcast(F32R), in_=pt[:, :]),
                    reads=[pn], writes=["xsT" + sfx])
            pg, ptg = pp.next()
            tk.ops("pe", [lambda e, kc=kc: e.matmul(ptg[:, :], lhsT=xsT[:, kc, :].bitcast(F32R),
                                                   rhs=wg[:, kc, :].bitcast(F32R), start=(kc == 0), stop=(kc == 7))
                          for kc in range(8)], reads=["xsT" + sfx, "wg" + sfx], writes=[pg], cost=3.0)
            pu, ptu = pp.next()
            tk.ops("pe", [lambda e, kc=kc: e.matmul(ptu[:, :], lhsT=xsT[:, kc, :].bitcast(F32R),
                                                   rhs=wu[:, kc, :].bitcast(F32R), start=(kc == 0), stop=(kc == 7))
                          for kc in range(8)], reads=["xsT" + sfx, "wu" + sfx], writes=[pu], cost=3.0)
            tk.op("act", lambda e: e.activation(out=gsil[:], in_=ptg[:, :], func=AF.Silu), reads=[pg],
                  writes=["gsil" + sfx])
            tk.op("dve", lambda e: e.tensor_tensor(out=hid[:], in0=gsil[:], in1=ptu[:, :], op=ALU.mult),
                  reads=["gsil" + sfx, pu], writes=["hid" + sfx])
            pn, pt = pp.next()
            tk.ops("pe", [lambda e, j=j, pt=pt: e.transpose(pt[:, j * 128:(j + 1) * 128],
                                                            hid[:, j * 128:(j + 1) * 128], ident[:])
                          for j in range(4)], reads=["hid" + sfx, "ident"], writes=[pn])
            tk.op("act", lambda e, pt=pt: e.copy(out=hidT[:].rearrange("p a b -> p (a b)").bitcast(F32R),
                                                 in_=pt[:, :]), reads=[pn], writes=["hidT" + sfx])
            for half in range(2):
                pn, pt = pp.next()
                tk.ops("pe", [lambda e, fc=fc, pt=pt, half=half: e.matmul(
                    pt[:, :], lhsT=hidT[:, fc, :].bitcast(F32R),
                    rhs=wd[:, fc, half * 512:(half + 1) * 512].bitcast(F32R), start=(fc == 0), stop=(fc == 3))
                    for fc in range(4)], reads=["hidT" + sfx, "wd" + sfx], writes=[pn], cost=1.5)
                if half == 0:
                    tk.op("act", lambda e, pt=pt: e.copy(out=ysb[:, 0:512], in_=pt[:, :]), reads=[pn],
                          writes=["ysb" + sfx])
                else:
                    tk.op("dve", lambda e, pt=pt: e.tensor_copy(out=ysb[:, 512:1024], in_=pt[:, :]), reads=[pn],
                          writes=["ysb" + sfx])
            tk.dma("sp", lambda e: e.dma_start(out=ys[b * 128:(b + 1) * 128, :], in_=ysb[:]), reads=["ysb" + sfx],
                   writes=[("ys", b)])
            if b % 16 == 15:
                tk.flush()
        tk.flush()
        real.barrier()
    ys_keys = [("ys", b) for b in range(nblk)]

    with ExitStack() as esC:
        sc_ = mk(esC, "c_")
        y02 = [sc_("y0%d" % i, [128, 1024]) for i in range(2)]
        y12 = [sc_("y1%d" % i, [128, 1024]) for i in range(2)]
        for k in range(ntile):
            par = k % 2
            x1, rr, xt, y0, y1 = x12[par], rr2[par], xt2[par], y02[par], y12[par]
            x1k, rrk, xtk, y0k, y1k = "x1%d" % par, "rr%d" % par, "xt%d" % par, "y0%d" % par, "y1%d" % par
            tsl = slice(k * 128, (k + 1) * 128)
            tk.dma("sp", lambda e: e.dma_start(out=x1[:], in_=x1d[tsl, :]), reads=["x1d"], writes=[x1k])
            for j, (yt_, yk) in enumerate(((y0, y0k), (y1, y1k))):
                tk.dma("pool", lambda e, j=j, yt_=yt_: e.indirect_dma_start(
                    out=yt_[:, :], out_offset=None, in_=ys[:, :],
                    in_offset=bass.IndirectOffsetOnAxis(ap=desti[:, j, k:k + 1], axis=0)),
                    reads=(ys_keys if k == 0 else []) + ["desti"], writes=[yk])
            tk.op("dve", lambda e: e.tensor_scalar(out=y0[:], in0=y0[:], scalar1=gts[:, k, 0:1], scalar2=None,
                                                   op0=ALU.mult), reads=[y0k, "gts"], writes=[y0k])
            tk.op("dve", lambda e: e.scalar_tensor_tensor(out=y0[:], in0=y1[:], scalar=gts[:, k, 1:2], in1=y0[:],
                                                          op0=ALU.mult, op1=ALU.add), reads=[y0k, y1k, "gts"],
                  writes=[y0k])
            tk.op("dve", lambda e: e.tensor_tensor(out=y0[:], in0=y0[:], in1=modb[:, 3, :], op=ALU.mult),
                  reads=[y0k, "modb"], writes=[y0k])
            tk.op("dve", lambda e: e.scalar_tensor_tensor(out=rr[:], in0=x1[:], scalar=ALPHA, in1=y0[:], op0=ALU.mult,
                                                          op1=ALU.add), reads=[x1k, y0k], writes=[rrk])
            layer_norm(rr, rrk, xt, xtk, 2)
            tk.dma("sp", lambda e: e.dma_start(out=out[tsl, :], in_=xt[:]), reads=[xtk], writes=["out"])
            if k % 8 == 7:
                tk.flush()
        tk.flush()


def consts2():
    p = np.arange(128)
    tri = (p[:, None] < p[None, :]).astype(np.float32)
    thr16 = np.broadcast_to((128.0 * np.arange(16, dtype=np.float32))[None, :], (128, 16))
    thr64 = np.broadcast_to((128.0 * np.arange(64, dtype=np.float32))[None, :], (128, 64))
    return {"c_ident": np.eye(128, dtype=np.float32), "c_tri": tri, "c_iota": p.astype(np.float32)[:, None].copy(),
            "c_thr16": np.ascontiguousarray(thr16), "c_thr64": np.ascontiguousarray(thr64)}


def build_phase2(ntile=NT2, nblk=NBLK):
    nc = bass.Bass("TRN2", target_bir_lowering=False)
    D = {}

    def din(name, shape, dt=F32):
        D[name] = nc.dram_tensor(name, shape, dt, kind="ExternalInput").ap()

    din("mixTf", [1024, 2048], BF16); din("x2", [2048, 1024]); din("cT", [128, 8])
    din("wada2", [128, 32, 8, 128]); din("bada2", [4096]); din("wo", [1024, 1024]); din("lnp", [4, 1024])
    din("wr", [1024, 36]); din("br", [36])
    din("wgate", [4096, 4096]); din("wup", [4096, 4096]); din("wdown", [4096, 4096])
    din("c_ident", [128, 128]); din("c_tri", [128, 128]); din("c_iota", [128, 1]); din("c_thr16", [128, 16])
    din("c_thr64", [128, 64])
    for nm in ("x1d", "h2d"):
        D[nm] = nc.dram_tensor(nm, [2048, 1024], F32, kind="Internal").ap()
    for nm in ("xs", "ys"):
        D[nm] = nc.dram_tensor(nm, [NBLK * 128, 1024], F32, kind="Internal").ap()
    D["out"] = nc.dram_tensor("out", [2048, 1024], F32, kind="ExternalOutput").ap()
    with ExitStack() as es:
        tk = Trk(nc, es)
        phase2(nc, tk, es, D, ntile=ntile, nblk=nblk)
        tk.finish("sp")
    return nc


def phase2_shared_inputs(inp):
    l = 0
    wg = inp["w_gate"][l].reshape(32, 8, 128, 512).transpose(0, 2, 1, 3).reshape(4096, 4096)
    wu = inp["w_up"][l].reshape(32, 8, 128, 512).transpose(0, 2, 1, 3).reshape(4096, 4096)
    wd = inp["w_down"][l].reshape(32, 4, 128, 1024).transpose(0, 2, 1, 3).reshape(4096, 4096)
    d = {"wada2": np.ascontiguousarray(inp["w_ada"][l][:, 2048:6144].reshape(8, 128, 32, 128).transpose(1, 2, 0, 3)),
         "bada2": np.ascontiguousarray(inp["b_ada"][l][2048:6144]),
         "wo": np.ascontiguousarray(inp["w_o"][l]),
         "lnp": np.ascontiguousarray(np.stack([inp["ln1_g"][l], inp["ln1_b"][l], inp["ln2_g"][l], inp["ln2_b"][l]])),
         "wr": np.ascontiguousarray(np.concatenate([inp["w_router_group"][l], inp["w_router_expert"][l]], 1)),
         "br": np.ascontiguousarray(np.concatenate([inp["b_router_group"][l], inp["b_router_expert"][l]])),
         "wgate": np.ascontiguousarray(wg), "wup": np.ascontiguousarray(wu), "wdown": np.ascontiguousarray(wd)}
    d.update(consts2())
    return d


def phase2_inputs(inp, core, shared, mixTf):
    b, q = core // 4, core % 4
    d = dict(shared)
    d["x2"] = np.ascontiguousarray(inp["x"][b, q * 2048:(q + 1) * 2048])
    d["cT"] = np.ascontiguousarray(inp["c"][b].reshape(8, 128).T)
    d["mixTf"] = mixTf
    return d


def build_fused():
    nc = bass.Bass("TRN2", target_bir_lowering=False)
    D = {}

    def din(name, shape, dt=F32):
        D[name] = nc.dram_tensor(name, shape, dt, kind="ExternalInput").ap()

    din("x", [D_MODEL, SEQ]); din("cT", [128, 8]); din("pos", [SEQ], I32)
    din("wada1", [128, 8, 8, 256]); din("bada1", [128, 16]); din("win", [128, 5, 8, 256])
    din("convw", [128, 3, 4]); din("pvec", [128, 4])
    din("c_ident", [128, 128]); din("c_masks", [128, 3, 128]); din("c_amask", [128, 256]); din("c_invf", [64, 2])
    din("x2", [2048, 1024]); din("qidx", [128, 8], I32)
    din("wada2", [128, 32, 8, 128]); din("bada2", [4096]); din("wo", [1024, 1024]); din("lnp", [4, 1024])
    din("wr", [1024, 36]); din("br", [36])
    din("wgate", [4096, 4096]); din("wup", [4096, 4096]); din("wdown", [4096, 4096])
    din("c_tri", [128, 128]); din("c_iota", [128, 1]); din("c_thr16", [128, 16]); din("c_thr64", [128, 64])
    cin = [nc.dram_tensor("cin%d" % q, [256, 2048], BF16, kind="Internal").ap() for q in range(4)]
    cout = nc.dram_tensor("cout", [4096, 2048], BF16, kind="Internal").ap()
    for nm in ("x1d", "h2d"):
        D[nm] = nc.dram_tensor(nm, [2048, 1024], F32, kind="Internal").ap()
    for nm in ("xs", "ys"):
        D[nm] = nc.dram_tensor(nm, [NBLK * 128, 1024], F32, kind="Internal").ap()
    D["out"] = nc.dram_tensor("out", [2048, 1024], F32, kind="ExternalOutput").ap()
    groups = [[0, 1, 2, 3], [4, 5, 6, 7]]
    with ExitStack() as es0:
        tk = Trk(nc, es0)

        def on_quarter(sc, tk):
            tk.cc(lambda e: e.collective_compute("AllGather", ALU.bypass, replica_groups=groups,
                                                 ins=[cin[sc][:, :]], outs=[cout[sc * 1024:(sc + 1) * 1024, :]]),
                  reads=[("cin_a", sc), ("cin_b", sc)], writes=[("cout", sc)])

        with ExitStack() as es1:
            phase1(nc, tk, es1, D, cin=cin, on_quarter=on_quarter)
        tk.barrier()
        with ExitStack() as es2:
            phase2(nc, tk, es2, D, cout=cout)
        tk.finish("sp")
    return nc


def fused_inputs(inp, core, shared):
    b, q = core // 4, core % 4
    d = dict(shared)
    d.update(phase1_inputs(inp, core))
    d["x2"] = np.ascontiguousarray(inp["x"][b, q * 2048:(q + 1) * 2048])
    kc = np.arange(8)[None, :]
    p = np.arange(128)[:, None]
    row = np.where(kc < 4, kc * 256 + p, (kc - 4) * 256 + 128 + p)
    d["qidx"] = np.ascontiguousarray((q * 1024 + row).astype(np.int32))
    return d


def kernel(**inputs):
    inp = {k: np.asarray(v) for k, v in inputs.items()}
    shared = phase2_shared_inputs(inp)
    nc = build_fused()
    maps = [fused_inputs(inp, core, shared) for core in range(NCORES)]
    res = run_bass_kernel_spmd(nc, maps, core_ids=list(range(NCORES)))
    out = np.zeros((BATCH, SEQ, D_MODEL), np.float32)
    for core in range(NCORES):
        b, q = core // 4, core % 4
        out[b, q * 2048:(q + 1) * 2048] = np.asarray(res.results[core]["out"])
    return out
```

```python
import math
from contextlib import ExitStack

import numpy as np
import concourse.bass as bass
import concourse.mybir as mybir
from concourse.bass_utils import run_bass_kernel_spmd

F32 = mybir.dt.float32
BF16 = mybir.dt.bfloat16
I32 = mybir.dt.int32
U32 = mybir.dt.uint32
F32R = mybir.dt.float32r
AF = mybir.ActivationFunctionType
ALU = mybir.AluOpType
AX = mybir.AxisListType

D_MODEL = 1024
SEQ = 8192
BATCH = 2
NCORES = 8
T = 512
NCH = SEQ // T
ALPHA = 2.0 ** 0.25
LN_EPS = 1e-5
RMS_EPS = 1e-6
ROPE_THETA = 500000.0
NEG = -30000.0
TWO_PI = 2.0 * math.pi


class Trk:
    def __init__(self, nc, es, n_dma_sems=24, same_engine_sync=True):
        self.nc = nc
        self.e = {"pe": nc.tensor, "dve": nc.vector, "act": nc.scalar, "pool": nc.gpsimd, "sp": nc.sync}
        self.sem = {k: es.enter_context(nc.semaphore("sem_" + k)) for k in self.e}
        self.cnt = {k: 0 for k in self.e}
        self.seen = {k: {} for k in self.e}
        self.last_w = {}
        self.reads = {}
        self.dsem = [es.enter_context(nc.semaphore("dsem%d" % i)) for i in range(n_dma_sems)]
        self.dval = [0] * n_dma_sems
        self.dnext = 0
        self.same = same_engine_sync
        self.all_dma_events = []
        self.ccsem = es.enter_context(nc.semaphore("ccsem"))
        self.ccval = 0

    def _wait(self, eng, ev):
        kind, src, n = ev
        key = (kind, src)
        if self.seen[eng].get(key, 0) >= n:
            return
        if kind == "e":
            if src == eng and not self.same:
                return
            self.e[eng].wait_ge(self.sem[src], n)
        elif kind == "c":
            self.e[eng].wait_ge(self.ccsem, n)
        else:
            self.e[eng].wait_ge(self.dsem[src], n)
        self.seen[eng][key] = n

    def _deps(self, eng, reads, writes):
        deps = []
        for r in reads:
            ev = self.last_w.get(r)
            if ev is not None:
                deps.append(ev)
        for w in writes:
            ev = self.last_w.get(w)
            if ev is not None:
                deps.append(ev)
            for ev2 in self.reads.get(w, {}).values():
                deps.append(ev2)
        for ev in deps:
            self._wait(eng, ev)

    def _record(self, ev, reads, writes):
        for r in reads:
            self.reads.setdefault(r, {})[(ev[0], ev[1])] = ev
        for w in writes:
            self.last_w[w] = ev
            self.reads[w] = {}

    def op(self, eng, fn, reads=(), writes=()):
        self._deps(eng, reads, writes)
        inst = fn(self.e[eng])
        self.cnt[eng] += 1
        inst.then_inc(self.sem[eng], 1)
        ev = ("e", eng, self.cnt[eng])
        self._record(ev, reads, writes)
        return ev

    def ops(self, eng, fns, reads=(), writes=()):
        self._deps(eng, reads, writes)
        inst = None
        for fn in fns:
            inst = fn(self.e[eng])
        self.cnt[eng] += 1
        inst.then_inc(self.sem[eng], 1)
        ev = ("e", eng, self.cnt[eng])
        self._record(ev, reads, writes)
        return ev

    def dma(self, eng, fn, reads=(), writes=()):
        self._deps(eng, reads, writes)
        i = self.dnext
        self.dnext = (self.dnext + 1) % len(self.dsem)
        if self.dval[i] > 0:
            self._wait(eng, ("d", i, self.dval[i]))
        inst = fn(self.e[eng])
        self.dval[i] += 16
        inst.then_inc(self.dsem[i], 16)
        ev = ("d", i, self.dval[i])
        self._record(ev, reads, writes)
        self.all_dma_events.append(ev)
        return ev

    def cc(self, fn, reads=(), writes=()):
        eng = "pool"
        self._deps(eng, reads, writes)
        if self.ccsem is None:
            raise RuntimeError("no cc semaphore")
        if self.ccval > 0:
            self._wait(eng, ("c", 0, self.ccval))
        inst = fn(self.e[eng])
        self.ccval += 1
        inst.then_inc(self.ccsem, 1)
        ev = ("c", 0, self.ccval)
        self._record(ev, reads, writes)
        return ev

    def barrier(self):
        for eng in self.e:
            for i, v in enumerate(self.dval):
                if v > 0:
                    self._wait(eng, ("d", i, v))
            if self.ccval > 0:
                self._wait(eng, ("c", 0, self.ccval))
            for k, n in self.cnt.items():
                if n > 0 and k != eng:
                    self._wait(eng, ("e", k, n))

    def finish(self, eng="sp"):
        for i, v in enumerate(self.dval):
            if v > 0:
                self._wait(eng, ("d", i, v))
        if self.ccval > 0:
            self._wait(eng, ("c", 0, self.ccval))
        for k, n in self.cnt.items():
            if n > 0 and k != eng:
                self._wait(eng, ("e", k, n))


class _Proxy:
    def __init__(self):
        self.calls = []

    def __getattr__(self, name):
        def f(*a, **k):
            self.calls.append((name, a, k))
            return None
        return f


def _freeze(fn):
    p = _Proxy()
    fn(p)
    assert len(p.calls) == 1
    name, a, k = p.calls[0]
    return lambda e: getattr(e, name)(*a, **k)


class Rec:
    HOP = 0.8

    def __init__(self, tk):
        self.tk = tk
        self.L = []

    def op(self, eng, fn, reads=(), writes=(), cost=None):
        self.L.append(("op", eng, _freeze(fn), tuple(reads), tuple(writes), cost if cost is not None else (0.3 if eng == "pe" else 0.6)))

    def ops(self, eng, fns, reads=(), writes=(), cost=None):
        fns = [_freeze(f) for f in fns]
        self.L.append(("ops", eng, fns, tuple(reads), tuple(writes), cost if cost is not None else 0.2 * len(fns)))

    def dma(self, eng, fn, reads=(), writes=(), cost=None):
        self.L.append(("dma", eng, _freeze(fn), tuple(reads), tuple(writes), cost if cost is not None else 2.0))

    def cc(self, fn, reads=(), writes=(), cost=None):
        self.L.append(("cc", "pool", _freeze(fn), tuple(reads), tuple(writes), 5.0))

    def flush(self):
        L = self.L
        self.L = []
        n = len(L)
        preds = [set() for _ in range(n)]
        last_w = {}
        readers = {}
        for i, (kind, eng, fn, reads, writes, cost) in enumerate(L):
            for r in reads:
                if r in last_w:
                    preds[i].add(last_w[r])
            for w in writes:
                if w in last_w:
                    preds[i].add(last_w[w])
                for j in readers.get(w, ()):
                    preds[i].add(j)
            for r in reads:
                readers.setdefault(r, []).append(i)
            for w in writes:
                last_w[w] = i
                readers[w] = []
            preds[i].discard(i)
        succs = [[] for _ in range(n)]
        npred = [len(p) for p in preds]
        for i in range(n):
            for j in preds[i]:
                succs[j].append(i)
        efree = {}
        finish = [0.0] * n
        ready_t = [0.0] * n
        ready = [i for i in range(n) if npred[i] == 0]
        order = []
        import heapq
        while ready:
            best = None
            bkey = None
            for i in ready:
                kind, eng, fn, reads, writes, cost = L[i]
                q = eng if kind in ("op", "ops") else ("q_" + eng)
                st = max(efree.get(q, 0.0), ready_t[i])
                key = (st, i)
                if bkey is None or key < bkey:
                    bkey = key
                    best = i
            i = best
            ready.remove(i)
            kind, eng, fn, reads, writes, cost = L[i]
            q = eng if kind in ("op", "ops") else ("q_" + eng)
            st = bkey[0]
            if kind in ("op", "ops"):
                efree[q] = st + cost
                finish[i] = st + cost
            else:
                efree[q] = st + 0.1
                finish[i] = st + cost
            order.append(i)
            for j in succs[i]:
                npred[j] -= 1
                same = (L[j][1] == eng and L[j][0] in ("op", "ops") and kind in ("op", "ops"))
                ready_t[j] = max(ready_t[j], finish[i] + (0.0 if same else self.HOP))
                if npred[j] == 0:
                    ready.append(j)
        assert len(order) == n
        for i in order:
            kind, eng, fn, reads, writes, cost = L[i]
            if kind == "op":
                self.tk.op(eng, fn, reads, writes)
            elif kind == "ops":
                self.tk.ops(eng, fn, reads, writes)
            elif kind == "dma":
                self.tk.dma(eng, fn, reads, writes)
            else:
                self.tk.cc(fn, reads, writes)


class PsumPool:
    _uid = [0]

    def __init__(self, nc, es, names):
        PsumPool._uid[0] += 1
        u = PsumPool._uid[0]
        self.t = {n: es.enter_context(nc.psum_tensor("ps%d_%s" % (u, n), [128, 512], F32)) for n in names}
        self.rot = [n for n in names if n.startswith("r")]
        self.i = 0

    def next(self):
        n = self.rot[self.i]
        self.i = (self.i + 1) % len(self.rot)
        return n, self.t[n]


def phase1(nc, tk, es, D, nch=NCH, cin=None, on_quarter=None):
    def sb(name, shape, dt=F32):
        return es.enter_context(nc.sbuf_tensor("s_" + name, shape, dt))

    def K(name, c):
        return (name, c % 8)

    ppall = PsumPool(nc, es, ["rb0", "rb1", "rb2", "rb3", "ra0", "ra1", "scan", "ot"])
    PS = ppall.t
    real_tk = tk
    tk = Rec(real_tk)

    class _Sub:
        def __init__(self, names):
            self.rot = names
            self.i = 0

        def next(self):
            n = self.rot[self.i]
            self.i = (self.i + 1) % len(self.rot)
            return n, PS[n]

    pp = _Sub(["rb0", "rb1", "rb2", "rb3"])
    ppa = _Sub(["ra0", "ra1"])

    ident = sb("ident", [128, 128])
    ident_b = sb("ident_b", [128, 128], BF16)
    ones_f = sb("ones_f", [128, 128])
    ones_b = sb("ones_b", [128, 128], BF16)
    mus_t = sb("mus_neg", [128, 128]); mui_t = sb("mu_inc", [128, 128]); mls_t = sb("mls_neg", [128, 128])
    mus_neg = mus_t[:].unsqueeze(1).to_broadcast([128, 4, 128])
    mu_inc = mui_t[:].unsqueeze(1).to_broadcast([128, 4, 128])
    mls_neg = mls_t[:].unsqueeze(1).to_broadcast([128, 4, 128])
    ident4 = ident[:].unsqueeze(1).to_broadcast([128, 4, 128])
    amask_f = sb("amask_f", [128, 256])
    amask = sb("amask", [128, 256], BF16)
    scanm = sb("scanm", [128, T])
    invf = sb("invf", [64, 2])
    win_b = sb("win_b", [128, 8, 1280], BF16)
    xtok = sb("xtok", [128, 8, T])
    wstage = [xtok[:, 4 * i:4 * i + 4, :].rearrange("p a (h c) -> p (a h) c", c=256) for i in range(2)]
    wkeys = ["xtok_a", "xtok_b"]
    sc1 = sb("sc1", [128, 8])
    sh1 = sb("sh1", [128, 8])
    convw = sb("convw", [128, 3, 4])
    pvec = sb("pvec", [128, 4])
    nalog = sb("nalog", [128, 1])
    cT = sb("cT", [128, 8])
    scT = sb("scT", [128, 8, 2])
    bada = sb("bada", [128, 16])
    mod1 = sb("mod1", [128, 16])

    tk.dma("sp", lambda e: e.dma_start(out=ident[:], in_=D["c_ident"][:, :]), writes=["ident"])
    tk.dma("sp", lambda e: e.dma_start(out=amask_f[:], in_=D["c_amask"][:, :]), writes=["amask_f"])
    tk.dma("sp", lambda e: e.dma_start(out=invf[:], in_=D["c_invf"][:, :]), writes=["invf"])
    tk.dma("sp", lambda e: e.dma_start(out=convw[:], in_=D["convw"][:, :, :]), writes=["convw"])
    tk.dma("sp", lambda e: e.dma_start(out=pvec[:], in_=D["pvec"][:, :]), writes=["pvec"])
    tk.dma("sp", lambda e: e.dma_start(out=cT[:], in_=D["cT"][:, :]), writes=["cT"])
    tk.dma("sp", lambda e: e.dma_start(out=bada[:], in_=D["bada1"][:, :]), writes=["bada"])
    for q, (nm, tl) in enumerate([("mus", mus_t), ("mui", mui_t), ("mls", mls_t)]):
        tk.dma("sp", lambda e, tl=tl, q=q: e.dma_start(out=tl[:], in_=D["c_masks"][:, q, :]), writes=[nm])
    tk.op("dve", lambda e: e.tensor_copy(out=ident_b[:], in_=ident[:]), reads=["ident"], writes=["ident_b"])
    tk.op("dve", lambda e: e.tensor_copy(out=amask[:], in_=amask_f[:]), reads=["amask_f"], writes=["amask"])
    tk.op("pool", lambda e: e.memset(ones_f[:], 1.0), writes=["ones_f"])
    tk.op("pool", lambda e: e.memset(ones_b[:], 1.0), writes=["ones_b"])
    tk.op("pool", lambda e: e.memset(scanm[:], 1.0), writes=["scanm"])
    tk.op("pool", lambda e: e.memset(scanm[:, 0:T:64], 0.0), writes=["scanm"])
    tk.op("act", lambda e: e.activation(out=nalog[:], in_=pvec[:, 0:1], func=AF.Exp), reads=["pvec"],
          writes=["nalog"])
    tk.op("dve", lambda e: e.tensor_scalar(out=nalog[:], in0=nalog[:], scalar1=-1.0, scalar2=None, op0=ALU.mult),
          reads=["nalog"], writes=["nalog"])

    for j in range(5):
        st = wstage[j % 2]
        key = wkeys[j % 2]
        tk.dma("sp" if j % 2 == 0 else "pool", lambda e, st=st, j=j: e.dma_start(out=st, in_=D["win"][:, j, :, :]),
               writes=[key])
        tk.op("dve" if j % 2 == 0 else "act", (lambda e, st=st, j=j: e.tensor_copy(out=win_b[:, :, j * 256:(j + 1) * 256], in_=st))
              if j % 2 == 0 else (lambda e, st=st, j=j: e.copy(out=win_b[:, :, j * 256:(j + 1) * 256], in_=st)),
              reads=[key], writes=[("win_b", j)])
    for c0 in (7 * 128, 7 * 128 + 32):
        tk.op("dve", lambda e, c0=c0: e.tensor_scalar(out=win_b[:, :, c0:c0 + 16], in0=win_b[:, :, c0:c0 + 16],
                                                       scalar1=-1.0, scalar2=None, op0=ALU.mult),
              reads=[("win_b", 3)], writes=[("win_b", 3)])

    tk.op("act", lambda e: e.activation(out=scT[:, :, 0], in_=cT[:], func=AF.Silu), reads=["cT"], writes=["scT"])
    tk.op("act", lambda e: e.activation(out=scT[:, :, 1], in_=cT[:], func=AF.Silu), reads=["cT"], writes=["scT"])
    for j in range(8):
        st = wstage[j % 2]
        key = wkeys[j % 2]
        tk.dma("sp" if j % 2 == 0 else "pool", lambda e, st=st, j=j: e.dma_start(out=st, in_=D["wada1"][:, j, :, :]),
               writes=[key])
        for fh in range(2):
            fc = 2 * j + fh
            pn, pt = pp.next()
            tk.ops("pe", [lambda e, st=st, kc=kc, pt=pt, fh=fh: e.matmul(
                pt[:, 0:2], lhsT=st[:, kc, fh * 128:(fh + 1) * 128], rhs=scT[:, kc, :], start=(kc == 0), stop=(kc == 7))
                for kc in range(8)], reads=[key, "scT"], writes=[pn])
            tk.op("dve", lambda e, pt=pt, fc=fc: e.tensor_tensor(out=mod1[:, fc:fc + 1], in0=pt[:, 0:1],
                                                                  in1=bada[:, fc:fc + 1], op=ALU.add),
                  reads=[pn, "bada"], writes=["mod1"])
    tk.op("dve", lambda e: e.tensor_copy(out=sh1[:], in_=mod1[:, 0:8]), reads=["mod1"], writes=["sh1"])
    tk.op("dve", lambda e: e.tensor_scalar(out=sc1[:], in0=mod1[:, 8:16], scalar1=1.0, scalar2=None, op0=ALU.add),
          reads=["mod1"], writes=["sc1"])

    RING = 4096
    qT = sb("qT", [128, RING], BF16)
    kT = sb("kT", [128, RING], BF16)
    vT = sb("vT", [128, RING], BF16)
    vtt = sb("vtt", [128, 4, 128], BF16)
    acc = sb("acc", [128, 2, 2048])
    S = [sb("S%d" % i, [128, 128]) for i in range(2)]
    tk.op("pool", lambda e: e.memset(S[0][:], 0.0), writes=["S0"])

    hT = [sb("hT%d" % i, [128, 8, T], BF16) for i in range(2)]
    craw = [sb("craw%d" % g, [128, T + 3]) for g in range(3)]
    cacc = sb("cacc", [128, T])
    qs = sb("qs", [128, T]); ks = sb("ks", [128, T]); vs = sb("vs", [128, T])
    zs2 = [sb("zs%d" % i, [128, T]) for i in range(2)]
    sq = sb("sq", [128, T]); rn = sb("rn", [128, T])
    p1 = sb("p1", [128, T]); p2 = sb("p2", [128, T]); p3 = sb("p3", [128, T])
    osq = p1; rs = p2; fsq = p1; fr = p2; oa = p3; fo = p3
    qn = sb("qn", [128, T]); kn = sb("kn", [128, T])
    betab = sb("betab", [128, T]); gb = sb("gb", [128, T]); gcb = sb("gcb", [128, T])
    egcb2 = [sb("egcb%d" % i, [128, T]) for i in range(2)]; eglb = sb("eglb", [128, T]); begb = gb
    cols = sb("cols", [128, 5, 4])
    Kbe = sb("Kbe", [128, 4, 128]); Kd2 = [sb("Kd%d" % i, [128, 4, 128]) for i in range(2)]
    Vb = sb("Vb", [128, 4, 128])
    tdf = sb("tdf", [128, 4, 128]); DT = sb("DT", [128, 4, 128]); Dm = sb("Dm", [128, 4, 128])
    E1 = tdf; QKT2 = [sb("QKT%d" % i, [128, 4, 128]) for i in range(2)]
    Bm = [sb("Bm%d" % i, [128, 4, 128]) for i in range(2)]
    BTm = [sb("BTm%d" % i, [128, 4, 128]) for i in range(2)]
    Pm = [sb("Pm%d" % i, [128, 4, 128]) for i in range(2)]
    U2 = [sb("U%d" % i, [128, 4, 128]) for i in range(2)]; WT2 = [sb("WT%d" % i, [128, T]) for i in range(2)]
    QdT2 = [sb("QdT%d" % i, [128, T]) for i in range(2)]
    vnew = sb("vnew", [128, 2, 128])
    oab = sb("oab", [128, T], BF16)
    posi = sb("posi", [64, T], I32); ra = sb("ra", [64, T]); rb = sb("rb", [64, T])
    ri = sb("ri", [64, T], I32)
    sincos = sb("sincos", [64, T])
    rt1 = sb("rt1", [32, T]); rt2 = sb("rt2", [32, T])
    pTt = [sb("pT%d" % i, [128, 256], BF16) for i in range(2)]
    fob = sb("fob", [128, T], BF16)

    x = D["x"]
    mixT = D.get("mixT")
    pos = D["pos"]

    def load_x(c):
        for i in range(2):
            tk.dma("sp", lambda e, i=i: e.dma_start(
                out=xtok[:, 4 * i:4 * i + 4, :],
                in_=x[i * 512:(i + 1) * 512, c * T:(c + 1) * T].rearrange("(kc p) t -> p kc t", p=128)),
                writes=[wkeys[i]])

    def stage_a(c):
        h = hT[c % 2]
        hk = "hT%d" % (c % 2)
        for kc in range(8):
            tk.op("act", lambda e, kc=kc: e.activation(out=h[:, kc, :], in_=xtok[:, kc, :], func=AF.Identity,
                                                       bias=sh1[:, kc:kc + 1], scale=sc1[:, kc:kc + 1]),
                  reads=[wkeys[kc // 4], "sh1", "sc1"], writes=[hk])

    def proj(c, m):
        h = hT[c % 2]
        hk = "hT%d" % (c % 2)
        pn, pt = pp.next()
        tk.ops("pe", [lambda e, kc=kc, pt=pt: e.matmul(pt[:, :], lhsT=win_b[:, kc, m * 128:(m + 1) * 128],
                                                      rhs=h[:, kc, :], start=(kc == 0), stop=(kc == 7))
                      for kc in range(8)], reads=[hk, ("win_b", m // 2)], writes=[pn])
        return pn, pt

    def rope_tables(c):
        tk.dma("sp", lambda e: e.dma_start(out=posi[:], in_=pos[c * T:(c + 1) * T].partition_broadcast(64)),
               writes=["posi"])
        tk.op("dve", lambda e: e.tensor_copy(out=ra[:], in_=posi[:]), reads=["posi"], writes=["ra"])
        tk.op("dve", lambda e: e.tensor_scalar(out=ra[:], in0=ra[:], scalar1=invf[:, 0:1], scalar2=invf[:, 1:2],
                                               op0=ALU.mult, op1=ALU.add), reads=["ra", "invf"], writes=["ra"])
        tk.op("dve", lambda e: e.tensor_scalar(out=ri[:], in0=ra[:], scalar1=1.0 / TWO_PI, scalar2=None,
                                               op0=ALU.mult), reads=["ra"], writes=["ri"])
        tk.op("dve", lambda e: e.tensor_copy(out=rb[:], in_=ri[:]), reads=["ri"], writes=["rb"])
        tk.op("dve", lambda e: e.scalar_tensor_tensor(out=ra[:], in0=rb[:], scalar=-TWO_PI, in1=ra[:], op0=ALU.mult,
                                                      op1=ALU.add), reads=["ra", "rb"], writes=["ra"])
        tk.op("dve", lambda e: e.tensor_scalar(out=rb[:], in0=ra[:], scalar1=math.pi, scalar2=-TWO_PI,
                                               op0=ALU.is_gt, op1=ALU.mult), reads=["ra"], writes=["rb"])
        tk.op("dve", lambda e: e.tensor_tensor(out=ra[:], in0=ra[:], in1=rb[:], op=ALU.add),
              reads=["ra", "rb"], writes=["ra"])
        tk.op("dve", lambda e: e.tensor_scalar(out=rb[:], in0=ra[:], scalar1=-math.pi, scalar2=TWO_PI,
                                               op0=ALU.is_lt, op1=ALU.mult), reads=["ra"], writes=["rb"])
        tk.op("dve", lambda e: e.tensor_tensor(out=ra[:], in0=ra[:], in1=rb[:], op=ALU.add),
              reads=["ra", "rb"], writes=["ra"])
        tk.op("act", lambda e: e.activation(out=sincos[:], in_=ra[:], func=AF.Sin), reads=["ra"], writes=["sincos"])

    def prep_main(c):
        par = c % 2
        U, WT, QdT, QKT, Kd, egcb, zs = U2[par], WT2[par], QdT2[par], QKT2[par], Kd2[par], egcb2[par], zs2[par]
        uk, wk, qdk, qkk, kdk, ek, zk = ("U%d" % par, "WT%d" % par, "QdT%d" % par, "QKT%d" % par, "Kd%d" % par,
                                         "egcb%d" % par, "zs%d" % par)
        stage_a(c)
        yield
        if c + 1 < nch:
            load_x(c + 1)
        yield
        for g, dst, dk_ in ((0, qs, "qs"), (1, ks, "ks"), (2, vs, "vs")):
            pn, pt = proj(c, g)
            ck = "craw%d" % g
            cr = craw[g]
            if c > 0:
                tk.op("dve", lambda e, cr=cr: e.tensor_copy(out=cr[:, 0:3], in_=cr[:, T:T + 3]), reads=[ck],
                      writes=[ck])
            else:
                tk.op("pool", lambda e, cr=cr: e.memset(cr[:, 0:3], 0.0), writes=[ck])
            tk.op("act", lambda e, pt=pt, cr=cr: e.copy(out=cr[:, 3:T + 3], in_=pt[:, :]), reads=[pn],
                  writes=[ck])
            tk.op("dve", lambda e, cr=cr, g=g: e.tensor_scalar(out=cacc[:], in0=cr[:, 0:T],
                                                               scalar1=convw[:, g, 0:1], scalar2=None, op0=ALU.mult),
                  reads=[ck, "convw"], writes=["cacc"])
            for j in range(1, 4):
                tk.op("dve", lambda e, cr=cr, g=g, j=j: e.scalar_tensor_tensor(
                    out=cacc[:], in0=cr[:, j:j + T], scalar=convw[:, g, j:j + 1], in1=cacc[:],
                    op0=ALU.mult, op1=ALU.add), reads=[ck, "convw", "cacc"], writes=["cacc"])
            tk.op("act", lambda e, dst=dst: e.activation(out=dst[:], in_=cacc[:], func=AF.Silu), reads=["cacc"],
                  writes=[dk_])
            yield
        yield
        pn, pt = proj(c, 3)
        tk.op("act", lambda e, pt=pt: e.activation(out=zs[:], in_=pt[:, :], func=AF.Silu), reads=[pn], writes=[zk])
        yield
        for src, sk, dst, dk_, scale in ((qs, "qs", qn, "qn", 128.0 ** -0.5), (ks, "ks", kn, "kn", 1.0)):
            tk.op("act", lambda e, src=src: e.activation(out=sq[:], in_=src[:], func=AF.Square), reads=[sk],
                  writes=["sq"])
            pn, pt = pp.next()
            tk.op("pe", lambda e, pt=pt: e.matmul(pt[:, :], lhsT=ones_f[:], rhs=sq[:], start=True, stop=True),
                  reads=["sq", "ones_f"], writes=[pn])
            tk.op("act", lambda e, pt=pt: e.activation(out=rn[:], in_=pt[:, :], func=AF.Ln, bias=RMS_EPS_AP[:, 0:1],
                                                       scale=1.0), reads=[pn, "epsc"], writes=["rn"])
            tk.op("act", lambda e: e.activation(out=rn[:], in_=rn[:], func=AF.Exp, scale=-0.5), reads=["rn"],
                  writes=["rn"])
            tk.op("dve", lambda e, src=src, dst=dst, scale=scale: e.scalar_tensor_tensor(
                out=dst[:].bitcast(F32R), in0=src[:], scalar=scale, in1=rn[:], op0=ALU.mult, op1=ALU.mult),
                reads=[sk, "rn"], writes=[dk_])
        yield
        pn, pt = proj(c, 8)
        tk.op("act", lambda e, pt=pt: e.activation(out=betab[:], in_=pt[:, :], func=AF.Sigmoid), reads=[pn],
              writes=["betab"])
        pn, pt = proj(c, 9)
        tk.op("act", lambda e, pt=pt: e.activation(out=gb[:], in_=pt[:, :], func=AF.Exp, bias=pvec[:, 1:2], scale=1.0),
              reads=[pn, "pvec"], writes=["gb"])
        tk.op("act", lambda e: e.activation(out=gb[:], in_=gb[:], func=AF.Ln, bias=ONE_AP[:, 0:1], scale=1.0),
              reads=["gb", "epsc"], writes=["gb"])
        tk.op("dve", lambda e: e.tensor_scalar(out=gb[:], in0=gb[:], scalar1=nalog[:, 0:1], scalar2=None, op0=ALU.mult),
              reads=["gb", "nalog"], writes=["gb"])
        tk.op("dve", lambda e: e.tensor_tensor_scan(out=gcb[:], data0=scanm[:], data1=gb[:], initial=0.0,
                                                    op0=ALU.mult, op1=ALU.add), reads=["gb", "scanm"], writes=["gcb"])
        tk.op("act", lambda e: e.activation(out=egcb[:], in_=gcb[:], func=AF.Exp), reads=["gcb"], writes=[ek])
        gc3 = gcb[:].rearrange("p (n t) -> p n t", t=64)
        tk.op("dve", lambda e: e.tensor_tensor(out=eglb[:].rearrange("p (n t) -> p n t", t=64),
                                               in0=gc3[:, :, 63:64].to_broadcast([128, 8, 64]), in1=gc3,
                                               op=ALU.subtract), reads=["gcb"], writes=["eglb"])
        tk.op("act", lambda e: e.activation(out=eglb[:], in_=eglb[:], func=AF.Exp), reads=["eglb"], writes=["eglb"])
        tk.op("dve", lambda e: e.tensor_tensor(out=begb[:], in0=betab[:], in1=egcb[:], op=ALU.mult),
              reads=["betab", ek], writes=["gb"])
        pn, pt = pp.next()
        srcs = [(betab, "betab"), (gcb, "gcb"), (egcb, ek), (eglb, "eglb"), (begb, "gb")]
        tk.ops("pe", [lambda e, q=q, s=s, pt=pt, src=src: e.transpose(
            pt[:, (q * 4 + s) * 16:(q * 4 + s + 1) * 16], src[0:16, s * 128:(s + 1) * 128], ident[0:16, 0:16])
            for q, (src, _) in enumerate(srcs) for s in range(4)],
            reads=[k_ for _, k_ in srcs] + ["ident"], writes=[pn])
        tk.op("dve", lambda e, pt=pt: e.tensor_copy(
            out=cols[:].rearrange("p q s -> p (q s)"),
            in_=pt[:, 0:320].rearrange("p (n w) -> p n w", w=16)[:, :, 0]), reads=[pn], writes=["cols"])

        def colb(q):
            return cols[:, q, :].unsqueeze(2).to_broadcast([128, 4, 128])

        yield
        pn, pt = pp.next()
        tk.ops("pe", [lambda e, s=s, pt=pt: e.transpose(pt[:, s * 128:(s + 1) * 128], kn[:, s * 128:(s + 1) * 128],
                                                        ident[:]) for s in range(4)], reads=["kn", "ident"],
               writes=[pn])
        pt3 = pt[:, :].rearrange("p (s d) -> p s d", d=128)
        tk.op("dve", lambda e, pt3=pt3: e.tensor_tensor(out=Kbe[:].bitcast(F32R), in0=pt3, in1=colb(4), op=ALU.mult),
              reads=[pn, "cols"], writes=["Kbe"])
        tk.op("dve", lambda e, pt3=pt3: e.tensor_tensor(out=Kd[:], in0=pt3, in1=colb(3), op=ALU.mult),
              reads=[pn, "cols"], writes=[kdk])
        pn, pt = pp.next()
        tk.ops("pe", [lambda e, s=s, pt=pt: e.transpose(pt[:, s * 128:(s + 1) * 128], vs[:, s * 128:(s + 1) * 128],
                                                        ident[:]) for s in range(4)], reads=["vs", "ident"],
               writes=[pn])
        pt3 = pt[:, :].rearrange("p (s d) -> p s d", d=128)
        tk.op("dve", lambda e, pt3=pt3: e.tensor_tensor(out=Vb[:].bitcast(F32R), in0=pt3, in1=colb(0), op=ALU.mult),
              reads=[pn, "cols"], writes=["Vb"])
        yield
        gcb3 = gcb[:].rearrange("p (s t) -> p s t", t=128)
        tk.op("dve", lambda e: e.tensor_tensor(out=tdf[:], in0=gcb3, in1=colb(1), op=ALU.subtract),
              reads=["gcb", "cols"], writes=["tdf"])
        tk.op("dve", lambda e: e.tensor_scalar(out=DT[:], in0=tdf[:], scalar1=0.0, scalar2=None, op0=ALU.min),
              reads=["tdf"], writes=["DT"])
        tk.op("act", lambda e: e.activation(out=DT[:], in_=DT[:], func=AF.Exp), reads=["DT"], writes=["DT"])
        tk.op("dve", lambda e: e.tensor_scalar(out=Dm[:], in0=tdf[:], scalar1=0.0, scalar2=None, op0=ALU.max),
              reads=["tdf"], writes=["Dm"])
        tk.op("act", lambda e: e.activation(out=Dm[:], in_=Dm[:], func=AF.Exp, scale=-1.0), reads=["Dm"],
              writes=["Dm"])
        yield
        pnk, ptk = pp.next()
        tk.ops("pe", [lambda e, s=s, ptk=ptk: e.matmul(ptk[:, s * 128:(s + 1) * 128], lhsT=kn[:, s * 128:(s + 1) * 128].bitcast(F32R),
                                                       rhs=kn[:, s * 128:(s + 1) * 128].bitcast(F32R), start=True, stop=True)
                      for s in range(4)], reads=["kn"], writes=[pnk])
        pnq, ptq = pp.next()
        tk.ops("pe", [lambda e, s=s, ptq=ptq: e.matmul(ptq[:, s * 128:(s + 1) * 128], lhsT=kn[:, s * 128:(s + 1) * 128].bitcast(F32R),
                                                       rhs=qn[:, s * 128:(s + 1) * 128].bitcast(F32R), start=True, stop=True)
                      for s in range(4)], reads=["kn", "qn"], writes=[pnq])
        kk3 = ptk[:, :].rearrange("p (s d) -> p s d", d=128)
        qk3 = ptq[:, :].rearrange("p (s d) -> p s d", d=128)
        b3 = betab[:].rearrange("p (s t) -> p s t", t=128)
        tk.op("dve", lambda e: e.tensor_tensor(out=E1[:], in0=DT[:], in1=b3, op=ALU.mult), reads=["DT", "betab"],
              writes=["tdf"])
        tk.op("dve", lambda e: e.tensor_tensor(out=E1[:], in0=E1[:], in1=mus_neg, op=ALU.mult),
              reads=["tdf", "mus"], writes=["tdf"])
        tk.op("dve", lambda e: e.tensor_tensor(out=Bm[0][:].bitcast(F32R), in0=kk3, in1=E1[:], op=ALU.mult), reads=[pnk, "tdf"],
              writes=["Bm0"])
        tk.op("dve", lambda e: e.tensor_tensor(out=DT[:], in0=DT[:], in1=mu_inc, op=ALU.mult),
              reads=["DT", "mui", "tdf"], writes=["DT"])
        tk.op("dve", lambda e: e.tensor_tensor(out=QKT[:], in0=qk3, in1=DT[:], op=ALU.mult), reads=[pnq, "DT"],
              writes=[qkk])
        tk.op("dve", lambda e: e.tensor_tensor(out=Dm[:], in0=Dm[:], in1=mls_neg, op=ALU.mult),
              reads=["Dm", "mls"], writes=["Dm"])
        tk.op("dve", lambda e: e.tensor_tensor(out=Dm[:], in0=Dm[:], in1=colb(0), op=ALU.mult),
              reads=["Dm", "cols"], writes=["Dm"])
        tk.op("dve", lambda e: e.tensor_tensor(out=BTm[0][:].bitcast(F32R), in0=kk3, in1=Dm[:], op=ALU.mult), reads=[pnk, "Dm"],
              writes=["BTm0"])
        tk.op("dve", lambda e: e.tensor_tensor(out=Pm[0][:].bitcast(F32R), in0=Bm[0][:], in1=ident4, op=ALU.add),
              reads=["Bm0", "ident"], writes=["Pm0"])
        yield
        cb = 0
        cp = 0
        for k in range(1, 6):
            nb = 1 - cb
            if k < 5:
                pn1, pt1 = pp.next()
                tk.ops("pe", [lambda e, s=s, pt1=pt1, cb=cb: e.matmul(
                    pt1[:, s * 128:(s + 1) * 128], lhsT=BTm[cb][:, s, :].bitcast(F32R), rhs=Bm[cb][:, s, :].bitcast(F32R), start=True, stop=True)
                    for s in range(4)], reads=["Bm%d" % cb, "BTm%d" % cb], writes=[pn1])
            pn2, pt2 = pp.next()
            tk.ops("pe", [lambda e, s=s, pt2=pt2, cb=cb: e.matmul(
                pt2[:, s * 128:(s + 1) * 128], lhsT=Bm[cb][:, s, :].bitcast(F32R), rhs=BTm[cb][:, s, :].bitcast(F32R), start=True, stop=True)
                for s in range(4)], reads=["Bm%d" % cb, "BTm%d" % cb], writes=[pn2])
            if k < 5:
                tk.op("act", lambda e, pt1=pt1, nb=nb: e.copy(out=Bm[nb][:].rearrange("p s d -> p (s d)").bitcast(F32R), in_=pt1[:, :]),
                      reads=[pn1], writes=["Bm%d" % nb])
            tk.op("dve", lambda e, pt2=pt2, nb=nb: e.tensor_copy(out=BTm[nb][:].rearrange("p s d -> p (s d)").bitcast(F32R),
                                                                 in_=pt2[:, :]), reads=[pn2], writes=["BTm%d" % nb])
            pn3, pt3_ = pp.next()
            tk.ops("pe", [lambda e, s=s, pt3_=pt3_, nb=nb, cp=cp: e.matmul(
                pt3_[:, s * 128:(s + 1) * 128], lhsT=BTm[nb][:, s, :].bitcast(F32R), rhs=Pm[cp][:, s, :].bitcast(F32R), start=True, stop=True)
                for s in range(4)], reads=["BTm%d" % nb, "Pm%d" % cp], writes=[pn3])
            tk.op("dve", lambda e, pt3_=pt3_, cp=cp: e.tensor_tensor(
                out=Pm[1 - cp][:].rearrange("p s d -> p (s d)").bitcast(F32R), in0=Pm[cp][:].rearrange("p s d -> p (s d)"),
                in1=pt3_[:, :], op=ALU.add), reads=[pn3, "Pm%d" % cp], writes=["Pm%d" % (1 - cp)])
            cb = nb
            cp = 1 - cp
            yield
        Pf = Pm[cp]
        pk = "Pm%d" % cp
        yield
        pn, pt = pp.next()
        tk.ops("pe", [lambda e, s=s, pt=pt: e.matmul(pt[:, s * 128:(s + 1) * 128], lhsT=Pf[:, s, :].bitcast(F32R), rhs=Vb[:, s, :].bitcast(F32R),
                                                     start=True, stop=True) for s in range(4)],
               reads=[pk, "Vb"], writes=[pn])
        tk.op("act", lambda e, pt=pt: e.copy(out=U[:].rearrange("p s d -> p (s d)"), in_=pt[:, :]), reads=[pn],
              writes=[uk])
        pn, pt = pp.next()
        tk.ops("pe", [lambda e, s=s, pt=pt: e.matmul(pt[:, s * 128:(s + 1) * 128], lhsT=Kbe[:, s, :].bitcast(F32R), rhs=Pf[:, s, :].bitcast(F32R),
                                                     start=True, stop=True) for s in range(4)],
               reads=[pk, "Kbe"], writes=[pn])
        tk.op("act", lambda e, pt=pt: e.copy(out=WT[:], in_=pt[:, :]), reads=[pn], writes=[wk])
        tk.op("dve", lambda e: e.tensor_tensor(out=QdT[:], in0=qn[:], in1=egcb[:], op=ALU.mult),
              reads=["qn", ek], writes=[qdk])
        yield

    def prep_attn(c):
        ts = slice((c * T) % 4096, (c * T) % 4096 + T)
        rope_tables(c)
        yield
        pnx, ptx = proj(c, 7)
        for m, dstT, dk_, xo in ((4, qT, "qT", 0), (5, kT, "kT", 32)):
            pn, pt = proj(c, m)
            tk.op("act", lambda e, pt=pt, dstT=dstT: e.copy(out=dstT[32:64, ts], in_=pt[32:64, :]), reads=[pn],
                  writes=[K(dk_, c)])
            tk.op("act", lambda e, pt=pt, dstT=dstT: e.copy(out=dstT[64:128, ts], in_=pt[64:128, :]), reads=[pn],
                  writes=[K(dk_, c)])
            tk.op("dve", lambda e, pt=pt: e.tensor_tensor(out=rt1[:], in0=pt[0:32, :], in1=sincos[32:64, :],
                                                          op=ALU.mult), reads=[pn, "sincos"], writes=["rt1"])
            tk.op("dve", lambda e, ptx=ptx, xo=xo: e.tensor_tensor(out=rt2[:], in0=ptx[xo:xo + 32, :],
                                                                   in1=sincos[0:32, :], op=ALU.mult),
                  reads=[pnx, "sincos"], writes=["rt2"])
            tk.op("dve", lambda e, dstT=dstT: e.tensor_tensor(out=dstT[0:32, ts], in0=rt1[:], in1=rt2[:], op=ALU.add),
                  reads=["rt1", "rt2"], writes=[K(dk_, c)])
            yield
        pn, pt = proj(c, 6)
        tk.op("act", lambda e, pt=pt: e.copy(out=vT[:, ts], in_=pt[:, :]), reads=[pn], writes=[K("vT", c)])

        yield

    def scan(c):
        ts = slice(c * T, (c + 1) * T)
        par = c % 2
        U, WT, QdT, QKT, Kd, egcb, zs = U2[par], WT2[par], QdT2[par], QKT2[par], Kd2[par], egcb2[par], zs2[par]
        uk, wk, qdk, qkk, kdk, ek, zk = ("U%d" % par, "WT%d" % par, "QdT%d" % par, "QKT%d" % par, "Kd%d" % par,
                                         "egcb%d" % par, "zs%d" % par)
        ot = PS["ot"]
        sc_ = PS["scan"]
        for n in range(8):
            gi = c * 8 + n
            s, half = n // 2, n % 2
            r0 = 64 * half
            rows = slice(r0, r0 + 64)
            tsl = slice(64 * n, 64 * n + 64)
            Sc, Sn = S[gi % 2], S[(gi + 1) % 2]
            sk, snk = "S%d" % (gi % 2), "S%d" % ((gi + 1) % 2)
            slot = (gi % 2) * 256
            tk.op("pe", lambda e: e.matmul(sc_[rows, slot:slot + 128], lhsT=WT[:, tsl], rhs=Sc[:], start=True, stop=True),
                  reads=[wk, sk], writes=["ps_scan_a%d" % (gi % 2)])
            tk.op("dve", lambda e: e.tensor_tensor(out=vnew[rows, gi % 2, :], in0=U[rows, s, :],
                                                   in1=sc_[rows, slot:slot + 128], op=ALU.subtract),
                  reads=[uk, "ps_scan_a%d" % (gi % 2)], writes=["vnew%d" % (gi % 2)])
            tk.ops("pe", [
                lambda e: e.matmul(ot[:, tsl], lhsT=Sc[:], rhs=QdT[:, tsl], start=True, stop=False),
                lambda e: e.matmul(ot[:, tsl], lhsT=vnew[rows, gi % 2, :], rhs=QKT[rows, s, r0:r0 + 64], start=False,
                                   stop=True),
                lambda e: e.matmul(sc_[:, slot + 128:slot + 256], lhsT=Kd[rows, s, :], rhs=vnew[rows, gi % 2, :],
                                   start=True, stop=True)],
                reads=[sk, qdk, qkk, kdk, "vnew%d" % (gi % 2)], writes=["ps_ot", "ps_scan_b%d" % (gi % 2)])
            tl = 64 * n + 63
            tk.op("dve", lambda e: e.scalar_tensor_tensor(out=Sn[:], in0=Sc[:], scalar=egcb[:, tl:tl + 1],
                                                          in1=sc_[:, slot + 128:slot + 256], op0=ALU.mult, op1=ALU.add),
                  reads=[sk, ek, "ps_scan_b%d" % (gi % 2)], writes=[snk])
            yield
        tk.op("act", lambda e: e.activation(out=osq[:], in_=ot[:, :], func=AF.Square), reads=["ps_ot"], writes=["p1"])
        pn, pt = ppa.next()
        tk.op("pe", lambda e: e.matmul(pt[:, :], lhsT=ones_f[:], rhs=osq[:], start=True, stop=True),
              reads=["p1", "ones_f"], writes=[pn])
        tk.op("act", lambda e: e.activation(out=rs[:], in_=pt[:, :], func=AF.Ln, bias=RMS_EPS_AP[:, 0:1],
                                            scale=1.0 / 128.0), reads=[pn, "epsc"], writes=["p2"])
        tk.op("act", lambda e: e.activation(out=rs[:], in_=rs[:], func=AF.Exp, scale=-0.5), reads=["p2"],
              writes=["p2"])
        tk.op("dve", lambda e: e.scalar_tensor_tensor(out=oa[:], in0=ot[:, :], scalar=pvec[:, 2:3], in1=rs[:],
                                                      op0=ALU.mult, op1=ALU.mult), reads=["ps_ot", "pvec", "p2"],
              writes=["p3"])
        tk.op("dve", lambda e: e.tensor_tensor(out=oab[:], in0=oa[:], in1=zs[:], op=ALU.mult), reads=["p3", zk],
              writes=["oab"])
        if cin is None:
            tk.dma("sp", lambda e: e.dma_start(out=mixT[0:128, ts], in_=oab[:]), reads=["oab"], writes=["mixT_a"])
        else:
            tk.dma("sp", lambda e: e.dma_start(out=cin[c // 4][0:128, (c % 4) * T:(c % 4 + 1) * T], in_=oab[:]),
                   reads=["oab"], writes=[("cin_a", c // 4)])
        yield

    att_scale = 128.0 ** -0.5
    pcount = [0]

    def qblock(qsel, kcur, kprev, vcur, vprev, vkeys, accv, first, rkeys):
        i = pcount[0] % 2
        pcount[0] += 1
        pTb = pTt[i]
        pk = "pT%d" % i
        pn, pt = ppa.next()
        if kprev is not None:
            tk.ops("pe", [
                lambda e: e.matmul(pt[:, 0:256], lhsT=ident_b[:], rhs=amask[:, 0:256], start=True, stop=False),
                lambda e: e.matmul(pt[:, 0:128], lhsT=kprev, rhs=qsel, start=False, stop=False),
                lambda e: e.matmul(pt[:, 128:256], lhsT=kcur, rhs=qsel, start=False, stop=True)],
                reads=rkeys + ["ident_b", "amask"], writes=[pn])
            tk.op("act", lambda e: e.activation(out=pTb[:, 0:256], in_=pt[:, 0:256], func=AF.Exp, scale=att_scale),
                  reads=[pn], writes=[pk])
            pn2, pt2 = ppa.next()
            tk.ops("pe", [
                lambda e: e.matmul(pt2[:, 0:128], lhsT=vprev, rhs=pTb[:, 0:128], start=True, stop=False),
                lambda e: e.matmul(pt2[:, 0:128], lhsT=vcur, rhs=pTb[:, 128:256], start=False, stop=True),
                lambda e: e.matmul(pt2[:, 128:256], lhsT=ones_b[:], rhs=pTb[:, 0:128], start=True, stop=False),
                lambda e: e.matmul(pt2[:, 128:256], lhsT=ones_b[:], rhs=pTb[:, 128:256], start=False, stop=True)],
                reads=[pk, "ones_b"] + vkeys, writes=[pn2])
        else:
            tk.ops("pe", [
                lambda e: e.matmul(pt[:, 128:256], lhsT=ident_b[:], rhs=amask[:, 128:256], start=True, stop=False),
                lambda e: e.matmul(pt[:, 128:256], lhsT=kcur, rhs=qsel, start=False, stop=True)],
                reads=rkeys + ["ident_b", "amask"], writes=[pn])
            tk.op("act", lambda e: e.activation(out=pTb[:, 128:256], in_=pt[:, 128:256], func=AF.Exp, scale=att_scale),
                  reads=[pn], writes=[pk])
            pn2, pt2 = ppa.next()
            tk.ops("pe", [
                lambda e: e.matmul(pt2[:, 0:128], lhsT=vcur, rhs=pTb[:, 128:256], start=True, stop=True),
                lambda e: e.matmul(pt2[:, 128:256], lhsT=ones_b[:], rhs=pTb[:, 128:256], start=True, stop=True)],
                reads=[pk, "ones_b"] + vkeys, writes=[pn2])
        src = pt2[:, 0:256].rearrange("p (a q) -> p a q", a=2)
        if first:
            tk.op("dve", lambda e: e.tensor_copy(out=accv, in_=src), reads=[pn2], writes=["acc"])
        else:
            tk.op("dve", lambda e: e.tensor_tensor(out=accv, in0=accv, in1=src, op=ALU.add), reads=[pn2, "acc"],
                  writes=["acc"])

    vcnt = [0]

    def vtrans(vsel, rk):
        i = vcnt[0] % 4
        vcnt[0] += 1
        pn, pt = ppa.next()
        ptb = pt[:, 0:64].bitcast(BF16)
        tk.op("pe", lambda e: e.transpose(ptb, vsel, ident_b[:]), reads=rk + ["ident_b"], writes=[pn])
        tk.op("act", lambda e: e.copy(out=vtt[:, i, :], in_=ptb), reads=[pn], writes=["vtt%d" % i])
        return vtt[:, i, :], "vtt%d" % i

    def rsl(t0, n, step=1):
        r0 = t0 % RING
        return slice(r0, r0 + (n - 1) * step + 1, step)

    def attn(c):
        sc = c // 4
        for jj in range(4):
            j = 4 * c + jj
            t0 = 128 * j
            cprev = (t0 - 128) // T
            vc, vck = vtrans(vT[:, rsl(t0, 128)], [K("vT", c)])
            if j > 0:
                vp, vpk = vtrans(vT[:, rsl(t0 - 128, 128)], [K("vT", cprev)])
            else:
                vp, vpk = None, None
            rk = [K("qT", c), K("kT", c)] + ([K("kT", cprev)] if j > 0 else [])
            qblock(qT[:, rsl(t0, 128)], kT[:, rsl(t0, 128)], kT[:, rsl(t0 - 128, 128)] if j > 0 else None,
                   vc, vp, [k_ for k_ in (vck, vpk) if k_], acc[:, :, (t0 % 2048):(t0 % 2048) + 128], True, rk)
            yield
        for r in range(4):
            t0 = T * c + r
            vc, vck = vtrans(vT[:, rsl(t0, 128, 4)], [K("vT", c)])
            if c > 0:
                vp, vpk = vtrans(vT[:, rsl(t0 - T, 128, 4)], [K("vT", c - 1)])
            else:
                vp, vpk = None, None
            rk = [K("qT", c), K("kT", c)] + ([K("kT", c - 1)] if c > 0 else [])
            a0 = (T * c) % 2048 + r
            qblock(qT[:, rsl(t0, 128, 4)], kT[:, rsl(t0, 128, 4)], kT[:, rsl(t0 - T, 128, 4)] if c > 0 else None,
                   vc, vp, [k_ for k_ in (vck, vpk) if k_], acc[:, :, a0:a0 + 509:4], False, rk)
            yield
        if c % 4 == 3:
            cs = [4 * sc + i for i in range(4)]
            csp = [4 * (sc - 1) + i for i in range(4)] if sc > 0 else []
            for r in range(16):
                t0 = 2048 * sc + r
                vc, vck = vtrans(vT[:, rsl(t0, 128, 16)], [K("vT", ci) for ci in cs])
                if sc > 0:
                    vp, vpk = vtrans(vT[:, rsl(t0 - 2048, 128, 16)], [K("vT", ci) for ci in csp])
                else:
                    vp, vpk = None, None
                rk = [K("qT", ci) for ci in cs] + [K("kT", ci) for ci in cs] + [K("kT", ci) for ci in csp]
                qblock(qT[:, rsl(t0, 128, 16)], kT[:, rsl(t0, 128, 16)],
                       kT[:, rsl(t0 - 2048, 128, 16)] if sc > 0 else None,
                       vc, vp, [k_ for k_ in (vck, vpk) if k_], acc[:, :, r:r + 2033:16], False, rk)
                yield
            for i in range(4):
                a = slice(i * T, (i + 1) * T)
                tsl = slice(2048 * sc + i * T, 2048 * sc + (i + 1) * T)
                tk.op("act", lambda e, a=a: e.activation(out=fr[:], in_=acc[:, 1, a], func=AF.Ln), reads=["acc"],
                      writes=["p2"])
                tk.op("act", lambda e: e.activation(out=fr[:], in_=fr[:], func=AF.Exp, scale=-1.0), reads=["p2"],
                      writes=["p2"])
                tk.op("dve", lambda e, a=a: e.tensor_tensor(out=fo[:], in0=acc[:, 0, a], in1=fr[:], op=ALU.mult),
                      reads=["acc", "p2"], writes=["p3"])
                tk.op("act", lambda e: e.activation(out=fsq[:], in_=fo[:], func=AF.Square), reads=["p3"],
                      writes=["p1"])
                pn, pt = ppa.next()
                tk.op("pe", lambda e, pt=pt: e.matmul(pt[:, :], lhsT=ones_f[:], rhs=fsq[:], start=True, stop=True),
                      reads=["p1", "ones_f"], writes=[pn])
                tk.op("act", lambda e, pt=pt: e.activation(out=fr[:], in_=pt[:, :], func=AF.Ln,
                                                           bias=RMS_EPS_AP[:, 0:1], scale=1.0 / 128.0),
                      reads=[pn, "epsc"], writes=["p2"])
                tk.op("act", lambda e: e.activation(out=fr[:], in_=fr[:], func=AF.Exp, scale=-0.5), reads=["p2"],
                      writes=["p2"])
                tk.op("dve", lambda e: e.scalar_tensor_tensor(out=fob[:], in0=fo[:], scalar=pvec[:, 3:4], in1=fr[:],
                                                              op0=ALU.mult, op1=ALU.mult), reads=["p3", "pvec", "p2"],
                      writes=["fob"])
                yield
                if cin is None:
                    tk.dma("sp", lambda e, tsl=tsl: e.dma_start(out=mixT[128:256, tsl], in_=fob[:]), reads=["fob"],
                           writes=["mixT_b"])
                else:
                    tk.dma("sp", lambda e, i=i: e.dma_start(out=cin[sc][128:256, i * T:(i + 1) * T], in_=fob[:]),
                           reads=["fob"], writes=[("cin_b", sc)])
            if on_quarter is not None:
                on_quarter(sc, tk)
        yield

    epsc = sb("epsc", [128, 2])
    tk.op("pool", lambda e: e.memset(epsc[:, 0:1], RMS_EPS), writes=["epsc"])
    tk.op("pool", lambda e: e.memset(epsc[:, 1:2], 1.0), writes=["epsc"])
    RMS_EPS_AP = epsc[:, 0:1]
    ONE_AP = epsc[:, 1:2]

    def run(gen):
        if gen is not None:
            for _ in gen:
                pass

    load_x(0)
    run(prep_main(0))
    tk.flush()
    for c in range(nch):
        run(scan(c))
        run(prep_attn(c))
        run(attn(c))
        if c + 1 < nch:
            run(prep_main(c + 1))
        if (c + 1) % 4 == 3 or c == nch - 1:
            tk.flush()
    tk = real_tk


def build_phase1(nch=NCH):
    nc = bass.Bass("TRN2", target_bir_lowering=False)
    D = {}

    def din(name, shape, dt=F32):
        D[name] = nc.dram_tensor(name, shape, dt, kind="ExternalInput").ap()

    din("x", [D_MODEL, SEQ]); din("cT", [128, 8]); din("pos", [SEQ], I32)
    din("wada1", [128, 8, 8, 256]); din("bada1", [128, 16]); din("win", [128, 5, 8, 256])
    din("convw", [128, 3, 4]); din("pvec", [128, 4])
    din("c_ident", [128, 128]); din("c_masks", [128, 3, 128]); din("c_amask", [128, 256]); din("c_invf", [64, 2])
    D["mixT"] = nc.dram_tensor("mixT", [256, SEQ], BF16, kind="ExternalOutput").ap()
    with ExitStack() as es:
        tk = Trk(nc, es)
        phase1(nc, tk, es, D, nch=nch)
        tk.finish("sp")
    return nc


def consts():
    p = np.arange(128)[:, None]
    f = np.arange(128)[None, :]
    same = (p // 64) == (f // 64)
    mus = -((f > p) & same).astype(np.float32)
    mui = ((f >= p) & same).astype(np.float32)
    mls = -((f < p) & same).astype(np.float32)
    masks = np.stack([mus, mui, mls], 1).astype(np.float32)
    prev = np.where(p >= f, 0.0, NEG).astype(np.float32)
    cur = np.where(p <= f, 0.0, NEG).astype(np.float32)
    amask = np.concatenate([prev, cur], 1)
    i = (np.arange(64) % 16).astype(np.float32)
    invf = (ROPE_THETA ** (-i * 2.0 / 32.0)).astype(np.float32)
    invf = np.stack([invf, np.where(np.arange(64) < 32, 0.0, math.pi / 2).astype(np.float32)], 1)
    return {"c_ident": np.eye(128, dtype=np.float32), "c_masks": masks, "c_amask": amask, "c_invf": invf}


def phase1_inputs(inp, core):
    b, h = core // 4, core % 4
    l = 0
    w_in = inp["w_in"][l]
    hs = slice(h * 128, (h + 1) * 128)
    qa, ka, va, z = w_in[:, 0:512][:, hs], w_in[:, 512:1024][:, hs], w_in[:, 1024:1536][:, hs], w_in[:, 1536:2048][:, hs]
    beta = w_in[:, 2048 + h:2049 + h]
    dec = w_in[:, 2052 + h:2053 + h]
    qb, kb, vb = w_in[:, 2056:2568][:, hs], w_in[:, 2568:3080][:, hs], w_in[:, 3080:3592][:, hs]
    extra = np.zeros((D_MODEL, 128), np.float32)
    extra[:, 0:16] = qb[:, 16:32]; extra[:, 16:32] = qb[:, 0:16]
    extra[:, 32:48] = kb[:, 16:32]; extra[:, 48:64] = kb[:, 0:16]
    win = np.concatenate([qa, ka, va, z, qb, kb, vb, extra, np.repeat(beta, 128, 1), np.repeat(dec, 128, 1)], 1)
    conv = inp["conv_w"][l]
    convw = np.stack([conv[:, g * 512:(g + 1) * 512][:, hs].T for g in range(3)], 1)
    pvec = np.stack([np.full(128, inp["a_log"][l, h]), np.full(128, inp["dt_bias"][l, h]),
                     inp["gdn_norm_w"][l], inp["attn_norm_w"][l]], 1).astype(np.float32)
    win_l = win.astype(np.float32).reshape(8, 128, 5, 256).transpose(1, 2, 0, 3)
    wada1_l = inp["w_ada"][l][:, 0:2048].reshape(8, 128, 8, 256).transpose(1, 2, 0, 3)
    d = {"x": np.ascontiguousarray(inp["x"][b].T), "cT": np.ascontiguousarray(inp["c"][b].reshape(8, 128).T),
         "pos": np.ascontiguousarray(inp["positions"][b]).astype(np.int32),
         "wada1": np.ascontiguousarray(wada1_l),
         "bada1": np.ascontiguousarray(inp["b_ada"][l][0:2048].reshape(16, 128).T),
         "win": np.ascontiguousarray(win_l), "convw": np.ascontiguousarray(convw.astype(np.float32)),
         "pvec": np.ascontiguousarray(pvec)}
    d.update(consts())
    return d


NT2 = 16
NBLK = 64


def phase2(nc, tk, es, D, ntile=NT2, nblk=NBLK, cout=None):
    real = tk
    tk = Rec(real)

    def mk(stack, pref):
        def sb(name, shape, dt=F32):
            return stack.enter_context(nc.sbuf_tensor(pref + name, shape, dt))
        return sb

    sb = mk(es, "t_")
    pp = PsumPool(nc, es, ["r0", "r1", "r2", "r3", "r4", "r5", "r6", "r7"])

    ident = sb("ident", [128, 128])
    ones_f = sb("ones_f", [128, 128])
    triS = sb("triS", [128, 128])
    kcoff = sb("kcoff", [128, 1])
    thr16 = sb("thr16", [128, 16])
    thr64 = sb("thr64", [128, 64])
    epsc = sb("epsc", [128, 1])
    cT = sb("cT", [128, 8]); scT = sb("scT", [128, 8])
    modb = sb("modb", [128, 4, 1024])
    lnp = sb("lnp", [128, 4, 1024])
    xt2 = [sb("xt%d" % i, [128, 1024]) for i in range(2)]
    rr2 = [sb("rr%d" % i, [128, 1024]) for i in range(2)]
    x12 = [sb("x1%d" % i, [128, 1024]) for i in range(2)]
    h22 = [sb("h2%d" % i, [128, 1024]) for i in range(2)]
    stt = sb("stt", [128, 2, 6]); mv = sb("mv", [128, 2]); rstd = sb("rstd", [128, 1])
    gts = sb("gts", [128, NT2, 2])
    desti = sb("desti", [128, 2, NT2], I32)
    idxw = sb("idxw", [128, 64], I32)

    x = D["x2"]; mixTf = D.get("mixTf"); out = D["out"]
    x1d = D["x1d"]; h2d = D["h2d"]; xs = D["xs"]; ys = D["ys"]

    def layer_norm(src, skey, dst, dkey, gi):
        tk.op("dve", lambda e: e.bn_stats(out=stt[:, 0, :], in_=src[:, 0:512]), reads=[skey], writes=["stt"])
        tk.op("dve", lambda e: e.bn_stats(out=stt[:, 1, :], in_=src[:, 512:1024]), reads=[skey], writes=["stt"])
        tk.op("dve", lambda e: e.bn_aggr(out=mv[:], in_=stt[:].rearrange("p a b -> p (a b)")), reads=["stt"],
              writes=["mv"])
        tk.op("act", lambda e: e.activation(out=rstd[:], in_=mv[:, 1:2], func=AF.Sqrt, bias=epsc[:, 0:1], scale=1.0),
              reads=["mv", "epsc"], writes=["rstd"])
        tk.op("dve", lambda e: e.reciprocal(out=rstd[:], in_=rstd[:]), reads=["rstd"], writes=["rstd"])
        tk.op("dve", lambda e: e.tensor_scalar(out=dst[:], in0=src[:], scalar1=mv[:, 0:1], scalar2=rstd[:, 0:1],
                                               op0=ALU.subtract, op1=ALU.mult), reads=[skey, "mv", "rstd"],
              writes=[dkey])
        tk.op("dve", lambda e: e.tensor_tensor(out=dst[:], in0=dst[:], in1=lnp[:, gi, :], op=ALU.mult),
              reads=[dkey, "lnp"], writes=[dkey])
        tk.op("dve", lambda e: e.tensor_tensor(out=dst[:], in0=dst[:], in1=lnp[:, gi + 1, :], op=ALU.add),
              reads=[dkey, "lnp"], writes=[dkey])

    tk.dma("sp", lambda e: e.dma_start(out=ident[:], in_=D["c_ident"][:, :]), writes=["ident"])
    tk.dma("sp", lambda e: e.dma_start(out=triS[:], in_=D["c_tri"][:, :]), writes=["triS"])
    tk.dma("sp", lambda e: e.dma_start(out=kcoff[:], in_=D["c_iota"][:, :]), writes=["kcoff"])
    tk.dma("sp", lambda e: e.dma_start(out=thr16[:], in_=D["c_thr16"][:, :]), writes=["thr16"])
    tk.dma("sp", lambda e: e.dma_start(out=thr64[:], in_=D["c_thr64"][:, :]), writes=["thr64"])
    tk.dma("sp", lambda e: e.dma_start(out=cT[:], in_=D["cT"][:, :]), writes=["cT"])
    for i in range(4):
        tk.dma("sp", lambda e, i=i: e.dma_start(out=lnp[:, i, :], in_=D["lnp"][i, :].partition_broadcast(128)),
               writes=["lnp"])
    tk.op("pool", lambda e: e.memset(ones_f[:], 1.0), writes=["ones_f"])
    tk.op("pool", lambda e: e.memset(epsc[:], LN_EPS), writes=["epsc"])

    with ExitStack() as esA:
        sa = mk(esA, "a_")
        scR = sa("scR", [128, 8, 128])
        badab = sa("badab", [128, 512])
        wo_b = sa("wo_b", [128, 8, 1024], BF16)
        wr = sa("wr", [128, 8, 36]); brb = sa("brb", [128, 36])
        h2T2 = [sa("h2T%d" % i, [128, 8, 128]) for i in range(2)]
        lg = sa("lg", [128, 36]); gmax = sa("gmax", [128, 1]); ngmax = sa("ngmax", [128, 1]); ohg = sa("ohg", [128, 4])
        ex4 = sa("ex4", [128, 4]); sume = sa("sume", [128, 1]); ggrp = sa("ggrp", [128, 1])
        tmp48 = sa("tmp48", [128, 4, 8]); lesel = sa("lesel", [128, 8]); m8 = sa("m8", [128, 8])
        oh8 = sa("oh8", [128, 2, 8]); e2 = sa("e2", [128, 1]); g1 = sa("g1", [128, 1])
        ohs = sa("ohs", [128, NT2, 2, 32]); ohany = sa("ohany", [128, 32]); cum = sa("cum", [128, 32])
        ranks = sa("ranks", [128, NT2, 32])
        cntb = sa("cntb", [128, 32]); cmp16 = sa("cmp16", [128, 32, 16]); padded = sa("padded", [128, 32])
        onesr = sa("onesr", [128, 32]); padend = sa("padend", [128, 32]); padstart = sa("padstart", [128, 32])
        tmpd = sa("tmpd", [128, NT2, 32]); tmpd2 = sa("tmpd2", [128, NT2, 32])
        destf = sa("destf", [128, 2, NT2])
        cmp64 = sa("cmp64", [128, 64, 32]); blkf = sa("blkf", [128, 64])

        tk.dma("sp", lambda e: e.dma_start(out=wr[:], in_=D["wr"][:, :].rearrange("(kc p) c -> p kc c", p=128)),
               writes=["wr"])
        tk.dma("sp", lambda e: e.dma_start(out=brb[:], in_=D["br"][:].partition_broadcast(128)), writes=["brb"])
        tk.op("pool", lambda e: e.memset(cum[:], 0.0), writes=["cum"])
        tk.op("pool", lambda e: e.memset(onesr[:], 1.0), writes=["onesr"])
        if cout is None:
            mt1 = sa("mt", [128, 8, 128], BF16)
        else:
            mtf = sa("mtf", [128, 8, 2048], BF16)
            qidx = sa("qidx", [128, 8], I32)
            tk.dma("sp", lambda e: e.dma_start(out=qidx[:], in_=D["qidx"][:, :]), writes=["qidx"])
            for kc in range(8):
                tk.dma("pool", lambda e, kc=kc: e.indirect_dma_start(
                    out=mtf[:, kc, :], out_offset=None, in_=cout[:, :],
                    in_offset=bass.IndirectOffsetOnAxis(ap=qidx[:, kc:kc + 1], axis=0)),
                    reads=["qidx"] + [("cout", q_) for q_ in range(4)], writes=["mt"])
        stg = [(xt2[0], "xt0"), (rr2[0], "rr0"), (xt2[1], "xt1"), (rr2[1], "rr1")]
        for kc in range(8):
            st, key = stg[kc % 4]
            tk.dma("sp", lambda e, st=st, kc=kc: e.dma_start(out=st[:], in_=D["wo"][kc * 128:(kc + 1) * 128, :]),
                   writes=[key])
            tk.op("dve", lambda e, st=st, kc=kc: e.tensor_copy(out=wo_b[:, kc, :], in_=st[:]), reads=[key],
                  writes=["wo_b"])
        tk.op("act", lambda e: e.activation(out=scT[:], in_=cT[:], func=AF.Silu), reads=["cT"], writes=["scT"])
        tk.op("dve", lambda e: e.tensor_copy(out=scR[:], in_=scT[:].unsqueeze(2).to_broadcast([128, 8, 128])),
              reads=["scT"], writes=["scR"])
        for blk in range(8):
            pn, pt = pp.next()
            for sub in range(4):
                st, key = stg[sub % 4]
                st3 = st[:].rearrange("p (kc c) -> p kc c", c=128)
                c0 = blk * 512 + sub * 128
                tk.dma("sp" if sub % 2 == 0 else "pool", lambda e, st3=st3, blk=blk, sub=sub: e.dma_start(
                    out=st3, in_=D["wada2"][:, blk * 4 + sub, :, :]), writes=[key])
                tk.ops("pe", [lambda e, kc=kc, pt=pt, st3=st3, sub=sub: e.matmul(
                    pt[:, sub * 128:(sub + 1) * 128], lhsT=scR[:, kc, :], rhs=st3[:, kc, :], start=(kc == 0),
                    stop=(kc == 7)) for kc in range(8)], reads=[key, "scR"], writes=[pn])
            tk.dma("sp", lambda e, blk=blk: e.dma_start(
                out=badab[:], in_=D["bada2"][blk * 512:(blk + 1) * 512].partition_broadcast(128)), writes=["badab"])
            dst = modb[:, blk // 2, (blk % 2) * 512:(blk % 2 + 1) * 512]
            tk.op("dve", lambda e, pt=pt, dst=dst: e.tensor_tensor(out=dst, in0=pt[:, :], in1=badab[:], op=ALU.add),
                  reads=[pn, "badab"], writes=["modb"])
            if blk // 2 != 1:
                tk.op("dve", lambda e, dst=dst: e.tensor_scalar(out=dst, in0=dst, scalar1=1.0, scalar2=None,
                                                                op0=ALU.add), reads=["modb"], writes=["modb"])
        tk.flush()

        for k in range(ntile):
            par = k % 2
            xt, rr, x1, h2, h2T = xt2[par], rr2[par], x12[par], h22[par], h2T2[par]
            xtk, rrk, x1k, h2k, h2Tk = "xt%d" % par, "rr%d" % par, "x1%d" % par, "h2%d" % par, "h2T%d" % par
            tsl = slice(k * 128, (k + 1) * 128)
            if cout is None:
                mt = mt1
                tk.dma("sp", lambda e: e.dma_start(out=mt[:], in_=mixTf[:, tsl].rearrange("(kc p) t -> p kc t", p=128)),
                       writes=["mt"])
            else:
                mt = mtf[:, :, tsl]
            tk.dma("sp", lambda e: e.dma_start(out=xt[:], in_=x[tsl, :]), writes=[xtk])
            pa, pta = pp.next()
            pb, ptb = pp.next()
            for (pn, pt, hs) in ((pa, pta, slice(0, 512)), (pb, ptb, slice(512, 1024))):
                tk.ops("pe", [lambda e, kc=kc, pt=pt, hs=hs: e.matmul(pt[:, :], lhsT=mt[:, kc, :], rhs=wo_b[:, kc, hs],
                                                                     start=(kc == 0), stop=(kc == 7))
                              for kc in range(8)], reads=["mt", "wo_b"], writes=[pn])
                tk.op("dve", lambda e, pt=pt, hs=hs: e.tensor_tensor(out=rr[:, hs], in0=pt[:, :], in1=modb[:, 0, hs],
                                                                     op=ALU.mult), reads=[pn, "modb"], writes=[rrk])
            tk.op("dve", lambda e: e.scalar_tensor_tensor(out=rr[:], in0=xt[:], scalar=ALPHA, in1=rr[:], op0=ALU.mult,
                                                          op1=ALU.add), reads=[xtk, rrk], writes=[rrk])
            layer_norm(rr, rrk, x1, x1k, 0)
            tk.dma("sp", lambda e: e.dma_start(out=x1d[tsl, :], in_=x1[:]), reads=[x1k], writes=["x1d"])
            tk.op("dve", lambda e: e.tensor_tensor(out=h2[:], in0=x1[:], in1=modb[:, 2, :], op=ALU.mult),
                  reads=[x1k, "modb"], writes=[h2k])
            tk.op("dve", lambda e: e.tensor_tensor(out=h2[:], in0=h2[:], in1=modb[:, 1, :], op=ALU.add),
                  reads=[h2k, "modb"], writes=[h2k])
            tk.dma("sp", lambda e: e.dma_start(out=h2d[tsl, :], in_=h2[:]), reads=[h2k], writes=["h2d"])
            for half in range(2):
                pn, pt = pp.next()
                tk.ops("pe", [lambda e, j=j, pt=pt, half=half: e.transpose(
                    pt[:, j * 128:(j + 1) * 128], h2[:, (half * 4 + j) * 128:(half * 4 + j + 1) * 128], ident[:])
                    for j in range(4)], reads=[h2k, "ident"], writes=[pn])
                tk.op("act", lambda e, pt=pt, half=half: e.copy(
                    out=h2T[:, half * 4:(half + 1) * 4, :].rearrange("p a b -> p (a b)"), in_=pt[:, :]), reads=[pn],
                    writes=[h2Tk])
            pn, pt = pp.next()
            tk.ops("pe", [lambda e, kc=kc, pt=pt: e.matmul(pt[:, 0:36], lhsT=h2T[:, kc, :], rhs=wr[:, kc, :],
                                                          start=(kc == 0), stop=(kc == 7)) for kc in range(8)],
                   reads=[h2Tk, "wr"], writes=[pn])
            tk.op("dve", lambda e, pt=pt: e.tensor_tensor(out=lg[:], in0=pt[:, 0:36], in1=brb[:], op=ALU.add),
                  reads=[pn, "brb"], writes=["lg"])
            tk.op("dve", lambda e: e.tensor_reduce(out=gmax[:], in_=lg[:, 0:4], axis=AX.X, op=ALU.max), reads=["lg"],
                  writes=["gmax"])
            tk.op("dve", lambda e: e.tensor_scalar(out=ohg[:], in0=lg[:, 0:4], scalar1=gmax[:, 0:1], scalar2=None,
                                                   op0=ALU.is_equal), reads=["lg", "gmax"], writes=["ohg"])
            tk.op("dve", lambda e: e.tensor_scalar(out=ngmax[:], in0=gmax[:], scalar1=-1.0, scalar2=None, op0=ALU.mult),
                  reads=["gmax"], writes=["ngmax"])
            tk.op("act", lambda e: e.activation(out=ex4[:], in_=lg[:, 0:4], func=AF.Exp, bias=ngmax[:, 0:1], scale=1.0),
                  reads=["lg", "ngmax"], writes=["ex4"])
            tk.op("dve", lambda e: e.tensor_reduce(out=sume[:], in_=ex4[:], axis=AX.X, op=ALU.add), reads=["ex4"],
                  writes=["sume"])
            tk.op("dve", lambda e: e.reciprocal(out=ggrp[:], in_=sume[:]), reads=["sume"], writes=["ggrp"])
            le3 = lg[:, 4:36].rearrange("p (g e) -> p g e", e=8)
            tk.op("dve", lambda e: e.tensor_tensor(out=tmp48[:], in0=le3,
                                                   in1=ohg[:].unsqueeze(2).to_broadcast([128, 4, 8]), op=ALU.mult),
                  reads=["lg", "ohg"], writes=["tmp48"])
            tk.op("dve", lambda e: e.tensor_reduce(out=lesel[:], in_=tmp48[:].rearrange("p g e -> p e g"), axis=AX.X,
                                                   op=ALU.add), reads=["tmp48"], writes=["lesel"])
            tk.op("dve", lambda e: e.max(out=m8[:], in_=lesel[:]), reads=["lesel"], writes=["m8"])
            for j in range(2):
                tk.op("dve", lambda e, j=j: e.tensor_scalar(out=oh8[:, j, :], in0=lesel[:], scalar1=m8[:, j:j + 1],
                                                            scalar2=None, op0=ALU.is_equal), reads=["lesel", "m8"],
                      writes=["oh8"])
            tk.op("dve", lambda e: e.tensor_tensor(out=e2[:], in0=m8[:, 1:2], in1=m8[:, 0:1], op=ALU.subtract),
                  reads=["m8"], writes=["e2"])
            tk.op("act", lambda e: e.activation(out=e2[:], in_=e2[:], func=AF.Exp), reads=["e2"], writes=["e2"])
            tk.op("dve", lambda e: e.tensor_scalar(out=g1[:], in0=e2[:], scalar1=1.0, scalar2=None, op0=ALU.add),
                  reads=["e2"], writes=["g1"])
            tk.op("dve", lambda e: e.reciprocal(out=g1[:], in_=g1[:]), reads=["g1"], writes=["g1"])
            tk.op("dve", lambda e: e.tensor_tensor(out=gts[:, k, 0:1], in0=g1[:], in1=ggrp[:], op=ALU.mult),
                  reads=["g1", "ggrp"], writes=["gts"])
            tk.op("dve", lambda e: e.tensor_tensor(out=e2[:], in0=e2[:], in1=g1[:], op=ALU.mult), reads=["e2", "g1"],
                  writes=["e2"])
            tk.op("dve", lambda e: e.tensor_tensor(out=gts[:, k, 1:2], in0=e2[:], in1=ggrp[:], op=ALU.mult),
                  reads=["e2", "ggrp"], writes=["gts"])
            for j in range(2):
                tk.op("dve", lambda e, j=j: e.tensor_tensor(
                    out=ohs[:, k, j, :].rearrange("p (g e) -> p g e", e=8),
                    in0=ohg[:].unsqueeze(2).to_broadcast([128, 4, 8]),
                    in1=oh8[:, j, :].unsqueeze(1).to_broadcast([128, 4, 8]), op=ALU.mult), reads=["ohg", "oh8"],
                    writes=["ohs"])
            tk.op("dve", lambda e: e.tensor_tensor(out=ohany[:], in0=ohs[:, k, 0, :], in1=ohs[:, k, 1, :], op=ALU.add),
                  reads=["ohs"], writes=["ohany"])
            pn, pt = pp.next()
            tk.ops("pe", [lambda e, pt=pt: e.matmul(pt[:, 0:32], lhsT=triS[:], rhs=ohany[:], start=True, stop=False),
                          lambda e, pt=pt: e.matmul(pt[:, 0:32], lhsT=ones_f[:], rhs=cum[:], start=False, stop=True)],
                   reads=["triS", "ohany", "ones_f", "cum"], writes=[pn])
            tk.op("act", lambda e, pt=pt: e.copy(out=ranks[:, k, :], in_=pt[:, 0:32]), reads=[pn], writes=["ranks"])
            tk.op("dve", lambda e: e.tensor_tensor(out=cum[:], in0=cum[:], in1=ohany[:], op=ALU.add),
                  reads=["cum", "ohany"], writes=["cum"])
            if k % 8 == 7:
                tk.flush()

        pn, pt = pp.next()
        tk.op("pe", lambda e: e.matmul(pt[:, 0:32], lhsT=ones_f[:], rhs=cum[:], start=True, stop=True),
              reads=["ones_f", "cum"], writes=[pn])
        tk.op("dve", lambda e: e.tensor_copy(out=cntb[:], in_=pt[:, 0:32]), reads=[pn], writes=["cntb"])
        tk.op("dve", lambda e: e.tensor_tensor(out=cmp16[:], in0=cntb[:].unsqueeze(2).to_broadcast([128, 32, 16]),
                                               in1=thr16[:].unsqueeze(1).to_broadcast([128, 32, 16]), op=ALU.is_gt),
              reads=["cntb", "thr16"], writes=["cmp16"])
        tk.op("dve", lambda e: e.tensor_reduce(out=padded[:], in_=cmp16[:], axis=AX.X, op=ALU.add), reads=["cmp16"],
              writes=["padded"])
        tk.op("dve", lambda e: e.tensor_scalar(out=padded[:], in0=padded[:], scalar1=128.0, scalar2=None, op0=ALU.mult),
              reads=["padded"], writes=["padded"])
        tk.op("dve", lambda e: e.tensor_tensor_scan(out=padend[:], data0=onesr[:], data1=padded[:], initial=0.0,
                                                    op0=ALU.mult, op1=ALU.add), reads=["onesr", "padded"],
              writes=["padend"])
        tk.op("dve", lambda e: e.tensor_tensor(out=padstart[:], in0=padend[:], in1=padded[:], op=ALU.subtract),
              reads=["padend", "padded"], writes=["padstart"])
        tk.op("dve", lambda e: e.tensor_tensor(out=tmpd[:, 0:ntile, :], in0=ranks[:, 0:ntile, :],
                                               in1=padstart[:].unsqueeze(1).to_broadcast([128, ntile, 32]), op=ALU.add),
              reads=["ranks", "padstart"], writes=["tmpd"])
        for j in range(2):
            tk.op("dve", lambda e, j=j: e.tensor_tensor(out=tmpd2[:, 0:ntile, :], in0=tmpd[:, 0:ntile, :],
                                                        in1=ohs[:, 0:ntile, j, :], op=ALU.mult), reads=["tmpd", "ohs"],
                  writes=["tmpd2"])
            tk.op("dve", lambda e, j=j: e.tensor_reduce(out=destf[:, j, 0:ntile], in_=tmpd2[:, 0:ntile, :], axis=AX.X,
                                                        op=ALU.add), reads=["tmpd2"], writes=["destf"])
        tk.op("dve", lambda e: e.tensor_copy(out=desti[:, :, 0:ntile], in_=destf[:, :, 0:ntile]), reads=["destf"],
              writes=["desti"])
        tk.op("dve", lambda e: e.tensor_tensor(out=cmp64[:], in0=padend[:].unsqueeze(1).to_broadcast([128, 64, 32]),
                                               in1=thr64[:].unsqueeze(2).to_broadcast([128, 64, 32]), op=ALU.is_le),
              reads=["padend", "thr64"], writes=["cmp64"])
        tk.op("dve", lambda e: e.tensor_reduce(out=blkf[:], in_=cmp64[:], axis=AX.X, op=ALU.add), reads=["cmp64"],
              writes=["blkf"])
        tk.op("dve", lambda e: e.tensor_scalar(out=blkf[:], in0=blkf[:], scalar1=128.0, scalar2=kcoff[:, 0:1],
                                               op0=ALU.mult, op1=ALU.add), reads=["blkf", "kcoff"], writes=["blkf"])
        tk.op("dve", lambda e: e.tensor_copy(out=idxw[:], in_=blkf[:]), reads=["blkf"], writes=["idxw"])
        for k in range(ntile):
            par = k % 2
            h2, h2k = h22[par], "h2%d" % par
            tsl = slice(k * 128, (k + 1) * 128)
            tk.dma("sp", lambda e: e.dma_start(out=h2[:], in_=h2d[tsl, :]), reads=["h2d"], writes=[h2k])
            for j in range(2):
                tk.dma("pool", lambda e, j=j: e.indirect_dma_start(
                    out=xs[:, :], out_offset=bass.IndirectOffsetOnAxis(ap=desti[:, j, k:k + 1], axis=0),
                    in_=h2[:, :], in_offset=None), reads=[h2k, "desti"], writes=[("xs", k, j)])
        tk.flush()
        real.barrier()
    xs_keys = [("xs", k, j) for k in range(ntile) for j in range(2)]

    with ExitStack() as esB:
        sbb = mk(esB, "b_")
        W = []
        for i in range(2):
            wg2 = sbb("wg%d" % i, [128, 4096]); wu2 = sbb("wu%d" % i, [128, 4096]); wd2 = sbb("wd%d" % i, [128, 4096])
            W.append((wg2, wu2, wd2))
        xsb2 = [sbb("xsb%d" % i, [128, 1024]) for i in range(2)]
        xsT2 = [sbb("xsT%d" % i, [128, 8, 128]) for i in range(2)]
        gsil2 = [sbb("gsil%d" % i, [128, 512]) for i in range(2)]
        hid2 = [sbb("hid%d" % i, [128, 512]) for i in range(2)]
        hidT2 = [sbb("hidT%d" % i, [128, 4, 128]) for i in range(2)]
        ysb2 = [sbb("ysb%d" % i, [128, 1024]) for i in range(2)]
        wgd = D["wgate"]; wud = D["wup"]; wdd = D["wdown"]
        breg = nc.gpsimd.to_reg(32 * 128 - 1)
        for b in range(nblk):
            par = b % 2
            wg2, wu2, wd2 = W[par]
            wg = wg2[:].rearrange("p (a b) -> p a b", b=512)
            wu = wu2[:].rearrange("p (a b) -> p a b", b=512)
            wd = wd2[:].rearrange("p (a b) -> p a b", b=1024)
            xsb, xsT, gsil, hid, hidT, ysb = xsb2[par], xsT2[par], gsil2[par], hid2[par], hidT2[par], ysb2[par]
            sfx = str(par)
            for (dst, dkey, src) in ((wg2, "wg" + sfx, wgd), (wu2, "wu" + sfx, wud), (wd2, "wd" + sfx, wdd)):
                tk.dma("pool", lambda e, dst=dst, src=src: e.indirect_dma_start(
                    out=dst[:, :].bitcast(F32R), out_offset=None, in_=src[:, :].bitcast(F32R),
                    in_offset=bass.IndirectOffsetOnAxis(ap=idxw[:, b:b + 1], axis=0), bounds_check=breg,
                    oob_is_err=False), reads=["idxw"], writes=[dkey], cost=8.0)
            tk.dma("sp", lambda e: e.dma_start(out=xsb[:], in_=xs[b * 128:(b + 1) * 128, :]),
                   reads=xs_keys if b == 0 else [], writes=["xsb" + sfx])
            for half in range(2):
                pn, pt = pp.next()
                tk.ops("pe", [lambda e, j=j, pt=pt, half=half: e.transpose(
                    pt[:, j * 128:(j + 1) * 128], xsb[:, (half * 4 + j) * 128:(half * 4 + j + 1) * 128], ident[:])
                    for j in range(4)], reads=["xsb" + sfx, "ident"], writes=[pn])
                tk.op("act", lambda e, pt=pt, half=half: e.copy(
                    out=xsT[:, half * 4:(half + 1) * 4, :].rearrange("p a b -> p (a b)").bitcast(F32R), in_=pt[:, :]),
                    reads=[pn], writes=["xsT" + sfx])
            pg, ptg = pp.next()
            tk.ops("pe", [lambda e, kc=kc: e.matmul(ptg[:, :], lhsT=xsT[:, kc, :].bitcast(F32R),
                                                   rhs=wg[:, kc, :].bitcast(F32R), start=(kc == 0), stop=(kc == 7))
                          for kc in range(8)], reads=["xsT" + sfx, "wg" + sfx], writes=[pg], cost=3.0)
            pu, ptu = pp.next()
            tk.ops("pe", [lambda e, kc=kc: e.matmul(ptu[:, :], lhsT=xsT[:, kc, :].bitcast(F32R),
                                                   rhs=wu[:, kc, :].bitcast(F32R), start=(kc == 0), stop=(kc == 7))
                          for kc in range(8)], reads=["xsT" + sfx, "wu" + sfx], writes=[pu], cost=3.0)
            tk.op("act", lambda e: e.activation(out=gsil[:], in_=ptg[:, :], func=AF.Silu), reads=[pg],
                  writes=["gsil" + sfx])
            tk.op("dve", lambda e: e.tensor_tensor(out=hid[:], in0=gsil[:], in1=ptu[:, :], op=ALU.mult),
                  reads=["gsil" + sfx, pu], writes=["hid" + sfx])
            pn, pt = pp.next()
            tk.ops("pe", [lambda e, j=j, pt=pt: e.transpose(pt[:, j * 128:(j + 1) * 128],
                                                            hid[:, j * 128:(j + 1) * 128], ident[:])
                          for j in range(4)], reads=["hid" + sfx, "ident"], writes=[pn])
            tk.op("act", lambda e, pt=pt: e.copy(out=hidT[:].rearrange("p a b -> p (a b)").bitcast(F32R),
                                                 in_=pt[:, :]), reads=[pn], writes=["hidT" + sfx])
            for half in range(2):
                pn, pt = pp.next()
                tk.ops("pe", [lambda e, fc=fc, pt=pt, half=half: e.matmul(
                    pt[:, :], lhsT=hidT[:, fc, :].bitcast(F32R),
                    rhs=wd[:, fc, half * 512:(half + 1) * 512].bitcast(F32R), start=(fc == 0), stop=(fc == 3))
                    for fc in range(4)], reads=["hidT" + sfx, "wd" + sfx], writes=[pn], cost=1.5)
                if half == 0:
                    tk.op("act", lambda e, pt=pt: e.copy(out=ysb[:, 0:512], in_=pt[:, :]), reads=[pn],
                          writes=["ysb" + sfx])
                else:
                    tk.op("dve", lambda e, pt=pt: e.tensor_copy(out=ysb[:, 512:1024], in_=pt[:, :]), reads=[pn],
                          writes=["ysb" + sfx])
            tk.dma("sp", lambda e: e.dma_start(out=ys[b * 128:(b + 1) * 128, :], in_=ysb[:]), reads=["ysb" + sfx],
                   writes=[("ys", b)])
            if b % 16 == 15:
                tk.flush()
        tk.flush()
        real.barrier()
    ys_keys = [("ys", b) for b in range(nblk)]

    with ExitStack() as esC:
        sc_ = mk(esC, "c_")
        y02 = [sc_("y0%d" % i, [128, 1024]) for i in range(2)]
        y12 = [sc_("y1%d" % i, [128, 1024]) for i in range(2)]
        for k in range(ntile):
            par = k % 2
            x1, rr, xt, y0, y1 = x12[par], rr2[par], xt2[par], y02[par], y12[par]
            x1k, rrk, xtk, y0k, y1k = "x1%d" % par, "rr%d" % par, "xt%d" % par, "y0%d" % par, "y1%d" % par
            tsl = slice(k * 128, (k + 1) * 128)
            tk.dma("sp", lambda e: e.dma_start(out=x1[:], in_=x1d[tsl, :]), reads=["x1d"], writes=[x1k])
            for j, (yt_, yk) in enumerate(((y0, y0k), (y1, y1k))):
                tk.dma("pool", lambda e, j=j, yt_=yt_: e.indirect_dma_start(
                    out=yt_[:, :], out_offset=None, in_=ys[:, :],
                    in_offset=bass.IndirectOffsetOnAxis(ap=desti[:, j, k:k + 1], axis=0)),
                    reads=(ys_keys if k == 0 else []) + ["desti"], writes=[yk])
            tk.op("dve", lambda e: e.tensor_scalar(out=y0[:], in0=y0[:], scalar1=gts[:, k, 0:1], scalar2=None,
                                                   op0=ALU.mult), reads=[y0k, "gts"], writes=[y0k])
            tk.op("dve", lambda e: e.scalar_tensor_tensor(out=y0[:], in0=y1[:], scalar=gts[:, k, 1:2], in1=y0[:],
                                                          op0=ALU.mult, op1=ALU.add), reads=[y0k, y1k, "gts"],
                  writes=[y0k])
            tk.op("dve", lambda e: e.tensor_tensor(out=y0[:], in0=y0[:], in1=modb[:, 3, :], op=ALU.mult),
                  reads=[y0k, "modb"], writes=[y0k])
            tk.op("dve", lambda e: e.scalar_tensor_tensor(out=rr[:], in0=x1[:], scalar=ALPHA, in1=y0[:], op0=ALU.mult,
                                                          op1=ALU.add), reads=[x1k, y0k], writes=[rrk])
            layer_norm(rr, rrk, xt, xtk, 2)
            tk.dma("sp", lambda e: e.dma_start(out=out[tsl, :], in_=xt[:]), reads=[xtk], writes=["out"])
            if k % 8 == 7:
                tk.flush()
        tk.flush()


def consts2():
    p = np.arange(128)
    tri = (p[:, None] < p[None, :]).astype(np.float32)
    thr16 = np.broadcast_to((128.0 * np.arange(16, dtype=np.float32))[None, :], (128, 16))
    thr64 = np.broadcast_to((128.0 * np.arange(64, dtype=np.float32))[None, :], (128, 64))
    return {"c_ident": np.eye(128, dtype=np.float32), "c_tri": tri, "c_iota": p.astype(np.float32)[:, None].copy(),
            "c_thr16": np.ascontiguousarray(thr16), "c_thr64": np.ascontiguousarray(thr64)}


def build_phase2(ntile=NT2, nblk=NBLK):
    nc = bass.Bass("TRN2", target_bir_lowering=False)
    D = {}

    def din(name, shape, dt=F32):
        D[name] = nc.dram_tensor(name, shape, dt, kind="ExternalInput").ap()

    din("mixTf", [1024, 2048], BF16); din("x2", [2048, 1024]); din("cT", [128, 8])
    din("wada2", [128, 32, 8, 128]); din("bada2", [4096]); din("wo", [1024, 1024]); din("lnp", [4, 1024])
    din("wr", [1024, 36]); din("br", [36])
    din("wgate", [4096, 4096]); din("wup", [4096, 4096]); din("wdown", [4096, 4096])
    din("c_ident", [128, 128]); din("c_tri", [128, 128]); din("c_iota", [128, 1]); din("c_thr16", [128, 16])
    din("c_thr64", [128, 64])
    for nm in ("x1d", "h2d"):
        D[nm] = nc.dram_tensor(nm, [2048, 1024], F32, kind="Internal").ap()
    for nm in ("xs", "ys"):
        D[nm] = nc.dram_tensor(nm, [NBLK * 128, 1024], F32, kind="Internal").ap()
    D["out"] = nc.dram_tensor("out", [2048, 1024], F32, kind="ExternalOutput").ap()
    with ExitStack() as es:
        tk = Trk(nc, es)
        phase2(nc, tk, es, D, ntile=ntile, nblk=nblk)
        tk.finish("sp")
    return nc


def phase2_shared_inputs(inp):
    l = 0
    wg = inp["w_gate"][l].reshape(32, 8, 128, 512).transpose(0, 2, 1, 3).reshape(4096, 4096)
    wu = inp["w_up"][l].reshape(32, 8, 128, 512).transpose(0, 2, 1, 3).reshape(4096, 4096)
    wd = inp["w_down"][l].reshape(32, 4, 128, 1024).transpose(0, 2, 1, 3).reshape(4096, 4096)
    d = {"wada2": np.ascontiguousarray(inp["w_ada"][l][:, 2048:6144].reshape(8, 128, 32, 128).transpose(1, 2, 0, 3)),
         "bada2": np.ascontiguousarray(inp["b_ada"][l][2048:6144]),
         "wo": np.ascontiguousarray(inp["w_o"][l]),
         "lnp": np.ascontiguousarray(np.stack([inp["ln1_g"][l], inp["ln1_b"][l], inp["ln2_g"][l], inp["ln2_b"][l]])),
         "wr": np.ascontiguousarray(np.concatenate([inp["w_router_group"][l], inp["w_router_expert"][l]], 1)),
         "br": np.ascontiguousarray(np.concatenate([inp["b_router_group"][l], inp["b_router_expert"][l]])),
         "wgate": np.ascontiguousarray(wg), "wup": np.ascontiguousarray(wu), "wdown": np.ascontiguousarray(wd)}
    d.update(consts2())
    return d


def phase2_inputs(inp, core, shared, mixTf):
    b, q = core // 4, core % 4
    d = dict(shared)
    d["x2"] = np.ascontiguousarray(inp["x"][b, q * 2048:(q + 1) * 2048])
    d["cT"] = np.ascontiguousarray(inp["c"][b].reshape(8, 128).T)
    d["mixTf"] = mixTf
    return d


def build_fused():
    nc = bass.Bass("TRN2", target_bir_lowering=False)
    D = {}

    def din(name, shape, dt=F32):
        D[name] = nc.dram_tensor(name, shape, dt, kind="ExternalInput").ap()

    din("x", [D_MODEL, SEQ]); din("cT", [128, 8]); din("pos", [SEQ], I32)
    din("wada1", [128, 8, 8, 256]); din("bada1", [128, 16]); din("win", [128, 5, 8, 256])
    din("convw", [128, 3, 4]); din("pvec", [128, 4])
    din("c_ident", [128, 128]); din("c_masks", [128, 3, 128]); din("c_amask", [128, 256]); din("c_invf", [64, 2])
    din("x2", [2048, 1024]); din("qidx", [128, 8], I32)
    din("wada2", [128, 32, 8, 128]); din("bada2", [4096]); din("wo", [1024, 1024]); din("lnp", [4, 1024])
    din("wr", [1024, 36]); din("br", [36])
    din("wgate", [4096, 4096]); din("wup", [4096, 4096]); din("wdown", [4096, 4096])
    din("c_tri", [128, 128]); din("c_iota", [128, 1]); din("c_thr16", [128, 16]); din("c_thr64", [128, 64])
    cin = [nc.dram_tensor("cin%d" % q, [256, 2048], BF16, kind="Internal").ap() for q in range(4)]
    cout = nc.dram_tensor("cout", [4096, 2048], BF16, kind="Internal").ap()
    for nm in ("x1d", "h2d"):
        D[nm] = nc.dram_tensor(nm, [2048, 1024], F32, kind="Internal").ap()
    for nm in ("xs", "ys"):
        D[nm] = nc.dram_tensor(nm, [NBLK * 128, 1024], F32, kind="Internal").ap()
    D["out"] = nc.dram_tensor("out", [2048, 1024], F32, kind="ExternalOutput").ap()
    groups = [[0, 1, 2, 3], [4, 5, 6, 7]]
    with ExitStack() as es0:
        tk = Trk(nc, es0)

        def on_quarter(sc, tk):
            tk.cc(lambda e: e.collective_compute("AllGather", ALU.bypass, replica_groups=groups,
                                                 ins=[cin[sc][:, :]], outs=[cout[sc * 1024:(sc + 1) * 1024, :]]),
                  reads=[("cin_a", sc), ("cin_b", sc)], writes=[("cout", sc)])

        with ExitStack() as es1:
            phase1(nc, tk, es1, D, cin=cin, on_quarter=on_quarter)
        tk.barrier()
        with ExitStack() as es2:
            phase2(nc, tk, es2, D, cout=cout)
        tk.finish("sp")
    return nc


def fused_inputs(inp, core, shared):
    b, q = core // 4, core % 4
    d = dict(shared)
    d.update(phase1_inputs(inp, core))
    d["x2"] = np.ascontiguousarray(inp["x"][b, q * 2048:(q + 1) * 2048])
    kc = np.arange(8)[None, :]
    p = np.arange(128)[:, None]
    row = np.where(kc < 4, kc * 256 + p, (kc - 4) * 256 + 128 + p)
    d["qidx"] = np.ascontiguousarray((q * 1024 + row).astype(np.int32))
    return d


def kernel(**inputs):
    inp = {k: np.asarray(v) for k, v in inputs.items()}
    shared = phase2_shared_inputs(inp)
    nc = build_fused()
    maps = [fused_inputs(inp, core, shared) for core in range(NCORES)]
    res = run_bass_kernel_spmd(nc, maps, core_ids=list(range(NCORES)))
    out = np.zeros((BATCH, SEQ, D_MODEL), np.float32)
    for core in range(NCORES):
        b, q = core // 4, core % 4
        out[b, q * 2048:(q + 1) * 2048] = np.asarray(res.results[core]["out"])
    return out
```

```python
import math
from contextlib import ExitStack

import numpy as np
import concourse.bass as bass
import concourse.mybir as mybir
from concourse.bass_utils import run_bass_kernel_spmd

F32 = mybir.dt.float32
BF16 = mybir.dt.bfloat16
I32 = mybir.dt.int32
U32 = mybir.dt.uint32
F32R = mybir.dt.float32r
AF = mybir.ActivationFunctionType
ALU = mybir.AluOpType
AX = mybir.AxisListType

D_MODEL = 1024
SEQ = 8192
BATCH = 2
NCORES = 8
T = 512
NCH = SEQ // T
ALPHA = 2.0 ** 0.25
LN_EPS = 1e-5
RMS_EPS = 1e-6
ROPE_THETA = 500000.0
NEG = -30000.0
TWO_PI = 2.0 * math.pi


class Trk:
    def __init__(self, nc, es, n_dma_sems=24, same_engine_sync=True):
        self.nc = nc
        self.e = {"pe": nc.tensor, "dve": nc.vector, "act": nc.scalar, "pool": nc.gpsimd, "sp": nc.sync}
        self.sem = {k: es.enter_context(nc.semaphore("sem_" + k)) for k in self.e}
        self.cnt = {k: 0 for k in self.e}
        self.seen = {k: {} for k in self.e}
        self.last_w = {}
        self.reads = {}
        self.dsem = [es.enter_context(nc.semaphore("dsem%d" % i)) for i in range(n_dma_sems)]
        self.dval = [0] * n_dma_sems
        self.dnext = 0
        self.same = same_engine_sync
        self.all_dma_events = []
        self.ccsem = es.enter_context(nc.semaphore("ccsem"))
        self.ccval = 0

    def _wait(self, eng, ev):
        kind, src, n = ev
        key = (kind, src)
        if self.seen[eng].get(key, 0) >= n:
            return
        if kind == "e":
            if src == eng and not self.same:
                return
            self.e[eng].wait_ge(self.sem[src], n)
        elif kind == "c":
            self.e[eng].wait_ge(self.ccsem, n)
        else:
            self.e[eng].wait_ge(self.dsem[src], n)
        self.seen[eng][key] = n

    def _deps(self, eng, reads, writes):
        deps = []
        for r in reads:
            ev = self.last_w.get(r)
            if ev is not None:
                deps.append(ev)
        for w in writes:
            ev = self.last_w.get(w)
            if ev is not None:
                deps.append(ev)
            for ev2 in self.reads.get(w, {}).values():
                deps.append(ev2)
        for ev in deps:
            self._wait(eng, ev)

    def _record(self, ev, reads, writes):
        for r in reads:
            self.reads.setdefault(r, {})[(ev[0], ev[1])] = ev
        for w in writes:
            self.last_w[w] = ev
            self.reads[w] = {}

    def op(self, eng, fn, reads=(), writes=()):
        self._deps(eng, reads, writes)
        inst = fn(self.e[eng])
        self.cnt[eng] += 1
        inst.then_inc(self.sem[eng], 1)
        ev = ("e", eng, self.cnt[eng])
        self._record(ev, reads, writes)
        return ev

    def ops(self, eng, fns, reads=(), writes=()):
        self._deps(eng, reads, writes)
        inst = None
        for fn in fns:
            inst = fn(self.e[eng])
        self.cnt[eng] += 1
        inst.then_inc(self.sem[eng], 1)
        ev = ("e", eng, self.cnt[eng])
        self._record(ev, reads, writes)
        return ev

    def dma(self, eng, fn, reads=(), writes=()):
        self._deps(eng, reads, writes)
        i = self.dnext
        self.dnext = (self.dnext + 1) % len(self.dsem)
        if self.dval[i] > 0:
            self._wait(eng, ("d", i, self.dval[i]))
        inst = fn(self.e[eng])
        self.dval[i] += 16
        inst.then_inc(self.dsem[i], 16)
        ev = ("d", i, self.dval[i])
        self._record(ev, reads, writes)
        self.all_dma_events.append(ev)
        return ev

    def cc(self, fn, reads=(), writes=()):
        eng = "pool"
        self._deps(eng, reads, writes)
        if self.ccsem is None:
            raise RuntimeError("no cc semaphore")
        if self.ccval > 0:
            self._wait(eng, ("c", 0, self.ccval))
        inst = fn(self.e[eng])
        self.ccval += 1
        inst.then_inc(self.ccsem, 1)
        ev = ("c", 0, self.ccval)
        self._record(ev, reads, writes)
        return ev

    def barrier(self):
        for eng in self.e:
            for i, v in enumerate(self.dval):
                if v > 0:
                    self._wait(eng, ("d", i, v))
            if self.ccval > 0:
                self._wait(eng, ("c", 0, self.ccval))
            for k, n in self.cnt.items():
                if n > 0 and k != eng:
                    self._wait(eng, ("e", k, n))

    def finish(self, eng="sp"):
        for i, v in enumerate(self.dval):
            if v > 0:
                self._wait(eng, ("d", i, v))
        if self.ccval > 0:
            self._wait(eng, ("c", 0, self.ccval))
        for k, n in self.cnt.items():
            if n > 0 and k != eng:
                self._wait(eng, ("e", k, n))


class _Proxy:
    def __init__(self):
        self.calls = []

    def __getattr__(self, name):
        def f(*a, **k):
            self.calls.append((name, a, k))
            return None
        return f


def _freeze(fn):
    p = _Proxy()
    fn(p)
    assert len(p.calls) == 1
    name, a, k = p.calls[0]
    return lambda e: getattr(e, name)(*a, **k)


class Rec:
    HOP = 0.8

    def __init__(self, tk):
        self.tk = tk
        self.L = []

    def op(self, eng, fn, reads=(), writes=(), cost=None):
        self.L.append(("op", eng, _freeze(fn), tuple(reads), tuple(writes), cost if cost is not None else (0.3 if eng == "pe" else 0.6)))

    def ops(self, eng, fns, reads=(), writes=(), cost=None):
        fns = [_freeze(f) for f in fns]
        self.L.append(("ops", eng, fns, tuple(reads), tuple(writes), cost if cost is not None else 0.2 * len(fns)))

    def dma(self, eng, fn, reads=(), writes=(), cost=None):
        self.L.append(("dma", eng, _freeze(fn), tuple(reads), tuple(writes), cost if cost is not None else 2.0))

    def cc(self, fn, reads=(), writes=(), cost=None):
        self.L.append(("cc", "pool", _freeze(fn), tuple(reads), tuple(writes), 5.0))

    def flush(self):
        L = self.L
        self.L = []
        n = len(L)
        preds = [set() for _ in range(n)]
        last_w = {}
        readers = {}
        for i, (kind, eng, fn, reads, writes, cost) in enumerate(L):
            for r in reads:
                if r in last_w:
                    preds[i].add(last_w[r])
            for w in writes:
                if w in last_w:
                    preds[i].add(last_w[w])
                for j in readers.get(w, ()):
                    preds[i].add(j)
            for r in reads:
                readers.setdefault(r, []).append(i)
            for w in writes:
                last_w[w] = i
                readers[w] = []
            preds[i].discard(i)
        succs = [[] for _ in range(n)]
        npred = [len(p) for p in preds]
        for i in range(n):
            for j in preds[i]:
                succs[j].append(i)
        efree = {}
        finish = [0.0] * n
        ready_t = [0.0] * n
        ready = [i for i in range(n) if npred[i] == 0]
        order = []
        import heapq
        while ready:
            best = None
            bkey = None
            for i in ready:
                kind, eng, fn, reads, writes, cost = L[i]
                q = eng if kind in ("op", "ops") else ("q_" + eng)
                st = max(efree.get(q, 0.0), ready_t[i])
                key = (st, i)
                if bkey is None or key < bkey:
                    bkey = key
                    best = i
            i = best
            ready.remove(i)
            kind, eng, fn, reads, writes, cost = L[i]
            q = eng if kind in ("op", "ops") else ("q_" + eng)
            st = bkey[0]
            if kind in ("op", "ops"):
                efree[q] = st + cost
                finish[i] = st + cost
            else:
                efree[q] = st + 0.1
                finish[i] = st + cost
            order.append(i)
            for j in succs[i]:
                npred[j] -= 1
                same = (L[j][1] == eng and L[j][0] in ("op", "ops") and kind in ("op", "ops"))
                ready_t[j] = max(ready_t[j], finish[i] + (0.0 if same else self.HOP))
                if npred[j] == 0:
                    ready.append(j)
        assert len(order) == n
        for i in order:
            kind, eng, fn, reads, writes, cost = L[i]
            if kind == "op":
                self.tk.op(eng, fn, reads, writes)
            elif kind == "ops":
                self.tk.ops(eng, fn, reads, writes)
            elif kind == "dma":
                self.tk.dma(eng, fn, reads, writes)
            else:
                self.tk.cc(fn, reads, writes)


class PsumPool:
    _uid = [0]

    def __init__(self, nc, es, names):
        PsumPool._uid[0] += 1
        u = PsumPool._uid[0]
        self.t = {n: es.enter_context(nc.psum_tensor("ps%d_%s" % (u, n), [128, 512], F32)) for n in names}
        self.rot = [n for n in names if n.startswith("r")]
        self.i = 0

    def next(self):
        n = self.rot[self.i]
        self.i = (self.i + 1) % len(self.rot)
        return n, self.t[n]


def phase1(nc, tk, es, D, nch=NCH, cin=None, on_quarter=None):
    def sb(name, shape, dt=F32):
        return es.enter_context(nc.sbuf_tensor("s_" + name, shape, dt))

    def K(name, c):
        return (name, c % 8)

    ppall = PsumPool(nc, es, ["rb0", "rb1", "rb2", "rb3", "ra0", "ra1", "scan", "ot"])
    PS = ppall.t
    real_tk = tk
    tk = Rec(real_tk)

    class _Sub:
        def __init__(self, names):
            self.rot = names
            self.i = 0

        def next(self):
            n = self.rot[self.i]
            self.i = (self.i + 1) % len(self.rot)
            return n, PS[n]

    pp = _Sub(["rb0", "rb1", "rb2", "rb3"])
    ppa = _Sub(["ra0", "ra1"])

    ident = sb("ident", [128, 128])
    ident_b = sb("ident_b", [128, 128], BF16)
    ones_f = sb("ones_f", [128, 128])
    ones_b = sb("ones_b", [128, 128], BF16)
    mus_t = sb("mus_neg", [128, 128]); mui_t = sb("mu_inc", [128, 128]); mls_t = sb("mls_neg", [128, 128])
    mus_neg = mus_t[:].unsqueeze(1).to_broadcast([128, 4, 128])
    mu_inc = mui_t[:].unsqueeze(1).to_broadcast([128, 4, 128])
    mls_neg = mls_t[:].unsqueeze(1).to_broadcast([128, 4, 128])
    ident4 = ident[:].unsqueeze(1).to_broadcast([128, 4, 128])
    amask_f = sb("amask_f", [128, 256])
    amask = sb("amask", [128, 256], BF16)
    scanm = sb("scanm", [128, T])
    invf = sb("invf", [64, 2])
    win_b = sb("win_b", [128, 8, 1280], BF16)
    xtok = sb("xtok", [128, 8, T])
    wstage = [xtok[:, 4 * i:4 * i + 4, :].rearrange("p a (h c) -> p (a h) c", c=256) for i in range(2)]
    wkeys = ["xtok_a", "xtok_b"]
    sc1 = sb("sc1", [128, 8])
    sh1 = sb("sh1", [128, 8])
    convw = sb("convw", [128, 3, 4])
    pvec = sb("pvec", [128, 4])
    nalog = sb("nalog", [128, 1])
    cT = sb("cT", [128, 8])
    scT = sb("scT", [128, 8, 2])
    bada = sb("bada", [128, 16])
    mod1 = sb("mod1", [128, 16])

    tk.dma("sp", lambda e: e.dma_start(out=ident[:], in_=D["c_ident"][:, :]), writes=["ident"])
    tk.dma("sp", lambda e: e.dma_start(out=amask_f[:], in_=D["c_amask"][:, :]), writes=["amask_f"])
    tk.dma("sp", lambda e: e.dma_start(out=invf[:], in_=D["c_invf"][:, :]), writes=["invf"])
    tk.dma("sp", lambda e: e.dma_start(out=convw[:], in_=D["convw"][:, :, :]), writes=["convw"])
    tk.dma("sp", lambda e: e.dma_start(out=pvec[:], in_=D["pvec"][:, :]), writes=["pvec"])
    tk.dma("sp", lambda e: e.dma_start(out=cT[:], in_=D["cT"][:, :]), writes=["cT"])
    tk.dma("sp", lambda e: e.dma_start(out=bada[:], in_=D["bada1"][:, :]), writes=["bada"])
    for q, (nm, tl) in enumerate([("mus", mus_t), ("mui", mui_t), ("mls", mls_t)]):
        tk.dma("sp", lambda e, tl=tl, q=q: e.dma_start(out=tl[:], in_=D["c_masks"][:, q, :]), writes=[nm])
    tk.op("dve", lambda e: e.tensor_copy(out=ident_b[:], in_=ident[:]), reads=["ident"], writes=["ident_b"])
    tk.op("dve", lambda e: e.tensor_copy(out=amask[:], in_=amask_f[:]), reads=["amask_f"], writes=["amask"])
    tk.op("pool", lambda e: e.memset(ones_f[:], 1.0), writes=["ones_f"])
    tk.op("pool", lambda e: e.memset(ones_b[:], 1.0), writes=["ones_b"])
    tk.op("pool", lambda e: e.memset(scanm[:], 1.0), writes=["scanm"])
    tk.op("pool", lambda e: e.memset(scanm[:, 0:T:64], 0.0), writes=["scanm"])
    tk.op("act", lambda e: e.activation(out=nalog[:], in_=pvec[:, 0:1], func=AF.Exp), reads=["pvec"],
          writes=["nalog"])
    tk.op("dve", lambda e: e.tensor_scalar(out=nalog[:], in0=nalog[:], scalar1=-1.0, scalar2=None, op0=ALU.mult),
          reads=["nalog"], writes=["nalog"])

    for j in range(5):
        st = wstage[j % 2]
        key = wkeys[j % 2]
        tk.dma("sp" if j % 2 == 0 else "pool", lambda e, st=st, j=j: e.dma_start(out=st, in_=D["win"][:, j, :, :]),
               writes=[key])
        tk.op("dve" if j % 2 == 0 else "act", (lambda e, st=st, j=j: e.tensor_copy(out=win_b[:, :, j * 256:(j + 1) * 256], in_=st))
              if j % 2 == 0 else (lambda e, st=st, j=j: e.copy(out=win_b[:, :, j * 256:(j + 1) * 256], in_=st)),
              reads=[key], writes=[("win_b", j)])
    for c0 in (7 * 128, 7 * 128 + 32):
        tk.op("dve", lambda e, c0=c0: e.tensor_scalar(out=win_b[:, :, c0:c0 + 16], in0=win_b[:, :, c0:c0 + 16],
                                                       scalar1=-1.0, scalar2=None, op0=ALU.mult),
              reads=[("win_b", 3)], writes=[("win_b", 3)])

    tk.op("act", lambda e: e.activation(out=scT[:, :, 0], in_=cT[:], func=AF.Silu), reads=["cT"], writes=["scT"])
    tk.op("act", lambda e: e.activation(out=scT[:, :, 1], in_=cT[:], func=AF.Silu), reads=["cT"], writes=["scT"])
    for j in range(8):
        st = wstage[j % 2]
        key = wkeys[j % 2]
        tk.dma("sp" if j % 2 == 0 else "pool", lambda e, st=st, j=j: e.dma_start(out=st, in_=D["wada1"][:, j, :, :]),
               writes=[key])
        for fh in range(2):
            fc = 2 * j + fh
            pn, pt = pp.next()
            tk.ops("pe", [lambda e, st=st, kc=kc, pt=pt, fh=fh: e.matmul(
                pt[:, 0:2], lhsT=st[:, kc, fh * 128:(fh + 1) * 128], rhs=scT[:, kc, :], start=(kc == 0), stop=(kc == 7))
                for kc in range(8)], reads=[key, "scT"], writes=[pn])
            tk.op("dve", lambda e, pt=pt, fc=fc: e.tensor_tensor(out=mod1[:, fc:fc + 1], in0=pt[:, 0:1],
                                                                  in1=bada[:, fc:fc + 1], op=ALU.add),
                  reads=[pn, "bada"], writes=["mod1"])
    tk.op("dve", lambda e: e.tensor_copy(out=sh1[:], in_=mod1[:, 0:8]), reads=["mod1"], writes=["sh1"])
    tk.op("dve", lambda e: e.tensor_scalar(out=sc1[:], in0=mod1[:, 8:16], scalar1=1.0, scalar2=None, op0=ALU.add),
          reads=["mod1"], writes=["sc1"])

    RING = 4096
    qT = sb("qT", [128, RING], BF16)
    kT = sb("kT", [128, RING], BF16)
    vT = sb("vT", [128, RING], BF16)
    vtt = sb("vtt", [128, 4, 128], BF16)
    acc = sb("acc", [128, 2, 2048])
    S = [sb("S%d" % i, [128, 128]) for i in range(2)]
    tk.op("pool", lambda e: e.memset(S[0][:], 0.0), writes=["S0"])

    hT = [sb("hT%d" % i, [128, 8, T], BF16) for i in range(2)]
    craw = [sb("craw%d" % g, [128, T + 3]) for g in range(3)]
    cacc = sb("cacc", [128, T])
    qs = sb("qs", [128, T]); ks = sb("ks", [128, T]); vs = sb("vs", [128, T])
    zs2 = [sb("zs%d" % i, [128, T]) for i in range(2)]
    sq = sb("sq", [128, T]); rn = sb("rn", [128, T])
    p1 = sb("p1", [128, T]); p2 = sb("p2", [128, T]); p3 = sb("p3", [128, T])
    osq = p1; rs = p2; fsq = p1; fr = p2; oa = p3; fo = p3
    qn = sb("qn", [128, T]); kn = sb("kn", [128, T])
    betab = sb("betab", [128, T]); gb = sb("gb", [128, T]); gcb = sb("gcb", [128, T])
    egcb2 = [sb("egcb%d" % i, [128, T]) for i in range(2)]; eglb = sb("eglb", [128, T]); begb = gb
    cols = sb("cols", [128, 5, 4])
    Kbe = sb("Kbe", [128, 4, 128]); Kd2 = [sb("Kd%d" % i, [128, 4, 128]) for i in range(2)]
    Vb = sb("Vb", [128, 4, 128])
    tdf = sb("tdf", [128, 4, 128]); DT = sb("DT", [128, 4, 128]); Dm = sb("Dm", [128, 4, 128])
    E1 = tdf; QKT2 = [sb("QKT%d" % i, [128, 4, 128]) for i in range(2)]
    Bm = [sb("Bm%d" % i, [128, 4, 128]) for i in range(2)]
    BTm = [sb("BTm%d" % i, [128, 4, 128]) for i in range(2)]
    Pm = [sb("Pm%d" % i, [128, 4, 128]) for i in range(2)]
    U2 = [sb("U%d" % i, [128, 4, 128]) for i in range(2)]; WT2 = [sb("WT%d" % i, [128, T]) for i in range(2)]
    QdT2 = [sb("QdT%d" % i, [128, T]) for i in range(2)]
    vnew = sb("vnew", [128, 2, 128])
    oab = sb("oab", [128, T], BF16)
    posi = sb("posi", [64, T], I32); ra = sb("ra", [64, T]); rb = sb("rb", [64, T])
    ri = sb("ri", [64, T], I32)
    sincos = sb("sincos", [64, T])
    rt1 = sb("rt1", [32, T]); rt2 = sb("rt2", [32, T])
    pTt = [sb("pT%d" % i, [128, 256], BF16) for i in range(2)]
    fob = sb("fob", [128, T], BF16)

    x = D["x"]
    mixT = D.get("mixT")
    pos = D["pos"]

    def load_x(c):
        for i in range(2):
            tk.dma("sp", lambda e, i=i: e.dma_start(
                out=xtok[:, 4 * i:4 * i + 4, :],
                in_=x[i * 512:(i + 1) * 512, c * T:(c + 1) * T].rearrange("(kc p) t -> p kc t", p=128)),
                writes=[wkeys[i]])

    def stage_a(c):
        h = hT[c % 2]
        hk = "hT%d" % (c % 2)
        for kc in range(8):
            tk.op("act", lambda e, kc=kc: e.activation(out=h[:, kc, :], in_=xtok[:, kc, :], func=AF.Identity,
                                                       bias=sh1[:, kc:kc + 1], scale=sc1[:, kc:kc + 1]),
                  reads=[wkeys[kc // 4], "sh1", "sc1"], writes=[hk])

    def proj(c, m):
        h = hT[c % 2]
        hk = "hT%d" % (c % 2)
        pn, pt = pp.next()
        tk.ops("pe", [lambda e, kc=kc, pt=pt: e.matmul(pt[:, :], lhsT=win_b[:, kc, m * 128:(m + 1) * 128],
                                                      rhs=h[:, kc, :], start=(kc == 0), stop=(kc == 7))
                      for kc in range(8)], reads=[hk, ("win_b", m // 2)], writes=[pn])
        return pn, pt

    def rope_tables(c):
        tk.dma("sp", lambda e: e.dma_start(out=posi[:], in_=pos[c * T:(c + 1) * T].partition_broadcast(64)),
               writes=["posi"])
        tk.op("dve", lambda e: e.tensor_copy(out=ra[:], in_=posi[:]), reads=["posi"], writes=["ra"])
        tk.op("dve", lambda e: e.tensor_scalar(out=ra[:], in0=ra[:], scalar1=invf[:, 0:1], scalar2=invf[:, 1:2],
                                               op0=ALU.mult, op1=ALU.add), reads=["ra", "invf"], writes=["ra"])
        tk.op("dve", lambda e: e.tensor_scalar(out=ri[:], in0=ra[:], scalar1=1.0 / TWO_PI, scalar2=None,
                                               op0=ALU.mult), reads=["ra"], writes=["ri"])
        tk.op("dve", lambda e: e.tensor_copy(out=rb[:], in_=ri[:]), reads=["ri"], writes=["rb"])
        tk.op("dve", lambda e: e.scalar_tensor_tensor(out=ra[:], in0=rb[:], scalar=-TWO_PI, in1=ra[:], op0=ALU.mult,
                                                      op1=ALU.add), reads=["ra", "rb"], writes=["ra"])
        tk.op("dve", lambda e: e.tensor_scalar(out=rb[:], in0=ra[:], scalar1=math.pi, scalar2=-TWO_PI,
                                               op0=ALU.is_gt, op1=ALU.mult), reads=["ra"], writes=["rb"])
        tk.op("dve", lambda e: e.tensor_tensor(out=ra[:], in0=ra[:], in1=rb[:], op=ALU.add),
              reads=["ra", "rb"], writes=["ra"])
        tk.op("dve", lambda e: e.tensor_scalar(out=rb[:], in0=ra[:], scalar1=-math.pi, scalar2=TWO_PI,
                                               op0=ALU.is_lt, op1=ALU.mult), reads=["ra"], writes=["rb"])
        tk.op("dve", lambda e: e.tensor_tensor(out=ra[:], in0=ra[:], in1=rb[:], op=ALU.add),
              reads=["ra", "rb"], writes=["ra"])
        tk.op("act", lambda e: e.activation(out=sincos[:], in_=ra[:], func=AF.Sin), reads=["ra"], writes=["sincos"])

    def prep_main(c):
        par = c % 2
        U, WT, QdT, QKT, Kd, egcb, zs = U2[par], WT2[par], QdT2[par], QKT2[par], Kd2[par], egcb2[par], zs2[par]
        uk, wk, qdk, qkk, kdk, ek, zk = ("U%d" % par, "WT%d" % par, "QdT%d" % par, "QKT%d" % par, "Kd%d" % par,
                                         "egcb%d" % par, "zs%d" % par)
        stage_a(c)
        yield
        if c + 1 < nch:
            load_x(c + 1)
        yield
        for g, dst, dk_ in ((0, qs, "qs"), (1, ks, "ks"), (2, vs, "vs")):
            pn, pt = proj(c, g)
            ck = "craw%d" % g
            cr = craw[g]
            if c > 0:
                tk.op("dve", lambda e, cr=cr: e.tensor_copy(out=cr[:, 0:3], in_=cr[:, T:T + 3]), reads=[ck],
                      writes=[ck])
            else:
                tk.op("pool", lambda e, cr=cr: e.memset(cr[:, 0:3], 0.0), writes=[ck])
            tk.op("act", lambda e, pt=pt, cr=cr: e.copy(out=cr[:, 3:T + 3], in_=pt[:, :]), reads=[pn],
                  writes=[ck])
            tk.op("dve", lambda e, cr=cr, g=g: e.tensor_scalar(out=cacc[:], in0=cr[:, 0:T],
                                                               scalar1=convw[:, g, 0:1], scalar2=None, op0=ALU.mult),
                  reads=[ck, "convw"], writes=["cacc"])
            for j in range(1, 4):
                tk.op("dve", lambda e, cr=cr, g=g, j=j: e.scalar_tensor_tensor(
                    out=cacc[:], in0=cr[:, j:j + T], scalar=convw[:, g, j:j + 1], in1=cacc[:],
                    op0=ALU.mult, op1=ALU.add), reads=[ck, "convw", "cacc"], writes=["cacc"])
            tk.op("act", lambda e, dst=dst: e.activation(out=dst[:], in_=cacc[:], func=AF.Silu), reads=["cacc"],
                  writes=[dk_])
            yield
        yield
        pn, pt = proj(c, 3)
        tk.op("act", lambda e, pt=pt: e.activation(out=zs[:], in_=pt[:, :], func=AF.Silu), reads=[pn], writes=[zk])
        yield
        for src, sk, dst, dk_, scale in ((qs, "qs", qn, "qn", 128.0 ** -0.5), (ks, "ks", kn, "kn", 1.0)):
            tk.op("act", lambda e, src=src: e.activation(out=sq[:], in_=src[:], func=AF.Square), reads=[sk],
                  writes=["sq"])
            pn, pt = pp.next()
            tk.op("pe", lambda e, pt=pt: e.matmul(pt[:, :], lhsT=ones_f[:], rhs=sq[:], start=True, stop=True),
                  reads=["sq", "ones_f"], writes=[pn])
            tk.op("act", lambda e, pt=pt: e.activation(out=rn[:], in_=pt[:, :], func=AF.Ln, bias=RMS_EPS_AP[:, 0:1],
                                                       scale=1.0), reads=[pn, "epsc"], writes=["rn"])
            tk.op("act", lambda e: e.activation(out=rn[:], in_=rn[:], func=AF.Exp, scale=-0.5), reads=["rn"],
                  writes=["rn"])
            tk.op("dve", lambda e, src=src, dst=dst, scale=scale: e.scalar_tensor_tensor(
                out=dst[:].bitcast(F32R), in0=src[:], scalar=scale, in1=rn[:], op0=ALU.mult, op1=ALU.mult),
                reads=[sk, "rn"], writes=[dk_])
        yield
        pn, pt = proj(c, 8)
        tk.op("act", lambda e, pt=pt: e.activation(out=betab[:], in_=pt[:, :], func=AF.Sigmoid), reads=[pn],
              writes=["betab"])
        pn, pt = proj(c, 9)
        tk.op("act", lambda e, pt=pt: e.activation(out=gb[:], in_=pt[:, :], func=AF.Exp, bias=pvec[:, 1:2], scale=1.0),
              reads=[pn, "pvec"], writes=["gb"])
        tk.op("act", lambda e: e.activation(out=gb[:], in_=gb[:], func=AF.Ln, bias=ONE_AP[:, 0:1], scale=1.0),
              reads=["gb", "epsc"], writes=["gb"])
        tk.op("dve", lambda e: e.tensor_scalar(out=gb[:], in0=gb[:], scalar1=nalog[:, 0:1], scalar2=None, op0=ALU.mult),
              reads=["gb", "nalog"], writes=["gb"])
        tk.op("dve", lambda e: e.tensor_tensor_scan(out=gcb[:], data0=scanm[:], data1=gb[:], initial=0.0,
                                                    op0=ALU.mult, op1=ALU.add), reads=["gb", "scanm"], writes=["gcb"])
        tk.op("act", lambda e: e.activation(out=egcb[:], in_=gcb[:], func=AF.Exp), reads=["gcb"], writes=[ek])
        gc3 = gcb[:].rearrange("p (n t) -> p n t", t=64)
        tk.op("dve", lambda e: e.tensor_tensor(out=eglb[:].rearrange("p (n t) -> p n t", t=64),
                                               in0=gc3[:, :, 63:64].to_broadcast([128, 8, 64]), in1=gc3,
                                               op=ALU.subtract), reads=["gcb"], writes=["eglb"])
        tk.op("act", lambda e: e.activation(out=eglb[:], in_=eglb[:], func=AF.Exp), reads=["eglb"], writes=["eglb"])
        tk.op("dve", lambda e: e.tensor_tensor(out=begb[:], in0=betab[:], in1=egcb[:], op=ALU.mult),
              reads=["betab", ek], writes=["gb"])
        pn, pt = pp.next()
        srcs = [(betab, "betab"), (gcb, "gcb"), (egcb, ek), (eglb, "eglb"), (begb, "gb")]
        tk.ops("pe", [lambda e, q=q, s=s, pt=pt, src=src: e.transpose(
            pt[:, (q * 4 + s) * 16:(q * 4 + s + 1) * 16], src[0:16, s * 128:(s + 1) * 128], ident[0:16, 0:16])
            for q, (src, _) in enumerate(srcs) for s in range(4)],
            reads=[k_ for _, k_ in srcs] + ["ident"], writes=[pn])
        tk.op("dve", lambda e, pt=pt: e.tensor_copy(
            out=cols[:].rearrange("p q s -> p (q s)"),
            in_=pt[:, 0:320].rearrange("p (n w) -> p n w", w=16)[:, :, 0]), reads=[pn], writes=["cols"])

        def colb(q):
            return cols[:, q, :].unsqueeze(2).to_broadcast([128, 4, 128])

        yield
        pn, pt = pp.next()
        tk.ops("pe", [lambda e, s=s, pt=pt: e.transpose(pt[:, s * 128:(s + 1) * 128], kn[:, s * 128:(s + 1) * 128],
                                                        ident[:]) for s in range(4)], reads=["kn", "ident"],
               writes=[pn])
        pt3 = pt[:, :].rearrange("p (s d) -> p s d", d=128)
        tk.op("dve", lambda e, pt3=pt3: e.tensor_tensor(out=Kbe[:].bitcast(F32R), in0=pt3, in1=colb(4), op=ALU.mult),
              reads=[pn, "cols"], writes=["Kbe"])
        tk.op("dve", lambda e, pt3=pt3: e.tensor_tensor(out=Kd[:], in0=pt3, in1=colb(3), op=ALU.mult),
              reads=[pn, "cols"], writes=[kdk])
        pn, pt = pp.next()
        tk.ops("pe", [lambda e, s=s, pt=pt: e.transpose(pt[:, s * 128:(s + 1) * 128], vs[:, s * 128:(s + 1) * 128],
                                                        ident[:]) for s in range(4)], reads=["vs", "ident"],
               writes=[pn])
        pt3 = pt[:, :].rearrange("p (s d) -> p s d", d=128)
        tk.op("dve", lambda e, pt3=pt3: e.tensor_tensor(out=Vb[:].bitcast(F32R), in0=pt3, in1=colb(0), op=ALU.mult),
              reads=[pn, "cols"], writes=["Vb"])
        yield
        gcb3 = gcb[:].rearrange("p (s t) -> p s t", t=128)
        tk.op("dve", lambda e: e.tensor_tensor(out=tdf[:], in0=gcb3, in1=colb(1), op=ALU.subtract),
              reads=["gcb", "cols"], writes=["tdf"])
        tk.op("dve", lambda e: e.tensor_scalar(out=DT[:], in0=tdf[:], scalar1=0.0, scalar2=None, op0=ALU.min),
              reads=["tdf"], writes=["DT"])
        tk.op("act", lambda e: e.activation(out=DT[:], in_=DT[:], func=AF.Exp), reads=["DT"], writes=["DT"])
        tk.op("dve", lambda e: e.tensor_scalar(out=Dm[:], in0=tdf[:], scalar1=0.0, scalar2=None, op0=ALU.max),
              reads=["tdf"], writes=["Dm"])
        tk.op("act", lambda e: e.activation(out=Dm[:], in_=Dm[:], func=AF.Exp, scale=-1.0), reads=["Dm"],
              writes=["Dm"])
        yield
        pnk, ptk = pp.next()
        tk.ops("pe", [lambda e, s=s, ptk=ptk: e.matmul(ptk[:, s * 128:(s + 1) * 128], lhsT=kn[:, s * 128:(s + 1) * 128].bitcast(F32R),
                                                       rhs=kn[:, s * 128:(s + 1) * 128].bitcast(F32R), start=True, stop=True)
                      for s in range(4)], reads=["kn"], writes=[pnk])
        pnq, ptq = pp.next()
        tk.ops("pe", [lambda e, s=s, ptq=ptq: e.matmul(ptq[:, s * 128:(s + 1) * 128], lhsT=kn[:, s * 128:(s + 1) * 128].bitcast(F32R),
                                                       rhs=qn[:, s * 128:(s + 1) * 128].bitcast(F32R), start=True, stop=True)
                      for s in range(4)], reads=["kn", "qn"], writes=[pnq])
        kk3 = ptk[:, :].rearrange("p (s d) -> p s d", d=128)
        qk3 = ptq[:, :].rearrange("p (s d) -> p s d", d=128)
        b3 = betab[:].rearrange("p (s t) -> p s t", t=128)
        tk.op("dve", lambda e: e.tensor_tensor(out=E1[:], in0=DT[:], in1=b3, op=ALU.mult), reads=["DT", "betab"],
              writes=["tdf"])
        tk.op("dve", lambda e: e.tensor_tensor(out=E1[:], in0=E1[:], in1=mus_neg, op=ALU.mult),
              reads=["tdf", "mus"], writes=["tdf"])
        tk.op("dve", lambda e: e.tensor_tensor(out=Bm[0][:].bitcast(F32R), in0=kk3, in1=E1[:], op=ALU.mult), reads=[pnk, "tdf"],
              writes=["Bm0"])
        tk.op("dve", lambda e: e.tensor_tensor(out=DT[:], in0=DT[:], in1=mu_inc, op=ALU.mult),
              reads=["DT", "mui", "tdf"], writes=["DT"])
        tk.op("dve", lambda e: e.tensor_tensor(out=QKT[:], in0=qk3, in1=DT[:], op=ALU.mult), reads=[pnq, "DT"],
              writes=[qkk])
        tk.op("dve", lambda e: e.tensor_tensor(out=Dm[:], in0=Dm[:], in1=mls_neg, op=ALU.mult),
              reads=["Dm", "mls"], writes=["Dm"])
        tk.op("dve", lambda e: e.tensor_tensor(out=Dm[:], in0=Dm[:], in1=colb(0), op=ALU.mult),
              reads=["Dm", "cols"], writes=["Dm"])
        tk.op("dve", lambda e: e.tensor_tensor(out=BTm[0][:].bitcast(F32R), in0=kk3, in1=Dm[:], op=ALU.mult), reads=[pnk, "Dm"],
              writes=["BTm0"])
        tk.op("dve", lambda e: e.tensor_tensor(out=Pm[0][:].bitcast(F32R), in0=Bm[0][:], in1=ident4, op=ALU.add),
              reads=["Bm0", "ident"], writes=["Pm0"])
        yield
        cb = 0
        cp = 0
        for k in range(1, 6):
            nb = 1 - cb
            if k < 5:
                pn1, pt1 = pp.next()
                tk.ops("pe", [lambda e, s=s, pt1=pt1, cb=cb: e.matmul(
                    pt1[:, s * 128:(s + 1) * 128], lhsT=BTm[cb][:, s, :].bitcast(F32R), rhs=Bm[cb][:, s, :].bitcast(F32R), start=True, stop=True)
                    for s in range(4)], reads=["Bm%d" % cb, "BTm%d" % cb], writes=[pn1])
            pn2, pt2 = pp.next()
            tk.ops("pe", [lambda e, s=s, pt2=pt2, cb=cb: e.matmul(
                pt2[:, s * 128:(s + 1) * 128], lhsT=Bm[cb][:, s, :].bitcast(F32R), rhs=BTm[cb][:, s, :].bitcast(F32R), start=True, stop=True)
                for s in range(4)], reads=["Bm%d" % cb, "BTm%d" % cb], writes=[pn2])
            if k < 5:
                tk.op("act", lambda e, pt1=pt1, nb=nb: e.copy(out=Bm[nb][:].rearrange("p s d -> p (s d)").bitcast(F32R), in_=pt1[:, :]),
                      reads=[pn1], writes=["Bm%d" % nb])
            tk.op("dve", lambda e, pt2=pt2, nb=nb: e.tensor_copy(out=BTm[nb][:].rearrange("p s d -> p (s d)").bitcast(F32R),
                                                                 in_=pt2[:, :]), reads=[pn2], writes=["BTm%d" % nb])
            pn3, pt3_ = pp.next()
            tk.ops("pe", [lambda e, s=s, pt3_=pt3_, nb=nb, cp=cp: e.matmul(
                pt3_[:, s * 128:(s + 1) * 128], lhsT=BTm[nb][:, s, :].bitcast(F32R), rhs=Pm[cp][:, s, :].bitcast(F32R), start=True, stop=True)
                for s in range(4)], reads=["BTm%d" % nb, "Pm%d" % cp], writes=[pn3])
            tk.op("dve", lambda e, pt3_=pt3_, cp=cp: e.tensor_tensor(
                out=Pm[1 - cp][:].rearrange("p s d -> p (s d)").bitcast(F32R), in0=Pm[cp][:].rearrange("p s d -> p (s d)"),
                in1=pt3_[:, :], op=ALU.add), reads=[pn3, "Pm%d" % cp], writes=["Pm%d" % (1 - cp)])
            cb = nb
            cp = 1 - cp
            yield
        Pf = Pm[cp]
        pk = "Pm%d" % cp
        yield
        pn, pt = pp.next()
        tk.ops("pe", [lambda e, s=s, pt=pt: e.matmul(pt[:, s * 128:(s + 1) * 128], lhsT=Pf[:, s, :].bitcast(F32R), rhs=Vb[:, s, :].bitcast(F32R),
                                                     start=True, stop=True) for s in range(4)],
               reads=[pk, "Vb"], writes=[pn])
        tk.op("act", lambda e, pt=pt: e.copy(out=U[:].rearrange("p s d -> p (s d)"), in_=pt[:, :]), reads=[pn],
              writes=[uk])
        pn, pt = pp.next()
        tk.ops("pe", [lambda e, s=s, pt=pt: e.matmul(pt[:, s * 128:(s + 1) * 128], lhsT=Kbe[:, s, :].bitcast(F32R), rhs=Pf[:, s, :].bitcast(F32R),
                                                     start=True, stop=True) for s in range(4)],
               reads=[pk, "Kbe"], writes=[pn])
        tk.op("act", lambda e, pt=pt: e.copy(out=WT[:], in_=pt[:, :]), reads=[pn], writes=[wk])
        tk.op("dve", lambda e: e.tensor_tensor(out=QdT[:], in0=qn[:], in1=egcb[:], op=ALU.mult),
              reads=["qn", ek], writes=[qdk])
        yield

    def prep_attn(c):
        ts = slice((c * T) % 4096, (c * T) % 4096 + T)
        rope_tables(c)
        yield
        pnx, ptx = proj(c, 7)
        for m, dstT, dk_, xo in ((4, qT, "qT", 0), (5, kT, "kT", 32)):
            pn, pt = proj(c, m)
            tk.op("act", lambda e, pt=pt, dstT=dstT: e.copy(out=dstT[32:64, ts], in_=pt[32:64, :]), reads=[pn],
                  writes=[K(dk_, c)])
            tk.op("act", lambda e, pt=pt, dstT=dstT: e.copy(out=dstT[64:128, ts], in_=pt[64:128, :]), reads=[pn],
                  writes=[K(dk_, c)])
            tk.op("dve", lambda e, pt=pt: e.tensor_tensor(out=rt1[:], in0=pt[0:32, :], in1=sincos[32:64, :],
                                                          op=ALU.mult), reads=[pn, "sincos"], writes=["rt1"])
            tk.op("dve", lambda e, ptx=ptx, xo=xo: e.tensor_tensor(out=rt2[:], in0=ptx[xo:xo + 32, :],
                                                                   in1=sincos[0:32, :], op=ALU.mult),
                  reads=[pnx, "sincos"], writes=["rt2"])
            tk.op("dve", lambda e, dstT=dstT: e.tensor_tensor(out=dstT[0:32, ts], in0=rt1[:], in1=rt2[:], op=ALU.add),
                  reads=["rt1", "rt2"], writes=[K(dk_, c)])
            yield
        pn, pt = proj(c, 6)
        tk.op("act", lambda e, pt=pt: e.copy(out=vT[:, ts], in_=pt[:, :]), reads=[pn], writes=[K("vT", c)])

        yield

    def scan(c):
        ts = slice(c * T, (c + 1) * T)
        par = c % 2
        U, WT, QdT, QKT, Kd, egcb, zs = U2[par], WT2[par], QdT2[par], QKT2[par], Kd2[par], egcb2[par], zs2[par]
        uk, wk, qdk, qkk, kdk, ek, zk = ("U%d" % par, "WT%d" % par, "QdT%d" % par, "QKT%d" % par, "Kd%d" % par,
                                         "egcb%d" % par, "zs%d" % par)
        ot = PS["ot"]
        sc_ = PS["scan"]
        for n in range(8):
            gi = c * 8 + n
            s, half = n // 2, n % 2
            r0 = 64 * half
            rows = slice(r0, r0 + 64)
            tsl = slice(64 * n, 64 * n + 64)
            Sc, Sn = S[gi % 2], S[(gi + 1) % 2]
            sk, snk = "S%d" % (gi % 2), "S%d" % ((gi + 1) % 2)
            slot = (gi % 2) * 256
            tk.op("pe", lambda e: e.matmul(sc_[rows, slot:slot + 128], lhsT=WT[:, tsl], rhs=Sc[:], start=True, stop=True),
                  reads=[wk, sk], writes=["ps_scan_a%d" % (gi % 2)])
            tk.op("dve", lambda e: e.tensor_tensor(out=vnew[rows, gi % 2, :], in0=U[rows, s, :],
                                                   in1=sc_[rows, slot:slot + 128], op=ALU.subtract),
                  reads=[uk, "ps_scan_a%d" % (gi % 2)], writes=["vnew%d" % (gi % 2)])
            tk.ops("pe", [
                lambda e: e.matmul(ot[:, tsl], lhsT=Sc[:], rhs=QdT[:, tsl], start=True, stop=False),
                lambda e: e.matmul(ot[:, tsl], lhsT=vnew[rows, gi % 2, :], rhs=QKT[rows, s, r0:r0 + 64], start=False,
                                   stop=True),
                lambda e: e.matmul(sc_[:, slot + 128:slot + 256], lhsT=Kd[rows, s, :], rhs=vnew[rows, gi % 2, :],
                                   start=True, stop=True)],
                reads=[sk, qdk, qkk, kdk, "vnew%d" % (gi % 2)], writes=["ps_ot", "ps_scan_b%d" % (gi % 2)])
            tl = 64 * n + 63
            tk.op("dve", lambda e: e.scalar_tensor_tensor(out=Sn[:], in0=Sc[:], scalar=egcb[:, tl:tl + 1],
                                                          in1=sc_[:, slot + 128:slot + 256], op0=ALU.mult, op1=ALU.add),
                  reads=[sk, ek, "ps_scan_b%d" % (gi % 2)], writes=[snk])
            yield
        tk.op("act", lambda e: e.activation(out=osq[:], in_=ot[:, :], func=AF.Square), reads=["ps_ot"], writes=["p1"])
        pn, pt = ppa.next()
        tk.op("pe", lambda e: e.matmul(pt[:, :], lhsT=ones_f[:], rhs=osq[:], start=True, stop=True),
              reads=["p1", "ones_f"], writes=[pn])
        tk.op("act", lambda e: e.activation(out=rs[:], in_=pt[:, :], func=AF.Ln, bias=RMS_EPS_AP[:, 0:1],
                                            scale=1.0 / 128.0), reads=[pn, "epsc"], writes=["p2"])
        tk.op("act", lambda e: e.activation(out=rs[:], in_=rs[:], func=AF.Exp, scale=-0.5), reads=["p2"],
              writes=["p2"])
        tk.op("dve", lambda e: e.scalar_tensor_tensor(out=oa[:], in0=ot[:, :], scalar=pvec[:, 2:3], in1=rs[:],
                                                      op0=ALU.mult, op1=ALU.mult), reads=["ps_ot", "pvec", "p2"],
              writes=["p3"])
        tk.op("dve", lambda e: e.tensor_tensor(out=oab[:], in0=oa[:], in1=zs[:], op=ALU.mult), reads=["p3", zk],
              writes=["oab"])
        if cin is None:
            tk.dma("sp", lambda e: e.dma_start(out=mixT[0:128, ts], in_=oab[:]), reads=["oab"], writes=["mixT_a"])
        else:
            tk.dma("sp", lambda e: e.dma_start(out=cin[c // 4][0:128, (c % 4) * T:(c % 4 + 1) * T], in_=oab[:]),
                   reads=["oab"], writes=[("cin_a", c // 4)])
        yield

    att_scale = 128.0 ** -0.5
    pcount = [0]

    def qblock(qsel, kcur, kprev, vcur, vprev, vkeys, accv, first, rkeys):
        i = pcount[0] % 2
        pcount[0] += 1
        pTb = pTt[i]
        pk = "pT%d" % i
        pn, pt = ppa.next()
        if kprev is not None:
            tk.ops("pe", [
                lambda e: e.matmul(pt[:, 0:256], lhsT=ident_b[:], rhs=amask[:, 0:256], start=True, stop=False),
                lambda e: e.matmul(pt[:, 0:128], lhsT=kprev, rhs=qsel, start=False, stop=False),
                lambda e: e.matmul(pt[:, 128:256], lhsT=kcur, rhs=qsel, start=False, stop=True)],
                reads=rkeys + ["ident_b", "amask"], writes=[pn])
            tk.op("act", lambda e: e.activation(out=pTb[:, 0:256], in_=pt[:, 0:256], func=AF.Exp, scale=att_scale),
                  reads=[pn], writes=[pk])
            pn2, pt2 = ppa.next()
            tk.ops("pe", [
                lambda e: e.matmul(pt2[:, 0:128], lhsT=vprev, rhs=pTb[:, 0:128], start=True, stop=False),
                lambda e: e.matmul(pt2[:, 0:128], lhsT=vcur, rhs=pTb[:, 128:256], start=False, stop=True),
                lambda e: e.matmul(pt2[:, 128:256], lhsT=ones_b[:], rhs=pTb[:, 0:128], start=True, stop=False),
                lambda e: e.matmul(pt2[:, 128:256], lhsT=ones_b[:], rhs=pTb[:, 128:256], start=False, stop=True)],
                reads=[pk, "ones_b"] + vkeys, writes=[pn2])
        else:
            tk.ops("pe", [
                lambda e: e.matmul(pt[:, 128:256], lhsT=ident_b[:], rhs=amask[:, 128:256], start=True, stop=False),
                lambda e: e.matmul(pt[:, 128:256], lhsT=kcur, rhs=qsel, start=False, stop=True)],
                reads=rkeys + ["ident_b", "amask"], writes=[pn])
            tk.op("act", lambda e: e.activation(out=pTb[:, 128:256], in_=pt[:, 128:256], func=AF.Exp, scale=att_scale),
                  reads=[pn], writes=[pk])
            pn2, pt2 = ppa.next()
            tk.ops("pe", [
                lambda e: e.matmul(pt2[:, 0:128], lhsT=vcur, rhs=pTb[:, 128:256], start=True, stop=True),
                lambda e: e.matmul(pt2[:, 128:256], lhsT=ones_b[:], rhs=pTb[:, 128:256], start=True, stop=True)],
                reads=[pk, "ones_b"] + vkeys, writes=[pn2])
        src = pt2[:, 0:256].rearrange("p (a q) -> p a q", a=2)
        if first:
            tk.op("dve", lambda e: e.tensor_copy(out=accv, in_=src), reads=[pn2], writes=["acc"])
        else:
            tk.op("dve", lambda e: e.tensor_tensor(out=accv, in0=accv, in1=src, op=ALU.add), reads=[pn2, "acc"],
                  writes=["acc"])

    vcnt = [0]

    def vtrans(vsel, rk):
        i = vcnt[0] % 4
        vcnt[0] += 1
        pn, pt = ppa.next()
        ptb = pt[:, 0:64].bitcast(BF16)
        tk.op("pe", lambda e: e.transpose(ptb, vsel, ident_b[:]), reads=rk + ["ident_b"], writes=[pn])
        tk.op("act", lambda e: e.copy(out=vtt[:, i, :], in_=ptb), reads=[pn], writes=["vtt%d" % i])
        return vtt[:, i, :], "vtt%d" % i

    def rsl(t0, n, step=1):
        r0 = t0 % RING
        return slice(r0, r0 + (n - 1) * step + 1, step)

    def attn(c):
        sc = c // 4
        for jj in range(4):
            j = 4 * c + jj
            t0 = 128 * j
            cprev = (t0 - 128) // T
            vc, vck = vtrans(vT[:, rsl(t0, 128)], [K("vT", c)])
            if j > 0:
                vp, vpk = vtrans(vT[:, rsl(t0 - 128, 128)], [K("vT", cprev)])
            else:
                vp, vpk = None, None
            rk = [K("qT", c), K("kT", c)] + ([K("kT", cprev)] if j > 0 else [])
            qblock(qT[:, rsl(t0, 128)], kT[:, rsl(t0, 128)], kT[:, rsl(t0 - 128, 128)] if j > 0 else None,
                   vc, vp, [k_ for k_ in (vck, vpk) if k_], acc[:, :, (t0 % 2048):(t0 % 2048) + 128], True, rk)
            yield
        for r in range(4):
            t0 = T * c + r
            vc, vck = vtrans(vT[:, rsl(t0, 128, 4)], [K("vT", c)])
            if c > 0:
                vp, vpk = vtrans(vT[:, rsl(t0 - T, 128, 4)], [K("vT", c - 1)])
            else:
                vp, vpk = None, None
            rk = [K("qT", c), K("kT", c)] + ([K("kT", c - 1)] if c > 0 else [])
            a0 = (T * c) % 2048 + r
            qblock(qT[:, rsl(t0, 128, 4)], kT[:, rsl(t0, 128, 4)], kT[:, rsl(t0 - T, 128, 4)] if c > 0 else None,
                   vc, vp, [k_ for k_ in (vck, vpk) if k_], acc[:, :, a0:a0 + 509:4], False, rk)
            yield
        if c % 4 == 3:
            cs = [4 * sc + i for i in range(4)]
            csp = [4 * (sc - 1) + i for i in range(4)] if sc > 0 else []
            for r in range(16):
                t0 = 2048 * sc + r
                vc, vck = vtrans(vT[:, rsl(t0, 128, 16)], [K("vT", ci) for ci in cs])
                if sc > 0:
                    vp, vpk = vtrans(vT[:, rsl(t0 - 2048, 128, 16)], [K("vT", ci) for ci in csp])
                else:
                    vp, vpk = None, None
                rk = [K("qT", ci) for ci in cs] + [K("kT", ci) for ci in cs] + [K("kT", ci) for ci in csp]
                qblock(qT[:, rsl(t0, 128, 16)], kT[:, rsl(t0, 128, 16)],
                       kT[:, rsl(t0 - 2048, 128, 16)] if sc > 0 else None,
                       vc, vp, [k_ for k_ in (vck, vpk) if k_], acc[:, :, r:r + 2033:16], False, rk)
                yield
            for i in range(4):
                a = slice(i * T, (i + 1) * T)
                tsl = slice(2048 * sc + i * T, 2048 * sc + (i + 1) * T)
                tk.op("act", lambda e, a=a: e.activation(out=fr[:], in_=acc[:, 1, a], func=AF.Ln), reads=["acc"],
                      writes=["p2"])
                tk.op("act", lambda e: e.activation(out=fr[:], in_=fr[:], func=AF.Exp, scale=-1.0), reads=["p2"],
                      writes=["p2"])
                tk.op("dve", lambda e, a=a: e.tensor_tensor(out=fo[:], in0=acc[:, 0, a], in1=fr[:], op=ALU.mult),
                      reads=["acc", "p2"], writes=["p3"])
                tk.op("act", lambda e: e.activation(out=fsq[:], in_=fo[:], func=AF.Square), reads=["p3"],
                      writes=["p1"])
                pn, pt = ppa.next()
                tk.op("pe", lambda e, pt=pt: e.matmul(pt[:, :], lhsT=ones_f[:], rhs=fsq[:], start=True, stop=True),
                      reads=["p1", "ones_f"], writes=[pn])
                tk.op("act", lambda e, pt=pt: e.activation(out=fr[:], in_=pt[:, :], func=AF.Ln,
                                                           bias=RMS_EPS_AP[:, 0:1], scale=1.0 / 128.0),
                      reads=[pn, "epsc"], writes=["p2"])
                tk.op("act", lambda e: e.activation(out=fr[:], in_=fr[:], func=AF.Exp, scale=-0.5), reads=["p2"],
                      writes=["p2"])
                tk.op("dve", lambda e: e.scalar_tensor_tensor(out=fob[:], in0=fo[:], scalar=pvec[:, 3:4], in1=fr[:],
                                                              op0=ALU.mult, op1=ALU.mult), reads=["p3", "pvec", "p2"],
                      writes=["fob"])
                yield
                if cin is None:
                    tk.dma("sp", lambda e, tsl=tsl: e.dma_start(out=mixT[128:256, tsl], in_=fob[:]), reads=["fob"],
                           writes=["mixT_b"])
                else:
                    tk.dma("sp", lambda e, i=i: e.dma_start(out=cin[sc][128:256, i * T:(i + 1) * T], in_=fob[:]),
                           reads=["fob"], writes=[("cin_b", sc)])
            if on_quarter is not None:
                on_quarter(sc, tk)
        yield

    epsc = sb("epsc", [128, 2])
    tk.op("pool", lambda e: e.memset(epsc[:, 0:1], RMS_EPS), writes=["epsc"])
    tk.op("pool", lambda e: e.memset(epsc[:, 1:2], 1.0), writes=["epsc"])
    RMS_EPS_AP = epsc[:, 0:1]
    ONE_AP = epsc[:, 1:2]

    def run(gen):
        if gen is not None:
            for _ in gen:
                pass

    load_x(0)
    run(prep_main(0))
    tk.flush()
    for c in range(nch):
        run(scan(c))
        run(prep_attn(c))
        run(attn(c))
        if c + 1 < nch:
            run(prep_main(c + 1))
        if (c + 1) % 4 == 3 or c == nch - 1:
            tk.flush()
    tk = real_tk


def build_phase1(nch=NCH):
    nc = bass.Bass("TRN2", target_bir_lowering=False)
    D = {}

    def din(name, shape, dt=F32):
        D[name] = nc.dram_tensor(name, shape, dt, kind="ExternalInput").ap()

    din("x", [D_MODEL, SEQ]); din("cT", [128, 8]); din("pos", [SEQ], I32)
    din("wada1", [128, 8, 8, 256]); din("bada1", [128, 16]); din("win", [128, 5, 8, 256])
    din("convw", [128, 3, 4]); din("pvec", [128, 4])
    din("c_ident", [128, 128]); din("c_masks", [128, 3, 128]); din("c_amask", [128, 256]); din("c_invf", [64, 2])
    D["mixT"] = nc.dram_tensor("mixT", [256, SEQ], BF16, kind="ExternalOutput").ap()
    with ExitStack() as es:
        tk = Trk(nc, es)
        phase1(nc, tk, es, D, nch=nch)
        tk.finish("sp")
    return nc


def consts():
    p = np.arange(128)[:, None]
    f = np.arange(128)[None, :]
    same = (p // 64) == (f // 64)
    mus = -((f > p) & same).astype(np.float32)
    mui = ((f >= p) & same).astype(np.float32)
    mls = -((f < p) & same).astype(np.float32)
    masks = np.stack([mus, mui, mls], 1).astype(np.float32)
    prev = np.where(p >= f, 0.0, NEG).astype(np.float32)
    cur = np.where(p <= f, 0.0, NEG).astype(np.float32)
    amask = np.concatenate([prev, cur], 1)
    i = (np.arange(64) % 16).astype(np.float32)
    invf = (ROPE_THETA ** (-i * 2.0 / 32.0)).astype(np.float32)
    invf = np.stack([invf, np.where(np.arange(64) < 32, 0.0, math.pi / 2).astype(np.float32)], 1)
    return {"c_ident": np.eye(128, dtype=np.float32), "c_masks": masks, "c_amask": amask, "c_invf": invf}


def phase1_inputs(inp, core):
    b, h = core // 4, core % 4
    l = 0
    w_in = inp["w_in"][l]
    hs = slice(h * 128, (h + 1) * 128)
    qa, ka, va, z = w_in[:, 0:512][:, hs], w_in[:, 512:1024][:, hs], w_in[:, 1024:1536][:, hs], w_in[:, 1536:2048][:, hs]
    beta = w_in[:, 2048 + h:2049 + h]
    dec = w_in[:, 2052 + h:2053 + h]
    qb, kb, vb = w_in[:, 2056:2568][:, hs], w_in[:, 2568:3080][:, hs], w_in[:, 3080:3592][:, hs]
    extra = np.zeros((D_MODEL, 128), np.float32)
    extra[:, 0:16] = qb[:, 16:32]; extra[:, 16:32] = qb[:, 0:16]
    extra[:, 32:48] = kb[:, 16:32]; extra[:, 48:64] = kb[:, 0:16]
    win = np.concatenate([qa, ka, va, z, qb, kb, vb, extra, np.repeat(beta, 128, 1), np.repeat(dec, 128, 1)], 1)
    conv = inp["conv_w"][l]
    convw = np.stack([conv[:, g * 512:(g + 1) * 512][:, hs].T for g in range(3)], 1)
    pvec = np.stack([np.full(128, inp["a_log"][l, h]), np.full(128, inp["dt_bias"][l, h]),
                     inp["gdn_norm_w"][l], inp["attn_norm_w"][l]], 1).astype(np.float32)
    win_l = win.astype(np.float32).reshape(8, 128, 5, 256).transpose(1, 2, 0, 3)
    wada1_l = inp["w_ada"][l][:, 0:2048].reshape(8, 128, 8, 256).transpose(1, 2, 0, 3)
    d = {"x": np.ascontiguousarray(inp["x"][b].T), "cT": np.ascontiguousarray(inp["c"][b].reshape(8, 128).T),
         "pos": np.ascontiguousarray(inp["positions"][b]).astype(np.int32),
         "wada1": np.ascontiguousarray(wada1_l),
         "bada1": np.ascontiguousarray(inp["b_ada"][l][0:2048].reshape(16, 128).T),
         "win": np.ascontiguousarray(win_l), "convw": np.ascontiguousarray(convw.astype(np.float32)),
         "pvec": np.ascontiguousarray(pvec)}
    d.update(consts())
    return d


NT2 = 16
NBLK = 64


def phase2(nc, tk, es, D, ntile=NT2, nblk=NBLK, cout=None):
    real = tk
    tk = Rec(real)

    def mk(stack, pref):
        def sb(name, shape, dt=F32):
            return stack.enter_context(nc.sbuf_tensor(pref + name, shape, dt))
        return sb

    sb = mk(es, "t_")
    pp = PsumPool(nc, es, ["r0", "r1", "r2", "r3", "r4", "r5", "r6", "r7"])

    ident = sb("ident", [128, 128])
    ones_f = sb("ones_f", [128, 128])
    triS = sb("triS", [128, 128])
    kcoff = sb("kcoff", [128, 1])
    thr16 = sb("thr16", [128, 16])
    thr64 = sb("thr64", [128, 64])
    epsc = sb("epsc", [128, 1])
    cT = sb("cT", [128, 8]); scT = sb("scT", [128, 8])
    modb = sb("modb", [128, 4, 1024])
    lnp = sb("lnp", [128, 4, 1024])
    xt2 = [sb("xt%d" % i, [128, 1024]) for i in range(2)]
    rr2 = [sb("rr%d" % i, [128, 1024]) for i in range(2)]
    x12 = [sb("x1%d" % i, [128, 1024]) for i in range(2)]
    h22 = [sb("h2%d" % i, [128, 1024]) for i in range(2)]
    stt = sb("stt", [128, 2, 6]); mv = sb("mv", [128, 2]); rstd = sb("rstd", [128, 1])
    gts = sb("gts", [128, NT2, 2])
    desti = sb("desti", [128, 2, NT2], I32)
    idxw = sb("idxw", [128, 64], I32)

    x = D["x2"]; mixTf = D.get("mixTf"); out = D["out"]
    x1d = D["x1d"]; h2d = D["h2d"]; xs = D["xs"]; ys = D["ys"]

    def layer_norm(src, skey, dst, dkey, gi):
        tk.op("dve", lambda e: e.bn_stats(out=stt[:, 0, :], in_=src[:, 0:512]), reads=[skey], writes=["stt"])
        tk.op("dve", lambda e: e.bn_stats(out=stt[:, 1, :], in_=src[:, 512:1024]), reads=[skey], writes=["stt"])
        tk.op("dve", lambda e: e.bn_aggr(out=mv[:], in_=stt[:].rearrange("p a b -> p (a b)")), reads=["stt"],
              writes=["mv"])
        tk.op("act", lambda e: e.activation(out=rstd[:], in_=mv[:, 1:2], func=AF.Sqrt, bias=epsc[:, 0:1], scale=1.0),
              reads=["mv", "epsc"], writes=["rstd"])
        tk.op("dve", lambda e: e.reciprocal(out=rstd[:], in_=rstd[:]), reads=["rstd"], writes=["rstd"])
        tk.op("dve", lambda e: e.tensor_scalar(out=dst[:], in0=src[:], scalar1=mv[:, 0:1], scalar2=rstd[:, 0:1],
                                               op0=ALU.subtract, op1=ALU.mult), reads=[skey, "mv", "rstd"],
              writes=[dkey])
        tk.op("dve", lambda e: e.tensor_tensor(out=dst[:], in0=dst[:], in1=lnp[:, gi, :], op=ALU.mult),
              reads=[dkey, "lnp"], writes=[dkey])
        tk.op("dve", lambda e: e.tensor_tensor(out=dst[:], in0=dst[:], in1=lnp[:, gi + 1, :], op=ALU.add),
              reads=[dkey, "lnp"], writes=[dkey])

    tk.dma("sp", lambda e: e.dma_start(out=ident[:], in_=D["c_ident"][:, :]), writes=["ident"])
    tk.dma("sp", lambda e: e.dma_start(out=triS[:], in_=D["c_tri"][:, :]), writes=["triS"])
    tk.dma("sp", lambda e: e.dma_start(out=kcoff[:], in_=D["c_iota"][:, :]), writes=["kcoff"])
    tk.dma("sp", lambda e: e.dma_start(out=thr16[:], in_=D["c_thr16"][:, :]), writes=["thr16"])
    tk.dma("sp", lambda e: e.dma_start(out=thr64[:], in_=D["c_thr64"][:, :]), writes=["thr64"])
    tk.dma("sp", lambda e: e.dma_start(out=cT[:], in_=D["cT"][:, :]), writes=["cT"])
    for i in range(4):
        tk.dma("sp", lambda e, i=i: e.dma_start(out=lnp[:, i, :], in_=D["lnp"][i, :].partition_broadcast(128)),
               writes=["lnp"])
    tk.op("pool", lambda e: e.memset(ones_f[:], 1.0), writes=["ones_f"])
    tk.op("pool", lambda e: e.memset(epsc[:], LN_EPS), writes=["epsc"])

    with ExitStack() as esA:
        sa = mk(esA, "a_")
        scR = sa("scR", [128, 8, 128])
        badab = sa("badab", [128, 512])
        wo_b = sa("wo_b", [128, 8, 1024], BF16)
        wr = sa("wr", [128, 8, 36]); brb = sa("brb", [128, 36])
        h2T2 = [sa("h2T%d" % i, [128, 8, 128]) for i in range(2)]
        lg = sa("lg", [128, 36]); gmax = sa("gmax", [128, 1]); ngmax = sa("ngmax", [128, 1]); ohg = sa("ohg", [128, 4])
        ex4 = sa("ex4", [128, 4]); sume = sa("sume", [128, 1]); ggrp = sa("ggrp", [128, 1])
        tmp48 = sa("tmp48", [128, 4, 8]); lesel = sa("lesel", [128, 8]); m8 = sa("m8", [128, 8])
        oh8 = sa("oh8", [128, 2, 8]); e2 = sa("e2", [128, 1]); g1 = sa("g1", [128, 1])
        ohs = sa("ohs", [128, NT2, 2, 32]); ohany = sa("ohany", [128, 32]); cum = sa("cum", [128, 32])
        ranks = sa("ranks", [128, NT2, 32])
        cntb = sa("cntb", [128, 32]); cmp16 = sa("cmp16", [128, 32, 16]); padded = sa("padded", [128, 32])
        onesr = sa("onesr", [128, 32]); padend = sa("padend", [128, 32]); padstart = sa("padstart", [128, 32])
        tmpd = sa("tmpd", [128, NT2, 32]); tmpd2 = sa("tmpd2", [128, NT2, 32])
        destf = sa("destf", [128, 2, NT2])
        cmp64 = sa("cmp64", [128, 64, 32]); blkf = sa("blkf", [128, 64])

        tk.dma("sp", lambda e: e.dma_start(out=wr[:], in_=D["wr"][:, :].rearrange("(kc p) c -> p kc c", p=128)),
               writes=["wr"])
        tk.dma("sp", lambda e: e.dma_start(out=brb[:], in_=D["br"][:].partition_broadcast(128)), writes=["brb"])
        tk.op("pool", lambda e: e.memset(cum[:], 0.0), writes=["cum"])
        tk.op("pool", lambda e: e.memset(onesr[:], 1.0), writes=["onesr"])
        if cout is None:
            mt1 = sa("mt", [128, 8, 128], BF16)
        else:
            mtf = sa("mtf", [128, 8, 2048], BF16)
            qidx = sa("qidx", [128, 8], I32)
            tk.dma("sp", lambda e: e.dma_start(out=qidx[:], in_=D["qidx"][:, :]), writes=["qidx"])
            for kc in range(8):
                tk.dma("pool", lambda e, kc=kc: e.indirect_dma_start(
                    out=mtf[:, kc, :], out_offset=None, in_=cout[:, :],
                    in_offset=bass.IndirectOffsetOnAxis(ap=qidx[:, kc:kc + 1], axis=0)),
                    reads=["qidx"] + [("cout", q_) for q_ in range(4)], writes=["mt"])
        stg = [(xt2[0], "xt0"), (rr2[0], "rr0"), (xt2[1], "xt1"), (rr2[1], "rr1")]
        for kc in range(8):
            st, key = stg[kc % 4]
            tk.dma("sp", lambda e, st=st, kc=kc: e.dma_start(out=st[:], in_=D["wo"][kc * 128:(kc + 1) * 128, :]),
                   writes=[key])
            tk.op("dve", lambda e, st=st, kc=kc: e.tensor_copy(out=wo_b[:, kc, :], in_=st[:]), reads=[key],
                  writes=["wo_b"])
        tk.op("act", lambda e: e.activation(out=scT[:], in_=cT[:], func=AF.Silu), reads=["cT"], writes=["scT"])
        tk.op("dve", lambda e: e.tensor_copy(out=scR[:], in_=scT[:].unsqueeze(2).to_broadcast([128, 8, 128])),
              reads=["scT"], writes=["scR"])
        for blk in range(8):
            pn, pt = pp.next()
            for sub in range(4):
                st, key = stg[sub % 4]
                st3 = st[:].rearrange("p (kc c) -> p kc c", c=128)
                c0 = blk * 512 + sub * 128
                tk.dma("sp" if sub % 2 == 0 else "pool", lambda e, st3=st3, blk=blk, sub=sub: e.dma_start(
                    out=st3, in_=D["wada2"][:, blk * 4 + sub, :, :]), writes=[key])
                tk.ops("pe", [lambda e, kc=kc, pt=pt, st3=st3, sub=sub: e.matmul(
                    pt[:, sub * 128:(sub + 1) * 128], lhsT=scR[:, kc, :], rhs=st3[:, kc, :], start=(kc == 0),
                    stop=(kc == 7)) for kc in range(8)], reads=[key, "scR"], writes=[pn])
            tk.dma("sp", lambda e, blk=blk: e.dma_start(
                out=badab[:], in_=D["bada2"][blk * 512:(blk + 1) * 512].partition_broadcast(128)), writes=["badab"])
            dst = modb[:, blk // 2, (blk % 2) * 512:(blk % 2 + 1) * 512]
            tk.op("dve", lambda e, pt=pt, dst=dst: e.tensor_tensor(out=dst, in0=pt[:, :], in1=badab[:], op=ALU.add),
                  reads=[pn, "badab"], writes=["modb"])
            if blk // 2 != 1:
                tk.op("dve", lambda e, dst=dst: e.tensor_scalar(out=dst, in0=dst, scalar1=1.0, scalar2=None,
                                                                op0=ALU.add), reads=["modb"], writes=["modb"])
        tk.flush()

        for k in range(ntile):
            par = k % 2
            xt, rr, x1, h2, h2T = xt2[par], rr2[par], x12[par], h22[par], h2T2[par]
            xtk, rrk, x1k, h2k, h2Tk = "xt%d" % par, "rr%d" % par, "x1%d" % par, "h2%d" % par, "h2T%d" % par
            tsl = slice(k * 128, (k + 1) * 128)
            if cout is None:
                mt = mt1
                tk.dma("sp", lambda e: e.dma_start(out=mt[:], in_=mixTf[:, tsl].rearrange("(kc p) t -> p kc t", p=128)),
                       writes=["mt"])
            else:
                mt = mtf[:, :, tsl]
            tk.dma("sp", lambda e: e.dma_start(out=xt[:], in_=x[tsl, :]), writes=[xtk])
            pa, pta = pp.next()
            pb, ptb = pp.next()
            for (pn, pt, hs) in ((pa, pta, slice(0, 512)), (pb, ptb, slice(512, 1024))):
                tk.ops("pe", [lambda e, kc=kc, pt=pt, hs=hs: e.matmul(pt[:, :], lhsT=mt[:, kc, :], rhs=wo_b[:, kc, hs],
                                                                     start=(kc == 0), stop=(kc == 7))
                              for kc in range(8)], reads=["mt", "wo_b"], writes=[pn])
                tk.op("dve", lambda e, pt=pt, hs=hs: e.tensor_tensor(out=rr[:, hs], in0=pt[:, :], in1=modb[:, 0, hs],
                                                                     op=ALU.mult), reads=[pn, "modb"], writes=[rrk])
            tk.op("dve", lambda e: e.scalar_tensor_tensor(out=rr[:], in0=xt[:], scalar=ALPHA, in1=rr[:], op0=ALU.mult,
                                                          op1=ALU.add), reads=[xtk, rrk], writes=[rrk])
            layer_norm(rr, rrk, x1, x1k, 0)
            tk.dma("sp", lambda e: e.dma_start(out=x1d[tsl, :], in_=x1[:]), reads=[x1k], writes=["x1d"])
            tk.op("dve", lambda e: e.tensor_tensor(out=h2[:], in0=x1[:], in1=modb[:, 2, :], op=ALU.mult),
                  reads=[x1k, "modb"], writes=[h2k])
            tk.op("dve", lambda e: e.tensor_tensor(out=h2[:], in0=h2[:], in1=modb[:, 1, :], op=ALU.add),
                  reads=[h2k, "modb"], writes=[h2k])
            tk.dma("sp", lambda e: e.dma_start(out=h2d[tsl, :], in_=h2[:]), reads=[h2k], writes=["h2d"])
            for half in range(2):
                pn, pt = pp.next()
                tk.ops("pe", [lambda e, j=j, pt=pt, half=half: e.transpose(
                    pt[:, j * 128:(j + 1) * 128], h2[:, (half * 4 + j) * 128:(half * 4 + j + 1) * 128], ident[:])
                    for j in range(4)], reads=[h2k, "ident"], writes=[pn])
                tk.op("act", lambda e, pt=pt, half=half: e.copy(
                    out=h2T[:, half * 4:(half + 1) * 4, :].rearrange("p a b -> p (a b)"), in_=pt[:, :]), reads=[pn],
                    writes=[h2Tk])
            pn, pt = pp.next()
            tk.ops("pe", [lambda e, kc=kc, pt=pt: e.matmul(pt[:, 0:36], lhsT=h2T[:, kc, :], rhs=wr[:, kc, :],
                                                          start=(kc == 0), stop=(kc == 7)) for kc in range(8)],
                   reads=[h2Tk, "wr"], writes=[pn])
            tk.op("dve", lambda e, pt=pt: e.tensor_tensor(out=lg[:], in0=pt[:, 0:36], in1=brb[:], op=ALU.add),
                  reads=[pn, "brb"], writes=["lg"])
            tk.op("dve", lambda e: e.tensor_reduce(out=gmax[:], in_=lg[:, 0:4], axis=AX.X, op=ALU.max), reads=["lg"],
                  writes=["gmax"])
            tk.op("dve", lambda e: e.tensor_scalar(out=ohg[:], in0=lg[:, 0:4], scalar1=gmax[:, 0:1], scalar2=None,
                                                   op0=ALU.is_equal), reads=["lg", "gmax"], writes=["ohg"])
            tk.op("dve", lambda e: e.tensor_scalar(out=ngmax[:], in0=gmax[:], scalar1=-1.0, scalar2=None, op0=ALU.mult),
                  reads=["gmax"], writes=["ngmax"])
            tk.op("act", lambda e: e.activation(out=ex4[:], in_=lg[:, 0:4], func=AF.Exp, bias=ngmax[:, 0:1], scale=1.0),
                  reads=["lg", "ngmax"], writes=["ex4"])
            tk.op("dve", lambda e: e.tensor_reduce(out=sume[:], in_=ex4[:], axis=AX.X, op=ALU.add), reads=["ex4"],
                  writes=["sume"])
            tk.op("dve", lambda e: e.reciprocal(out=ggrp[:], in_=sume[:]), reads=["sume"], writes=["ggrp"])
            le3 = lg[:, 4:36].rearrange("p (g e) -> p g e", e=8)
            tk.op("dve", lambda e: e.tensor_tensor(out=tmp48[:], in0=le3,
                                                   in1=ohg[:].unsqueeze(2).to_broadcast([128, 4, 8]), op=ALU.mult),
                  reads=["lg", "ohg"], writes=["tmp48"])
            tk.op("dve", lambda e: e.tensor_reduce(out=lesel[:], in_=tmp48[:].rearrange("p g e -> p e g"), axis=AX.X,
                                                   op=ALU.add), reads=["tmp48"], writes=["lesel"])
            tk.op("dve", lambda e: e.max(out=m8[:], in_=lesel[:]), reads=["lesel"], writes=["m8"])
            for j in range(2):
                tk.op("dve", lambda e, j=j: e.tensor_scalar(out=oh8[:, j, :], in0=lesel[:], scalar1=m8[:, j:j + 1],
                                                            scalar2=None, op0=ALU.is_equal), reads=["lesel", "m8"],
                      writes=["oh8"])
            tk.op("dve", lambda e: e.tensor_tensor(out=e2[:], in0=m8[:, 1:2], in1=m8[:, 0:1], op=ALU.subtract),
                  reads=["m8"], writes=["e2"])
            tk.op("act", lambda e: e.activation(out=e2[:], in_=e2[:], func=AF.Exp), reads=["e2"], writes=["e2"])
            tk.op("dve", lambda e: e.tensor_scalar(out=g1[:], in0=e2[:], scalar1=1.0, scalar2=None, op0=ALU.add),
                  reads=["e2"], writes=["g1"])
            tk.op("dve", lambda e: e.reciprocal(out=g1[:], in_=g1[:]), reads=["g1"], writes=["g1"])
            tk.op("dve", lambda e: e.tensor_tensor(out=gts[:, k, 0:1], in0=g1[:], in1=ggrp[:], op=ALU.mult),
                  reads=["g1", "ggrp"], writes=["gts"])
            tk.op("dve", lambda e: e.tensor_tensor(out=e2[:], in0=e2[:], in1=g1[:], op=ALU.mult), reads=["e2", "g1"],
                  writes=["e2"])
            tk.op("dve", lambda e: e.tensor_tensor(out=gts[:, k, 1:2], in0=e2[:], in1=ggrp[:], op=ALU.mult),
                  reads=["e2", "ggrp"], writes=["gts"])
            for j in range(2):
                tk.op("dve", lambda e, j=j: e.tensor_tensor(
                    out=ohs[:, k, j, :].rearrange("p (g e) -> p g e", e=8),
                    in0=ohg[:].unsqueeze(2).to_broadcast([128, 4, 8]),
                    in1=oh8[:, j, :].unsqueeze(1).to_broadcast([128, 4, 8]), op=ALU.mult), reads=["ohg", "oh8"],
                    writes=["ohs"])
            tk.op("dve", lambda e: e.tensor_tensor(out=ohany[:], in0=ohs[:, k, 0, :], in1=ohs[:, k, 1, :], op=ALU.add),
                  reads=["ohs"], writes=["ohany"])
            pn, pt = pp.next()
            tk.ops("pe", [lambda e, pt=pt: e.matmul(pt[:, 0:32], lhsT=triS[:], rhs=ohany[:], start=True, stop=False),
                          lambda e, pt=pt: e.matmul(pt[:, 0:32], lhsT=ones_f[:], rhs=cum[:], start=False, stop=True)],
                   reads=["triS", "ohany", "ones_f", "cum"], writes=[pn])
            tk.op("act", lambda e, pt=pt: e.copy(out=ranks[:, k, :], in_=pt[:, 0:32]), reads=[pn], writes=["ranks"])
            tk.op("dve", lambda e: e.tensor_tensor(out=cum[:], in0=cum[:], in1=ohany[:], op=ALU.add),
                  reads=["cum", "ohany"], writes=["cum"])
            if k % 16 == 15:
                tk.flush()

        pn, pt = pp.next()
        tk.op("pe", lambda e: e.matmul(pt[:, 0:32], lhsT=ones_f[:], rhs=cum[:], start=True, stop=True),
              reads=["ones_f", "cum"], writes=[pn])
        tk.op("dve", lambda e: e.tensor_copy(out=cntb[:], in_=pt[:, 0:32]), reads=[pn], writes=["cntb"])
        tk.op("dve", lambda e: e.tensor_tensor(out=cmp16[:], in0=cntb[:].unsqueeze(2).to_broadcast([128, 32, 16]),
                                               in1=thr16[:].unsqueeze(1).to_broadcast([128, 32, 16]), op=ALU.is_gt),
              reads=["cntb", "thr16"], writes=["cmp16"])
        tk.op("dve", lambda e: e.tensor_reduce(out=padded[:], in_=cmp16[:], axis=AX.X, op=ALU.add), reads=["cmp16"],
              writes=["padded"])
        tk.op("dve", lambda e: e.tensor_scalar(out=padded[:], in0=padded[:], scalar1=128.0, scalar2=None, op0=ALU.mult),
              reads=["padded"], writes=["padded"])
        tk.op("dve", lambda e: e.tensor_tensor_scan(out=padend[:], data0=onesr[:], data1=padded[:], initial=0.0,
                                                    op0=ALU.mult, op1=ALU.add), reads=["onesr", "padded"],
              writes=["padend"])
        tk.op("dve", lambda e: e.tensor_tensor(out=padstart[:], in0=padend[:], in1=padded[:], op=ALU.subtract),
              reads=["padend", "padded"], writes=["padstart"])
        tk.op("dve", lambda e: e.tensor_tensor(out=tmpd[:, 0:ntile, :], in0=ranks[:, 0:ntile, :],
                                               in1=padstart[:].unsqueeze(1).to_broadcast([128, ntile, 32]), op=ALU.add),
              reads=["ranks", "padstart"], writes=["tmpd"])
        for j in range(2):
            tk.op("dve", lambda e, j=j: e.tensor_tensor(out=tmpd2[:, 0:ntile, :], in0=tmpd[:, 0:ntile, :],
                                                        in1=ohs[:, 0:ntile, j, :], op=ALU.mult), reads=["tmpd", "ohs"],
                  writes=["tmpd2"])
            tk.op("dve", lambda e, j=j: e.tensor_reduce(out=destf[:, j, 0:ntile], in_=tmpd2[:, 0:ntile, :], axis=AX.X,
                                                        op=ALU.add), reads=["tmpd2"], writes=["destf"])
        tk.op("dve", lambda e: e.tensor_copy(out=desti[:, :, 0:ntile], in_=destf[:, :, 0:ntile]), reads=["destf"],
              writes=["desti"])
        tk.op("dve", lambda e: e.tensor_tensor(out=cmp64[:], in0=padend[:].unsqueeze(1).to_broadcast([128, 64, 32]),
                                               in1=thr64[:].unsqueeze(2).to_broadcast([128, 64, 32]), op=ALU.is_le),
              reads=["padend", "thr64"], writes=["cmp64"])
        tk.op("dve", lambda e: e.tensor_reduce(out=blkf[:], in_=cmp64[:], axis=AX.X, op=ALU.add), reads=["cmp64"],
              writes=["blkf"])
        tk.op("dve", lambda e: e.tensor_scalar(out=blkf[:], in0=blkf[:], scalar1=128.0, scalar2=kcoff[:, 0:1],
                                               op0=ALU.mult, op1=ALU.add), reads=["blkf", "kcoff"], writes=["blkf"])
        tk.op("dve", lambda e: e.tensor_copy(out=idxw[:], in_=blkf[:]), reads=["blkf"], writes=["idxw"])
        for k in range(ntile):
            par = k % 2
            h2, h2k = h22[par], "h2%d" % par
            tsl = slice(k * 128, (k + 1) * 128)
            tk.dma("sp", lambda e: e.dma_start(out=h2[:], in_=h2d[tsl, :]), reads=["h2d"], writes=[h2k])
            for j in range(2):
                tk.dma("pool", lambda e, j=j: e.indirect_dma_start(
                    out=xs[:, :], out_offset=bass.IndirectOffsetOnAxis(ap=desti[:, j, k:k + 1], axis=0),
                    in_=h2[:, :], in_offset=None), reads=[h2k, "desti"], writes=[("xs", k, j)])
        tk.flush()
        real.barrier()
    xs_keys = [("xs", k, j) for k in range(ntile) for j in range(2)]

    with ExitStack() as esB:
        sbb = mk(esB, "b_")
        W = []
        for i in range(2):
            wg2 = sbb("wg%d" % i, [128, 4096]); wu2 = sbb("wu%d" % i, [128, 4096]); wd2 = sbb("wd%d" % i, [128, 4096])
            W.append((wg2, wu2, wd2))
        xsb2 = [sbb("xsb%d" % i, [128, 1024]) for i in range(2)]
        xsT2 = [sbb("xsT%d" % i, [128, 8, 128]) for i in range(2)]
        gsil2 = [sbb("gsil%d" % i, [128, 512]) for i in range(2)]
        hid2 = [sbb("hid%d" % i, [128, 512]) for i in range(2)]
        hidT2 = [sbb("hidT%d" % i, [128, 4, 128]) for i in range(2)]
        ysb2 = [sbb("ysb%d" % i, [128, 1024]) for i in range(2)]
        wgd = D["wgate"]; wud = D["wup"]; wdd = D["wdown"]
        breg = nc.gpsimd.to_reg(32 * 128 - 1)
        for b in range(nblk):
            par = b % 2
            wg2, wu2, wd2 = W[par]
            wg = wg2[:].rearrange("p (a b) -> p a b", b=512)
            wu = wu2[:].rearrange("p (a b) -> p a b", b=512)
            wd = wd2[:].rearrange("p (a b) -> p a b", b=1024)
            xsb, xsT, gsil, hid, hidT, ysb = xsb2[par], xsT2[par], gsil2[par], hid2[par], hidT2[par], ysb2[par]
            sfx = str(par)
            for (dst, dkey, src) in ((wg2, "wg" + sfx, wgd), (wu2, "wu" + sfx, wud), (wd2, "wd" + sfx, wdd)):
                tk.dma("pool", lambda e, dst=dst, src=src: e.indirect_dma_start(
                    out=dst[:, :].bitcast(F32R), out_offset=None, in_=src[:, :].bitcast(F32R),
                    in_offset=bass.IndirectOffsetOnAxis(ap=idxw[:, b:b + 1], axis=0), bounds_check=breg,
                    oob_is_err=False), reads=["idxw"], writes=[dkey], cost=8.0)
            tk.dma("sp", lambda e: e.dma_start(out=xsb[:], in_=xs[b * 128:(b + 1) * 128, :]),
                   reads=xs_keys if b == 0 else [], writes=["xsb" + sfx])
            for half in range(2):
                pn, pt = pp.next()
                tk.ops("pe", [lambda e, j=j, pt=pt, half=half: e.transpose(
                    pt[:, j * 128:(j + 1) * 128], xsb[:, (half * 4 + j) * 128:(half * 4 + j + 1) * 128], ident[:])
                    for j in range(4)], reads=["xsb" + sfx, "ident"], writes=[pn])
                tk.op("act", lambda e, pt=pt, half=half: e.copy(
                    out=xsT[:, half * 4:(half + 1) * 4, :].rearrange("p a b -> p (a b)").bitcast(F32R), in_=pt[:, :]),
                    reads=[pn], writes=["xsT" + sfx])
            pg, ptg = pp.next()
            tk.ops("pe", [lambda e, kc=kc: e.matmul(ptg[:, :], lhsT=xsT[:, kc, :].bitcast(F32R),
                                                   rhs=wg[:, kc, :].bitcast(F32R), start=(kc == 0), stop=(kc == 7))
                          for kc in range(8)], reads=["xsT" + sfx, "wg" + sfx], writes=[pg], cost=3.0)
            pu, ptu = pp.next()
            tk.ops("pe", [lambda e, kc=kc: e.matmul(ptu[:, :], lhsT=xsT[:, kc, :].bitcast(F32R),
                                                   rhs=wu[:, kc, :].bitcast(F32R), start=(kc == 0), stop=(kc == 7))
                          for kc in range(8)], reads=["xsT" + sfx, "wu" + sfx], writes=[pu], cost=3.0)
            tk.op("act", lambda e: e.activation(out=gsil[:], in_=ptg[:, :], func=AF.Silu), reads=[pg],
                  writes=["gsil" + sfx])
            tk.op("dve", lambda e: e.tensor_tensor(out=hid[:], in0=gsil[:], in1=ptu[:, :], op=ALU.mult),
                  reads=["gsil" + sfx, pu], writes=["hid" + sfx])
            pn, pt = pp.next()
            tk.ops("pe", [lambda e, j=j, pt=pt: e.transpose(pt[:, j * 128:(j + 1) * 128],
                                                            hid[:, j * 128:(j + 1) * 128], ident[:])
                          for j in range(4)], reads=["hid" + sfx, "ident"], writes=[pn])
            tk.op("act", lambda e, pt=pt: e.copy(out=hidT[:].rearrange("p a b -> p (a b)").bitcast(F32R),
                                                 in_=pt[:, :]), reads=[pn], writes=["hidT" + sfx])
            for half in range(2):
                pn, pt = pp.next()
                tk.ops("pe", [lambda e, fc=fc, pt=pt, half=half: e.matmul(
                    pt[:, :], lhsT=hidT[:, fc, :].bitcast(F32R),
                    rhs=wd[:, fc, half * 512:(half + 1) * 512].bitcast(F32R), start=(fc == 0), stop=(fc == 3))
                    for fc in range(4)], reads=["hidT" + sfx, "wd" + sfx], writes=[pn], cost=1.5)
                if half == 0:
                    tk.op("act", lambda e, pt=pt: e.copy(out=ysb[:, 0:512], in_=pt[:, :]), reads=[pn],
                          writes=["ysb" + sfx])
                else:
                    tk.op("dve", lambda e, pt=pt: e.tensor_copy(out=ysb[:, 512:1024], in_=pt[:, :]), reads=[pn],
                          writes=["ysb" + sfx])
            tk.dma("sp", lambda e: e.dma_start(out=ys[b * 128:(b + 1) * 128, :], in_=ysb[:]), reads=["ysb" + sfx],
                   writes=[("ys", b)])
            if b % 32 == 31:
                tk.flush()
        tk.flush()
        real.barrier()
    ys_keys = [("ys", b) for b in range(nblk)]

    with ExitStack() as esC:
        sc_ = mk(esC, "c_")
        y02 = [sc_("y0%d" % i, [128, 1024]) for i in range(2)]
        y12 = [sc_("y1%d" % i, [128, 1024]) for i in range(2)]
        for k in range(ntile):
            par = k % 2
            x1, rr, xt, y0, y1 = x12[par], rr2[par], xt2[par], y02[par], y12[par]
            x1k, rrk, xtk, y0k, y1k = "x1%d" % par, "rr%d" % par, "xt%d" % par, "y0%d" % par, "y1%d" % par
            tsl = slice(k * 128, (k + 1) * 128)
            tk.dma("sp", lambda e: e.dma_start(out=x1[:], in_=x1d[tsl, :]), reads=["x1d"], writes=[x1k])
            for j, (yt_, yk) in enumerate(((y0, y0k), (y1, y1k))):
                tk.dma("pool", lambda e, j=j, yt_=yt_: e.indirect_dma_start(
                    out=yt_[:, :], out_offset=None, in_=ys[:, :],
                    in_offset=bass.IndirectOffsetOnAxis(ap=desti[:, j, k:k + 1], axis=0)),
                    reads=(ys_keys if k == 0 else []) + ["desti"], writes=[yk])
            tk.op("dve", lambda e: e.tensor_scalar(out=y0[:], in0=y0[:], scalar1=gts[:, k, 0:1], scalar2=None,
                                                   op0=ALU.mult), reads=[y0k, "gts"], writes=[y0k])
            tk.op("dve", lambda e: e.scalar_tensor_tensor(out=y0[:], in0=y1[:], scalar=gts[:, k, 1:2], in1=y0[:],
                                                          op0=ALU.mult, op1=ALU.add), reads=[y0k, y1k, "gts"],
                  writes=[y0k])
            tk.op("dve", lambda e: e.tensor_tensor(out=y0[:], in0=y0[:], in1=modb[:, 3, :], op=ALU.mult),
                  reads=[y0k, "modb"], writes=[y0k])
            tk.op("dve", lambda e: e.scalar_tensor_tensor(out=rr[:], in0=x1[:], scalar=ALPHA, in1=y0[:], op0=ALU.mult,
                                                          op1=ALU.add), reads=[x1k, y0k], writes=[rrk])
            layer_norm(rr, rrk, xt, xtk, 2)
            tk.dma("sp", lambda e: e.dma_start(out=out[tsl, :], in_=xt[:]), reads=[xtk], writes=["out"])
            if k % 16 == 15:
                tk.flush()
        tk.flush()


def consts2():
    p = np.arange(128)
    tri = (p[:, None] < p[None, :]).astype(np.float32)
    thr16 = np.broadcast_to((128.0 * np.arange(16, dtype=np.float32))[None, :], (128, 16))
    thr64 = np.broadcast_to((128.0 * np.arange(64, dtype=np.float32))[None, :], (128, 64))
    return {"c_ident": np.eye(128, dtype=np.float32), "c_tri": tri, "c_iota": p.astype(np.float32)[:, None].copy(),
            "c_thr16": np.ascontiguousarray(thr16), "c_thr64": np.ascontiguousarray(thr64)}


def build_phase2(ntile=NT2, nblk=NBLK):
    nc = bass.Bass("TRN2", target_bir_lowering=False)
    D = {}

    def din(name, shape, dt=F32):
        D[name] = nc.dram_tensor(name, shape, dt, kind="ExternalInput").ap()

    din("mixTf", [1024, 2048], BF16); din("x2", [2048, 1024]); din("cT", [128, 8])
    din("wada2", [128, 32, 8, 128]); din("bada2", [4096]); din("wo", [1024, 1024]); din("lnp", [4, 1024])
    din("wr", [1024, 36]); din("br", [36])
    din("wgate", [4096, 4096]); din("wup", [4096, 4096]); din("wdown", [4096, 4096])
    din("c_ident", [128, 128]); din("c_tri", [128, 128]); din("c_iota", [128, 1]); din("c_thr16", [128, 16])
    din("c_thr64", [128, 64])
    for nm in ("x1d", "h2d"):
        D[nm] = nc.dram_tensor(nm, [2048, 1024], F32, kind="Internal").ap()
    for nm in ("xs", "ys"):
        D[nm] = nc.dram_tensor(nm, [NBLK * 128, 1024], F32, kind="Internal").ap()
    D["out"] = nc.dram_tensor("out", [2048, 1024], F32, kind="ExternalOutput").ap()
    with ExitStack() as es:
        tk = Trk(nc, es)
        phase2(nc, tk, es, D, ntile=ntile, nblk=nblk)
        tk.finish("sp")
    return nc


def phase2_shared_inputs(inp):
    l = 0
    wg = inp["w_gate"][l].reshape(32, 8, 128, 512).transpose(0, 2, 1, 3).reshape(4096, 4096)
    wu = inp["w_up"][l].reshape(32, 8, 128, 512).transpose(0, 2, 1, 3).reshape(4096, 4096)
    wd = inp["w_down"][l].reshape(32, 4, 128, 1024).transpose(0, 2, 1, 3).reshape(4096, 4096)
    d = {"wada2": np.ascontiguousarray(inp["w_ada"][l][:, 2048:6144].reshape(8, 128, 32, 128).transpose(1, 2, 0, 3)),
         "bada2": np.ascontiguousarray(inp["b_ada"][l][2048:6144]),
         "wo": np.ascontiguousarray(inp["w_o"][l]),
         "lnp": np.ascontiguousarray(np.stack([inp["ln1_g"][l], inp["ln1_b"][l], inp["ln2_g"][l], inp["ln2_b"][l]])),
         "wr": np.ascontiguousarray(np.concatenate([inp["w_router_group"][l], inp["w_router_expert"][l]], 1)),
         "br": np.ascontiguousarray(np.concatenate([inp["b_router_group"][l], inp["b_router_expert"][l]])),
         "wgate": np.ascontiguousarray(wg), "wup": np.ascontiguousarray(wu), "wdown": np.ascontiguousarray(wd)}
    d.update(consts2())
    return d


def phase2_inputs(inp, core, shared, mixTf):
    b, q = core // 4, core % 4
    d = dict(shared)
    d["x2"] = np.ascontiguousarray(inp["x"][b, q * 2048:(q + 1) * 2048])
    d["cT"] = np.ascontiguousarray(inp["c"][b].reshape(8, 128).T)
    d["mixTf"] = mixTf
    return d


def build_fused():
    nc = bass.Bass("TRN2", target_bir_lowering=False)
    D = {}

    def din(name, shape, dt=F32):
        D[name] = nc.dram_tensor(name, shape, dt, kind="ExternalInput").ap()

    din("x", [D_MODEL, SEQ]); din("cT", [128, 8]); din("pos", [SEQ], I32)
    din("wada1", [128, 8, 8, 256]); din("bada1", [128, 16]); din("win", [128, 5, 8, 256])
    din("convw", [128, 3, 4]); din("pvec", [128, 4])
    din("c_ident", [128, 128]); din("c_masks", [128, 3, 128]); din("c_amask", [128, 256]); din("c_invf", [64, 2])
    din("x2", [2048, 1024]); din("qidx", [128, 8], I32)
    din("wada2", [128, 32, 8, 128]); din("bada2", [4096]); din("wo", [1024, 1024]); din("lnp", [4, 1024])
    din("wr", [1024, 36]); din("br", [36])
    din("wgate", [4096, 4096]); din("wup", [4096, 4096]); din("wdown", [4096, 4096])
    din("c_tri", [128, 128]); din("c_iota", [128, 1]); din("c_thr16", [128, 16]); din("c_thr64", [128, 64])
    cin = [nc.dram_tensor("cin%d" % q, [256, 2048], BF16, kind="Internal").ap() for q in range(4)]
    cout = nc.dram_tensor("cout", [4096, 2048], BF16, kind="Internal").ap()
    for nm in ("x1d", "h2d"):
        D[nm] = nc.dram_tensor(nm, [2048, 1024], F32, kind="Internal").ap()
    for nm in ("xs", "ys"):
        D[nm] = nc.dram_tensor(nm, [NBLK * 128, 1024], F32, kind="Internal").ap()
    D["out"] = nc.dram_tensor("out", [2048, 1024], F32, kind="ExternalOutput").ap()
    groups = [[0, 1, 2, 3], [4, 5, 6, 7]]
    with ExitStack() as es0:
        tk = Trk(nc, es0)

        def on_quarter(sc, tk):
            tk.cc(lambda e: e.collective_compute("AllGather", ALU.bypass, replica_groups=groups,
                                                 ins=[cin[sc][:, :]], outs=[cout[sc * 1024:(sc + 1) * 1024, :]]),
                  reads=[("cin_a", sc), ("cin_b", sc)], writes=[("cout", sc)])

        with ExitStack() as es1:
            phase1(nc, tk, es1, D, cin=cin, on_quarter=on_quarter)
        tk.barrier()
        with ExitStack() as es2:
            phase2(nc, tk, es2, D, cout=cout)
        tk.finish("sp")
    return nc


def fused_inputs(inp, core, shared):
    b, q = core // 4, core % 4
    d = dict(shared)
    d.update(phase1_inputs(inp, core))
    d["x2"] = np.ascontiguousarray(inp["x"][b, q * 2048:(q + 1) * 2048])
    kc = np.arange(8)[None, :]
    p = np.arange(128)[:, None]
    row = np.where(kc < 4, kc * 256 + p, (kc - 4) * 256 + 128 + p)
    d["qidx"] = np.ascontiguousarray((q * 1024 + row).astype(np.int32))
    return d


def kernel(**inputs):
    inp = {k: np.asarray(v) for k, v in inputs.items()}
    shared = phase2_shared_inputs(inp)
    nc = build_fused()
    maps = [fused_inputs(inp, core, shared) for core in range(NCORES)]
    res = run_bass_kernel_spmd(nc, maps, core_ids=list(range(NCORES)))
    out = np.zeros((BATCH, SEQ, D_MODEL), np.float32)
    for core in range(NCORES):
        b, q = core // 4, core % 4
        out[b, q * 2048:(q + 1) * 2048] = np.asarray(res.results[core]["out"])
    return out
```
